# Optimizing a Trainium2 kernel written in Bass

```python
import math
import jax
import jax.numpy as jnp
from jax import lax
import numpy as np

D_MODEL = 2048
BATCH = 2
SEQ = 8192
DEPTH = 2

N_META = 16
EPS = 1e-6
NEG_INF = -1e30
CONV_K = 4

GDN_HEADS = 4
GDN_DK = 128
GDN_DV = 128
GDN_CHUNK = 64
GDN_QK = GDN_HEADS * GDN_DK
GDN_VW = GDN_HEADS * GDN_DV

MLA_HEADS = 4
MLA_Q_RANK = 512
MLA_KV_RANK = 256
MLA_NOPE = 128
MLA_ROPE = 64
MLA_V = 128
ROPE_BASE = 10000.0

FOX_HEADS = 4
FOX_DH = 128
FOX_W = FOX_HEADS * FOX_DH
ATTN_BLOCK = 128

SSD_HEADS = 8
SSD_HEADDIM = 64
SSD_GROUPS = 2
SSD_STATE = 128
SSD_CHUNK = 256
SSD_INNER = SSD_HEADS * SSD_HEADDIM

N_BRANCH = 4
BRANCH_W = 512

D_FF = 5632
N_EXPERTS = 8
TOP_K = 2
D_FF_EXPERT = 1408
N_DENSE = (DEPTH + 1) // 2
N_MOE = DEPTH // 2

IN_WIDTHS = (
    GDN_QK, GDN_QK, GDN_VW, GDN_VW, GDN_HEADS, GDN_HEADS,
    MLA_Q_RANK, MLA_KV_RANK, MLA_ROPE,
    FOX_W, FOX_W, FOX_W, FOX_HEADS,
    SSD_INNER, SSD_INNER, SSD_GROUPS * SSD_STATE, SSD_GROUPS * SSD_STATE, SSD_HEADS,
)
D_IN = sum(IN_WIDTHS)

kernel_name = 'hybrid_gated_parallel_mixer_moe'


def rms_norm(x, g):
    xf = x.astype(jnp.float32)
    y = xf * lax.rsqrt(jnp.mean(xf * xf, axis=-1, keepdims=True) + EPS)
    return (y * g.astype(jnp.float32)).astype(x.dtype)


def l2norm(t):
    return t * lax.rsqrt(jnp.sum(t * t, axis=-1, keepdims=True) + EPS)


def causal_dwconv(x, w):
    ch = x.shape[-1]
    return lax.conv_general_dilated(
        x, w.astype(x.dtype)[:, None, :], window_strides=(1,), padding=[(w.shape[0] - 1, 0)],
        dimension_numbers=('NWC', 'WIO', 'NWC'), feature_group_count=ch)


def pad_to_chunks(t, chunk):
    front = chunk - N_META
    back = (-(t.shape[1] + front)) % chunk
    pad = [(0, 0), (front, back)] + [(0, 0)] * (t.ndim - 2)
    return jnp.pad(t, pad), front


def rope_tables(L):
    inv = 1.0 / (ROPE_BASE ** (jnp.arange(0, MLA_ROPE, 2, dtype=jnp.float32) / MLA_ROPE))
    ang = jnp.arange(L, dtype=jnp.float32)[:, None] * inv[None, :]
    ang = jnp.concatenate([ang, ang], axis=-1)
    return jnp.cos(ang), jnp.sin(ang)


def apply_rope(t, cos, sin):
    half = t.shape[-1] // 2
    rot = jnp.concatenate([-t[..., half:], t[..., :half]], axis=-1)
    return t * cos[None, :, None, :] + rot * sin[None, :, None, :]


def block_causal_attention(q, k, v, log_f=None):
    Bsz, L, H, dq = q.shape
    dv = v.shape[-1]
    (q, front), (k, _), (v, _) = pad_to_chunks(q, ATTN_BLOCK), pad_to_chunks(k, ATTN_BLOCK), pad_to_chunks(v, ATTN_BLOCK)
    T = q.shape[1]
    nb = T // ATTN_BLOCK
    q = jnp.moveaxis(q, 2, 1).astype(jnp.float32) * dq ** -0.5
    k = jnp.moveaxis(k, 2, 1).astype(jnp.float32)
    v = jnp.moveaxis(v, 2, 1).astype(jnp.float32)
    qb = jnp.moveaxis(q.reshape(Bsz, H, nb, ATTN_BLOCK, dq), 2, 0)
    if log_f is None:
        cum = None
        cb = None
    else:
        lf, _ = pad_to_chunks(log_f.astype(jnp.float32), ATTN_BLOCK)
        cum = jnp.cumsum(jnp.moveaxis(lf, 2, 1), axis=-1)
        cb = jnp.moveaxis(cum.reshape(Bsz, H, nb, ATTN_BLOCK), 2, 0)
    kpos = jnp.arange(T)

    def one_block(args):
        i, q_i, c_i = args
        s = jnp.einsum('bhqd,bhkd->bhqk', q_i, k)
        if c_i is not None:
            s = s + c_i[..., :, None] - cum[..., None, :]
        qpos = i * ATTN_BLOCK + jnp.arange(ATTN_BLOCK)
        mask = (kpos[None, :] <= qpos[:, None]) & (kpos[None, :] >= front)
        p = jax.nn.softmax(jnp.where(mask, s, NEG_INF), axis=-1)
        return jnp.einsum('bhqk,bhkd->bhqd', p, v)

    o = lax.map(one_block, (jnp.arange(nb), qb, cb))
    o = jnp.moveaxis(o, 0, 2).reshape(Bsz, H, T, dv)[:, :, front:front + L]
    return jnp.moveaxis(o, 1, 2)


def gated_deltanet(q, k, v, z, b_logit, a_logit, conv_w, A_log, dt_bias, norm_g):
    Bsz, L, _ = q.shape
    C = GDN_CHUNK
    f32 = jnp.float32
    qkv = jax.nn.silu(causal_dwconv(jnp.concatenate([q, k, v], axis=-1), conv_w))
    q, k, v = jnp.split(qkv, [GDN_QK, 2 * GDN_QK], axis=-1)
    q = l2norm(q.reshape(Bsz, L, GDN_HEADS, GDN_DK).astype(f32)) * GDN_DK ** -0.5
    k = l2norm(k.reshape(Bsz, L, GDN_HEADS, GDN_DK).astype(f32))
    v = v.reshape(Bsz, L, GDN_HEADS, GDN_DV).astype(f32)
    beta = jax.nn.sigmoid(b_logit.astype(f32))
    g = -jnp.exp(A_log.astype(f32)) * jax.nn.softplus(a_logit.astype(f32) + dt_bias.astype(f32))

    def to_chunks(t):
        t, _ = pad_to_chunks(t, C)
        t = jnp.moveaxis(t, 2, 1)
        return t.reshape(t.shape[0], t.shape[1], -1, C, *t.shape[3:])

    front = C - N_META
    qc, kc, vc, bc, gc = (to_chunks(t) for t in (q, k, v, beta, g))
    gcs = jnp.cumsum(gc, axis=-1)
    causal = jnp.tril(jnp.ones((C, C), dtype=bool))
    strict = jnp.tril(jnp.ones((C, C), dtype=bool), -1)
    decay = jnp.exp(jnp.where(causal, gcs[..., :, None] - gcs[..., None, :], -jnp.inf))
    kb = kc * bc[..., None]
    a_strict = jnp.where(strict, jnp.einsum('bhncd,bhnsd->bhncs', kb, kc) * decay, 0.0)
    eye = jnp.eye(C, dtype=f32)
    tinv = lax.linalg.triangular_solve(eye + a_strict, jnp.broadcast_to(eye, a_strict.shape),
                                       left_side=True, lower=True)
    u = tinv @ (vc * bc[..., None])
    w = tinv @ (kb * jnp.exp(gcs)[..., None])
    attn_intra = jnp.einsum('bhncd,bhnsd->bhncs', qc, kc) * decay
    q_dec = qc * jnp.exp(gcs)[..., None]
    k_to_end = kc * jnp.exp(gcs[..., -1:] - gcs)[..., None]
    chunk_decay = jnp.exp(gcs[..., -1])

    def step(S, inp):
        u_i, w_i, qd_i, kd_i, att_i, cd_i = inp
        v_new = u_i - jnp.einsum('bhcd,bhde->bhce', w_i, S)
        o = jnp.einsum('bhcd,bhde->bhce', qd_i, S) + jnp.einsum('bhcs,bhse->bhce', att_i, v_new)
        S = S * cd_i[..., None, None] + jnp.einsum('bhcd,bhce->bhde', kd_i, v_new)
        return S, o

    xs = tuple(jnp.moveaxis(t, 2, 0) for t in (u, w, q_dec, k_to_end, attn_intra, chunk_decay))
    S0 = jnp.zeros((Bsz, GDN_HEADS, GDN_DK, GDN_DV), f32)
    _, o = lax.scan(step, S0, xs)
    o = jnp.moveaxis(o, 0, 2).reshape(Bsz, GDN_HEADS, -1, GDN_DV)[:, :, front:front + L]
    o = jnp.moveaxis(o, 1, 2)
    o = rms_norm(o, norm_g) * jax.nn.silu(z.reshape(Bsz, L, GDN_HEADS, GDN_DV).astype(f32))
    return o.reshape(Bsz, L, GDN_VW)


def mla_attention(qa, kva, kpe, cos, sin, qa_g, wq_b, kva_g, wkv_b, qn_g, kn_g):
    Bsz, L, _ = qa.shape
    q = (rms_norm(qa, qa_g) @ wq_b).reshape(Bsz, L, MLA_HEADS, MLA_NOPE + MLA_ROPE)
    kv = (rms_norm(kva, kva_g) @ wkv_b).reshape(Bsz, L, MLA_HEADS, MLA_NOPE + MLA_V)
    k_nope, v = kv[..., :MLA_NOPE], kv[..., MLA_NOPE:]
    k = jnp.concatenate([k_nope, jnp.broadcast_to(kpe[:, :, None, :], (Bsz, L, MLA_HEADS, MLA_ROPE))], axis=-1)
    q = rms_norm(q, qn_g)
    k = rms_norm(k, kn_g)
    q = jnp.concatenate([q[..., :MLA_NOPE], apply_rope(q[..., MLA_NOPE:], cos, sin)], axis=-1)
    k = jnp.concatenate([k[..., :MLA_NOPE], apply_rope(k[..., MLA_NOPE:], cos, sin)], axis=-1)
    o = block_causal_attention(q, k, v)
    return o.reshape(Bsz, L, MLA_HEADS * MLA_V)


def forgetting_attention(q, k, v, f_logit, qn_g, kn_g, b_f):
    Bsz, L, _ = q.shape
    q = rms_norm(q.reshape(Bsz, L, FOX_HEADS, FOX_DH), qn_g)
    k = rms_norm(k.reshape(Bsz, L, FOX_HEADS, FOX_DH), kn_g)
    v = v.reshape(Bsz, L, FOX_HEADS, FOX_DH)
    log_f = jax.nn.log_sigmoid(f_logit.astype(jnp.float32) + b_f.astype(jnp.float32))
    o = block_causal_attention(q, k, v, log_f)
    return o.reshape(Bsz, L, FOX_W)


def mamba2_ssd(z, xs, Bm, Cm, dt_raw, conv_w, conv_b, dt_bias, A_log, D, norm_g):
    Bsz, L, _ = z.shape
    C, G, HG, P, N = SSD_CHUNK, SSD_GROUPS, SSD_HEADS // SSD_GROUPS, SSD_HEADDIM, SSD_STATE
    f32 = jnp.float32
    xBC = jax.nn.silu(causal_dwconv(jnp.concatenate([xs, Bm, Cm], axis=-1), conv_w) + conv_b)
    xs, Bm, Cm = jnp.split(xBC, [SSD_INNER, SSD_INNER + G * N], axis=-1)
    X = xs.reshape(Bsz, L, G, HG, P).astype(f32)
    Bm = Bm.reshape(Bsz, L, G, N).astype(f32)
    Cm = Cm.reshape(Bsz, L, G, N).astype(f32)
    dt = jax.nn.softplus(dt_raw.astype(f32) + dt_bias.astype(f32)).reshape(Bsz, L, G, HG)
    a = dt * (-jnp.exp(A_log.astype(f32))).reshape(G, HG)
    Xdt = X * dt[..., None]

    def to_chunks(t):
        t, _ = pad_to_chunks(t, C)
        return t.reshape(t.shape[0], -1, C, *t.shape[2:])

    front = C - N_META
    Xc, Bc, Cc, ac = (to_chunks(t) for t in (Xdt, Bm, Cm, a))
    acs = jnp.cumsum(ac, axis=2)
    causal = jnp.tril(jnp.ones((C, C), dtype=bool))
    seg = acs[:, :, :, None] - acs[:, :, None, :]
    Lmat = jnp.exp(jnp.where(causal[:, :, None, None], seg, -jnp.inf))
    CB = jnp.einsum('bclgn,bcsgn->bclsg', Cc, Bc)
    y_diag = jnp.einsum('bclsgh,bcsghp->bclghp', CB[..., None] * Lmat, Xc)
    decay_to_end = jnp.exp(acs[:, :, -1:] - acs)
    states = jnp.einsum('bclgn,bclghp->bcghpn', Bc, Xc * decay_to_end[..., None])
    chunk_decay = jnp.exp(acs[:, :, -1])

    def step(hs, inp):
        st, cd = inp
        return hs * cd[..., None, None] + st, hs

    h0 = jnp.zeros((Bsz, G, HG, P, N), f32)
    _, h_in = lax.scan(step, h0, (jnp.moveaxis(states, 1, 0), jnp.moveaxis(chunk_decay, 1, 0)))
    h_in = jnp.moveaxis(h_in, 0, 1)
    y_off = jnp.einsum('bclgn,bcghpn->bclghp', Cc, h_in) * jnp.exp(acs)[..., None]
    y = (y_diag + y_off).reshape(Bsz, -1, G, HG, P)[:, front:front + L]
    y = y + X * D.astype(f32).reshape(G, HG)[..., None]
    y = y.reshape(Bsz, L, G, HG * P) * jax.nn.silu(z.reshape(Bsz, L, G, HG * P).astype(f32))
    y = rms_norm(y, norm_g.reshape(G, HG * P))
    return y.reshape(Bsz, L, SSD_INNER)


def hybrid_mixer(h, cos, sin, w_in,
                 gdn_conv_w, gdn_A_log, gdn_dt_bias, gdn_norm_g,
                 mla_qa_g, mla_wq_b, mla_kva_g, mla_wkv_b, mla_qn_g, mla_kn_g,
                 fox_qn_g, fox_kn_g, fox_b_f,
                 ssd_conv_w, ssd_conv_b, ssd_dt_bias, ssd_A_log, ssd_D, ssd_norm_g,
                 w_gate, w_branch, w_o):
    proj = h @ w_in
    (g_q, g_k, g_v, g_z, g_b, g_a, m_qa, m_kva, m_kpe, f_q, f_k, f_v, f_f,
     s_z, s_x, s_B, s_C, s_dt) = jnp.split(proj, np.cumsum(IN_WIDTHS)[:-1].tolist(), axis=-1)
    branches = (
        gated_deltanet(g_q, g_k, g_v, g_z, g_b, g_a, gdn_conv_w, gdn_A_log, gdn_dt_bias, gdn_norm_g),
        mla_attention(m_qa, m_kva, m_kpe, cos, sin, mla_qa_g, mla_wq_b, mla_kva_g, mla_wkv_b, mla_qn_g, mla_kn_g),
        forgetting_attention(f_q, f_k, f_v, f_f, fox_qn_g, fox_kn_g, fox_b_f),
        mamba2_ssd(s_z, s_x, s_B, s_C, s_dt, ssd_conv_w, ssd_conv_b, ssd_dt_bias, ssd_A_log, ssd_D, ssd_norm_g),
    )
    merged = sum(jax.nn.sigmoid(h @ w_gate[b]) * (branches[b] @ w_branch[b]) for b in range(N_BRANCH))
    return merged @ w_o


def swiglu(h, w_g, w_u, w_d):
    return (jax.nn.silu(h @ w_g) * (h @ w_u)) @ w_d


def moe_swiglu(h, router_w, w_g, w_u, w_d):
    logits = (h @ router_w).astype(jnp.float32)
    top_v, top_i = lax.top_k(logits, TOP_K)
    top_p = jax.nn.softmax(top_v, axis=-1)
    combine = jnp.sum(jax.nn.one_hot(top_i, N_EXPERTS, dtype=jnp.float32) * top_p[..., None], axis=-2)
    return sum(combine[..., e:e + 1] * swiglu(h, w_g[e], w_u[e], w_d[e]) for e in range(N_EXPERTS))


def setup_inputs(seed: int = 0) -> dict:
    key = jax.random.key(seed)
    keys = iter(jax.random.split(key, 48))
    f32 = jnp.float32

    def nrm(shape, scale):
        return jax.random.normal(next(keys), shape, f32) * scale

    def gain(shape):
        return 1.0 + nrm(shape, 0.02)

    def a_log(shape):
        return jnp.log(jax.random.uniform(next(keys), shape, f32, 1.0, 16.0))

    def dt_bias(shape):
        dt = jnp.exp(jax.random.uniform(next(keys), shape, f32, math.log(1e-3), math.log(1e-1)))
        return dt + jnp.log(-jnp.expm1(-dt))

    Dm = D_MODEL
    return {
        'x': nrm((BATCH, SEQ, Dm), 1.0),
        'meta_tokens': nrm((N_META, Dm), 1.0),
        'mix_norm_g': gain((DEPTH, Dm)),
        'w_in': nrm((DEPTH, Dm, D_IN), Dm ** -0.5),
        'gdn_conv_w': nrm((DEPTH, CONV_K, 2 * GDN_QK + GDN_VW), CONV_K ** -0.5),
        'gdn_A_log': a_log((DEPTH, GDN_HEADS)),
        'gdn_dt_bias': dt_bias((DEPTH, GDN_HEADS)),
        'gdn_norm_g': gain((DEPTH, GDN_DV)),
        'mla_qa_g': gain((DEPTH, MLA_Q_RANK)),
        'mla_wq_b': nrm((DEPTH, MLA_Q_RANK, MLA_HEADS * (MLA_NOPE + MLA_ROPE)), MLA_Q_RANK ** -0.5),
        'mla_kva_g': gain((DEPTH, MLA_KV_RANK)),
        'mla_wkv_b': nrm((DEPTH, MLA_KV_RANK, MLA_HEADS * (MLA_NOPE + MLA_V)), MLA_KV_RANK ** -0.5),
        'mla_qn_g': gain((DEPTH, MLA_NOPE + MLA_ROPE)),
        'mla_kn_g': gain((DEPTH, MLA_NOPE + MLA_ROPE)),
        'fox_qn_g': gain((DEPTH, FOX_DH)),
        'fox_kn_g': gain((DEPTH, FOX_DH)),
        'fox_b_f': 2.0 + nrm((DEPTH, FOX_HEADS), 0.1),
        'ssd_conv_w': nrm((DEPTH, CONV_K, SSD_INNER + 2 * SSD_GROUPS * SSD_STATE), CONV_K ** -0.5),
        'ssd_conv_b': nrm((DEPTH, SSD_INNER + 2 * SSD_GROUPS * SSD_STATE), 0.02),
        'ssd_dt_bias': dt_bias((DEPTH, SSD_HEADS)),
        'ssd_A_log': a_log((DEPTH, SSD_HEADS)),
        'ssd_D': gain((DEPTH, SSD_HEADS)),
        'ssd_norm_g': gain((DEPTH, SSD_INNER)),
        'w_gate': nrm((DEPTH, N_BRANCH, Dm, Dm), Dm ** -0.5),
        'w_branch': nrm((DEPTH, N_BRANCH, BRANCH_W, Dm), BRANCH_W ** -0.5),
        'w_o': nrm((DEPTH, Dm, Dm), Dm ** -0.5),
        'ffn_norm_g': gain((DEPTH, Dm)),
        'dense_w_gate': nrm((N_DENSE, Dm, D_FF), Dm ** -0.5),
        'dense_w_up': nrm((N_DENSE, Dm, D_FF), Dm ** -0.5),
        'dense_w_down': nrm((N_DENSE, D_FF, Dm), D_FF ** -0.5),
        'router_w': nrm((N_MOE, Dm, N_EXPERTS), Dm ** -0.5),
        'moe_w_gate': nrm((N_MOE, N_EXPERTS, Dm, D_FF_EXPERT), Dm ** -0.5),
        'moe_w_up': nrm((N_MOE, N_EXPERTS, Dm, D_FF_EXPERT), Dm ** -0.5),
        'moe_w_down': nrm((N_MOE, N_EXPERTS, D_FF_EXPERT, Dm), D_FF_EXPERT ** -0.5),
    }


def reference(x, meta_tokens, mix_norm_g, w_in,
              gdn_conv_w, gdn_A_log, gdn_dt_bias, gdn_norm_g,
              mla_qa_g, mla_wq_b, mla_kva_g, mla_wkv_b, mla_qn_g, mla_kn_g,
              fox_qn_g, fox_kn_g, fox_b_f,
              ssd_conv_w, ssd_conv_b, ssd_dt_bias, ssd_A_log, ssd_D, ssd_norm_g,
              w_gate, w_branch, w_o, ffn_norm_g,
              dense_w_gate, dense_w_up, dense_w_down,
              router_w, moe_w_gate, moe_w_up, moe_w_down):
    Bsz = x.shape[0]
    meta = jnp.broadcast_to(meta_tokens[None].astype(x.dtype), (Bsz, N_META, D_MODEL))
    h = jnp.concatenate([meta, x], axis=1)
    cos, sin = rope_tables(h.shape[1])
    for layer in range(DEPTH):
        hn = rms_norm(h, mix_norm_g[layer])
        h = h + hybrid_mixer(
            hn, cos, sin, w_in[layer],
            gdn_conv_w[layer], gdn_A_log[layer], gdn_dt_bias[layer], gdn_norm_g[layer],
            mla_qa_g[layer], mla_wq_b[layer], mla_kva_g[layer], mla_wkv_b[layer], mla_qn_g[layer], mla_kn_g[layer],
            fox_qn_g[layer], fox_kn_g[layer], fox_b_f[layer],
            ssd_conv_w[layer], ssd_conv_b[layer], ssd_dt_bias[layer], ssd_A_log[layer], ssd_D[layer], ssd_norm_g[layer],
            w_gate[layer], w_branch[layer], w_o[layer])
        hn = rms_norm(h, ffn_norm_g[layer])
        i = layer // 2
        if layer % 2 == 0:
            h = h + swiglu(hn, dense_w_gate[i], dense_w_up[i], dense_w_down[i])
        else:
            h = h + moe_swiglu(hn, router_w[i], moe_w_gate[i], moe_w_up[i], moe_w_down[i])
    return h[:, N_META:].astype(x.dtype)
```

```python
import os
import numpy as np
import concourse.bass as bass
import concourse.mybir as mybir
from concourse.bass_utils import run_bass_kernel_spmd

F32 = mybir.dt.float32
BF16 = mybir.dt.bfloat16
AF = mybir.ActivationFunctionType
ALU = mybir.AluOpType
AX = mybir.AxisListType


class Res:
    __slots__ = ("w", "r", "dsem", "dcnt")

    def __init__(self):
        self.w = None
        self.r = {}
        self.dsem = None
        self.dcnt = 0


class V:
    __slots__ = ("ap", "res", "space")

    def __init__(self, ap, res, space="sbuf"):
        self.ap = ap
        self.res = res
        self.space = space

    def __getitem__(self, idx):
        return V(self.ap[idx], self.res, self.space)


class Buf:
    def __init__(self, kb, name, shape, dtype, space="sbuf", kind=None, nres=1, stack=None):
        self.kb = kb
        nc = kb.nc
        self.name = name
        if space == "sbuf":
            if stack is not None:
                self.t = stack.enter_context(nc.sbuf_tensor(name, list(shape), dtype))
            else:
                self.t = nc.alloc_sbuf_tensor(name, list(shape), dtype)
        elif space == "psum":
            self.t = nc.alloc_psum_tensor(name, list(shape), dtype)
        else:
            self.t = nc.dram_tensor(name, list(shape), dtype, kind=kind or "Internal").ap()
        self.res = [Res() for _ in range(nres)]
        self.space = space

    def __getitem__(self, idx):
        return V(self.t[idx], self.res, self.space)

    def sub(self, ri, idx):
        return V(self.t[idx], [self.res[ri]], self.space)


def bc(v, ap):
    return V(ap, v.res, v.space)


class StopBuild(Exception):
    pass


class KB:
    ENG = ("pe", "act", "dve", "pool", "sp")

    def ck(self, name=""):
        import os
        lim = int(os.environ.get("STOP_AT", "0"))
        self._ck = getattr(self, "_ck", 0) + 1
        if lim and self._ck >= lim:
            print("STOP at checkpoint", self._ck, name)
            raise StopBuild()

    def __init__(self):
        self.nc = bass.Bass("TRN2", target_bir_lowering=False)
        nc = self.nc
        self.e = {"pe": nc.tensor, "act": nc.scalar, "dve": nc.vector, "pool": nc.gpsimd, "sp": nc.sync}
        self.sem = {}
        self.cnt = {}
        self.seen = {k: {} for k in self.ENG}
        self.semh = {}
        for k in self.ENG:
            h = nc.alloc_semaphore("sem_" + k)
            self.semh[k] = h
            self.cnt[k] = 0
        self.ndsem = 0
        self.dma_max = {}
        self.out_tokens = []
        self.ninstr = 0

    def _dsem(self, res):
        if res.dsem is None:
            key = "d%d" % self.ndsem
            self.ndsem += 1
            self.semh[key] = self.nc.alloc_semaphore("sem_" + key)
            res.dsem = key
        return res.dsem

    def _collect(self, eng, outs, ins):
        waits = {}

        def need(tok):
            if tok is None:
                return
            s, val = tok
            if waits.get(s, 0) < val:
                waits[s] = val

        for v in ins:
            for r in v.res:
                need(r.w)
                if v.space == "psum":
                    for s, val in r.r.items():
                        if s != eng:
                            need((s, val))
        for v in outs:
            for r in v.res:
                if r.w is not None and not (r.w[0] == eng):
                    need(r.w)
                for s, val in r.r.items():
                    if s != eng:
                        need((s, val))
        if eng == "pe" and "pe" in waits:
            del waits["pe"]
        return waits

    def _dowaits(self, eng, waits):
        E = self.e[eng]
        seen = self.seen[eng]
        for s, val in waits.items():
            if seen.get(s, 0) >= val:
                continue
            E.wait_ge(self.semh[s], val)
            seen[s] = val

    def op(self, eng, fn, outs, ins):
        waits = self._collect(eng, outs, ins)
        self._dowaits(eng, waits)
        ins_ = fn()
        self.cnt[eng] += 1
        c = self.cnt[eng]
        ins_.then_inc(self.semh[eng], 1)
        for v in ins:
            for r in v.res:
                r.r[eng] = c
        for v in outs:
            for r in v.res:
                r.w = (eng, c)
                r.r = {}
        self.ninstr += 1
        return ins_

    def dma(self, q, out, in_, final=False):
        waits = self._collect("__dma__", [out], [in_])
        self._dowaits(q, waits)
        if out.space == "sbuf":
            sres = out.res[0]
        elif in_.space == "sbuf":
            sres = in_.res[0]
        else:
            sres = out.res[0]
        key = self._dsem(sres)
        ins_ = self.e[q].dma_start(out=out.ap, in_=in_.ap)
        sres.dcnt += 16
        ins_.then_inc(self.semh[key], 16)
        tok = (key, sres.dcnt)
        self.dma_max[key] = sres.dcnt
        for r in in_.res:
            r.r[key] = max(r.r.get(key, 0), sres.dcnt)
        for r in out.res:
            r.w = tok
            r.r = {}
        if final:
            self.out_tokens.append(tok)
        self.ninstr += 1
        return ins_

    def transfer(self, olds, news):
        toks = {}
        for b in olds:
            for r in b.res:
                if r.w is not None:
                    toks[r.w[0]] = max(toks.get(r.w[0], 0), r.w[1])
                for s, val in r.r.items():
                    toks[s] = max(toks.get(s, 0), val)
        for b in news:
            for r in b.res:
                r.w = None
                r.r = dict(toks)

    def barrier(self):
        allw = {k: self.cnt[k] for k in ("pe", "act", "dve", "pool") if self.cnt[k] > 0}
        for key, val in self.dma_max.items():
            allw[key] = val
        for e in self.ENG:
            w = {k: v for k, v in allw.items() if k != e}
            self._dowaits(e, w)

    def finish(self):
        waits = {}
        for s, val in self.out_tokens:
            waits[s] = max(waits.get(s, 0), val)
        self._dowaits("sp", waits)
        for k in ("pe", "act", "dve", "pool"):
            if self.cnt[k] > 0:
                self._dowaits("sp", {k: self.cnt[k]})

    def mm(self, out, lhsT, rhs, start=True, stop=True):
        nc = self.nc
        return self.op("pe", lambda: nc.tensor.matmul(out.ap, lhsT.ap, rhs.ap, start=start, stop=stop),
                       [out], [lhsT, rhs])

    def tr(self, out, in_, ident):
        nc = self.nc
        return self.op("pe", lambda: nc.tensor.transpose(out.ap, in_.ap, ident.ap), [out], [in_, ident])

    def act(self, out, in_, func, bias=None, scale=1.0, accum=None, eng="act"):
        nc = self.nc
        ins = [in_]
        kw = {}
        if bias is not None:
            if isinstance(bias, V):
                ins.append(bias)
                kw["bias"] = bias.ap
            else:
                kw["bias"] = bias
        if isinstance(scale, V):
            ins.append(scale)
            kw["scale"] = scale.ap
        else:
            kw["scale"] = scale
        outs = [out]
        if accum is not None:
            outs.append(accum)
            kw["accum_out"] = accum.ap
        return self.op("act", lambda: nc.scalar.activation(out=out.ap, in_=in_.ap, func=func, **kw), outs, ins)

    def tt(self, eng, out, a, b, op):
        E = self.e[eng]
        return self.op(eng, lambda: E.tensor_tensor(out=out.ap, in0=a.ap, in1=b.ap, op=op), [out], [a, b])

    def ts(self, eng, out, a, s1, s2=None, op0=ALU.mult, op1=None, accum=None):
        E = self.e[eng]
        ins = [a]
        a1 = s1
        a2 = s2
        if isinstance(s1, V):
            ins.append(s1)
            a1 = s1.ap
        if isinstance(s2, V):
            ins.append(s2)
            a2 = s2.ap
        kw = {}
        if op1 is not None:
            kw["op1"] = op1
        outs = [out]
        if accum is not None:
            outs.append(accum)
            kw["accum_out"] = accum.ap
        return self.op(eng, lambda: E.tensor_scalar(out=out.ap, in0=a.ap, scalar1=a1, scalar2=a2, op0=op0, **kw),
                       outs, ins)

    def stt(self, eng, out, a, s, b, op0, op1):
        E = self.e[eng]
        ins = [a, b]
        sa = s
        if isinstance(s, V):
            ins.append(s)
            sa = s.ap
        return self.op(eng, lambda: E.scalar_tensor_tensor(out=out.ap, in0=a.ap, scalar=sa, in1=b.ap, op0=op0, op1=op1),
                       [out], ins)

    def copy(self, eng, out, in_):
        if eng == "act":
            nc = self.nc
            return self.op("act", lambda: nc.scalar.copy(out=out.ap, in_=in_.ap), [out], [in_])
        E = self.e[eng]
        return self.op(eng, lambda: E.tensor_copy(out=out.ap, in_=in_.ap), [out], [in_])

    def recip(self, out, in_):
        nc = self.nc
        return self.op("dve", lambda: nc.vector.reciprocal(out=out.ap, in_=in_.ap), [out], [in_])

    def eps_tile(self, val):
        if not hasattr(self, "_eps"):
            self._eps = {}
        if val not in self._eps:
            b = Buf(self, "cst%d" % len(self._eps), [128, 1], F32)
            self.memset("dve", b[:, :], float(val))
            self._eps[val] = b
        return self._eps[val][:, :]

    def memset(self, eng, out, val):
        E = self.e[eng]
        return self.op(eng, lambda: E.memset(out.ap, val), [out], [])


class ABuf:
    def __init__(self, ap, space="sbuf", nres=1):
        self.t = ap
        self.res = [Res() for _ in range(nres)]
        self.space = space

    def __getitem__(self, idx):
        return V(self.t[idx], self.res, self.space)


D_MODEL = 2048
KC = 16
EPS = 1e-6
D_FF = 5632
NFF = D_FF // 128
N_EXP = 8
D_FFE = 1408
NFE = D_FFE // 128


def token_tiles(NT, W=512):
    out = []
    t = 0
    while t < NT:
        w = min(W, NT - t)
        out.append((t, w))
        t += w
    return out


def rmsnorm_fm(kb, h_sb, hn, g_sb, ones_bf, sqs, ps_ss, rstd, W, D=D_MODEL, post=None):
    nkc = D // 128
    for kc in range(nkc):
        sq = sqs[kc % len(sqs)]
        kb.act(sq[:, 0:W], h_sb[:, kc, 0:W], AF.Square)
        kb.mm(ps_ss[:, 0:W], ones_bf[:, :], sq[:, 0:W], start=(kc == 0), stop=(kc == nkc - 1))
    kb.act(rstd[:, 0:W], ps_ss[:, 0:W], AF.Sqrt, bias=kb.eps_tile(EPS))
    kb.recip(rstd[:, 0:W], rstd[:, 0:W])
    for kc in range(nkc):
        eng = "dve"
        if post is None:
            kb.stt(eng, hn[:, kc, 0:W], h_sb[:, kc, 0:W], g_sb[:, kc:kc + 1], rstd[:, 0:W], ALU.mult, ALU.mult)
        else:
            post(kc, eng)


def build_phaseB(NTB, kind):
    kb = KB()
    nc = kb.nc
    NT = NTB * 128
    D = D_MODEL
    dr = lambda n, s, k="ExternalInput": Buf(kb, n, s, F32, "dram", k)
    hT = dr("hT", [KC, 128, NT])
    br = dr("br", [16, 128, NT])
    wgate = dr("wgate", [4, 128, KC, D])
    wbr = dr("wbr", [4, 128, 4, D])
    wo = dr("wo", [128, KC, D])
    g1d = dr("g1", [128, KC])
    g2d = dr("g2", [128, KC])
    if kind == "dense":
        wfg = dr("wfg", [128, KC, D_FF])
        wfu = dr("wfu", [128, KC, D_FF])
        wfd = dr("wfd", [128, NFF, D])
    else:
        wr = dr("wr", [128, KC, N_EXP])
        weg = dr("weg", [N_EXP, 128, KC, D_FFE])
        weu = dr("weu", [N_EXP, 128, KC, D_FFE])
        wed = dr("wed", [N_EXP, 128, NFE, D])
        seld = dr("sel", [N_EXP, N_EXP, 128])
        identd = dr("ident", [128, 128])
    hTo = dr("hTo", [KC, 128, NT], "ExternalOutput")

    sb = lambda n, s, d=F32: Buf(kb, n, s, d)
    h_sb = sb("h_sb", [128, KC, 512])
    hn = sb("hn", [128, KC, 512], BF16)
    merged = sb("merged", [128, KC, 512], BF16)
    sqs = [sb("sq%d" % i, [128, 512], BF16) for i in range(2)]
    rstd = sb("rstd", [128, 512])
    X = nc.alloc_sbuf_tensor("X", [128, 12288], F32)
    macc = ABuf(X[:, 0:8192].rearrange("p (a b) -> p a b", b=512))
    br_sb = ABuf(X[:, 8192:12288].bitcast(BF16).rearrange("p (a b) -> p a b", b=512))
    actb = ABuf(X[:, 0:11264].bitcast(BF16).rearrange("p (a b) -> p a b", b=512))
    NWP = 3
    wps = [sb("wp%d" % i, [128, KC, 256], BF16) for i in range(NWP)]
    wbs = sb("wbs", [128, 4, D], BF16)
    wds = [sb("wd%d" % i, [128, NFF, 128], BF16) for i in range(2)]
    g1 = sb("g1s", [128, KC])
    g2 = sb("g2s", [128, KC])
    ones_bf = sb("ones_bf", [128, 128], BF16)
    sgs = [sb("sg%d" % i, [128, 512]) for i in range(2)]
    tmps = [sb("tmp%d" % i, [128, 512]) for i in range(2)]
    pss = [Buf(kb, "ps%d" % i, [128, 512], F32, "psum") for i in range(8)]
    if kind == "moe":
        stg = [sb("stg%d" % i, [128, 512]) for i in range(2)]
        wr_sb = sb("wr_sb", [128, KC, N_EXP])
        lg = sb("lg", [128, 4, 8])
        top8 = sb("top8", [128, 4, 8])
        comb = sb("comb", [128, 4, 8])
        cw = sb("cw", [128, 4, 8])
        combT = sb("combT", [8, 512], BF16)
        sel = sb("sel_sb", [N_EXP, N_EXP, 128], BF16)
        combB = [sb("combB%d" % i, [128, 512]) for i in range(2)]
        ident = sb("ident_sb", [128, 128])
        kb.dma("sp", wr_sb[:, :, :], wr[:, :, :])
        kb.dma("pool", sel[:, :, :], seld[:, :, :])
        kb.dma("sp", ident[:, :], identd[:, :])

    kb.dma("sp", g1[:, :], g1d[:, :])
    kb.dma("sp", g2[:, :], g2d[:, :])
    kb.memset("dve", ones_bf[:, :], 1.0 / D)

    wpi = [0]

    def next_wp():
        b = wps[wpi[0] % NWP]
        wpi[0] += 1
        return b

    psi = [0]

    def next_ps():
        p = pss[psi[0] % 6]
        psi[0] += 1
        return p

    ps_ss = pss[6]
    ps_misc = pss[7]
    cnt = [0]

    for (t0, W) in token_tiles(NT):
        for kc in range(KC):
            kb.dma("sp", h_sb[:, kc, 0:W], hT[kc, :, t0:t0 + W])
        rmsnorm_fm(kb, h_sb, hn, g1, ones_bf, sqs, ps_ss, rstd, W)
        kb.transfer([actb], [macc, br_sb])
        for i in range(16):
            kb.dma("pool", br_sb[:, i, 0:W], br[i, :, t0:t0 + W])
        for b in range(4):
            kb.dma("pool", wbs[:, :, :], wbr[b, :, :, :])
            for pc in range(D // 256):
                wp = next_wp()
                kb.dma("pool", wp[:, :, :], wgate[b, :, :, pc * 256:(pc + 1) * 256])
                for mm_ in range(2):
                    m = pc * 2 + mm_
                    pg = next_ps()
                    for kc in range(KC):
                        kb.mm(pg[:, 0:W], wp[:, kc, mm_ * 128:(mm_ + 1) * 128], hn[:, kc, 0:W],
                              start=(kc == 0), stop=(kc == KC - 1))
                    pb = next_ps()
                    for hh in range(4):
                        kb.mm(pb[:, 0:W], wbs[:, hh, m * 128:(m + 1) * 128], br_sb[:, b * 4 + hh, 0:W],
                              start=(hh == 0), stop=(hh == 3))
                    sg = sgs[cnt[0] % 2]
                    tmp = tmps[cnt[0] % 2]
                    cnt[0] += 1
                    kb.act(sg[:, 0:W], pg[:, 0:W], AF.Sigmoid)
                    if b == 0:
                        kb.tt("dve", macc[:, m, 0:W], sg[:, 0:W], pb[:, 0:W], ALU.mult)
                    elif b < 3:
                        kb.tt("dve", tmp[:, 0:W], sg[:, 0:W], pb[:, 0:W], ALU.mult)
                        kb.tt("pool", macc[:, m, 0:W], macc[:, m, 0:W], tmp[:, 0:W], ALU.add)
                    else:
                        kb.tt("dve", tmp[:, 0:W], sg[:, 0:W], pb[:, 0:W], ALU.mult)
                        kb.tt("pool", merged[:, m, 0:W], macc[:, m, 0:W], tmp[:, 0:W], ALU.add)
        for pc in range(D // 256):
            wp = next_wp()
            kb.dma("pool", wp[:, :, :], wo[:, :, pc * 256:(pc + 1) * 256])
            for mm_ in range(2):
                m = pc * 2 + mm_
                po = next_ps()
                for kc in range(KC):
                    kb.mm(po[:, 0:W], wp[:, kc, mm_ * 128:(mm_ + 1) * 128], merged[:, kc, 0:W],
                          start=(kc == 0), stop=(kc == KC - 1))
                kb.tt("dve", h_sb[:, m, 0:W], h_sb[:, m, 0:W], po[:, 0:W], ALU.add)
        kb.transfer([macc, br_sb], [actb])
        if kind == "dense":
            rmsnorm_fm(kb, h_sb, hn, g2, ones_bf, sqs, ps_ss, rstd, W)
            for pc in range(D_FF // 256):
                wpg = next_wp()
                kb.dma("pool", wpg[:, :, :], wfg[:, :, pc * 256:(pc + 1) * 256])
                wpu = next_wp()
                kb.dma("pool", wpu[:, :, :], wfu[:, :, pc * 256:(pc + 1) * 256])
                for mm_ in range(2):
                    fc = pc * 2 + mm_
                    pg = next_ps()
                    for kc in range(KC):
                        kb.mm(pg[:, 0:W], wpg[:, kc, mm_ * 128:(mm_ + 1) * 128], hn[:, kc, 0:W],
                              start=(kc == 0), stop=(kc == KC - 1))
                    pu = next_ps()
                    for kc in range(KC):
                        kb.mm(pu[:, 0:W], wpu[:, kc, mm_ * 128:(mm_ + 1) * 128], hn[:, kc, 0:W],
                              start=(kc == 0), stop=(kc == KC - 1))
                    sg = sgs[cnt[0] % 2]
                    cnt[0] += 1
                    kb.act(sg[:, 0:W], pg[:, 0:W], AF.Silu)
                    kb.tt("dve", actb[:, fc, 0:W], sg[:, 0:W], pu[:, 0:W], ALU.mult)
            for m in range(KC):
                wd = wds[m % 2]
                kb.dma("pool", wd[:, :, :], wfd[:, :, m * 128:(m + 1) * 128])
                po = next_ps()
                for fc in range(NFF):
                    kb.mm(po[:, 0:W], wd[:, fc, :], actb[:, fc, 0:W], start=(fc == 0), stop=(fc == NFF - 1))
                kb.tt("dve", h_sb[:, m, 0:W], h_sb[:, m, 0:W], po[:, 0:W], ALU.add)
        else:
            nb = W // 128
            def post(kc, eng):
                st = stg[kc % 2]
                kb.stt("dve", st[:, 0:W], h_sb[:, kc, 0:W], g2[:, kc:kc + 1], rstd[:, 0:W], ALU.mult, ALU.mult)
                kb.copy("act", hn[:, kc, 0:W], st[:, 0:W])
                for tb in range(nb):
                    kb.mm(pss[tb][:, 0:8], st[:, tb * 128:(tb + 1) * 128], wr_sb[:, kc, :],
                          start=(kc == 0), stop=(kc == KC - 1))
            rmsnorm_fm(kb, h_sb, hn, g2, ones_bf, sqs, ps_ss, rstd, W, post=post)
            for tb in range(nb):
                kb.copy("dve", lg[:, tb, :], pss[tb][:, 0:8])
            for tb in range(nb):
                kb.op("dve", lambda tb=tb: nc.vector.max(out=top8.t[:, tb, :], in_=lg.t[:, tb, :]),
                      [top8[:, tb, :]], [lg[:, tb, :]])
            kb.tt("dve", cw[:, 0:nb, 0:1], top8[:, 0:nb, 0:1], top8[:, 0:nb, 1:2], ALU.subtract)
            kb.act(cw[:, 0:nb, 0:1], cw[:, 0:nb, 0:1], AF.Exp)
            kb.ts("dve", cw[:, 0:nb, 0:1], cw[:, 0:nb, 0:1], 1.0, None, op0=ALU.add)
            kb.op("dve", lambda: nc.vector.reciprocal(out=cw.t[:, 0:nb, 1:2], in_=cw.t[:, 0:nb, 0:1]),
                  [cw[:, 0:nb, 1:2]], [cw[:, 0:nb, 0:1]])
            kb.ts("dve", cw[:, 0:nb, 0:1], cw[:, 0:nb, 1:2], -1.0, 1.0, op0=ALU.mult, op1=ALU.add)
            for tb in range(nb):
                kb.ts("dve", comb[:, tb, :], lg[:, tb, :], top8[:, tb, 0:1], cw[:, tb, 0:1],
                      op0=ALU.is_equal, op1=ALU.mult)
                kb.ts("dve", lg[:, tb, :], lg[:, tb, :], top8[:, tb, 1:2], cw[:, tb, 1:2],
                      op0=ALU.is_equal, op1=ALU.mult)
                kb.tt("dve", comb[:, tb, :], comb[:, tb, :], lg[:, tb, :], ALU.add)
            pT = next_ps()
            for tb in range(nb):
                kb.mm(pT[0:8, tb * 128:(tb + 1) * 128], comb[:, tb, :], ident[:, :], start=True, stop=True)
            kb.copy("dve", combT[:, 0:W], pT[0:8, 0:W])
            for half in range(2):
                for el in range(4):
                    e = half * 4 + el
                    pcb = next_ps()
                    kb.mm(pcb[:, 0:W], sel[:, e, :], combT[:, 0:W], start=True, stop=True)
                    cb = combB[e % 2]
                    kb.copy("act", cb[:, 0:W], pcb[:, 0:W])
                    for pc in range(6):
                        c0 = pc * 256
                        cw_ = min(256, D_FFE - c0)
                        wpg = next_wp()
                        kb.dma("pool", wpg[:, :, 0:cw_], weg[e, :, :, c0:c0 + cw_])
                        wpu = next_wp()
                        kb.dma("pool", wpu[:, :, 0:cw_], weu[e, :, :, c0:c0 + cw_])
                        for mm_ in range(cw_ // 128):
                            fc = pc * 2 + mm_
                            pg = next_ps()
                            for kc in range(KC):
                                kb.mm(pg[:, 0:W], wpg[:, kc, mm_ * 128:(mm_ + 1) * 128], hn[:, kc, 0:W],
                                      start=(kc == 0), stop=(kc == KC - 1))
                            pu = next_ps()
                            for kc in range(KC):
                                kb.mm(pu[:, 0:W], wpu[:, kc, mm_ * 128:(mm_ + 1) * 128], hn[:, kc, 0:W],
                                      start=(kc == 0), stop=(kc == KC - 1))
                            sg = sgs[cnt[0] % 2]
                            tmp = tmps[cnt[0] % 2]
                            cnt[0] += 1
                            kb.act(sg[:, 0:W], pg[:, 0:W], AF.Silu)
                            kb.tt("dve", tmp[:, 0:W], sg[:, 0:W], pu[:, 0:W], ALU.mult)
                            kb.tt("pool", actb[:, el * NFE + fc, 0:W], tmp[:, 0:W], cb[:, 0:W], ALU.mult)
                for m in range(KC):
                    wd = wds[m % 2]
                    for el in range(4):
                        kb.dma("pool", wd[:, el * NFE:(el + 1) * NFE, :], wed[half * 4 + el, :, :, m * 128:(m + 1) * 128])
                    po = next_ps()
                    for fc in range(NFF):
                        kb.mm(po[:, 0:W], wd[:, fc, :], actb[:, fc, 0:W], start=(fc == 0), stop=(fc == NFF - 1))
                    kb.tt("dve", h_sb[:, m, 0:W], h_sb[:, m, 0:W], po[:, 0:W], ALU.add)
        for kc in range(KC):
            kb.dma("sp", hTo[kc, :, t0:t0 + W], h_sb[:, kc, 0:W], final=True)
    kb.finish()
    return kb


FRONT = 112
W_SMALL = 8


def build_phaseA(NBLK, mixers=("gdn", "mla", "fox", "ssd")):
    from contextlib import ExitStack
    kb = KB()
    nc = kb.nc
    T = NBLK * 128
    dr = lambda n, s, k="ExternalInput", d=F32: Buf(kb, n, s, d, "dram", k)
    hT = dr("hT", [KC, 128, T])
    g1d = dr("g1", [128, KC])
    w_small_d = dr("w_small", [128, KC, W_SMALL])
    cst_d = dr("cst", [6, 128, 128])
    bo = dr("bo", [5, 128, T], "ExternalOutput")
    hnT = dr("hnT_scr", [KC, 128, T], "Internal", BF16)
    w_gdn_d = dr("w_gdn", [128, KC, 512])
    gdn_conv_d = dr("gdn_conv", [128, 3, 4])
    gdn_vec_d = dr("gdn_vec", [128, 2])
    gdn_ng_d = dr("gdn_ng", [128, 128])
    w_mla_d = dr("w_mla", [128, KC, 832])
    mla_wq_d = dr("mla_wq", [128, 4, 192])
    mla_wkv_d = dr("mla_wkv", [128, 2, 256])
    mla_g_d = dr("mla_g", [128, 10])
    rope_d = dr("rope", [2, 64, T])
    rotm_d = dr("rotm", [64, 64])
    w_fox_d = dr("w_fox", [128, KC, 384])
    fox_g_d = dr("fox_g", [128, 3])
    w_ssd_d = dr("w_ssd", [128, KC, 768])
    ssd_conv_d = dr("ssd_conv", [128, 4, 5])
    ssd_vec_d = dr("ssd_vec", [128, 8])
    ssd_row_d = dr("ssd_row", [2, 128, 256])

    sb = lambda n, s, d=F32, st=None: Buf(kb, n, s, d, stack=st)
    kb.eps_tile(EPS)
    kb.eps_tile(1.0)
    cst = sb("cst_sb", [128, 6, 128])
    kb.dma("sp", cst[:, :, :], bc(cst_d[:, :, :], cst_d.t.rearrange("c p f -> p c f")))
    cstb = sb("cst_bf", [128, 6, 128], BF16)
    kb.copy("dve", cstb[:, :, :], cst[:, :, :])
    ident, Umat, MnegT, strictT, causT, ones = [cst[:, i, :] for i in range(6)]
    ident_b, _, _, _, causT_b, ones_b = [cstb[:, i, :] for i in range(6)]
    g1 = sb("g1s", [128, KC])
    kb.dma("sp", g1[:, :], g1d[:, :])
    small_all = sb("small_all", [128, NBLK, W_SMALL])
    pss = [Buf(kb, "ps%d" % i, [128, 512], F32, "psum") for i in range(8)]
    psi = [0]

    def next_ps(n=8):
        p = pss[psi[0] % n]
        psi[0] += 1
        return p

    tiles = token_tiles(T)

    def sq_sum_rstd(srcs, W, scale, rstd, sqs, ps):
        for i, (v, P) in enumerate(srcs):
            sq = sqs[i % len(sqs)]
            kb.act(sq[0:P, 0:W], v, AF.Square)
            kb.mm(ps[:, 0:W], bc(ones_b, ones_b.ap[0:P, :]), sq[0:P, 0:W], start=(i == 0), stop=(i == len(srcs) - 1))
        kb.act(rstd[:, 0:W], ps[:, 0:W], AF.Sqrt, bias=kb.eps_tile(EPS), scale=scale)
        kb.recip(rstd[:, 0:W], rstd[:, 0:W])

    with ExitStack() as st:
        h_sb = sb("h_sb", [128, KC, 512], F32, st)
        hn = sb("hn0", [128, KC, 512], BF16, st)
        sqs = [sb("sq%d" % i, [128, 512], BF16, st) for i in range(2)]
        rstd = sb("rstd", [128, 512], F32, st)
        wsm = sb("wsm", [128, KC, W_SMALL], BF16, st)
        kb.dma("pool", wsm[:, :, :], w_small_d[:, :, :])
        import os
        STOP = int(os.environ.get("A0_STOP", "9"))
        for (t0, W) in tiles:
            if STOP < 1:
                break
            kb.dma("sp", h_sb[:, :, 0:W], bc(hT[:, :, t0:t0 + W], hT.t[:, :, t0:t0 + W].rearrange("k p w -> p k w")))
            if STOP < 2:
                continue
            sq_sum_rstd([(h_sb[:, kc, 0:W], 128) for kc in range(KC)], W, 1.0 / D_MODEL, rstd, sqs, pss[7])
            if STOP < 3:
                continue
            for kc in range(KC):
                kb.stt("dve", hn[:, kc, 0:W], h_sb[:, kc, 0:W], g1[:, kc:kc + 1], rstd[:, 0:W], ALU.mult, ALU.mult)
            if STOP < 4:
                continue
            for kc in range(KC):
                kb.dma("sp", hnT[kc, :, t0:t0 + W], hn[:, kc, 0:W])
            if STOP < 5:
                continue
            for tb in range(W // 128):
                blk = t0 // 128 + tb
                ps = next_ps(4)
                for kc in range(KC):
                    kb.mm(ps[:, 0:W_SMALL], hn[:, kc, tb * 128:(tb + 1) * 128], wsm[:, kc, :],
                          start=(kc == 0), stop=(kc == KC - 1))
                kb.copy("act", small_all[:, blk, :], ps[:, 0:W_SMALL])
    kb.barrier()

    def load_hn(hn_t, t0, W):
        for kc in range(KC):
            kb.dma("sp", hn_t[:, kc, 0:W], hnT[kc, :, t0:t0 + W])

    def proj_fm(ps, w, c0, ncols, hn_t, W):
        for kc in range(KC):
            kb.mm(ps[0:ncols, 0:W], w[:, kc, c0:c0 + ncols], hn_t[:, kc, 0:W], start=(kc == 0), stop=(kc == KC - 1))

    def proj_tm(psv, hn_t, tb, w, c0, ncols):
        for kc in range(KC):
            kb.mm(psv, hn_t[:, kc, tb * 128:(tb + 1) * 128], w[:, kc, c0:c0 + ncols], start=(kc == 0), stop=(kc == KC - 1))

    def softplus(out, in_, bias):
        kb.act(out, in_, AF.Exp, bias=bias)
        kb.act(out, out, AF.Ln, bias=kb.eps_tile(1.0))

    def conv4(acc, pre, wv, W):
        kb.ts("dve", acc[:, 0:W], pre[:, 3:3 + W], wv[:, 3:4], None, op0=ALU.mult)
        for i in (2, 1, 0):
            kb.stt("dve", acc[:, 0:W], pre[:, i:i + W], wv[:, i:i + 1], acc[:, 0:W], ALU.mult, ALU.add)

    def decay_prep(st, g_all, name):
        d = {}
        for nm in ("gcs", "ngcs", "eg", "e2e", "cd"):
            d[nm] = sb(name + nm, [128, NBLK], F32, st)
        ps = next_ps()
        kb.mm(ps[:, 0:NBLK], Umat, g_all[:, :], start=True, stop=True)
        kb.copy("dve", d["gcs"][:, :], ps[:, 0:NBLK])
        kb.ts("dve", d["ngcs"][:, :], d["gcs"][:, :], -1.0, None, op0=ALU.mult)
        kb.act(d["eg"][:, :], d["gcs"][:, :], AF.Exp)
        ps2 = next_ps()
        kb.mm(ps2[:, 0:NBLK], ones, g_all[:, :], start=True, stop=True)
        kb.act(d["cd"][:, :], ps2[:, 0:NBLK], AF.Exp)
        kb.tt("dve", d["e2e"][:, :], ps2[:, 0:NBLK], d["gcs"][:, :], ALU.subtract)
        kb.act(d["e2e"][:, :], d["e2e"][:, :], AF.Exp)
        return d

    def decay_mats(g_all, dec, n, grep, DmT, EGrow):
        kb.ts("dve", grep[:, :], ones, g_all[:, n:n + 1], None, op0=ALU.mult)
        ps = next_ps()
        kb.mm(ps[:, 0:128], grep[:, :], Umat, start=True, stop=True)
        kb.mm(ps[:, 128:256], grep[:, :], Umat, start=True, stop=False)
        kb.mm(ps[:, 128:256], ident, MnegT, start=False, stop=True)
        if EGrow is not None:
            kb.act(EGrow[:, :], ps[:, 0:128], AF.Exp)
        kb.act(DmT[:, :], ps[:, 128:256], AF.Exp, bias=dec["ngcs"][:, n:n + 1])

    def emit_out(slot, src_tm, ncol_chunks, blk, stage, stage_i, psl=None):
        for c in range(ncol_chunks):
            if psl is None:
                ps = next_ps()
            else:
                ps = psl[stage_i[0] % len(psl)]
            kb.mm(ps[:, 0:128], src_tm[:, c * 128:(c + 1) * 128], ident, start=True, stop=True)
            so = stage[stage_i[0] % len(stage)]
            stage_i[0] += 1
            kb.copy("act", so[:, :], ps[:, 0:128])
            kb.dma("sp", bo[slot + c, :, blk * 128:(blk + 1) * 128], so[:, :], final=True)

    try:
      if "gdn" in mixers:
          with ExitStack() as st:
              w = sb("w_gdn_s", [128, KC, 512], BF16, st)
              kb.dma("pool", w[:, :, :], w_gdn_d[:, :, :])
              convw = sb("gdn_convw", [128, 3, 4], F32, st)
              kb.dma("sp", convw[:, :, :], gdn_conv_d[:, :, :])
              gvec = sb("gdn_vec_s", [128, 2], F32, st)
              kb.dma("sp", gvec[:, :], gdn_vec_d[:, :])
              ngt = sb("gdn_ng_s", [128, 128], F32, st)
              kb.dma("sp", ngt[:, :], gdn_ng_d[:, :])
              hns = [sb("ghn%d" % i, [128, KC, 512], BF16, st) for i in range(2)]
              pre = sb("gpre", [128, 3, 515], F32, st)
              acc = [sb("gacc%d" % i, [128, 512], F32, st) for i in range(2)]
              sqs = [sb("gsq%d" % i, [128, 512], BF16, st) for i in range(2)]
              rstd = sb("grstd", [128, 512], F32, st)
              qT = sb("g_qT", [128, T], BF16, st)
              kT = sb("g_kT", [128, T], BF16, st)
              vT = sb("g_vT", [128, T], BF16, st)
              zg = sb("g_zg", [128, NBLK, 128], BF16, st)
              kb.memset("dve", pre[:, :, 0:3], 0.0)
              for ti, (t0, W) in enumerate(tiles):
                  hn_t = hns[ti % 2]
                  load_hn(hn_t, t0, W)
                  for c, dst in enumerate((qT, kT, vT)):
                      ps = next_ps()
                      proj_fm(ps, w, c * 128, 128, hn_t, W)
                      kb.copy("act", pre[:, c, 3:3 + W], ps[:, 0:W])
                      a = acc[c % 2]
                      conv4(a, bc(pre[:, c, :], pre.t[:, c, :]), bc(convw[:, c, :], convw.t[:, c, :]), W)
                      kb.copy("pool", pre[:, c, 0:3], pre[:, c, W:W + 3])
                      if c < 2:
                          kb.act(a[:, 0:W], a[:, 0:W], AF.Silu)
                          sq_sum_rstd([(a[:, 0:W], 128)], W, 1.0, rstd, sqs, pss[7])
                          kb.stt("dve", dst[:, t0:t0 + W], a[:, 0:W], (128.0 ** -0.5) if c == 0 else 1.0, rstd[:, 0:W],
                                 ALU.mult, ALU.mult)
                      else:
                          kb.act(dst[:, t0:t0 + W], a[:, 0:W], AF.Silu)
                  for tb in range(W // 128):
                      blk = t0 // 128 + tb
                      ps = next_ps()
                      proj_tm(ps[:, 0:128], hn_t, tb, w, 384, 128)
                      a = acc[tb % 2]
                      kb.act(a[:, 0:128], ps[:, 0:128], AF.Silu)
                      kb.tt("pool", zg[:, blk, :], a[:, 0:128], ngt[:, :], ALU.mult)
              kb.ck("gdn prep done")
              beta = sb("g_beta", [128, NBLK], F32, st)
              nbeta = sb("g_nbeta", [128, NBLK], F32, st)
              g_all = sb("g_gall", [128, NBLK], F32, st)
              expA = sb("g_expA", [128, 1], F32, st)
              kb.act(beta[:, :], bc(small_all[:, :, 0], small_all.t[:, :, 0]), AF.Sigmoid)
              kb.ts("dve", nbeta[:, :], beta[:, :], -1.0, None, op0=ALU.mult)
              kb.act(expA[:, :], gvec[:, 0:1], AF.Exp)
              kb.ts("dve", expA[:, :], expA[:, :], -1.0, None, op0=ALU.mult)
              softplus(g_all[:, :], bc(small_all[:, :, 1], small_all.t[:, :, 1]), gvec[:, 1:2])
              kb.ts("dve", g_all[:, :], g_all[:, :], expA[:, 0:1], None, op0=ALU.mult)
              kb.ck("gdn scalars")
              dec = decay_prep(st, g_all, "gd_")
              kb.ck("gdn decay_prep")
              NB2 = 2
              mk = lambda n, d=F32, shp=(128, 128): [sb("%s%d" % (n, i), list(shp), d, st) for i in range(NB2)]
              grep_, DmT_, EG_ = mk("g_grep"), mk("g_DmT"), mk("g_EG")
              t1_, Q_, P_, R_ = mk("g_t1"), [mk("g_Q%d" % k) for k in range(2)], [mk("g_P%d" % k) for k in range(2)], [mk("g_R%d" % k) for k in range(2)]
              attnT_, KeT_, QeT_ = mk("g_attnT", BF16), mk("g_KeT", BF16), mk("g_QeT", BF16)
              k2e_, Vt_ = mk("g_k2e", BF16), mk("g_Vt")
              R1_, vnew_ = mk("g_resid"), mk("g_vnew", BF16)
              o_, junk_ = mk("g_o"), mk("g_junk")
              ss_ = mk("g_ss", F32, (128, 1))
              S = sb("g_S", [128, 128], F32, st)
              S_bf = sb("g_Sbf", [128, 128], BF16, st)
              kb.memset("dve", S[:, :], 0.0)
              kb.memset("dve", S_bf[:, :], 0.0)
              stage = [sb("g_stage%d" % i, [128, 128], F32, st) for i in range(2)]
              stage_i = [0]
              for n in range(NBLK):
                  i2 = n % NB2
                  cs = slice(n * 128, (n + 1) * 128)
                  grep, DmT, EG = grep_[i2], DmT_[i2], EG_[i2]
                  decay_mats(g_all, dec, n, grep, DmT, EG)
                  kb.ck("decay_mats")
                  psk = next_ps()
                  kb.mm(psk[:, 0:128], kT[:, cs], kT[:, cs], start=True, stop=True)
                  kb.mm(psk[:, 128:256], kT[:, cs], qT[:, cs], start=True, stop=True)
                  t1 = t1_[i2]
                  kb.tt("dve", t1[:, :], DmT[:, :], psk[:, 0:128], ALU.mult)
                  Q0 = Q_[0][i2]
                  kb.stt("dve", Q0[:, :], t1[:, :], nbeta[:, n:n + 1], strictT, ALU.mult, ALU.mult)
                  attnT = attnT_[i2]
                  kb.tt("dve", attnT[:, :], DmT[:, :], psk[:, 128:256], ALU.mult)
                  kb.ck("t1/Q0/attnT")
                  pst = next_ps()
                  kb.mm(pst[:, 0:128], Q0[:, :], ident, start=True, stop=True)
                  P0 = P_[0][i2]
                  kb.copy("act", P0[:, :], pst[:, 0:128])
                  kb.ck("P0")
                  Rc = R_[0][i2]
                  kb.tt("pool", Rc[:, :], Q0[:, :], ident, ALU.add)
                  kb.ck("R0")
                  Qc, Pc = Q0, P0
                  for k in range(1, 7):
                      psq = next_ps()
                      Pn = P_[k % 2][i2]
                      kb.mm(psq[:, 0:128], Qc[:, :], Pc[:, :], start=True, stop=True)
                      if k < 6:
                          kb.mm(psq[:, 128:256], Pc[:, :], Qc[:, :], start=True, stop=True)
                      kb.copy("act", Pn[:, :], psq[:, 0:128])
                      if k < 6:
                          Qn = Q_[k % 2][i2]
                          kb.copy("dve", Qn[:, :], psq[:, 128:256])
                      kb.ck("PQ k=%d" % k)
                      psr = next_ps()
                      kb.mm(psr[:, 0:128], Pn[:, :], Rc[:, :], start=True, stop=True)
                      Rn = R_[k % 2][i2]
                      kb.tt("dve", Rn[:, :], Rc[:, :], psr[:, 0:128], ALU.add)
                      kb.ck("R k=%d" % k)
                      Rc, Pc = Rn, Pn
                      if k < 6:
                          Qc = Qn
                  kb.ck("inverse")
                  KeT, QeT = KeT_[i2], QeT_[i2]
                  kb.tt("pool", KeT[:, :], kT[:, cs], EG[:, :], ALU.mult)
                  kb.tt("pool", QeT[:, :], qT[:, cs], EG[:, :], ALU.mult)
                  pstk = next_ps()
                  kb.mm(pstk[:, 0:128], kT[:, cs], ident_b, start=True, stop=True)
                  kb.mm(pstk[:, 128:256], vT[:, cs], ident_b, start=True, stop=True)
                  k2e, Vt = k2e_[i2], Vt_[i2]
                  kb.ts("dve", k2e[:, :], pstk[:, 0:128], dec["e2e"][:, n:n + 1], None, op0=ALU.mult)
                  kb.copy("act", Vt[:, :], pstk[:, 128:256])
                  kb.ck("pre-seq")
                  psa = next_ps()
                  kb.mm(psa[:, 0:128], KeT[:, :], S_bf[:, :], start=True, stop=True)
                  R1 = R1_[i2]
                  kb.tt("dve", R1[:, :], Vt[:, :], psa[:, 0:128], ALU.subtract)
                  kb.mm(psa[:, 128:256], Rc[:, :], R1[:, :], start=True, stop=True)
                  vnew = vnew_[i2]
                  kb.ts("dve", vnew[:, :], psa[:, 128:256], beta[:, n:n + 1], None, op0=ALU.mult)
                  pso = next_ps()
                  kb.mm(pso[:, 0:128], QeT[:, :], S_bf[:, :], start=True, stop=False)
                  kb.mm(pso[:, 0:128], attnT[:, :], vnew[:, :], start=False, stop=True)
                  kb.mm(pso[:, 128:256], k2e[:, :], vnew[:, :], start=True, stop=True)
                  kb.stt("dve", S[:, :], S[:, :], dec["cd"][:, n:n + 1], pso[:, 128:256], ALU.mult, ALU.add)
                  kb.copy("act", S_bf[:, :], S[:, :])
                  kb.ck("seq")
                  ss, junk, o = ss_[i2], junk_[i2], o_[i2]
                  kb.memset("pool", ss[:, :], 0.0)
                  kb.act(junk[:, :], pso[:, 0:128], AF.Square, accum=ss[:, :])
                  kb.ck("accum")
                  kb.act(ss[:, :], ss[:, :], AF.Sqrt, bias=kb.eps_tile(EPS), scale=1.0 / 128)
                  kb.recip(ss[:, :], ss[:, :])
                  kb.ck("rstd")
                  kb.stt("dve", o[:, :], pso[:, 0:128], ss[:, 0:1], zg[:, n, :], ALU.mult, ALU.mult)
                  kb.ck("o")
                  emit_out(0, o, 1, n, stage, stage_i)
          kb.barrier()
    except StopBuild:
        pass

    def attention(st, slot, q_parts, k_parts, Vaug, tab, name):
        G = 4
        pts = [sb("%s_pt%d" % (name, i), [128, 512], BF16, st) for i in range(3)]
        ot = [sb("%s_ot%d" % (name, i), [128, 128], F32, st) for i in range(2)]
        rc = [sb("%s_rc%d" % (name, i), [128, 1], F32, st) for i in range(2)]
        stage = [sb("%s_stage%d" % (name, i), [128, 128], F32, st) for i in range(2)]
        stage_i = [0]
        pti = [0]
        oi = [0]
        ps_s = [pss[0], pss[1]]
        ps_o4 = [pss[2], pss[3], pss[4], pss[5]]
        si = [0]
        for gi, i0 in enumerate(range(0, NBLK, G)):
            i1 = min(i0 + G, NBLK) - 1
            for j in range(0, i1 + 1):
                is_ = max(i0, j)
                Wq = (i1 - is_ + 1) * 128
                q0 = is_ * 128
                ps = ps_s[si[0] % 2]
                si[0] += 1
                for pi, ((qb, P), (kbuf, _)) in enumerate(zip(q_parts, k_parts)):
                    kb.mm(ps[:, 0:Wq], kbuf[0:P, j * 128:(j + 1) * 128], qb[0:P, q0:q0 + Wq],
                          start=(pi == 0), stop=(pi == len(q_parts) - 1))
                pt = pts[pti[0] % 3]
                pti[0] += 1
                if tab is None and not os.environ.get("NARROW"):
                    kb.act(pt[:, 0:Wq], ps[:, 0:Wq], AF.Exp)
                elif tab is None:
                    for ii in range(is_, i1 + 1):
                        c = (ii - is_) * 128
                        kb.act(pt[:, c:c + 128], ps[:, c:c + 128], AF.Exp)
                else:
                    for ii in range(is_, i1 + 1):
                        c = (ii - is_) * 128
                        kb.act(pt[:, c:c + 128], ps[:, c:c + 128], AF.Exp, bias=tab[:, ii, j:j + 1])
                if j >= i0:
                    kb.tt("pool", pt[:, 0:128], pt[:, 0:128], causT_b, ALU.mult)
                for ii in range(is_, i1 + 1):
                    c = (ii - is_) * 128
                    li = ii - i0
                    dst = ps_o4[li]
                    kb.mm(dst[:, 0:129], pt[:, c:c + 128],
                          bc(Vaug[:, j, :], Vaug.t[:, j, :]), start=(j == 0), stop=(j == ii))
            for ii in range(i0, i1 + 1):
                li = ii - i0
                src = ps_o4[li]
                c0 = 0
                r = rc[oi[0] % 2]
                o = ot[oi[0] % 2]
                oi[0] += 1
                kb.ts("dve", r[:, :], src[:, c0 + 128:c0 + 129], 1e-30, None, op0=ALU.max)
                kb.recip(r[:, :], r[:, :])
                kb.ts("dve", o[:, :], src[:, c0:c0 + 128], r[:, 0:1], None, op0=ALU.mult)
                emit_out(slot, o, 1, ii, stage, stage_i, psl=[pss[6], pss[7]])

    def make_vaug(st, name):
        Vaug = sb(name, [128, NBLK, 129], BF16, st)
        kb.memset("dve", bc(Vaug[:, :, 128:129], Vaug.t[:, :, 128:129]), 1.0)
        return Vaug

    def finish_vaug(Vaug):
        for p0, p1 in ((0, 32), (32, 64), (64, 96), (96, FRONT)):
            kb.memset("dve", bc(Vaug[:, 0, :], Vaug.t[p0:p1, 0, :]), 0.0)

    if "mla" in mixers:
        with ExitStack() as st:
            w = sb("w_mla_s", [128, KC, 832], BF16, st)
            kb.dma("pool", w[:, :, :], w_mla_d[:, :, :])
            wq = sb("mla_wq_s", [128, 4, 192], BF16, st)
            kb.dma("pool", wq[:, :, :], mla_wq_d[:, :, :])
            wkv = sb("mla_wkv_s", [128, 2, 256], BF16, st)
            kb.dma("pool", wkv[:, :, :], mla_wkv_d[:, :, :])
            mg = sb("mla_g_s", [128, 10], F32, st)
            kb.dma("sp", mg[:, :], mla_g_d[:, :])
            mgq = sb("mla_gq_s", [128, 2], F32, st)
            kb.ts("dve", mgq[:, :], mg[:, 6:8], 192.0 ** -0.5, None, op0=ALU.mult)
            ropes = [sb("rope_s%d" % i, [64, 2, 512], F32, st) for i in range(2)]
            rotm = sb("rotm_s", [64, 64], F32, st)
            kb.dma("sp", rotm[:, :], rotm_d[:, :])
            hns = [sb("mhn%d" % i, [128, KC, 512], BF16, st) for i in range(1)]
            lat = sb("m_lat", [128, 6, 512], F32, st)
            latn = sb("m_latn", [128, 6, 512], BF16, st)
            sqs = [sb("msq%d" % i, [128, 512], BF16, st) for i in range(2)]
            rstd = sb("mrstd", [128, 512], F32, st)
            raw = sb("m_raw", [128, 2, 512], F32, st)
            tr_ = sb("m_tr", [64, 512], F32, st)
            ta_ = sb("m_ta", [64, 512], F32, st)
            tb_ = sb("m_tb", [64, 512], F32, st)
            QTn = sb("m_QTn", [128, T], BF16, st)
            QTr = sb("m_QTr", [128, T], BF16, st)
            kb.memset("pool", QTr[64:128, :], 0.0)
            KTn = sb("m_KTn", [128, T], BF16, st)
            KTr = sb("m_KTr", [128, T], BF16, st)
            kb.memset("pool", KTr[64:128, :], 0.0)
            Vaug = make_vaug(st, "m_Vaug")

            def qk_finish(raw, gn, gr, dn, dr_, t0, W, rope):
                sq_sum_rstd([(raw[:, 0, 0:W], 128), (raw[0:64, 1, 0:W], 64)], W, 1.0 / 192, rstd, sqs, pss[7])
                kb.stt("dve", dn[:, t0:t0 + W], raw[:, 0, 0:W], gn, rstd[:, 0:W], ALU.mult, ALU.mult)
                kb.stt("dve", tr_[:, 0:W], raw[0:64, 1, 0:W], gr, rstd[0:64, 0:W], ALU.mult, ALU.mult)
                ps = next_ps(7)
                for c0 in range(0, W, 128):
                    kb.mm(ps[0:64, c0:c0 + 128], rotm[:, :], tr_[:, c0:c0 + 128], start=True, stop=True)
                kb.tt("dve", ta_[:, 0:W], tr_[:, 0:W], rope[:, 0, 0:W], ALU.mult)
                kb.tt("dve", tb_[:, 0:W], ps[0:64, 0:W], rope[:, 1, 0:W], ALU.mult)
                kb.tt("pool", dr_[0:64, t0:t0 + W], ta_[:, 0:W], tb_[:, 0:W], ALU.add)

            for ti, (t0, W) in enumerate(tiles):
                hn_t = hns[0]
                load_hn(hn_t, t0, W)
                rope = ropes[ti % 2]
                kb.dma("sp", rope[:, :, 0:W], bc(rope_d[:, :, t0:t0 + W], rope_d.t[:, :, t0:t0 + W].rearrange("c p t -> p c t")))
                for c in range(6):
                    ps = next_ps(7)
                    proj_fm(ps, w, c * 128, 128, hn_t, W)
                    kb.copy("act", lat[:, c, 0:W], ps[:, 0:W])
                sq_sum_rstd([(lat[:, c, 0:W], 128) for c in range(4)], W, 1.0 / 512, rstd, sqs, pss[7])
                for c in range(4):
                    kb.stt("dve", latn[:, c, 0:W], lat[:, c, 0:W], mg[:, c:c + 1], rstd[:, 0:W], ALU.mult, ALU.mult)
                sq_sum_rstd([(lat[:, c, 0:W], 128) for c in (4, 5)], W, 1.0 / 256, rstd, sqs, pss[7])
                for c in (4, 5):
                    kb.stt("dve", latn[:, c, 0:W], lat[:, c, 0:W], mg[:, c:c + 1], rstd[:, 0:W], ALU.mult, ALU.mult)
                ps = next_ps(7)
                for c in range(4):
                    kb.mm(ps[:, 0:W], wq[:, c, 0:128], latn[:, c, 0:W], start=(c == 0), stop=(c == 3))
                kb.copy("act", raw[:, 0, 0:W], ps[:, 0:W])
                ps = next_ps(7)
                for c in range(4):
                    kb.mm(ps[0:64, 0:W], wq[:, c, 128:192], latn[:, c, 0:W], start=(c == 0), stop=(c == 3))
                kb.copy("act", raw[0:64, 1, 0:W], ps[0:64, 0:W])
                qk_finish(raw, mgq[:, 0:1], mgq[0:64, 1:2], QTn, QTr, t0, W, rope)
                ps = next_ps(7)
                for c in range(2):
                    kb.mm(ps[:, 0:W], wkv[:, c, 0:128], latn[:, 4 + c, 0:W], start=(c == 0), stop=(c == 1))
                kb.copy("act", raw[:, 0, 0:W], ps[:, 0:W])
                ps = next_ps(7)
                proj_fm(ps, w, 768, 64, hn_t, W)
                kb.copy("act", raw[0:64, 1, 0:W], ps[0:64, 0:W])
                qk_finish(raw, mg[:, 8:9], mg[0:64, 9:10], KTn, KTr, t0, W, rope)
                for tb in range(W // 128):
                    blk = t0 // 128 + tb
                    ps = next_ps(7)
                    for c in range(2):
                        kb.mm(ps[:, 0:128], latn[:, 4 + c, tb * 128:(tb + 1) * 128], wkv[:, c, 128:256],
                              start=(c == 0), stop=(c == 1))
                    kb.copy("act", Vaug[:, blk, 0:128], ps[:, 0:128])
            finish_vaug(Vaug)
            if os.environ.get("DUMPQ"):
                kb.dma("pool", bo[3, :, :], QTn[:, :], final=True)
                kb.dma("pool", bo[4, :, :], QTr[:, :], final=True)
            attention(st, 1, [(QTn, 128), (QTr, 128)], [(KTn, 128), (KTr, 128)], Vaug, None, "ma")
        kb.barrier()

    if "fox" in mixers:
        with ExitStack() as st:
            w = sb("w_fox_s", [128, KC, 384], BF16, st)
            kb.dma("pool", w[:, :, :], w_fox_d[:, :, :])
            fg = sb("fox_g_s", [128, 3], F32, st)
            kb.dma("sp", fg[:, :], fox_g_d[:, :])
            fgq = sb("fox_gq", [128, 1], F32, st)
            kb.ts("dve", fgq[:, :], fg[:, 0:1], 128.0 ** -0.5, None, op0=ALU.mult)
            nbf = sb("fox_nbf", [128, 1], F32, st)
            kb.ts("dve", nbf[:, :], fg[:, 2:3], -1.0, None, op0=ALU.mult)
            hns = [sb("fhn%d" % i, [128, KC, 512], BF16, st) for i in range(2)]
            raws = [sb("f_raw%d" % i, [128, 512], F32, st) for i in range(2)]
            sqs = [sb("fsq%d" % i, [128, 512], BF16, st) for i in range(2)]
            rstd = sb("frstd", [128, 512], F32, st)
            QT = sb("f_QT", [128, T], BF16, st)
            KT = sb("f_KT", [128, T], BF16, st)
            Vaug = make_vaug(st, "f_Vaug")
            for ti, (t0, W) in enumerate(tiles):
                hn_t = hns[ti % 2]
                load_hn(hn_t, t0, W)
                for c, (dst, gv) in enumerate(((QT, fgq[:, 0:1]), (KT, fg[:, 1:2]))):
                    ps = next_ps(7)
                    proj_fm(ps, w, c * 128, 128, hn_t, W)
                    raw = raws[c]
                    kb.copy("act", raw[:, 0:W], ps[:, 0:W])
                    sq_sum_rstd([(raw[:, 0:W], 128)], W, 1.0 / 128, rstd, sqs, pss[7])
                    kb.stt("dve", dst[:, t0:t0 + W], raw[:, 0:W], gv, rstd[:, 0:W], ALU.mult, ALU.mult)
                for tb in range(W // 128):
                    blk = t0 // 128 + tb
                    ps = next_ps(7)
                    proj_tm(ps[:, 0:128], hn_t, tb, w, 256, 128)
                    kb.copy("act", Vaug[:, blk, 0:128], ps[:, 0:128])
            finish_vaug(Vaug)
            lf = sb("f_lf", [128, NBLK], F32, st)
            kb.act(lf[:, :], bc(small_all[:, :, 2], small_all.t[:, :, 2]), AF.Exp, bias=nbf[:, 0:1], scale=-1.0)
            kb.act(lf[:, :], lf[:, :], AF.Ln, bias=kb.eps_tile(1.0))
            kb.ts("dve", lf[:, :], lf[:, :], -1.0, None, op0=ALU.mult)
            within = sb("f_within", [128, NBLK], F32, st)
            totB = sb("f_totB", [128, NBLK], F32, st)
            ps = next_ps(7)
            kb.mm(ps[:, 0:NBLK], Umat, lf[:, :], start=True, stop=True)
            kb.copy("dve", within[:, :], ps[:, 0:NBLK])
            ps = next_ps(7)
            kb.mm(ps[:, 0:NBLK], ones, lf[:, :], start=True, stop=True)
            kb.copy("dve", totB[:, :], ps[:, 0:NBLK])
            tab = sb("f_tab", [128, NBLK, NBLK], F32, st)
            for i in range(NBLK):
                if i > 0:
                    kb.ts("dve", tab[:, i, 0:i], tab[:, i - 1, 0:i], totB[:, i:i + 1], None, op0=ALU.add)
                kb.tt("dve", tab[:, i, i:i + 1], totB[:, i:i + 1], within[:, i:i + 1], ALU.subtract)
            attention(st, 2, [(QT, 128)], [(KT, 128)], Vaug, tab, "fa")
        kb.barrier()

    if "ssd" in mixers:
        with ExitStack() as st:
            w = sb("w_ssd_s", [128, KC, 768], BF16, st)
            kb.dma("pool", w[:, :, :], w_ssd_d[:, :, :])
            scv = sb("ssd_conv_s", [128, 4, 5], F32, st)
            kb.dma("sp", scv[:, :, :], ssd_conv_d[:, :, :])
            svec = sb("ssd_vec_s", [128, 8], F32, st)
            kb.dma("sp", svec[:, :], ssd_vec_d[:, :])
            srow = sb("ssd_row_s", [128, 2, 256], F32, st)
            kb.dma("sp", srow[:, :, :], bc(ssd_row_d[:, :, :], ssd_row_d.t.rearrange("c p f -> p c f")))
            hns = [sb("shn%d" % i, [128, KC, 512], BF16, st) for i in range(1)]
            pre = sb("spre", [128, 4, 515], F32, st)
            acc = [sb("sacc%d" % i, [128, 512], F32, st) for i in range(2)]
            xT = [sb("s_xT%d" % i, [128, T], BF16, st) for i in range(2)]
            BT = sb("s_BT", [128, T], BF16, st)
            CT = sb("s_CT", [128, T], BF16, st)
            zs = sb("s_zs", [128, NBLK, 256], BF16, st)
            kb.memset("dve", pre[:, :, 0:3], 0.0)
            dsts = (xT[0], xT[1], BT, CT)
            for ti, (t0, W) in enumerate(tiles):
                hn_t = hns[0]
                load_hn(hn_t, t0, W)
                for c in range(4):
                    ps = next_ps()
                    proj_fm(ps, w, 256 + c * 128, 128, hn_t, W)
                    kb.copy("act", pre[:, c, 3:3 + W], ps[:, 0:W])
                    a = acc[c % 2]
                    conv4(a, bc(pre[:, c, :], pre.t[:, c, :]), bc(scv[:, c, :], scv.t[:, c, :]), W)
                    kb.copy("pool", pre[:, c, 0:3], pre[:, c, W:W + 3])
                    kb.act(dsts[c][:, t0:t0 + W], a[:, 0:W], AF.Silu, bias=scv[:, c, 4:5])
                for tb in range(W // 128):
                    blk = t0 // 128 + tb
                    ps = next_ps()
                    proj_tm(ps[:, 0:256], hn_t, tb, w, 0, 256)
                    kb.act(zs[:, blk, :], ps[:, 0:256], AF.Silu)
            for b_ in (xT[0], xT[1], BT):
                kb.memset("dve", b_[:, 0:FRONT], 0.0)
            negA = sb("s_negA", [128, 4], F32, st)
            kb.act(negA[:, :], svec[:, 4:8], AF.Exp)
            kb.ts("dve", negA[:, :], negA[:, :], -1.0, None, op0=ALU.mult)
            dts, decs, a_alls = [], [], []
            for h in range(4):
                dt = sb("s_dt%d" % h, [128, NBLK], F32, st)
                a_all = sb("s_a%d" % h, [128, NBLK], F32, st)
                softplus(dt[:, :], bc(small_all[:, :, 3 + h], small_all.t[:, :, 3 + h]), svec[:, h:h + 1])
                kb.ts("dve", a_all[:, :], dt[:, :], negA[:, h:h + 1], None, op0=ALU.mult)
                dts.append(dt)
                a_alls.append(a_all)
                decs.append(decay_prep(st, a_all, "sd%d_" % h))
            mk = lambda n, d=F32, shp=(128, 128), k=2: [sb("%s%d" % (n, i), list(shp), d, st) for i in range(k)]
            grep_, DmT_, EG_ = mk("s_grep"), mk("s_DmT"), mk("s_EG")
            attnT_, CeT_ = mk("s_attnT", BF16), mk("s_CeT", BF16)
            Btok_ = mk("s_Btok", BF16)
            Xtok_ = mk("s_Xtok", F32, (128, 256))
            Xdt_ = mk("s_Xdt", BF16, (128, 256))
            Xdec_ = mk("s_Xdec", BF16, (128, 256))
            CBt_ = mk("s_CBt")
            y1_ = mk("s_y1", F32, (128, 256))
            y2_ = mk("s_y2", F32, (128, 256))
            junk_ = mk("s_junk", F32, (128, 256))
            ss_ = mk("s_ss", F32, (128, 1))
            S = sb("s_S", [128, 256], F32, st)
            S_bf = sb("s_Sbf", [128, 256], BF16, st)
            kb.memset("dve", S[:, :], 0.0)
            kb.memset("dve", S_bf[:, :], 0.0)
            stage = [sb("s_stage%d" % i, [128, 128], F32, st) for i in range(2)]
            stage_i = [0]
            hh = [0]
            for n in range(NBLK):
                i2 = n % 2
                cs = slice(n * 128, (n + 1) * 128)
                pst = next_ps()
                kb.mm(pst[:, 0:128], BT[:, cs], ident_b, start=True, stop=True)
                kb.mm(pst[:, 128:256], xT[0][:, cs], ident_b, start=True, stop=True)
                kb.mm(pst[:, 256:384], xT[1][:, cs], ident_b, start=True, stop=True)
                Btok, Xtok, Xdt, Xdec = Btok_[i2], Xtok_[i2], Xdt_[i2], Xdec_[i2]
                kb.copy("act", Btok[:, :], pst[:, 0:128])
                kb.copy("act", Xtok[:, :], pst[:, 128:384])
                for h in range(4):
                    hs = slice(h * 64, (h + 1) * 64)
                    kb.ts("dve", Xdt[:, hs], Xtok[:, hs], dts[h][:, n:n + 1], None, op0=ALU.mult)
                    kb.ts("dve", Xdec[:, hs], Xtok[:, hs], dts[h][:, n:n + 1], decs[h]["e2e"][:, n:n + 1],
                          op0=ALU.mult, op1=ALU.mult)
                psc = next_ps()
                kb.mm(psc[:, 0:128], BT[:, cs], CT[:, cs], start=True, stop=True)
                CBt = CBt_[i2]
                kb.copy("act", CBt[:, :], psc[:, 0:128])
                psy = next_ps()
                for h in range(4):
                    hs = slice(h * 64, (h + 1) * 64)
                    k2 = hh[0] % 2
                    hh[0] += 1
                    grep, DmT, EG = grep_[k2], DmT_[k2], EG_[k2]
                    decay_mats(a_alls[h], decs[h], n, grep, DmT, EG)
                    attnT, CeT = attnT_[k2], CeT_[k2]
                    kb.tt("dve", attnT[:, :], CBt[:, :], DmT[:, :], ALU.mult)
                    kb.tt("pool", CeT[:, :], CT[:, cs], EG[:, :], ALU.mult)
                    kb.mm(psy[:, hs], CeT[:, :], S_bf[:, hs], start=True, stop=False)
                    kb.mm(psy[:, hs], attnT[:, :], Xdt[:, hs], start=False, stop=True)
                psn = next_ps()
                kb.mm(psn[:, 0:256], Btok[:, :], Xdec[:, :], start=True, stop=True)
                for h in range(4):
                    hs = slice(h * 64, (h + 1) * 64)
                    kb.stt("dve", S[:, hs], S[:, hs], decs[h]["cd"][:, n:n + 1], psn[:, hs], ALU.mult, ALU.add)
                kb.copy("act", S_bf[:, :], S[:, :])
                y1, y2, junk, ss = y1_[i2], y2_[i2], junk_[i2], ss_[i2]
                kb.tt("pool", y1[:, :], Xtok[:, :], bc(srow[:, 0, :], srow.t[:, 0, :]), ALU.mult)
                kb.tt("dve", y1[:, :], y1[:, :], psy[:, 0:256], ALU.add)
                kb.tt("pool", y2[:, :], y1[:, :], bc(zs[:, n, :], zs.t[:, n, :]), ALU.mult)
                kb.memset("pool", ss[:, :], 0.0)
                kb.act(junk[:, :], y2[:, :], AF.Square, accum=ss[:, :])
                kb.act(ss[:, :], ss[:, :], AF.Sqrt, bias=kb.eps_tile(EPS), scale=1.0 / 256)
                kb.recip(ss[:, :], ss[:, :])
                kb.stt("dve", y1[:, :], y2[:, :], ss[:, 0:1], bc(srow[:, 1, :], srow.t[:, 1, :]), ALU.mult, ALU.mult)
                emit_out(3, y1, 2, n, stage, stage_i)
        kb.barrier()
    kb.finish()
    return kb, locals()


IN_WIDTHS = (512, 512, 512, 512, 4, 4, 512, 256, 64, 512, 512, 512, 4, 512, 512, 256, 256, 8)
OFF = [0]
for _w in IN_WIDTHS:
    OFF.append(OFF[-1] + _w)


def r3(w):
    K, C = w.shape
    return np.ascontiguousarray(w.reshape(K // 128, 128, C).transpose(1, 0, 2))


def rep(v, n=128):
    v = np.asarray(v, np.float32).reshape(1, -1)
    return np.ascontiguousarray(np.broadcast_to(v, (n, v.shape[1])))


def make_consts(T):
    p = np.arange(128)[:, None]
    f = np.arange(128)[None, :]
    cst = np.stack([
        (p == f), (p <= f), np.where(f >= p, 0.0, -30000.0), (f > p), (f >= p), np.ones((128, 128)),
    ]).astype(np.float32)
    pos = np.maximum(np.arange(T) - FRONT, 0).astype(np.float32)
    inv = (1.0 / (10000.0 ** (np.arange(0, 64, 2, dtype=np.float32) / 64.0))).astype(np.float32)
    ang = pos[None, :] * np.concatenate([inv, inv])[:, None]
    rope = np.stack([np.cos(ang), np.sin(ang)]).astype(np.float32)
    rot = np.zeros((64, 64), np.float32)
    for ff in range(32):
        rot[ff, ff + 32] = -1.0
        rot[ff + 32, ff] = 1.0
    return cst, rope, np.ascontiguousarray(rot.T)


def phaseA_inputs(P, l, j, hT_b, T):
    G = j // 2
    w_in = P["w_in"][l]
    col = lambda o, a, n: w_in[:, OFF[o] + a:OFF[o] + a + n]
    cst, rope, rotT = make_consts(T)
    small = np.concatenate([col(4, j, 1), col(5, j, 1), col(12, j, 1), col(17, G * 4, 4),
                            np.zeros((2048, 1), np.float32)], axis=1)
    w_gdn = np.concatenate([col(0, j * 128, 128), col(1, j * 128, 128), col(2, j * 128, 128), col(3, j * 128, 128)], 1)
    cw = P["gdn_conv_w"][l]
    gdn_conv = np.stack([cw[:, j * 128:(j + 1) * 128].T, cw[:, 512 + j * 128:512 + (j + 1) * 128].T,
                         cw[:, 1024 + j * 128:1024 + (j + 1) * 128].T], axis=1)
    w_mla = np.concatenate([col(6, 0, 512), col(7, 0, 256), col(8, 0, 64)], 1)
    mla_g = np.zeros((128, 10), np.float32)
    mla_g[:, 0:4] = P["mla_qa_g"][l].reshape(4, 128).T
    mla_g[:, 4:6] = P["mla_kva_g"][l].reshape(2, 128).T
    mla_g[:, 6] = P["mla_qn_g"][l][0:128]
    mla_g[0:64, 7] = P["mla_qn_g"][l][128:192]
    mla_g[:, 8] = P["mla_kn_g"][l][0:128]
    mla_g[0:64, 9] = P["mla_kn_g"][l][128:192]
    w_fox = np.concatenate([col(9, j * 128, 128), col(10, j * 128, 128), col(11, j * 128, 128)], 1)
    fox_g = np.stack([P["fox_qn_g"][l], P["fox_kn_g"][l], np.full(128, P["fox_b_f"][l][j], np.float32)], 1)
    w_ssd = np.concatenate([col(13, G * 256, 256), col(14, G * 256, 256), col(15, G * 128, 128), col(16, G * 128, 128)], 1)
    sw = P["ssd_conv_w"][l]
    sbias = P["ssd_conv_b"][l]
    chs = [slice(G * 256, G * 256 + 128), slice(G * 256 + 128, G * 256 + 256),
           slice(512 + G * 128, 512 + (G + 1) * 128), slice(768 + G * 128, 768 + (G + 1) * 128)]
    ssd_conv = np.stack([np.concatenate([sw[:, c].T, sbias[c][:, None]], 1) for c in chs], axis=1)
    ssd_vec = rep(np.concatenate([P["ssd_dt_bias"][l][G * 4:G * 4 + 4], P["ssd_A_log"][l][G * 4:G * 4 + 4]]))
    ssd_row = np.stack([rep(np.repeat(P["ssd_D"][l][G * 4:G * 4 + 4], 64)), rep(P["ssd_norm_g"][l][G * 256:(G + 1) * 256])])
    f32c = lambda a: np.ascontiguousarray(a, dtype=np.float32)
    return {
        "hT": f32c(hT_b), "g1": f32c(P["mix_norm_g"][l].reshape(KC, 128).T), "w_small": r3(f32c(small)), "cst": cst,
        "w_gdn": r3(f32c(w_gdn)), "gdn_conv": f32c(gdn_conv),
        "gdn_vec": rep([P["gdn_A_log"][l][j], P["gdn_dt_bias"][l][j]]), "gdn_ng": rep(P["gdn_norm_g"][l]),
        "w_mla": r3(f32c(w_mla)), "mla_wq": r3(f32c(P["mla_wq_b"][l][:, j * 192:(j + 1) * 192])),
        "mla_wkv": r3(f32c(P["mla_wkv_b"][l][:, j * 256:(j + 1) * 256])), "mla_g": mla_g,
        "rope": rope, "rotm": rotT,
        "w_fox": r3(f32c(w_fox)), "fox_g": f32c(fox_g),
        "w_ssd": r3(f32c(w_ssd)), "ssd_conv": f32c(ssd_conv), "ssd_vec": ssd_vec, "ssd_row": f32c(ssd_row),
    }


def seq_to_hT(h_seq, T):
    L = h_seq.shape[0]
    out = np.zeros((2048, T), np.float32)
    out[:, FRONT:FRONT + L] = h_seq.T
    return out.reshape(KC, 128, T)


_PROGS = {}


def _prog(key, fn):
    if key not in _PROGS:
        _PROGS[key] = fn()
    return _PROGS[key]


def phaseB_weights(P, l):
    f32c = lambda a: np.ascontiguousarray(a, dtype=np.float32)
    d = {
        "wgate": np.stack([r3(f32c(P["w_gate"][l][b])) for b in range(4)]),
        "wbr": np.stack([r3(f32c(P["w_branch"][l][b])) for b in range(4)]),
        "wo": r3(f32c(P["w_o"][l])),
        "g1": f32c(P["mix_norm_g"][l].reshape(KC, 128).T),
        "g2": f32c(P["ffn_norm_g"][l].reshape(KC, 128).T),
    }
    i = l // 2
    if l % 2 == 0:
        d.update({"wfg": r3(f32c(P["dense_w_gate"][i])), "wfu": r3(f32c(P["dense_w_up"][i])),
                  "wfd": r3(f32c(P["dense_w_down"][i]))})
    else:
        sel = np.zeros((N_EXP, N_EXP, 128), np.float32)
        for e in range(N_EXP):
            sel[e, e, :] = 1.0
        d.update({"wr": r3(f32c(P["router_w"][i])),
                  "weg": np.stack([r3(f32c(P["moe_w_gate"][i][e])) for e in range(N_EXP)]),
                  "weu": np.stack([r3(f32c(P["moe_w_up"][i][e])) for e in range(N_EXP)]),
                  "wed": np.stack([r3(f32c(P["moe_w_down"][i][e])) for e in range(N_EXP)]),
                  "sel": sel, "ident": np.eye(128, dtype=np.float32)})
    return d


def kernel(**inputs):
    P = {k: np.asarray(v) for k, v in inputs.items()}
    x = P["x"].astype(np.float32, copy=False)
    Bsz, S, D = x.shape
    NX = S // 128
    NBLK = NX + 1
    T = NBLK * 128
    NXB = NX // 4
    NTB = NXB + 1
    depth = P["w_in"].shape[0]
    meta = P["meta_tokens"].astype(np.float32)
    hT = [seq_to_hT(np.concatenate([meta, x[b]], 0), T) for b in range(Bsz)]
    cores = list(range(8))
    for l in range(depth):
        kbA, _ = _prog(("A", NBLK), lambda: build_phaseA(NBLK))
        in_maps = [phaseA_inputs(P, l, c % 4, hT[c // 4], T) for c in cores]
        resA = run_bass_kernel_spmd(kbA.nc, in_maps, core_ids=cores)
        br = []
        for b in range(Bsz):
            bos = [resA.results[b * 4 + j]["bo"] for j in range(4)]
            rows = [bos[j][0] for j in range(4)] + [bos[j][1] for j in range(4)] + [bos[j][2] for j in range(4)] \
                + [bos[j][3 + (j % 2)] for j in range(4)]
            br.append(np.stack(rows))
        del resA
        kind = "dense" if l % 2 == 0 else "moe"
        kbB = _prog(("B", NTB, kind), lambda: build_phaseB(NTB, kind))
        wts = phaseB_weights(P, l)
        in_maps = []
        for c in cores:
            b, q = c // 4, c % 4
            cols = np.r_[0:128, 128 + q * NXB * 128:128 + (q + 1) * NXB * 128]
            m = dict(wts)
            m["hT"] = np.ascontiguousarray(hT[b][:, :, cols])
            m["br"] = np.ascontiguousarray(br[b][:, :, cols])
            in_maps.append(m)
        resB = run_bass_kernel_spmd(kbB.nc, in_maps, core_ids=cores)
        for c in cores:
            b, q = c // 4, c % 4
            o = resB.results[c]["hTo"]
            if q == 0:
                hT[b][:, :, FRONT:128] = o[:, :, FRONT:128]
            hT[b][:, :, 128 + q * NXB * 128:128 + (q + 1) * NXB * 128] = o[:, :, 128:]
        del resB
    out = np.stack([np.ascontiguousarray(hT[b].reshape(D, T)[:, 128:].T) for b in range(Bsz)])
    return out.astype(np.float32)
```

```python
import os
import numpy as np
import concourse.bass as bass
import concourse.mybir as mybir
from concourse.bass_utils import run_bass_kernel_spmd

F32 = mybir.dt.float32
BF16 = mybir.dt.bfloat16
AF = mybir.ActivationFunctionType
ALU = mybir.AluOpType
AX = mybir.AxisListType


class Res:
    __slots__ = ("w", "r", "dsem", "dcnt")

    def __init__(self):
        self.w = None
        self.r = {}
        self.dsem = None
        self.dcnt = 0


class V:
    __slots__ = ("ap", "res", "space")

    def __init__(self, ap, res, space="sbuf"):
        self.ap = ap
        self.res = res
        self.space = space

    def __getitem__(self, idx):
        return V(self.ap[idx], self.res, self.space)


class Buf:
    def __init__(self, kb, name, shape, dtype, space="sbuf", kind=None, nres=1, stack=None):
        self.kb = kb
        nc = kb.nc
        self.name = name
        if space == "sbuf":
            if stack is not None:
                self.t = stack.enter_context(nc.sbuf_tensor(name, list(shape), dtype))
            else:
                self.t = nc.alloc_sbuf_tensor(name, list(shape), dtype)
        elif space == "psum":
            self.t = nc.alloc_psum_tensor(name, list(shape), dtype)
        else:
            self.t = nc.dram_tensor(name, list(shape), dtype, kind=kind or "Internal").ap()
        self.res = [Res() for _ in range(nres)]
        self.space = space

    def __getitem__(self, idx):
        return V(self.t[idx], self.res, self.space)

    def sub(self, ri, idx):
        return V(self.t[idx], [self.res[ri]], self.space)


def bc(v, ap):
    return V(ap, v.res, v.space)


class StopBuild(Exception):
    pass


class KB:
    ENG = ("pe", "act", "dve", "pool", "sp")

    def ck(self, name=""):
        import os
        lim = int(os.environ.get("STOP_AT", "0"))
        self._ck = getattr(self, "_ck", 0) + 1
        if lim and self._ck >= lim:
            print("STOP at checkpoint", self._ck, name)
            raise StopBuild()

    def __init__(self):
        self.nc = bass.Bass("TRN2", target_bir_lowering=False)
        nc = self.nc
        self.e = {"pe": nc.tensor, "act": nc.scalar, "dve": nc.vector, "pool": nc.gpsimd, "sp": nc.sync}
        self.sem = {}
        self.cnt = {}
        self.seen = {k: {} for k in self.ENG}
        self.semh = {}
        for k in self.ENG:
            h = nc.alloc_semaphore("sem_" + k)
            self.semh[k] = h
            self.cnt[k] = 0
        self.ndsem = 0
        self.dma_max = {}
        self.out_tokens = []
        self.ninstr = 0

    def _dsem(self, res):
        if res.dsem is None:
            key = "d%d" % self.ndsem
            self.ndsem += 1
            self.semh[key] = self.nc.alloc_semaphore("sem_" + key)
            res.dsem = key
        return res.dsem

    def _collect(self, eng, outs, ins):
        waits = {}

        def need(tok):
            if tok is None:
                return
            s, val = tok
            if waits.get(s, 0) < val:
                waits[s] = val

        for v in ins:
            for r in v.res:
                need(r.w)
                if v.space == "psum":
                    for s, val in r.r.items():
                        if s != eng:
                            need((s, val))
        for v in outs:
            for r in v.res:
                if r.w is not None:
                    need(r.w)
                for s, val in r.r.items():
                    need((s, val))
        if eng == "pe" and "pe" in waits:
            del waits["pe"]
        return waits

    def _dowaits(self, eng, waits):
        E = self.e[eng]
        seen = self.seen[eng]
        for s, val in waits.items():
            if seen.get(s, 0) >= val:
                continue
            E.wait_ge(self.semh[s], val)
            seen[s] = val

    def op(self, eng, fn, outs, ins):
        waits = self._collect(eng, outs, ins)
        self._dowaits(eng, waits)
        ins_ = fn()
        self.cnt[eng] += 1
        c = self.cnt[eng]
        ins_.then_inc(self.semh[eng], 1)
        for v in ins:
            for r in v.res:
                r.r[eng] = c
        for v in outs:
            for r in v.res:
                r.w = (eng, c)
                r.r = {}
        self.ninstr += 1
        return ins_

    def dma(self, q, out, in_, final=False):
        waits = self._collect("__dma__", [out], [in_])
        self._dowaits(q, waits)
        if out.space == "sbuf":
            sres = out.res[0]
        elif in_.space == "sbuf":
            sres = in_.res[0]
        else:
            sres = out.res[0]
        key = self._dsem(sres)
        ins_ = self.e[q].dma_start(out=out.ap, in_=in_.ap)
        sres.dcnt += 16
        ins_.then_inc(self.semh[key], 16)
        tok = (key, sres.dcnt)
        self.dma_max[key] = sres.dcnt
        for r in in_.res:
            r.r[key] = max(r.r.get(key, 0), sres.dcnt)
        for r in out.res:
            r.w = tok
            r.r = {}
        if final:
            self.out_tokens.append(tok)
        self.ninstr += 1
        return ins_

    def transfer(self, olds, news):
        toks = {}
        for b in olds:
            for r in b.res:
                if r.w is not None:
                    toks[r.w[0]] = max(toks.get(r.w[0], 0), r.w[1])
                for s, val in r.r.items():
                    toks[s] = max(toks.get(s, 0), val)
        for b in news:
            for r in b.res:
                r.w = None
                r.r = dict(toks)

    def barrier(self):
        allw = {k: self.cnt[k] for k in ("pe", "act", "dve", "pool") if self.cnt[k] > 0}
        for key, val in self.dma_max.items():
            allw[key] = val
        for e in self.ENG:
            w = {k: v for k, v in allw.items() if k != e}
            self._dowaits(e, w)

    def finish(self):
        waits = {}
        for s, val in self.out_tokens:
            waits[s] = max(waits.get(s, 0), val)
        self._dowaits("sp", waits)
        for k in ("pe", "act", "dve", "pool"):
            if self.cnt[k] > 0:
                self._dowaits("sp", {k: self.cnt[k]})

    def mm(self, out, lhsT, rhs, start=True, stop=True):
        nc = self.nc
        return self.op("pe", lambda: nc.tensor.matmul(out.ap, lhsT.ap, rhs.ap, start=start, stop=stop),
                       [out], [lhsT, rhs])

    def tr(self, out, in_, ident):
        nc = self.nc
        return self.op("pe", lambda: nc.tensor.transpose(out.ap, in_.ap, ident.ap), [out], [in_, ident])

    def act(self, out, in_, func, bias=None, scale=1.0, accum=None, eng="act"):
        nc = self.nc
        ins = [in_]
        kw = {}
        if bias is not None:
            if isinstance(bias, V):
                ins.append(bias)
                kw["bias"] = bias.ap
            else:
                kw["bias"] = bias
        if isinstance(scale, V):
            ins.append(scale)
            kw["scale"] = scale.ap
        else:
            kw["scale"] = scale
        outs = [out]
        if accum is not None:
            outs.append(accum)
            kw["accum_out"] = accum.ap
        return self.op("act", lambda: nc.scalar.activation(out=out.ap, in_=in_.ap, func=func, **kw), outs, ins)

    def tt(self, eng, out, a, b, op):
        E = self.e[eng]
        return self.op(eng, lambda: E.tensor_tensor(out=out.ap, in0=a.ap, in1=b.ap, op=op), [out], [a, b])

    def ts(self, eng, out, a, s1, s2=None, op0=ALU.mult, op1=None, accum=None):
        E = self.e[eng]
        ins = [a]
        a1 = s1
        a2 = s2
        if isinstance(s1, V):
            ins.append(s1)
            a1 = s1.ap
        if isinstance(s2, V):
            ins.append(s2)
            a2 = s2.ap
        kw = {}
        if op1 is not None:
            kw["op1"] = op1
        outs = [out]
        if accum is not None:
            outs.append(accum)
            kw["accum_out"] = accum.ap
        return self.op(eng, lambda: E.tensor_scalar(out=out.ap, in0=a.ap, scalar1=a1, scalar2=a2, op0=op0, **kw),
                       outs, ins)

    def stt(self, eng, out, a, s, b, op0, op1):
        E = self.e[eng]
        ins = [a, b]
        sa = s
        if isinstance(s, V):
            ins.append(s)
            sa = s.ap
        return self.op(eng, lambda: E.scalar_tensor_tensor(out=out.ap, in0=a.ap, scalar=sa, in1=b.ap, op0=op0, op1=op1),
                       [out], ins)

    def copy(self, eng, out, in_):
        if eng == "act":
            nc = self.nc
            return self.op("act", lambda: nc.scalar.copy(out=out.ap, in_=in_.ap), [out], [in_])
        E = self.e[eng]
        return self.op(eng, lambda: E.tensor_copy(out=out.ap, in_=in_.ap), [out], [in_])

    def recip(self, out, in_):
        nc = self.nc
        return self.op("dve", lambda: nc.vector.reciprocal(out=out.ap, in_=in_.ap), [out], [in_])

    def eps_tile(self, val):
        if not hasattr(self, "_eps"):
            self._eps = {}
        if val not in self._eps:
            b = Buf(self, "cst%d" % len(self._eps), [128, 1], F32)
            self.memset("dve", b[:, :], float(val))
            self._eps[val] = b
        return self._eps[val][:, :]

    def memset(self, eng, out, val):
        E = self.e[eng]
        return self.op(eng, lambda: E.memset(out.ap, val), [out], [])


class ABuf:
    def __init__(self, ap, space="sbuf", nres=1):
        self.t = ap
        self.res = [Res() for _ in range(nres)]
        self.space = space

    def __getitem__(self, idx):
        return V(self.t[idx], self.res, self.space)


D_MODEL = 2048
KC = 16
EPS = 1e-6
D_FF = 5632
NFF = D_FF // 128
N_EXP = 8
D_FFE = 1408
NFE = D_FFE // 128


def token_tiles(NT, W=512):
    out = []
    t = 0
    while t < NT:
        w = min(W, NT - t)
        out.append((t, w))
        t += w
    return out


def rmsnorm_fm(kb, h_sb, hn, g_sb, ones_bf, sqs, ps_ss, rstd, W, D=D_MODEL, post=None):
    nkc = D // 128
    for kc in range(nkc):
        sq = sqs[kc % len(sqs)]
        kb.act(sq[:, 0:W], h_sb[:, kc, 0:W], AF.Square)
        kb.mm(ps_ss[:, 0:W], ones_bf[:, :], sq[:, 0:W], start=(kc == 0), stop=(kc == nkc - 1))
    kb.act(rstd[:, 0:W], ps_ss[:, 0:W], AF.Sqrt, bias=kb.eps_tile(EPS))
    kb.recip(rstd[:, 0:W], rstd[:, 0:W])
    for kc in range(nkc):
        eng = "dve"
        if post is None:
            kb.stt(eng, hn[:, kc, 0:W], h_sb[:, kc, 0:W], g_sb[:, kc:kc + 1], rstd[:, 0:W], ALU.mult, ALU.mult)
        else:
            post(kc, eng)


def build_phaseB(NTB, kind):
    kb = KB()
    nc = kb.nc
    NT = NTB * 128
    D = D_MODEL
    dr = lambda n, s, k="ExternalInput": Buf(kb, n, s, F32, "dram", k)
    hT = dr("hT", [KC, 128, NT])
    br = dr("br", [16, 128, NT])
    wgate = dr("wgate", [4, D // 256, 128, KC, 256])
    wbr = dr("wbr", [4, 128, 4, D])
    wo = dr("wo", [D // 256, 128, KC, 256])
    g1d = dr("g1", [128, KC])
    g2d = dr("g2", [128, KC])
    if kind == "dense":
        wfg = dr("wfg", [D_FF // 256, 128, KC, 256])
        wfu = dr("wfu", [D_FF // 256, 128, KC, 256])
        wfd = dr("wfd", [KC, 128, NFF, 128])
    else:
        wr = dr("wr", [128, KC, N_EXP])
        weg = dr("weg", [N_EXP, 6, 128, KC, 256])
        weu = dr("weu", [N_EXP, 6, 128, KC, 256])
        wed = dr("wed", [N_EXP, KC, 128, NFE, 128])
        seld = dr("sel", [N_EXP, N_EXP, 128])
        identd = dr("ident", [128, 128])
    hTo = dr("hTo", [KC, 128, NT], "ExternalOutput")

    sb = lambda n, s, d=F32: Buf(kb, n, s, d)
    h_sb = sb("h_sb", [128, KC, 512])
    hn = sb("hn", [128, KC, 512], BF16)
    merged = sb("merged", [128, KC, 512], BF16)
    sqs = [sb("sq%d" % i, [128, 512], BF16) for i in range(2)]
    rstd = sb("rstd", [128, 512])
    X = nc.alloc_sbuf_tensor("X", [128, 12288], F32)
    macc = ABuf(X[:, 0:8192].rearrange("p (a b) -> p a b", b=512))
    br_sb = ABuf(X[:, 8192:12288].bitcast(BF16).rearrange("p (a b) -> p a b", b=512))
    actb = ABuf(X[:, 0:11264].bitcast(BF16).rearrange("p (a b) -> p a b", b=512))
    NWP = 3
    wps = [sb("wp%d" % i, [128, KC, 256], BF16) for i in range(NWP)]
    wbs = sb("wbs", [128, 4, D], BF16)
    wds = [sb("wd%d" % i, [128, NFF, 128], BF16) for i in range(2)]
    g1 = sb("g1s", [128, KC])
    g2 = sb("g2s", [128, KC])
    ones_bf = sb("ones_bf", [128, 128], BF16)
    sgs = [sb("sg%d" % i, [128, 512]) for i in range(2)]
    tmps = [sb("tmp%d" % i, [128, 512]) for i in range(2)]
    pss = [Buf(kb, "ps%d" % i, [128, 512], F32, "psum") for i in range(8)]
    if kind == "moe":
        stg = [sb("stg%d" % i, [128, 512]) for i in range(2)]
        wr_sb = sb("wr_sb", [128, KC, N_EXP])
        lg = sb("lg", [128, 4, 8])
        top8 = sb("top8", [128, 4, 8])
        comb = sb("comb", [128, 4, 8])
        cw = sb("cw", [128, 4, 8])
        combT = sb("combT", [8, 512], BF16)
        sel = sb("sel_sb", [N_EXP, N_EXP, 128], BF16)
        combB = [sb("combB%d" % i, [128, 512]) for i in range(2)]
        ident = sb("ident_sb", [128, 128])
        kb.dma("sp", wr_sb[:, :, :], wr[:, :, :])
        kb.dma("pool", sel[:, :, :], seld[:, :, :])
        kb.dma("sp", ident[:, :], identd[:, :])

    kb.dma("sp", g1[:, :], g1d[:, :])
    kb.dma("sp", g2[:, :], g2d[:, :])
    kb.memset("dve", ones_bf[:, :], 1.0 / D)

    wpi = [0]

    def next_wp():
        b = wps[wpi[0] % NWP]
        wpi[0] += 1
        return b

    psi = [0]

    def next_ps():
        p = pss[psi[0] % 6]
        psi[0] += 1
        return p

    ps_ss = pss[6]
    ps_misc = pss[7]
    cnt = [0]

    for (t0, W) in token_tiles(NT):
        for kc in range(KC):
            kb.dma("sp", h_sb[:, kc, 0:W], hT[kc, :, t0:t0 + W])
        rmsnorm_fm(kb, h_sb, hn, g1, ones_bf, sqs, ps_ss, rstd, W)
        kb.transfer([actb], [macc, br_sb])
        for i in range(16):
            kb.dma("pool", br_sb[:, i, 0:W], br[i, :, t0:t0 + W])
        for b in range(4):
            kb.dma("pool", wbs[:, :, :], wbr[b, :, :, :])
            for pc in range(D // 256):
                wp = next_wp()
                kb.dma("pool", wp[:, :, :], wgate[b, pc, :, :, :])
                for mm_ in range(2):
                    m = pc * 2 + mm_
                    pg = next_ps()
                    for kc in range(KC):
                        kb.mm(pg[:, 0:W], wp[:, kc, mm_ * 128:(mm_ + 1) * 128], hn[:, kc, 0:W],
                              start=(kc == 0), stop=(kc == KC - 1))
                    pb = next_ps()
                    for hh in range(4):
                        kb.mm(pb[:, 0:W], wbs[:, hh, m * 128:(m + 1) * 128], br_sb[:, b * 4 + hh, 0:W],
                              start=(hh == 0), stop=(hh == 3))
                    sg = sgs[cnt[0] % 2]
                    tmp = tmps[cnt[0] % 2]
                    cnt[0] += 1
                    kb.act(sg[:, 0:W], pg[:, 0:W], AF.Sigmoid)
                    if b == 0:
                        kb.tt("dve", macc[:, m, 0:W], sg[:, 0:W], pb[:, 0:W], ALU.mult)
                    elif b < 3:
                        kb.tt("dve", tmp[:, 0:W], sg[:, 0:W], pb[:, 0:W], ALU.mult)
                        kb.tt("dve", macc[:, m, 0:W], macc[:, m, 0:W], tmp[:, 0:W], ALU.add)
                    else:
                        kb.tt("dve", tmp[:, 0:W], sg[:, 0:W], pb[:, 0:W], ALU.mult)
                        kb.tt("dve", merged[:, m, 0:W], macc[:, m, 0:W], tmp[:, 0:W], ALU.add)
        for pc in range(D // 256):
            wp = next_wp()
            kb.dma("pool", wp[:, :, :], wo[pc, :, :, :])
            for mm_ in range(2):
                m = pc * 2 + mm_
                po = next_ps()
                for kc in range(KC):
                    kb.mm(po[:, 0:W], wp[:, kc, mm_ * 128:(mm_ + 1) * 128], merged[:, kc, 0:W],
                          start=(kc == 0), stop=(kc == KC - 1))
                kb.tt("dve", h_sb[:, m, 0:W], h_sb[:, m, 0:W], po[:, 0:W], ALU.add)
        kb.transfer([macc, br_sb], [actb])
        if kind == "dense":
            rmsnorm_fm(kb, h_sb, hn, g2, ones_bf, sqs, ps_ss, rstd, W)
            for pc in range(D_FF // 256):
                wpg = next_wp()
                kb.dma("pool", wpg[:, :, :], wfg[pc, :, :, :])
                wpu = next_wp()
                kb.dma("pool", wpu[:, :, :], wfu[pc, :, :, :])
                for mm_ in range(2):
                    fc = pc * 2 + mm_
                    pg = next_ps()
                    for kc in range(KC):
                        kb.mm(pg[:, 0:W], wpg[:, kc, mm_ * 128:(mm_ + 1) * 128], hn[:, kc, 0:W],
                              start=(kc == 0), stop=(kc == KC - 1))
                    pu = next_ps()
                    for kc in range(KC):
                        kb.mm(pu[:, 0:W], wpu[:, kc, mm_ * 128:(mm_ + 1) * 128], hn[:, kc, 0:W],
                              start=(kc == 0), stop=(kc == KC - 1))
                    sg = sgs[cnt[0] % 2]
                    cnt[0] += 1
                    kb.act(sg[:, 0:W], pg[:, 0:W], AF.Silu)
                    kb.tt("dve", actb[:, fc, 0:W], sg[:, 0:W], pu[:, 0:W], ALU.mult)
            for m in range(KC):
                wd = wds[m % 2]
                kb.dma("pool", wd[:, :, :], wfd[m, :, :, :])
                po = next_ps()
                for fc in range(NFF):
                    kb.mm(po[:, 0:W], wd[:, fc, :], actb[:, fc, 0:W], start=(fc == 0), stop=(fc == NFF - 1))
                kb.tt("dve", h_sb[:, m, 0:W], h_sb[:, m, 0:W], po[:, 0:W], ALU.add)
        else:
            nb = W // 128
            def post(kc, eng):
                st = stg[kc % 2]
                kb.stt("dve", st[:, 0:W], h_sb[:, kc, 0:W], g2[:, kc:kc + 1], rstd[:, 0:W], ALU.mult, ALU.mult)
                kb.copy("act", hn[:, kc, 0:W], st[:, 0:W])
                for tb in range(nb):
                    kb.mm(pss[tb][:, 0:8], st[:, tb * 128:(tb + 1) * 128], wr_sb[:, kc, :],
                          start=(kc == 0), stop=(kc == KC - 1))
            rmsnorm_fm(kb, h_sb, hn, g2, ones_bf, sqs, ps_ss, rstd, W, post=post)
            for tb in range(nb):
                kb.copy("dve", lg[:, tb, :], pss[tb][:, 0:8])
            for tb in range(nb):
                kb.op("dve", lambda tb=tb: nc.vector.max(out=top8.t[:, tb, :], in_=lg.t[:, tb, :]),
                      [top8[:, tb, :]], [lg[:, tb, :]])
            kb.tt("dve", cw[:, 0:nb, 0:1], top8[:, 0:nb, 0:1], top8[:, 0:nb, 1:2], ALU.subtract)
            kb.act(cw[:, 0:nb, 0:1], cw[:, 0:nb, 0:1], AF.Exp)
            kb.ts("dve", cw[:, 0:nb, 0:1], cw[:, 0:nb, 0:1], 1.0, None, op0=ALU.add)
            kb.op("dve", lambda: nc.vector.reciprocal(out=cw.t[:, 0:nb, 1:2], in_=cw.t[:, 0:nb, 0:1]),
                  [cw[:, 0:nb, 1:2]], [cw[:, 0:nb, 0:1]])
            kb.ts("dve", cw[:, 0:nb, 0:1], cw[:, 0:nb, 1:2], -1.0, 1.0, op0=ALU.mult, op1=ALU.add)
            for tb in range(nb):
                kb.ts("dve", comb[:, tb, :], lg[:, tb, :], top8[:, tb, 0:1], cw[:, tb, 0:1],
                      op0=ALU.is_equal, op1=ALU.mult)
                kb.ts("dve", lg[:, tb, :], lg[:, tb, :], top8[:, tb, 1:2], cw[:, tb, 1:2],
                      op0=ALU.is_equal, op1=ALU.mult)
                kb.tt("dve", comb[:, tb, :], comb[:, tb, :], lg[:, tb, :], ALU.add)
            pT = next_ps()
            for tb in range(nb):
                kb.mm(pT[0:8, tb * 128:(tb + 1) * 128], comb[:, tb, :], ident[:, :], start=True, stop=True)
            kb.copy("dve", combT[:, 0:W], pT[0:8, 0:W])
            for half in range(2):
                for el in range(4):
                    e = half * 4 + el
                    pcb = next_ps()
                    kb.mm(pcb[:, 0:W], sel[:, e, :], combT[:, 0:W], start=True, stop=True)
                    cb = combB[e % 2]
                    kb.copy("act", cb[:, 0:W], pcb[:, 0:W])
                    for pc in range(6):
                        c0 = pc * 256
                        cw_ = min(256, D_FFE - c0)
                        wpg = next_wp()
                        kb.dma("pool", wpg[:, :, 0:cw_], weg[e, pc, :, :, 0:cw_])
                        wpu = next_wp()
                        kb.dma("pool", wpu[:, :, 0:cw_], weu[e, pc, :, :, 0:cw_])
                        for mm_ in range(cw_ // 128):
                            fc = pc * 2 + mm_
                            pg = next_ps()
                            for kc in range(KC):
                                kb.mm(pg[:, 0:W], wpg[:, kc, mm_ * 128:(mm_ + 1) * 128], hn[:, kc, 0:W],
                                      start=(kc == 0), stop=(kc == KC - 1))
                            pu = next_ps()
                            for kc in range(KC):
                                kb.mm(pu[:, 0:W], wpu[:, kc, mm_ * 128:(mm_ + 1) * 128], hn[:, kc, 0:W],
                                      start=(kc == 0), stop=(kc == KC - 1))
                            sg = sgs[cnt[0] % 2]
                            tmp = tmps[cnt[0] % 2]
                            cnt[0] += 1
                            kb.act(sg[:, 0:W], pg[:, 0:W], AF.Silu)
                            kb.tt("dve", tmp[:, 0:W], sg[:, 0:W], pu[:, 0:W], ALU.mult)
                            kb.tt("dve", actb[:, el * NFE + fc, 0:W], tmp[:, 0:W], cb[:, 0:W], ALU.mult)
                for m in range(KC):
                    wd = wds[m % 2]
                    for el in range(4):
                        kb.dma("pool", wd[:, el * NFE:(el + 1) * NFE, :], wed[half * 4 + el, m, :, :, :])
                    po = next_ps()
                    for fc in range(NFF):
                        kb.mm(po[:, 0:W], wd[:, fc, :], actb[:, fc, 0:W], start=(fc == 0), stop=(fc == NFF - 1))
                    kb.tt("dve", h_sb[:, m, 0:W], h_sb[:, m, 0:W], po[:, 0:W], ALU.add)
        for kc in range(KC):
            kb.dma("sp", hTo[kc, :, t0:t0 + W], h_sb[:, kc, 0:W], final=True)
    kb.finish()
    return kb


FRONT = 112
W_SMALL = 8


def build_phaseA(NBLK, mixers=("gdn", "mla", "fox", "ssd")):
    from contextlib import ExitStack
    kb = KB()
    nc = kb.nc
    T = NBLK * 128
    dr = lambda n, s, k="ExternalInput", d=F32: Buf(kb, n, s, d, "dram", k)
    hT = dr("hT", [KC, 128, T])
    g1d = dr("g1", [128, KC])
    w_small_d = dr("w_small", [128, KC, W_SMALL])
    cst_d = dr("cst", [6, 128, 128])
    bo = dr("bo", [5, 128, T], "ExternalOutput")
    hnT = dr("hnT_scr", [KC, 128, T], "Internal", BF16)
    w_gdn_d = dr("w_gdn", [128, KC, 512])
    gdn_conv_d = dr("gdn_conv", [128, 3, 4])
    gdn_vec_d = dr("gdn_vec", [128, 2])
    gdn_ng_d = dr("gdn_ng", [128, 128])
    w_mla_d = dr("w_mla", [128, KC, 832])
    mla_wq_d = dr("mla_wq", [128, 4, 192])
    mla_wkv_d = dr("mla_wkv", [128, 2, 256])
    mla_g_d = dr("mla_g", [128, 10])
    rope_d = dr("rope", [2, 64, T])
    rotm_d = dr("rotm", [64, 64])
    w_fox_d = dr("w_fox", [128, KC, 384])
    fox_g_d = dr("fox_g", [128, 3])
    w_ssd_d = dr("w_ssd", [128, KC, 768])
    ssd_conv_d = dr("ssd_conv", [128, 4, 5])
    ssd_vec_d = dr("ssd_vec", [128, 8])
    ssd_row_d = dr("ssd_row", [2, 128, 256])

    sb = lambda n, s, d=F32, st=None: Buf(kb, n, s, d, stack=st)
    kb.eps_tile(EPS)
    kb.eps_tile(1.0)
    cst = sb("cst_sb", [128, 6, 128])
    kb.dma("sp", cst[:, :, :], bc(cst_d[:, :, :], cst_d.t.rearrange("c p f -> p c f")))
    cstb = sb("cst_bf", [128, 6, 128], BF16)
    kb.copy("dve", cstb[:, :, :], cst[:, :, :])
    ident, Umat, MnegT, strictT, causT, ones = [cst[:, i, :] for i in range(6)]
    ident_b, _, _, _, causT_b, ones_b = [cstb[:, i, :] for i in range(6)]
    g1 = sb("g1s", [128, KC])
    kb.dma("sp", g1[:, :], g1d[:, :])
    small_all = sb("small_all", [128, NBLK, W_SMALL])
    pss = [Buf(kb, "ps%d" % i, [128, 512], F32, "psum") for i in range(8)]
    psi = [0]

    def next_ps(n=8):
        p = pss[psi[0] % n]
        psi[0] += 1
        return p

    tiles = token_tiles(T)

    def sq_sum_rstd(srcs, W, scale, rstd, sqs, ps):
        for i, (v, P) in enumerate(srcs):
            sq = sqs[i % len(sqs)]
            kb.act(sq[0:P, 0:W], v, AF.Square)
            kb.mm(ps[:, 0:W], bc(ones_b, ones_b.ap[0:P, :]), sq[0:P, 0:W], start=(i == 0), stop=(i == len(srcs) - 1))
        kb.act(rstd[:, 0:W], ps[:, 0:W], AF.Sqrt, bias=kb.eps_tile(EPS), scale=scale)
        kb.recip(rstd[:, 0:W], rstd[:, 0:W])

    with ExitStack() as st:
        h_sb = sb("h_sb", [128, KC, 512], F32, st)
        hn = sb("hn0", [128, KC, 512], BF16, st)
        sqs = [sb("sq%d" % i, [128, 512], BF16, st) for i in range(2)]
        rstd = sb("rstd", [128, 512], F32, st)
        wsm = sb("wsm", [128, KC, W_SMALL], BF16, st)
        kb.dma("pool", wsm[:, :, :], w_small_d[:, :, :])
        import os
        STOP = int(os.environ.get("A0_STOP", "9"))
        for (t0, W) in tiles:
            if STOP < 1:
                break
            kb.dma("sp", h_sb[:, :, 0:W], bc(hT[:, :, t0:t0 + W], hT.t[:, :, t0:t0 + W].rearrange("k p w -> p k w")))
            if STOP < 2:
                continue
            sq_sum_rstd([(h_sb[:, kc, 0:W], 128) for kc in range(KC)], W, 1.0 / D_MODEL, rstd, sqs, pss[7])
            if STOP < 3:
                continue
            for kc in range(KC):
                kb.stt("dve", hn[:, kc, 0:W], h_sb[:, kc, 0:W], g1[:, kc:kc + 1], rstd[:, 0:W], ALU.mult, ALU.mult)
            if STOP < 4:
                continue
            for kc in range(KC):
                kb.dma("sp", hnT[kc, :, t0:t0 + W], hn[:, kc, 0:W])
            if STOP < 5:
                continue
            for tb in range(W // 128):
                blk = t0 // 128 + tb
                ps = next_ps(4)
                for kc in range(KC):
                    kb.mm(ps[:, 0:W_SMALL], hn[:, kc, tb * 128:(tb + 1) * 128], wsm[:, kc, :],
                          start=(kc == 0), stop=(kc == KC - 1))
                kb.copy("act", small_all[:, blk, :], ps[:, 0:W_SMALL])
    kb.barrier()

    def load_hn(hn_t, t0, W):
        for kc in range(KC):
            kb.dma("sp", hn_t[:, kc, 0:W], hnT[kc, :, t0:t0 + W])

    def proj_fm(ps, w, c0, ncols, hn_t, W):
        for kc in range(KC):
            kb.mm(ps[0:ncols, 0:W], w[:, kc, c0:c0 + ncols], hn_t[:, kc, 0:W], start=(kc == 0), stop=(kc == KC - 1))

    def proj_tm(psv, hn_t, tb, w, c0, ncols):
        for kc in range(KC):
            kb.mm(psv, hn_t[:, kc, tb * 128:(tb + 1) * 128], w[:, kc, c0:c0 + ncols], start=(kc == 0), stop=(kc == KC - 1))

    def softplus(out, in_, bias):
        kb.act(out, in_, AF.Exp, bias=bias)
        kb.act(out, out, AF.Ln, bias=kb.eps_tile(1.0))

    def conv4(acc, pre, wv, W):
        kb.ts("dve", acc[:, 0:W], pre[:, 3:3 + W], wv[:, 3:4], None, op0=ALU.mult)
        for i in (2, 1, 0):
            kb.stt("dve", acc[:, 0:W], pre[:, i:i + W], wv[:, i:i + 1], acc[:, 0:W], ALU.mult, ALU.add)

    def decay_prep(st, g_all, name):
        d = {}
        for nm in ("gcs", "ngcs", "eg", "e2e", "cd"):
            d[nm] = sb(name + nm, [128, NBLK], F32, st)
        ps = next_ps()
        kb.mm(ps[:, 0:NBLK], Umat, g_all[:, :], start=True, stop=True)
        kb.copy("dve", d["gcs"][:, :], ps[:, 0:NBLK])
        kb.ts("dve", d["ngcs"][:, :], d["gcs"][:, :], -1.0, None, op0=ALU.mult)
        kb.act(d["eg"][:, :], d["gcs"][:, :], AF.Exp)
        ps2 = next_ps()
        kb.mm(ps2[:, 0:NBLK], ones, g_all[:, :], start=True, stop=True)
        kb.act(d["cd"][:, :], ps2[:, 0:NBLK], AF.Exp)
        kb.tt("dve", d["e2e"][:, :], ps2[:, 0:NBLK], d["gcs"][:, :], ALU.subtract)
        kb.act(d["e2e"][:, :], d["e2e"][:, :], AF.Exp)
        return d

    def decay_mats(g_all, dec, n, grep, DmT, EGrow):
        kb.ts("dve", grep[:, :], ones, g_all[:, n:n + 1], None, op0=ALU.mult)
        ps = next_ps()
        kb.mm(ps[:, 0:128], grep[:, :], Umat, start=True, stop=True)
        kb.mm(ps[:, 128:256], grep[:, :], Umat, start=True, stop=False)
        kb.mm(ps[:, 128:256], ident, MnegT, start=False, stop=True)
        if EGrow is not None:
            kb.act(EGrow[:, :], ps[:, 0:128], AF.Exp)
        kb.act(DmT[:, :], ps[:, 128:256], AF.Exp, bias=dec["ngcs"][:, n:n + 1])

    def emit_out(slot, src_tm, ncol_chunks, blk, stage, stage_i, psl=None):
        for c in range(ncol_chunks):
            if psl is None:
                ps = next_ps()
            else:
                ps = psl[stage_i[0] % len(psl)]
            kb.mm(ps[:, 0:128], src_tm[:, c * 128:(c + 1) * 128], ident, start=True, stop=True)
            so = stage[stage_i[0] % len(stage)]
            stage_i[0] += 1
            kb.copy("act", so[:, :], ps[:, 0:128])
            kb.dma("sp", bo[slot + c, :, blk * 128:(blk + 1) * 128], so[:, :], final=True)

    try:
      if "gdn" in mixers:
          with ExitStack() as st:
              w = sb("w_gdn_s", [128, KC, 512], BF16, st)
              kb.dma("pool", w[:, :, :], w_gdn_d[:, :, :])
              convw = sb("gdn_convw", [128, 3, 4], F32, st)
              kb.dma("sp", convw[:, :, :], gdn_conv_d[:, :, :])
              gvec = sb("gdn_vec_s", [128, 2], F32, st)
              kb.dma("sp", gvec[:, :], gdn_vec_d[:, :])
              ngt = sb("gdn_ng_s", [128, 128], F32, st)
              kb.dma("sp", ngt[:, :], gdn_ng_d[:, :])
              hns = [sb("ghn%d" % i, [128, KC, 512], BF16, st) for i in range(2)]
              pre = sb("gpre", [128, 3, 515], F32, st)
              acc = [sb("gacc%d" % i, [128, 512], F32, st) for i in range(2)]
              sqs = [sb("gsq%d" % i, [128, 512], BF16, st) for i in range(2)]
              rstd = sb("grstd", [128, 512], F32, st)
              qT = sb("g_qT", [128, T], BF16, st)
              kT = sb("g_kT", [128, T], BF16, st)
              vT = sb("g_vT", [128, T], BF16, st)
              zg = sb("g_zg", [128, NBLK, 128], BF16, st)
              kb.memset("dve", pre[:, :, 0:3], 0.0)
              for ti, (t0, W) in enumerate(tiles):
                  hn_t = hns[ti % 2]
                  load_hn(hn_t, t0, W)
                  for c, dst in enumerate((qT, kT, vT)):
                      ps = next_ps()
                      proj_fm(ps, w, c * 128, 128, hn_t, W)
                      kb.copy("act", pre[:, c, 3:3 + W], ps[:, 0:W])
                      a = acc[c % 2]
                      conv4(a, bc(pre[:, c, :], pre.t[:, c, :]), bc(convw[:, c, :], convw.t[:, c, :]), W)
                      kb.copy("pool", pre[:, c, 0:3], pre[:, c, W:W + 3])
                      if c < 2:
                          kb.act(a[:, 0:W], a[:, 0:W], AF.Silu)
                          sq_sum_rstd([(a[:, 0:W], 128)], W, 1.0, rstd, sqs, pss[7])
                          kb.stt("dve", dst[:, t0:t0 + W], a[:, 0:W], (128.0 ** -0.5) if c == 0 else 1.0, rstd[:, 0:W],
                                 ALU.mult, ALU.mult)
                      else:
                          kb.act(dst[:, t0:t0 + W], a[:, 0:W], AF.Silu)
                  for tb in range(W // 128):
                      blk = t0 // 128 + tb
                      ps = next_ps()
                      proj_tm(ps[:, 0:128], hn_t, tb, w, 384, 128)
                      a = acc[tb % 2]
                      kb.act(a[:, 0:128], ps[:, 0:128], AF.Silu)
                      kb.tt("pool", zg[:, blk, :], a[:, 0:128], ngt[:, :], ALU.mult)
              kb.ck("gdn prep done")
              beta = sb("g_beta", [128, NBLK], F32, st)
              nbeta = sb("g_nbeta", [128, NBLK], F32, st)
              g_all = sb("g_gall", [128, NBLK], F32, st)
              expA = sb("g_expA", [128, 1], F32, st)
              kb.act(beta[:, :], bc(small_all[:, :, 0], small_all.t[:, :, 0]), AF.Sigmoid)
              kb.ts("dve", nbeta[:, :], beta[:, :], -1.0, None, op0=ALU.mult)
              kb.act(expA[:, :], gvec[:, 0:1], AF.Exp)
              kb.ts("dve", expA[:, :], expA[:, :], -1.0, None, op0=ALU.mult)
              softplus(g_all[:, :], bc(small_all[:, :, 1], small_all.t[:, :, 1]), gvec[:, 1:2])
              kb.ts("dve", g_all[:, :], g_all[:, :], expA[:, 0:1], None, op0=ALU.mult)
              kb.ck("gdn scalars")
              dec = decay_prep(st, g_all, "gd_")
              kb.ck("gdn decay_prep")
              NB2 = 4
              mk = lambda n, d=F32, shp=(128, 128): [sb("%s%d" % (n, i), list(shp), d, st) for i in range(NB2)]
              grep_, DmT_, EG_ = mk("g_grep"), mk("g_DmT"), mk("g_EG")
              t1_, Q_, P_, R_ = mk("g_t1"), [mk("g_Q%d" % k) for k in range(2)], [mk("g_P%d" % k) for k in range(2)], [mk("g_R%d" % k) for k in range(2)]
              attnT_, KeT_, QeT_ = mk("g_attnT", BF16), mk("g_KeT", BF16), mk("g_QeT", BF16)
              k2e_, Vt_ = mk("g_k2e", BF16), mk("g_Vt")
              R1_, vnew_ = mk("g_resid"), mk("g_vnew", BF16)
              o_, junk_ = mk("g_o"), mk("g_junk")
              ss_ = mk("g_ss", F32, (128, 1))
              S = sb("g_S", [128, 128], F32, st)
              S_bf = sb("g_Sbf", [128, 128], BF16, st)
              kb.memset("dve", S[:, :], 0.0)
              kb.memset("dve", S_bf[:, :], 0.0)
              stage = [sb("g_stage%d" % i, [128, 128], F32, st) for i in range(2)]
              stage_i = [0]
              def g_stage1(n, ctx):
                  i2 = n % NB2
                  cs = slice(n * 128, (n + 1) * 128)
                  grep, DmT, EG = grep_[i2], DmT_[i2], EG_[i2]
                  decay_mats(g_all, dec, n, grep, DmT, EG)
                  yield
                  psk = next_ps()
                  kb.mm(psk[:, 0:128], kT[:, cs], kT[:, cs], start=True, stop=True)
                  kb.mm(psk[:, 128:256], kT[:, cs], qT[:, cs], start=True, stop=True)
                  t1 = t1_[i2]
                  kb.tt("dve", t1[:, :], DmT[:, :], psk[:, 0:128], ALU.mult)
                  Q0 = Q_[0][i2]
                  kb.stt("dve", Q0[:, :], t1[:, :], nbeta[:, n:n + 1], strictT, ALU.mult, ALU.mult)
                  attnT = attnT_[i2]
                  kb.tt("dve", attnT[:, :], DmT[:, :], psk[:, 128:256], ALU.mult)
                  yield
                  pst = next_ps()
                  kb.mm(pst[:, 0:128], Q0[:, :], ident, start=True, stop=True)
                  P0 = P_[0][i2]
                  kb.copy("act", P0[:, :], pst[:, 0:128])
                  yield
                  Rc = R_[0][i2]
                  kb.tt("pool", Rc[:, :], Q0[:, :], ident, ALU.add)
                  yield
                  Qc, Pc = Q0, P0
                  for k in range(1, 7):
                      psq = next_ps()
                      Pn = P_[k % 2][i2]
                      kb.mm(psq[:, 0:128], Qc[:, :], Pc[:, :], start=True, stop=True)
                      if k < 6:
                          kb.mm(psq[:, 128:256], Pc[:, :], Qc[:, :], start=True, stop=True)
                      kb.copy("act", Pn[:, :], psq[:, 0:128])
                      if k < 6:
                          Qn = Q_[k % 2][i2]
                          kb.copy("dve", Qn[:, :], psq[:, 128:256])
                      yield
                      psr = next_ps()
                      kb.mm(psr[:, 0:128], Pn[:, :], Rc[:, :], start=True, stop=True)
                      Rn = R_[k % 2][i2]
                      kb.tt("dve", Rn[:, :], Rc[:, :], psr[:, 0:128], ALU.add)
                      yield
                      Rc, Pc = Rn, Pn
                      if k < 6:
                          Qc = Qn
                  yield
                  KeT, QeT = KeT_[i2], QeT_[i2]
                  kb.tt("pool", KeT[:, :], kT[:, cs], EG[:, :], ALU.mult)
                  kb.tt("pool", QeT[:, :], qT[:, cs], EG[:, :], ALU.mult)
                  pstk = next_ps()
                  kb.mm(pstk[:, 0:128], kT[:, cs], ident_b, start=True, stop=True)
                  kb.mm(pstk[:, 128:256], vT[:, cs], ident_b, start=True, stop=True)
                  k2e, Vt = k2e_[i2], Vt_[i2]
                  kb.ts("dve", k2e[:, :], pstk[:, 0:128], dec["e2e"][:, n:n + 1], None, op0=ALU.mult)
                  kb.copy("act", Vt[:, :], pstk[:, 128:256])
                  ctx.update(dict(KeT=KeT, QeT=QeT, k2e=k2e, Vt=Vt, Rc=Rc, attnT=attnT))
                  yield

              def g_stage2(n, ctx):
                  i2 = n % NB2
                  KeT, QeT, k2e, Vt, Rc, attnT = (ctx[k_] for k_ in ('KeT', 'QeT', 'k2e', 'Vt', 'Rc', 'attnT'))
                  psa = next_ps()
                  kb.mm(psa[:, 0:128], KeT[:, :], S_bf[:, :], start=True, stop=True)
                  R1 = R1_[i2]
                  kb.tt("dve", R1[:, :], Vt[:, :], psa[:, 0:128], ALU.subtract)
                  kb.mm(psa[:, 128:256], Rc[:, :], R1[:, :], start=True, stop=True)
                  vnew = vnew_[i2]
                  kb.ts("dve", vnew[:, :], psa[:, 128:256], beta[:, n:n + 1], None, op0=ALU.mult)
                  pso = next_ps()
                  kb.mm(pso[:, 0:128], QeT[:, :], S_bf[:, :], start=True, stop=False)
                  kb.mm(pso[:, 0:128], attnT[:, :], vnew[:, :], start=False, stop=True)
                  kb.mm(pso[:, 128:256], k2e[:, :], vnew[:, :], start=True, stop=True)
                  kb.stt("dve", S[:, :], S[:, :], dec["cd"][:, n:n + 1], pso[:, 128:256], ALU.mult, ALU.add)
                  kb.copy("act", S_bf[:, :], S[:, :])
                  ss, junk, o = ss_[i2], junk_[i2], o_[i2]
                  kb.memset("pool", ss[:, :], 0.0)
                  kb.act(junk[:, :], pso[:, 0:128], AF.Square, accum=ss[:, :])
                  kb.act(ss[:, :], ss[:, :], AF.Sqrt, bias=kb.eps_tile(EPS), scale=1.0 / 128)
                  kb.recip(ss[:, :], ss[:, :])
                  kb.stt("dve", o[:, :], pso[:, 0:128], ss[:, 0:1], zg[:, n, :], ALU.mult, ALU.mult)
                  emit_out(0, o, 1, n, stage, stage_i)


              for n0 in range(0, NBLK, NB2):
                  ns = [n for n in range(n0, min(n0 + NB2, NBLK))]
                  ctxs = [dict() for _ in ns]
                  gens = [g_stage1(n, c_) for n, c_ in zip(ns, ctxs)]
                  live = list(gens)
                  while live:
                      for g_ in list(live):
                          try:
                              next(g_)
                          except StopIteration:
                              live.remove(g_)
                  for n, c_ in zip(ns, ctxs):
                      g_stage2(n, c_)
          kb.barrier()
    except StopBuild:
        pass

    def attention(st, slot, q_parts, k_parts, Vaug, tab, name):
        G = 4
        pts = [sb("%s_pt%d" % (name, i), [128, 512], BF16, st) for i in range(3)]
        ot = [sb("%s_ot%d" % (name, i), [128, 128], F32, st) for i in range(2)]
        rc = [sb("%s_rc%d" % (name, i), [128, 1], F32, st) for i in range(2)]
        stage = [sb("%s_stage%d" % (name, i), [128, 128], F32, st) for i in range(2)]
        stage_i = [0]
        pti = [0]
        oi = [0]
        ps_s = [pss[0], pss[1]]
        ps_o4 = [pss[2], pss[3], pss[4], pss[5]]
        si = [0]
        for gi, i0 in enumerate(range(0, NBLK, G)):
            i1 = min(i0 + G, NBLK) - 1
            for j in range(0, i1 + 1):
                is_ = max(i0, j)
                Wq = (i1 - is_ + 1) * 128
                q0 = is_ * 128
                ps = ps_s[si[0] % 2]
                si[0] += 1
                for pi, ((qb, P), (kbuf, _)) in enumerate(zip(q_parts, k_parts)):
                    kb.mm(ps[:, 0:Wq], kbuf[0:P, j * 128:(j + 1) * 128], qb[0:P, q0:q0 + Wq],
                          start=(pi == 0), stop=(pi == len(q_parts) - 1))
                pt = pts[pti[0] % 3]
                pti[0] += 1
                if tab is None and not os.environ.get("NARROW"):
                    kb.act(pt[:, 0:Wq], ps[:, 0:Wq], AF.Exp)
                elif tab is None:
                    for ii in range(is_, i1 + 1):
                        c = (ii - is_) * 128
                        kb.act(pt[:, c:c + 128], ps[:, c:c + 128], AF.Exp)
                else:
                    for ii in range(is_, i1 + 1):
                        c = (ii - is_) * 128
                        kb.act(pt[:, c:c + 128], ps[:, c:c + 128], AF.Exp, bias=tab[:, ii, j:j + 1])
                if j >= i0:
                    kb.tt("pool", pt[:, 0:128], pt[:, 0:128], causT_b, ALU.mult)
                for ii in range(is_, i1 + 1):
                    c = (ii - is_) * 128
                    li = ii - i0
                    dst = ps_o4[li]
                    kb.mm(dst[:, 0:129], pt[:, c:c + 128],
                          bc(Vaug[:, j, :], Vaug.t[:, j, :]), start=(j == 0), stop=(j == ii))
            for ii in range(i0, i1 + 1):
                li = ii - i0
                src = ps_o4[li]
                c0 = 0
                r = rc[oi[0] % 2]
                o = ot[oi[0] % 2]
                oi[0] += 1
                kb.ts("dve", r[:, :], src[:, c0 + 128:c0 + 129], 1e-30, None, op0=ALU.max)
                kb.recip(r[:, :], r[:, :])
                kb.ts("dve", o[:, :], src[:, c0:c0 + 128], r[:, 0:1], None, op0=ALU.mult)
                emit_out(slot, o, 1, ii, stage, stage_i, psl=[pss[6], pss[7]])

    def make_vaug(st, name):
        Vaug = sb(name, [128, NBLK, 129], BF16, st)
        kb.memset("dve", bc(Vaug[:, :, 128:129], Vaug.t[:, :, 128:129]), 1.0)
        return Vaug

    def finish_vaug(Vaug):
        for p0, p1 in ((0, 32), (32, 64), (64, 96), (96, FRONT)):
            kb.memset("dve", bc(Vaug[:, 0, :], Vaug.t[p0:p1, 0, :]), 0.0)

    if "mla" in mixers:
        with ExitStack() as st:
            w = sb("w_mla_s", [128, KC, 832], BF16, st)
            kb.dma("pool", w[:, :, :], w_mla_d[:, :, :])
            wq = sb("mla_wq_s", [128, 4, 192], BF16, st)
            kb.dma("pool", wq[:, :, :], mla_wq_d[:, :, :])
            wkv = sb("mla_wkv_s", [128, 2, 256], BF16, st)
            kb.dma("pool", wkv[:, :, :], mla_wkv_d[:, :, :])
            mg = sb("mla_g_s", [128, 10], F32, st)
            kb.dma("sp", mg[:, :], mla_g_d[:, :])
            mgq = sb("mla_gq_s", [128, 2], F32, st)
            kb.ts("dve", mgq[:, :], mg[:, 6:8], 192.0 ** -0.5, None, op0=ALU.mult)
            ropes = [sb("rope_s%d" % i, [64, 2, 512], F32, st) for i in range(2)]
            rotm = sb("rotm_s", [64, 64], F32, st)
            kb.dma("sp", rotm[:, :], rotm_d[:, :])
            hns = [sb("mhn%d" % i, [128, KC, 512], BF16, st) for i in range(1)]
            lat = sb("m_lat", [128, 6, 512], F32, st)
            latn = sb("m_latn", [128, 6, 512], BF16, st)
            sqs = [sb("msq%d" % i, [128, 512], BF16, st) for i in range(2)]
            rstd = sb("mrstd", [128, 512], F32, st)
            raw = sb("m_raw", [128, 2, 512], F32, st)
            tr_ = sb("m_tr", [64, 512], F32, st)
            ta_ = sb("m_ta", [64, 512], F32, st)
            tb_ = sb("m_tb", [64, 512], F32, st)
            QTn = sb("m_QTn", [128, T], BF16, st)
            QTr = sb("m_QTr", [128, T], BF16, st)
            kb.memset("pool", QTr[64:128, :], 0.0)
            KTn = sb("m_KTn", [128, T], BF16, st)
            KTr = sb("m_KTr", [128, T], BF16, st)
            kb.memset("pool", KTr[64:128, :], 0.0)
            Vaug = make_vaug(st, "m_Vaug")

            def qk_finish(raw, gn, gr, dn, dr_, t0, W, rope):
                sq_sum_rstd([(raw[:, 0, 0:W], 128), (raw[0:64, 1, 0:W], 64)], W, 1.0 / 192, rstd, sqs, pss[7])
                kb.stt("dve", dn[:, t0:t0 + W], raw[:, 0, 0:W], gn, rstd[:, 0:W], ALU.mult, ALU.mult)
                kb.stt("dve", tr_[:, 0:W], raw[0:64, 1, 0:W], gr, rstd[0:64, 0:W], ALU.mult, ALU.mult)
                ps = next_ps(7)
                for c0 in range(0, W, 128):
                    kb.mm(ps[0:64, c0:c0 + 128], rotm[:, :], tr_[:, c0:c0 + 128], start=True, stop=True)
                kb.tt("dve", ta_[:, 0:W], tr_[:, 0:W], rope[:, 0, 0:W], ALU.mult)
                kb.tt("dve", tb_[:, 0:W], ps[0:64, 0:W], rope[:, 1, 0:W], ALU.mult)
                kb.tt("pool", dr_[0:64, t0:t0 + W], ta_[:, 0:W], tb_[:, 0:W], ALU.add)

            for ti, (t0, W) in enumerate(tiles):
                hn_t = hns[0]
                load_hn(hn_t, t0, W)
                rope = ropes[ti % 2]
                kb.dma("sp", rope[:, :, 0:W], bc(rope_d[:, :, t0:t0 + W], rope_d.t[:, :, t0:t0 + W].rearrange("c p t -> p c t")))
                for c in range(6):
                    ps = next_ps(7)
                    proj_fm(ps, w, c * 128, 128, hn_t, W)
                    kb.copy("act", lat[:, c, 0:W], ps[:, 0:W])
                sq_sum_rstd([(lat[:, c, 0:W], 128) for c in range(4)], W, 1.0 / 512, rstd, sqs, pss[7])
                for c in range(4):
                    kb.stt("dve", latn[:, c, 0:W], lat[:, c, 0:W], mg[:, c:c + 1], rstd[:, 0:W], ALU.mult, ALU.mult)
                sq_sum_rstd([(lat[:, c, 0:W], 128) for c in (4, 5)], W, 1.0 / 256, rstd, sqs, pss[7])
                for c in (4, 5):
                    kb.stt("dve", latn[:, c, 0:W], lat[:, c, 0:W], mg[:, c:c + 1], rstd[:, 0:W], ALU.mult, ALU.mult)
                ps = next_ps(7)
                for c in range(4):
                    kb.mm(ps[:, 0:W], wq[:, c, 0:128], latn[:, c, 0:W], start=(c == 0), stop=(c == 3))
                kb.copy("act", raw[:, 0, 0:W], ps[:, 0:W])
                ps = next_ps(7)
                for c in range(4):
                    kb.mm(ps[0:64, 0:W], wq[:, c, 128:192], latn[:, c, 0:W], start=(c == 0), stop=(c == 3))
                kb.copy("act", raw[0:64, 1, 0:W], ps[0:64, 0:W])
                qk_finish(raw, mgq[:, 0:1], mgq[0:64, 1:2], QTn, QTr, t0, W, rope)
                ps = next_ps(7)
                for c in range(2):
                    kb.mm(ps[:, 0:W], wkv[:, c, 0:128], latn[:, 4 + c, 0:W], start=(c == 0), stop=(c == 1))
                kb.copy("act", raw[:, 0, 0:W], ps[:, 0:W])
                ps = next_ps(7)
                proj_fm(ps, w, 768, 64, hn_t, W)
                kb.copy("act", raw[0:64, 1, 0:W], ps[0:64, 0:W])
                qk_finish(raw, mg[:, 8:9], mg[0:64, 9:10], KTn, KTr, t0, W, rope)
                for tb in range(W // 128):
                    blk = t0 // 128 + tb
                    ps = next_ps(7)
                    for c in range(2):
                        kb.mm(ps[:, 0:128], latn[:, 4 + c, tb * 128:(tb + 1) * 128], wkv[:, c, 128:256],
                              start=(c == 0), stop=(c == 1))
                    kb.copy("act", Vaug[:, blk, 0:128], ps[:, 0:128])
            finish_vaug(Vaug)
            if os.environ.get("DUMPQ"):
                kb.dma("pool", bo[3, :, :], QTn[:, :], final=True)
                kb.dma("pool", bo[4, :, :], QTr[:, :], final=True)
            attention(st, 1, [(QTn, 128), (QTr, 128)], [(KTn, 128), (KTr, 128)], Vaug, None, "ma")
        kb.barrier()

    if "fox" in mixers:
        with ExitStack() as st:
            w = sb("w_fox_s", [128, KC, 384], BF16, st)
            kb.dma("pool", w[:, :, :], w_fox_d[:, :, :])
            fg = sb("fox_g_s", [128, 3], F32, st)
            kb.dma("sp", fg[:, :], fox_g_d[:, :])
            fgq = sb("fox_gq", [128, 1], F32, st)
            kb.ts("dve", fgq[:, :], fg[:, 0:1], 128.0 ** -0.5, None, op0=ALU.mult)
            nbf = sb("fox_nbf", [128, 1], F32, st)
            kb.ts("dve", nbf[:, :], fg[:, 2:3], -1.0, None, op0=ALU.mult)
            hns = [sb("fhn%d" % i, [128, KC, 512], BF16, st) for i in range(2)]
            raws = [sb("f_raw%d" % i, [128, 512], F32, st) for i in range(2)]
            sqs = [sb("fsq%d" % i, [128, 512], BF16, st) for i in range(2)]
            rstd = sb("frstd", [128, 512], F32, st)
            QT = sb("f_QT", [128, T], BF16, st)
            KT = sb("f_KT", [128, T], BF16, st)
            Vaug = make_vaug(st, "f_Vaug")
            for ti, (t0, W) in enumerate(tiles):
                hn_t = hns[ti % 2]
                load_hn(hn_t, t0, W)
                for c, (dst, gv) in enumerate(((QT, fgq[:, 0:1]), (KT, fg[:, 1:2]))):
                    ps = next_ps(7)
                    proj_fm(ps, w, c * 128, 128, hn_t, W)
                    raw = raws[c]
                    kb.copy("act", raw[:, 0:W], ps[:, 0:W])
                    sq_sum_rstd([(raw[:, 0:W], 128)], W, 1.0 / 128, rstd, sqs, pss[7])
                    kb.stt("dve", dst[:, t0:t0 + W], raw[:, 0:W], gv, rstd[:, 0:W], ALU.mult, ALU.mult)
                for tb in range(W // 128):
                    blk = t0 // 128 + tb
                    ps = next_ps(7)
                    proj_tm(ps[:, 0:128], hn_t, tb, w, 256, 128)
                    kb.copy("act", Vaug[:, blk, 0:128], ps[:, 0:128])
            finish_vaug(Vaug)
            lf = sb("f_lf", [128, NBLK], F32, st)
            kb.act(lf[:, :], bc(small_all[:, :, 2], small_all.t[:, :, 2]), AF.Exp, bias=nbf[:, 0:1], scale=-1.0)
            kb.act(lf[:, :], lf[:, :], AF.Ln, bias=kb.eps_tile(1.0))
            kb.ts("dve", lf[:, :], lf[:, :], -1.0, None, op0=ALU.mult)
            within = sb("f_within", [128, NBLK], F32, st)
            totB = sb("f_totB", [128, NBLK], F32, st)
            ps = next_ps(7)
            kb.mm(ps[:, 0:NBLK], Umat, lf[:, :], start=True, stop=True)
            kb.copy("dve", within[:, :], ps[:, 0:NBLK])
            ps = next_ps(7)
            kb.mm(ps[:, 0:NBLK], ones, lf[:, :], start=True, stop=True)
            kb.copy("dve", totB[:, :], ps[:, 0:NBLK])
            tab = sb("f_tab", [128, NBLK, NBLK], F32, st)
            for i in range(NBLK):
                if i > 0:
                    kb.ts("dve", tab[:, i, 0:i], tab[:, i - 1, 0:i], totB[:, i:i + 1], None, op0=ALU.add)
                kb.tt("dve", tab[:, i, i:i + 1], totB[:, i:i + 1], within[:, i:i + 1], ALU.subtract)
            attention(st, 2, [(QT, 128)], [(KT, 128)], Vaug, tab, "fa")
        kb.barrier()

    if "ssd" in mixers:
        with ExitStack() as st:
            w = sb("w_ssd_s", [128, KC, 768], BF16, st)
            kb.dma("pool", w[:, :, :], w_ssd_d[:, :, :])
            scv = sb("ssd_conv_s", [128, 4, 5], F32, st)
            kb.dma("sp", scv[:, :, :], ssd_conv_d[:, :, :])
            svec = sb("ssd_vec_s", [128, 8], F32, st)
            kb.dma("sp", svec[:, :], ssd_vec_d[:, :])
            srow = sb("ssd_row_s", [128, 2, 256], F32, st)
            kb.dma("sp", srow[:, :, :], bc(ssd_row_d[:, :, :], ssd_row_d.t.rearrange("c p f -> p c f")))
            hns = [sb("shn%d" % i, [128, KC, 512], BF16, st) for i in range(1)]
            pre = sb("spre", [128, 4, 515], F32, st)
            acc = [sb("sacc%d" % i, [128, 512], F32, st) for i in range(2)]
            xT = [sb("s_xT%d" % i, [128, T], BF16, st) for i in range(2)]
            BT = sb("s_BT", [128, T], BF16, st)
            CT = sb("s_CT", [128, T], BF16, st)
            zs = sb("s_zs", [128, NBLK, 256], BF16, st)
            kb.memset("dve", pre[:, :, 0:3], 0.0)
            dsts = (xT[0], xT[1], BT, CT)
            for ti, (t0, W) in enumerate(tiles):
                hn_t = hns[0]
                load_hn(hn_t, t0, W)
                for c in range(4):
                    ps = next_ps()
                    proj_fm(ps, w, 256 + c * 128, 128, hn_t, W)
                    kb.copy("act", pre[:, c, 3:3 + W], ps[:, 0:W])
                    a = acc[c % 2]
                    conv4(a, bc(pre[:, c, :], pre.t[:, c, :]), bc(scv[:, c, :], scv.t[:, c, :]), W)
                    kb.copy("pool", pre[:, c, 0:3], pre[:, c, W:W + 3])
                    kb.act(dsts[c][:, t0:t0 + W], a[:, 0:W], AF.Silu, bias=scv[:, c, 4:5])
                for tb in range(W // 128):
                    blk = t0 // 128 + tb
                    ps = next_ps()
                    proj_tm(ps[:, 0:256], hn_t, tb, w, 0, 256)
                    kb.act(zs[:, blk, :], ps[:, 0:256], AF.Silu)
            for b_ in (xT[0], xT[1], BT):
                kb.memset("dve", b_[:, 0:FRONT], 0.0)
            negA = sb("s_negA", [128, 4], F32, st)
            kb.act(negA[:, :], svec[:, 4:8], AF.Exp)
            kb.ts("dve", negA[:, :], negA[:, :], -1.0, None, op0=ALU.mult)
            dts, decs, a_alls = [], [], []
            for h in range(4):
                dt = sb("s_dt%d" % h, [128, NBLK], F32, st)
                a_all = sb("s_a%d" % h, [128, NBLK], F32, st)
                softplus(dt[:, :], bc(small_all[:, :, 3 + h], small_all.t[:, :, 3 + h]), svec[:, h:h + 1])
                kb.ts("dve", a_all[:, :], dt[:, :], negA[:, h:h + 1], None, op0=ALU.mult)
                dts.append(dt)
                a_alls.append(a_all)
                decs.append(decay_prep(st, a_all, "sd%d_" % h))
            mk = lambda n, d=F32, shp=(128, 128), k=2: [sb("%s%d" % (n, i), list(shp), d, st) for i in range(k)]
            grep_, DmT_, EG_ = mk("s_grep", k=4), mk("s_DmT", k=4), mk("s_EG", k=4)
            attnT_, CeT_ = mk("s_attnT", BF16, k=4), mk("s_CeT", BF16, k=4)
            Btok_ = mk("s_Btok", BF16)
            Xtok_ = mk("s_Xtok", F32, (128, 256))
            Xdt_ = mk("s_Xdt", BF16, (128, 256))
            Xdec_ = mk("s_Xdec", BF16, (128, 256))
            CBt_ = mk("s_CBt")
            y1_ = mk("s_y1", F32, (128, 256))
            y2_ = mk("s_y2", F32, (128, 256))
            junk_ = mk("s_junk", F32, (128, 256))
            ss_ = mk("s_ss", F32, (128, 1))
            S = sb("s_S", [128, 256], F32, st)
            S_bf = sb("s_Sbf", [128, 256], BF16, st)
            kb.memset("dve", S[:, :], 0.0)
            kb.memset("dve", S_bf[:, :], 0.0)
            stage = [sb("s_stage%d" % i, [128, 128], F32, st) for i in range(2)]
            stage_i = [0]
            hh = [0]
            for n in range(NBLK):
                i2 = n % 2
                cs = slice(n * 128, (n + 1) * 128)
                pst = next_ps()
                kb.mm(pst[:, 0:128], BT[:, cs], ident_b, start=True, stop=True)
                kb.mm(pst[:, 128:256], xT[0][:, cs], ident_b, start=True, stop=True)
                kb.mm(pst[:, 256:384], xT[1][:, cs], ident_b, start=True, stop=True)
                Btok, Xtok, Xdt, Xdec = Btok_[i2], Xtok_[i2], Xdt_[i2], Xdec_[i2]
                kb.copy("act", Btok[:, :], pst[:, 0:128])
                kb.copy("act", Xtok[:, :], pst[:, 128:384])
                for h in range(4):
                    hs = slice(h * 64, (h + 1) * 64)
                    kb.ts("dve", Xdt[:, hs], Xtok[:, hs], dts[h][:, n:n + 1], None, op0=ALU.mult)
                    kb.ts("dve", Xdec[:, hs], Xtok[:, hs], dts[h][:, n:n + 1], decs[h]["e2e"][:, n:n + 1],
                          op0=ALU.mult, op1=ALU.mult)
                psc = next_ps()
                kb.mm(psc[:, 0:128], BT[:, cs], CT[:, cs], start=True, stop=True)
                CBt = CBt_[i2]
                kb.copy("act", CBt[:, :], psc[:, 0:128])
                for h in range(4):
                    decay_mats(a_alls[h], decs[h], n, grep_[h], DmT_[h], EG_[h])
                for h in range(4):
                    kb.tt("dve", attnT_[h][:, :], CBt[:, :], DmT_[h][:, :], ALU.mult)
                    kb.tt("pool", CeT_[h][:, :], CT[:, cs], EG_[h][:, :], ALU.mult)
                psy = next_ps()
                for h in range(4):
                    hs = slice(h * 64, (h + 1) * 64)
                    kb.mm(psy[:, hs], CeT_[h][:, :], S_bf[:, hs], start=True, stop=False)
                    kb.mm(psy[:, hs], attnT_[h][:, :], Xdt[:, hs], start=False, stop=True)
                psn = next_ps()
                kb.mm(psn[:, 0:256], Btok[:, :], Xdec[:, :], start=True, stop=True)
                for h in range(4):
                    hs = slice(h * 64, (h + 1) * 64)
                    kb.stt("dve", S[:, hs], S[:, hs], decs[h]["cd"][:, n:n + 1], psn[:, hs], ALU.mult, ALU.add)
                kb.copy("act", S_bf[:, :], S[:, :])
                y1, y2, junk, ss = y1_[i2], y2_[i2], junk_[i2], ss_[i2]
                kb.tt("pool", y1[:, :], Xtok[:, :], bc(srow[:, 0, :], srow.t[:, 0, :]), ALU.mult)
                kb.tt("dve", y1[:, :], y1[:, :], psy[:, 0:256], ALU.add)
                kb.tt("pool", y2[:, :], y1[:, :], bc(zs[:, n, :], zs.t[:, n, :]), ALU.mult)
                kb.memset("pool", ss[:, :], 0.0)
                kb.act(junk[:, :], y2[:, :], AF.Square, accum=ss[:, :])
                kb.act(ss[:, :], ss[:, :], AF.Sqrt, bias=kb.eps_tile(EPS), scale=1.0 / 256)
                kb.recip(ss[:, :], ss[:, :])
                kb.stt("dve", y1[:, :], y2[:, :], ss[:, 0:1], bc(srow[:, 1, :], srow.t[:, 1, :]), ALU.mult, ALU.mult)
                emit_out(3, y1, 2, n, stage, stage_i)
        kb.barrier()
    kb.finish()
    return kb, locals()


IN_WIDTHS = (512, 512, 512, 512, 4, 4, 512, 256, 64, 512, 512, 512, 4, 512, 512, 256, 256, 8)
OFF = [0]
for _w in IN_WIDTHS:
    OFF.append(OFF[-1] + _w)


def r3(w):
    K, C = w.shape
    return np.ascontiguousarray(w.reshape(K // 128, 128, C).transpose(1, 0, 2))


def rep(v, n=128):
    v = np.asarray(v, np.float32).reshape(1, -1)
    return np.ascontiguousarray(np.broadcast_to(v, (n, v.shape[1])))


def make_consts(T):
    p = np.arange(128)[:, None]
    f = np.arange(128)[None, :]
    cst = np.stack([
        (p == f), (p <= f), np.where(f >= p, 0.0, -30000.0), (f > p), (f >= p), np.ones((128, 128)),
    ]).astype(np.float32)
    pos = np.maximum(np.arange(T) - FRONT, 0).astype(np.float32)
    inv = (1.0 / (10000.0 ** (np.arange(0, 64, 2, dtype=np.float32) / 64.0))).astype(np.float32)
    ang = pos[None, :] * np.concatenate([inv, inv])[:, None]
    rope = np.stack([np.cos(ang), np.sin(ang)]).astype(np.float32)
    rot = np.zeros((64, 64), np.float32)
    for ff in range(32):
        rot[ff, ff + 32] = -1.0
        rot[ff + 32, ff] = 1.0
    return cst, rope, np.ascontiguousarray(rot.T)


def phaseA_inputs(P, l, j, hT_b, T):
    G = j // 2
    w_in = P["w_in"][l]
    col = lambda o, a, n: w_in[:, OFF[o] + a:OFF[o] + a + n]
    cst, rope, rotT = make_consts(T)
    small = np.concatenate([col(4, j, 1), col(5, j, 1), col(12, j, 1), col(17, G * 4, 4),
                            np.zeros((2048, 1), np.float32)], axis=1)
    w_gdn = np.concatenate([col(0, j * 128, 128), col(1, j * 128, 128), col(2, j * 128, 128), col(3, j * 128, 128)], 1)
    cw = P["gdn_conv_w"][l]
    gdn_conv = np.stack([cw[:, j * 128:(j + 1) * 128].T, cw[:, 512 + j * 128:512 + (j + 1) * 128].T,
                         cw[:, 1024 + j * 128:1024 + (j + 1) * 128].T], axis=1)
    w_mla = np.concatenate([col(6, 0, 512), col(7, 0, 256), col(8, 0, 64)], 1)
    mla_g = np.zeros((128, 10), np.float32)
    mla_g[:, 0:4] = P["mla_qa_g"][l].reshape(4, 128).T
    mla_g[:, 4:6] = P["mla_kva_g"][l].reshape(2, 128).T
    mla_g[:, 6] = P["mla_qn_g"][l][0:128]
    mla_g[0:64, 7] = P["mla_qn_g"][l][128:192]
    mla_g[:, 8] = P["mla_kn_g"][l][0:128]
    mla_g[0:64, 9] = P["mla_kn_g"][l][128:192]
    w_fox = np.concatenate([col(9, j * 128, 128), col(10, j * 128, 128), col(11, j * 128, 128)], 1)
    fox_g = np.stack([P["fox_qn_g"][l], P["fox_kn_g"][l], np.full(128, P["fox_b_f"][l][j], np.float32)], 1)
    w_ssd = np.concatenate([col(13, G * 256, 256), col(14, G * 256, 256), col(15, G * 128, 128), col(16, G * 128, 128)], 1)
    sw = P["ssd_conv_w"][l]
    sbias = P["ssd_conv_b"][l]
    chs = [slice(G * 256, G * 256 + 128), slice(G * 256 + 128, G * 256 + 256),
           slice(512 + G * 128, 512 + (G + 1) * 128), slice(768 + G * 128, 768 + (G + 1) * 128)]
    ssd_conv = np.stack([np.concatenate([sw[:, c].T, sbias[c][:, None]], 1) for c in chs], axis=1)
    ssd_vec = rep(np.concatenate([P["ssd_dt_bias"][l][G * 4:G * 4 + 4], P["ssd_A_log"][l][G * 4:G * 4 + 4]]))
    ssd_row = np.stack([rep(np.repeat(P["ssd_D"][l][G * 4:G * 4 + 4], 64)), rep(P["ssd_norm_g"][l][G * 256:(G + 1) * 256])])
    f32c = lambda a: np.ascontiguousarray(a, dtype=np.float32)
    return {
        "hT": f32c(hT_b), "g1": f32c(P["mix_norm_g"][l].reshape(KC, 128).T), "w_small": r3(f32c(small)), "cst": cst,
        "w_gdn": r3(f32c(w_gdn)), "gdn_conv": f32c(gdn_conv),
        "gdn_vec": rep([P["gdn_A_log"][l][j], P["gdn_dt_bias"][l][j]]), "gdn_ng": rep(P["gdn_norm_g"][l]),
        "w_mla": r3(f32c(w_mla)), "mla_wq": r3(f32c(P["mla_wq_b"][l][:, j * 192:(j + 1) * 192])),
        "mla_wkv": r3(f32c(P["mla_wkv_b"][l][:, j * 256:(j + 1) * 256])), "mla_g": mla_g,
        "rope": rope, "rotm": rotT,
        "w_fox": r3(f32c(w_fox)), "fox_g": f32c(fox_g),
        "w_ssd": r3(f32c(w_ssd)), "ssd_conv": f32c(ssd_conv), "ssd_vec": ssd_vec, "ssd_row": f32c(ssd_row),
    }


def seq_to_hT(h_seq, T):
    L = h_seq.shape[0]
    out = np.zeros((2048, T), np.float32)
    out[:, FRONT:FRONT + L] = h_seq.T
    return out.reshape(KC, 128, T)


_PROGS = {}


def _prog(key, fn):
    if key not in _PROGS:
        _PROGS[key] = fn()
    return _PROGS[key]


def panels(w3, pw=256):
    p, K, C = w3.shape
    n = (C + pw - 1) // pw
    if n * pw != C:
        w3 = np.concatenate([w3, np.zeros((p, K, n * pw - C), w3.dtype)], axis=2)
    return np.ascontiguousarray(w3.reshape(p, K, n, pw).transpose(2, 0, 1, 3))


def phaseB_weights(P, l):
    f32c = lambda a: np.ascontiguousarray(a, dtype=np.float32)
    d = {
        "wgate": np.stack([panels(r3(f32c(P["w_gate"][l][b]))) for b in range(4)]),
        "wbr": np.stack([r3(f32c(P["w_branch"][l][b])) for b in range(4)]),
        "wo": panels(r3(f32c(P["w_o"][l]))),
        "g1": f32c(P["mix_norm_g"][l].reshape(KC, 128).T),
        "g2": f32c(P["ffn_norm_g"][l].reshape(KC, 128).T),
    }
    i = l // 2
    if l % 2 == 0:
        d.update({"wfg": panels(r3(f32c(P["dense_w_gate"][i]))), "wfu": panels(r3(f32c(P["dense_w_up"][i]))),
                  "wfd": panels(r3(f32c(P["dense_w_down"][i])), 128)})
    else:
        sel = np.zeros((N_EXP, N_EXP, 128), np.float32)
        for e in range(N_EXP):
            sel[e, e, :] = 1.0
        d.update({"wr": r3(f32c(P["router_w"][i])),
                  "weg": np.stack([panels(r3(f32c(P["moe_w_gate"][i][e]))) for e in range(N_EXP)]),
                  "weu": np.stack([panels(r3(f32c(P["moe_w_up"][i][e]))) for e in range(N_EXP)]),
                  "wed": np.stack([panels(r3(f32c(P["moe_w_down"][i][e])), 128) for e in range(N_EXP)]),
                  "sel": sel, "ident": np.eye(128, dtype=np.float32)})
    return d


def kernel(**inputs):
    P = {k: np.asarray(v) for k, v in inputs.items()}
    x = P["x"].astype(np.float32, copy=False)
    Bsz, S, D = x.shape
    NX = S // 128
    NBLK = NX + 1
    T = NBLK * 128
    NXB = NX // 4
    NTB = NXB + 1
    depth = P["w_in"].shape[0]
    meta = P["meta_tokens"].astype(np.float32)
    hT = [seq_to_hT(np.concatenate([meta, x[b]], 0), T) for b in range(Bsz)]
    cores = list(range(8))
    for l in range(depth):
        kbA, _ = _prog(("A", NBLK), lambda: build_phaseA(NBLK))
        in_maps = [phaseA_inputs(P, l, c % 4, hT[c // 4], T) for c in cores]
        resA = run_bass_kernel_spmd(kbA.nc, in_maps, core_ids=cores)
        br = []
        for b in range(Bsz):
            bos = [resA.results[b * 4 + j]["bo"] for j in range(4)]
            rows = [bos[j][0] for j in range(4)] + [bos[j][1] for j in range(4)] + [bos[j][2] for j in range(4)] \
                + [bos[j][3 + (j % 2)] for j in range(4)]
            br.append(np.stack(rows))
        del resA
        kind = "dense" if l % 2 == 0 else "moe"
        last = (l == depth - 1)
        ntb = NXB if last else NTB
        kbB = _prog(("B", ntb, kind), lambda: build_phaseB(ntb, kind))
        wts = phaseB_weights(P, l)
        in_maps = []
        for c in cores:
            b, q = c // 4, c % 4
            if last:
                cols = np.r_[128 + q * NXB * 128:128 + (q + 1) * NXB * 128]
            else:
                cols = np.r_[0:128, 128 + q * NXB * 128:128 + (q + 1) * NXB * 128]
            m = dict(wts)
            m["hT"] = np.ascontiguousarray(hT[b][:, :, cols])
            m["br"] = np.ascontiguousarray(br[b][:, :, cols])
            in_maps.append(m)
        resB = run_bass_kernel_spmd(kbB.nc, in_maps, core_ids=cores)
        for c in cores:
            b, q = c // 4, c % 4
            o = resB.results[c]["hTo"]
            if last:
                hT[b][:, :, 128 + q * NXB * 128:128 + (q + 1) * NXB * 128] = o
                continue
            if q == 0:
                hT[b][:, :, FRONT:128] = o[:, :, FRONT:128]
            hT[b][:, :, 128 + q * NXB * 128:128 + (q + 1) * NXB * 128] = o[:, :, 128:]
        del resB
    out = np.stack([np.ascontiguousarray(hT[b].reshape(D, T)[:, 128:].T) for b in range(Bsz)])
    return out.astype(np.float32)
```

```python
import os
import numpy as np
import concourse.bass as bass
import concourse.mybir as mybir
from concourse.bass_utils import run_bass_kernel_spmd

F32 = mybir.dt.float32
BF16 = mybir.dt.bfloat16
AF = mybir.ActivationFunctionType
ALU = mybir.AluOpType
AX = mybir.AxisListType


class Res:
    __slots__ = ("w", "r", "dsem", "dcnt")

    def __init__(self):
        self.w = None
        self.r = {}
        self.dsem = None
        self.dcnt = 0


class V:
    __slots__ = ("ap", "res", "space")

    def __init__(self, ap, res, space="sbuf"):
        self.ap = ap
        self.res = res
        self.space = space

    def __getitem__(self, idx):
        return V(self.ap[idx], self.res, self.space)


class Buf:
    def __init__(self, kb, name, shape, dtype, space="sbuf", kind=None, nres=1, stack=None):
        self.kb = kb
        nc = kb.nc
        self.name = name
        if space == "sbuf":
            if stack is not None:
                self.t = stack.enter_context(nc.sbuf_tensor(name, list(shape), dtype))
            else:
                self.t = nc.alloc_sbuf_tensor(name, list(shape), dtype)
        elif space == "psum":
            self.t = nc.alloc_psum_tensor(name, list(shape), dtype)
        else:
            self.t = nc.dram_tensor(name, list(shape), dtype, kind=kind or "Internal").ap()
        self.res = [Res() for _ in range(nres)]
        self.space = space

    def __getitem__(self, idx):
        return V(self.t[idx], self.res, self.space)

    def sub(self, ri, idx):
        return V(self.t[idx], [self.res[ri]], self.space)


def bc(v, ap):
    return V(ap, v.res, v.space)


class StopBuild(Exception):
    pass


class KB:
    ENG = ("pe", "act", "dve", "pool", "sp")

    def ck(self, name=""):
        import os
        lim = int(os.environ.get("STOP_AT", "0"))
        self._ck = getattr(self, "_ck", 0) + 1
        if lim and self._ck >= lim:
            print("STOP at checkpoint", self._ck, name)
            raise StopBuild()

    def __init__(self):
        self.nc = bass.Bass("TRN2", target_bir_lowering=False)
        nc = self.nc
        self.e = {"pe": nc.tensor, "act": nc.scalar, "dve": nc.vector, "pool": nc.gpsimd, "sp": nc.sync}
        self.sem = {}
        self.cnt = {}
        self.seen = {k: {} for k in self.ENG}
        self.semh = {}
        for k in self.ENG:
            h = nc.alloc_semaphore("sem_" + k)
            self.semh[k] = h
            self.cnt[k] = 0
        self.ndsem = 0
        self.dma_max = {}
        self.out_tokens = []
        self.ninstr = 0

    def _dsem(self, res):
        if res.dsem is None:
            key = "d%d" % self.ndsem
            self.ndsem += 1
            self.semh[key] = self.nc.alloc_semaphore("sem_" + key)
            res.dsem = key
        return res.dsem

    def _collect(self, eng, outs, ins):
        waits = {}

        def need(tok):
            if tok is None:
                return
            s, val = tok
            if waits.get(s, 0) < val:
                waits[s] = val

        for v in ins:
            for r in v.res:
                need(r.w)
                if v.space == "psum":
                    for s, val in r.r.items():
                        if s != eng:
                            need((s, val))
        for v in outs:
            for r in v.res:
                if r.w is not None:
                    need(r.w)
                for s, val in r.r.items():
                    need((s, val))
        if eng == "pe" and "pe" in waits:
            del waits["pe"]
        return waits

    def _dowaits(self, eng, waits):
        E = self.e[eng]
        seen = self.seen[eng]
        for s, val in waits.items():
            if seen.get(s, 0) >= val:
                continue
            E.wait_ge(self.semh[s], val)
            seen[s] = val

    def op(self, eng, fn, outs, ins):
        waits = self._collect(eng, outs, ins)
        self._dowaits(eng, waits)
        ins_ = fn()
        self.cnt[eng] += 1
        c = self.cnt[eng]
        ins_.then_inc(self.semh[eng], 1)
        for v in ins:
            for r in v.res:
                r.r[eng] = c
        for v in outs:
            for r in v.res:
                r.w = (eng, c)
                r.r = {}
        self.ninstr += 1
        return ins_

    def dma(self, q, out, in_, final=False):
        waits = self._collect("__dma__", [out], [in_])
        self._dowaits(q, waits)
        if out.space == "sbuf":
            sres = out.res[0]
        elif in_.space == "sbuf":
            sres = in_.res[0]
        else:
            sres = out.res[0]
        key = self._dsem(sres)
        ins_ = self.e[q].dma_start(out=out.ap, in_=in_.ap)
        sres.dcnt += 16
        ins_.then_inc(self.semh[key], 16)
        tok = (key, sres.dcnt)
        self.dma_max[key] = sres.dcnt
        for r in in_.res:
            r.r[key] = max(r.r.get(key, 0), sres.dcnt)
        for r in out.res:
            r.w = tok
            r.r = {}
        if final:
            self.out_tokens.append(tok)
        self.ninstr += 1
        return ins_

    def transfer(self, olds, news):
        toks = {}
        for b in olds:
            for r in b.res:
                if r.w is not None:
                    toks[r.w[0]] = max(toks.get(r.w[0], 0), r.w[1])
                for s, val in r.r.items():
                    toks[s] = max(toks.get(s, 0), val)
        for b in news:
            for r in b.res:
                r.w = None
                r.r = dict(toks)

    def barrier(self):
        allw = {k: self.cnt[k] for k in ("pe", "act", "dve", "pool") if self.cnt[k] > 0}
        for key, val in self.dma_max.items():
            allw[key] = val
        for e in self.ENG:
            w = {k: v for k, v in allw.items() if k != e}
            self._dowaits(e, w)

    def finish(self):
        waits = {}
        for s, val in self.out_tokens:
            waits[s] = max(waits.get(s, 0), val)
        self._dowaits("sp", waits)
        for k in ("pe", "act", "dve", "pool"):
            if self.cnt[k] > 0:
                self._dowaits("sp", {k: self.cnt[k]})

    def mm(self, out, lhsT, rhs, start=True, stop=True):
        nc = self.nc
        return self.op("pe", lambda: nc.tensor.matmul(out.ap, lhsT.ap, rhs.ap, start=start, stop=stop),
                       [out], [lhsT, rhs])

    def tr(self, out, in_, ident):
        nc = self.nc
        return self.op("pe", lambda: nc.tensor.transpose(out.ap, in_.ap, ident.ap), [out], [in_, ident])

    def act(self, out, in_, func, bias=None, scale=1.0, accum=None, eng="act"):
        nc = self.nc
        ins = [in_]
        kw = {}
        if bias is not None:
            if isinstance(bias, V):
                ins.append(bias)
                kw["bias"] = bias.ap
            else:
                kw["bias"] = bias
        if isinstance(scale, V):
            ins.append(scale)
            kw["scale"] = scale.ap
        else:
            kw["scale"] = scale
        outs = [out]
        if accum is not None:
            outs.append(accum)
            kw["accum_out"] = accum.ap
        return self.op("act", lambda: nc.scalar.activation(out=out.ap, in_=in_.ap, func=func, **kw), outs, ins)

    def tt(self, eng, out, a, b, op):
        E = self.e[eng]
        return self.op(eng, lambda: E.tensor_tensor(out=out.ap, in0=a.ap, in1=b.ap, op=op), [out], [a, b])

    def ts(self, eng, out, a, s1, s2=None, op0=ALU.mult, op1=None, accum=None):
        E = self.e[eng]
        ins = [a]
        a1 = s1
        a2 = s2
        if isinstance(s1, V):
            ins.append(s1)
            a1 = s1.ap
        if isinstance(s2, V):
            ins.append(s2)
            a2 = s2.ap
        kw = {}
        if op1 is not None:
            kw["op1"] = op1
        outs = [out]
        if accum is not None:
            outs.append(accum)
            kw["accum_out"] = accum.ap
        return self.op(eng, lambda: E.tensor_scalar(out=out.ap, in0=a.ap, scalar1=a1, scalar2=a2, op0=op0, **kw),
                       outs, ins)

    def stt(self, eng, out, a, s, b, op0, op1):
        E = self.e[eng]
        ins = [a, b]
        sa = s
        if isinstance(s, V):
            ins.append(s)
            sa = s.ap
        return self.op(eng, lambda: E.scalar_tensor_tensor(out=out.ap, in0=a.ap, scalar=sa, in1=b.ap, op0=op0, op1=op1),
                       [out], ins)

    def copy(self, eng, out, in_):
        if eng == "act":
            nc = self.nc
            return self.op("act", lambda: nc.scalar.copy(out=out.ap, in_=in_.ap), [out], [in_])
        E = self.e[eng]
        return self.op(eng, lambda: E.tensor_copy(out=out.ap, in_=in_.ap), [out], [in_])

    def recip(self, out, in_):
        nc = self.nc
        return self.op("dve", lambda: nc.vector.reciprocal(out=out.ap, in_=in_.ap), [out], [in_])

    def eps_tile(self, val):
        if not hasattr(self, "_eps"):
            self._eps = {}
        if val not in self._eps:
            b = Buf(self, "cst%d" % len(self._eps), [128, 1], F32)
            self.memset("dve", b[:, :], float(val))
            self._eps[val] = b
        return self._eps[val][:, :]

    def memset(self, eng, out, val):
        E = self.e[eng]
        return self.op(eng, lambda: E.memset(out.ap, val), [out], [])


class ABuf:
    def __init__(self, ap, space="sbuf", nres=1):
        self.t = ap
        self.res = [Res() for _ in range(nres)]
        self.space = space

    def __getitem__(self, idx):
        return V(self.t[idx], self.res, self.space)


D_MODEL = 2048
KC = 16
EPS = 1e-6
D_FF = 5632
NFF = D_FF // 128
N_EXP = 8
D_FFE = 1408
NFE = D_FFE // 128


def token_tiles(NT, W=512):
    out = []
    t = 0
    while t < NT:
        w = min(W, NT - t)
        out.append((t, w))
        t += w
    return out


def rmsnorm_fm(kb, h_sb, hn, g_sb, ones_bf, sqs, ps_ss, rstd, W, D=D_MODEL, post=None):
    nkc = D // 128
    for kc in range(nkc):
        sq = sqs[kc % len(sqs)]
        kb.act(sq[:, 0:W], h_sb[:, kc, 0:W], AF.Square)
        kb.mm(ps_ss[:, 0:W], ones_bf[:, :], sq[:, 0:W], start=(kc == 0), stop=(kc == nkc - 1))
    kb.act(rstd[:, 0:W], ps_ss[:, 0:W], AF.Sqrt, bias=kb.eps_tile(EPS))
    kb.recip(rstd[:, 0:W], rstd[:, 0:W])
    for kc in range(nkc):
        eng = "dve"
        if post is None:
            kb.stt(eng, hn[:, kc, 0:W], h_sb[:, kc, 0:W], g_sb[:, kc:kc + 1], rstd[:, 0:W], ALU.mult, ALU.mult)
        else:
            post(kc, eng)


def build_phaseB(NTB, kind):
    kb = KB()
    nc = kb.nc
    NT = NTB * 128
    D = D_MODEL
    dr = lambda n, s, k="ExternalInput": Buf(kb, n, s, F32, "dram", k)
    hT = dr("hT", [KC, 128, NT])
    br = dr("br", [16, 128, NT])
    wgate = dr("wgate", [4, D // 256, 128, KC, 256])
    wbr = dr("wbr", [4, 128, 4, D])
    wo = dr("wo", [D // 256, 128, KC, 256])
    g1d = dr("g1", [128, KC])
    g2d = dr("g2", [128, KC])
    if kind == "dense":
        wfg = dr("wfg", [D_FF // 256, 128, KC, 256])
        wfu = dr("wfu", [D_FF // 256, 128, KC, 256])
        wfd = dr("wfd", [KC, 128, NFF, 128])
    else:
        wr = dr("wr", [128, KC, N_EXP])
        weg = dr("weg", [N_EXP, 6, 128, KC, 256])
        weu = dr("weu", [N_EXP, 6, 128, KC, 256])
        wed = dr("wed", [N_EXP, KC, 128, NFE, 128])
        seld = dr("sel", [N_EXP, N_EXP, 128])
        identd = dr("ident", [128, 128])
    hTo = dr("hTo", [KC, 128, NT], "ExternalOutput")

    sb = lambda n, s, d=F32: Buf(kb, n, s, d)
    h_sb = sb("h_sb", [128, KC, 512])
    hn = sb("hn", [128, KC, 512], BF16)
    merged = sb("merged", [128, KC, 512], BF16)
    sqs = [sb("sq%d" % i, [128, 512], BF16) for i in range(2)]
    rstd = sb("rstd", [128, 512])
    X = nc.alloc_sbuf_tensor("X", [128, 12288], F32)
    macc = ABuf(X[:, 0:8192].rearrange("p (a b) -> p a b", b=512))
    br_sb = ABuf(X[:, 8192:12288].bitcast(BF16).rearrange("p (a b) -> p a b", b=512))
    actb = ABuf(X[:, 0:11264].bitcast(BF16).rearrange("p (a b) -> p a b", b=512))
    NWP = 3
    wps = [sb("wp%d" % i, [128, KC, 256], BF16) for i in range(NWP)]
    wbs = sb("wbs", [128, 4, D], BF16)
    wds = [sb("wd%d" % i, [128, NFF, 128], BF16) for i in range(2)]
    g1 = sb("g1s", [128, KC])
    g2 = sb("g2s", [128, KC])
    ones_bf = sb("ones_bf", [128, 128], BF16)
    sgs = [sb("sg%d" % i, [128, 512]) for i in range(2)]
    tmps = [sb("tmp%d" % i, [128, 512]) for i in range(2)]
    pss = [Buf(kb, "ps%d" % i, [128, 512], F32, "psum") for i in range(8)]
    if kind == "moe":
        stg = [sb("stg%d" % i, [128, 512]) for i in range(2)]
        wr_sb = sb("wr_sb", [128, KC, N_EXP])
        lg = sb("lg", [128, 4, 8])
        top8 = sb("top8", [128, 4, 8])
        comb = sb("comb", [128, 4, 8])
        cw = sb("cw", [128, 4, 8])
        combT = sb("combT", [8, 512], BF16)
        sel = sb("sel_sb", [N_EXP, N_EXP, 128], BF16)
        combB = [sb("combB%d" % i, [128, 512]) for i in range(2)]
        ident = sb("ident_sb", [128, 128])
        kb.dma("sp", wr_sb[:, :, :], wr[:, :, :])
        kb.dma("pool", sel[:, :, :], seld[:, :, :])
        kb.dma("sp", ident[:, :], identd[:, :])

    kb.dma("sp", g1[:, :], g1d[:, :])
    kb.dma("sp", g2[:, :], g2d[:, :])
    kb.memset("dve", ones_bf[:, :], 1.0 / D)

    wpi = [0]

    def next_wp():
        b = wps[wpi[0] % NWP]
        wpi[0] += 1
        return b

    psi = [0]

    def next_ps():
        p = pss[psi[0] % 6]
        psi[0] += 1
        return p

    ps_ss = pss[6]
    ps_misc = pss[7]
    cnt = [0]

    for (t0, W) in token_tiles(NT):
        for kc in range(KC):
            kb.dma("sp", h_sb[:, kc, 0:W], hT[kc, :, t0:t0 + W])
        rmsnorm_fm(kb, h_sb, hn, g1, ones_bf, sqs, ps_ss, rstd, W)
        kb.transfer([actb], [macc, br_sb])
        for i in range(16):
            kb.dma("pool", br_sb[:, i, 0:W], br[i, :, t0:t0 + W])
        for b in range(4):
            kb.dma("pool", wbs[:, :, :], wbr[b, :, :, :])
            for pc in range(D // 256):
                wp = next_wp()
                kb.dma("pool", wp[:, :, :], wgate[b, pc, :, :, :])
                for mm_ in range(2):
                    m = pc * 2 + mm_
                    pg = next_ps()
                    for kc in range(KC):
                        kb.mm(pg[:, 0:W], wp[:, kc, mm_ * 128:(mm_ + 1) * 128], hn[:, kc, 0:W],
                              start=(kc == 0), stop=(kc == KC - 1))
                    pb = next_ps()
                    for hh in range(4):
                        kb.mm(pb[:, 0:W], wbs[:, hh, m * 128:(m + 1) * 128], br_sb[:, b * 4 + hh, 0:W],
                              start=(hh == 0), stop=(hh == 3))
                    sg = sgs[cnt[0] % 2]
                    tmp = tmps[cnt[0] % 2]
                    cnt[0] += 1
                    kb.act(sg[:, 0:W], pg[:, 0:W], AF.Sigmoid)
                    if b == 0:
                        kb.tt("dve", macc[:, m, 0:W], sg[:, 0:W], pb[:, 0:W], ALU.mult)
                    elif b < 3:
                        kb.tt("dve", tmp[:, 0:W], sg[:, 0:W], pb[:, 0:W], ALU.mult)
                        kb.tt("dve", macc[:, m, 0:W], macc[:, m, 0:W], tmp[:, 0:W], ALU.add)
                    else:
                        kb.tt("dve", tmp[:, 0:W], sg[:, 0:W], pb[:, 0:W], ALU.mult)
                        kb.tt("dve", merged[:, m, 0:W], macc[:, m, 0:W], tmp[:, 0:W], ALU.add)
        for pc in range(D // 256):
            wp = next_wp()
            kb.dma("pool", wp[:, :, :], wo[pc, :, :, :])
            for mm_ in range(2):
                m = pc * 2 + mm_
                po = next_ps()
                for kc in range(KC):
                    kb.mm(po[:, 0:W], wp[:, kc, mm_ * 128:(mm_ + 1) * 128], merged[:, kc, 0:W],
                          start=(kc == 0), stop=(kc == KC - 1))
                kb.tt("dve", h_sb[:, m, 0:W], h_sb[:, m, 0:W], po[:, 0:W], ALU.add)
        kb.transfer([macc, br_sb], [actb])
        if kind == "dense":
            rmsnorm_fm(kb, h_sb, hn, g2, ones_bf, sqs, ps_ss, rstd, W)
            for pc in range(D_FF // 256):
                wpg = next_wp()
                kb.dma("pool", wpg[:, :, :], wfg[pc, :, :, :])
                wpu = next_wp()
                kb.dma("pool", wpu[:, :, :], wfu[pc, :, :, :])
                for mm_ in range(2):
                    fc = pc * 2 + mm_
                    pg = next_ps()
                    for kc in range(KC):
                        kb.mm(pg[:, 0:W], wpg[:, kc, mm_ * 128:(mm_ + 1) * 128], hn[:, kc, 0:W],
                              start=(kc == 0), stop=(kc == KC - 1))
                    pu = next_ps()
                    for kc in range(KC):
                        kb.mm(pu[:, 0:W], wpu[:, kc, mm_ * 128:(mm_ + 1) * 128], hn[:, kc, 0:W],
                              start=(kc == 0), stop=(kc == KC - 1))
                    sg = sgs[cnt[0] % 2]
                    cnt[0] += 1
                    kb.act(sg[:, 0:W], pg[:, 0:W], AF.Silu)
                    kb.tt("dve", actb[:, fc, 0:W], sg[:, 0:W], pu[:, 0:W], ALU.mult)
            for m in range(KC):
                wd = wds[m % 2]
                kb.dma("pool", wd[:, :, :], wfd[m, :, :, :])
                po = next_ps()
                for fc in range(NFF):
                    kb.mm(po[:, 0:W], wd[:, fc, :], actb[:, fc, 0:W], start=(fc == 0), stop=(fc == NFF - 1))
                kb.tt("dve", h_sb[:, m, 0:W], h_sb[:, m, 0:W], po[:, 0:W], ALU.add)
        else:
            nb = W // 128
            def post(kc, eng):
                st = stg[kc % 2]
                kb.stt("dve", st[:, 0:W], h_sb[:, kc, 0:W], g2[:, kc:kc + 1], rstd[:, 0:W], ALU.mult, ALU.mult)
                kb.copy("act", hn[:, kc, 0:W], st[:, 0:W])
                for tb in range(nb):
                    kb.mm(pss[tb][:, 0:8], st[:, tb * 128:(tb + 1) * 128], wr_sb[:, kc, :],
                          start=(kc == 0), stop=(kc == KC - 1))
            rmsnorm_fm(kb, h_sb, hn, g2, ones_bf, sqs, ps_ss, rstd, W, post=post)
            for tb in range(nb):
                kb.copy("dve", lg[:, tb, :], pss[tb][:, 0:8])
            for tb in range(nb):
                kb.op("dve", lambda tb=tb: nc.vector.max(out=top8.t[:, tb, :], in_=lg.t[:, tb, :]),
                      [top8[:, tb, :]], [lg[:, tb, :]])
            kb.tt("dve", cw[:, 0:nb, 0:1], top8[:, 0:nb, 0:1], top8[:, 0:nb, 1:2], ALU.subtract)
            kb.act(cw[:, 0:nb, 0:1], cw[:, 0:nb, 0:1], AF.Exp)
            kb.ts("dve", cw[:, 0:nb, 0:1], cw[:, 0:nb, 0:1], 1.0, None, op0=ALU.add)
            kb.op("dve", lambda: nc.vector.reciprocal(out=cw.t[:, 0:nb, 1:2], in_=cw.t[:, 0:nb, 0:1]),
                  [cw[:, 0:nb, 1:2]], [cw[:, 0:nb, 0:1]])
            kb.ts("dve", cw[:, 0:nb, 0:1], cw[:, 0:nb, 1:2], -1.0, 1.0, op0=ALU.mult, op1=ALU.add)
            for tb in range(nb):
                kb.ts("dve", comb[:, tb, :], lg[:, tb, :], top8[:, tb, 0:1], cw[:, tb, 0:1],
                      op0=ALU.is_equal, op1=ALU.mult)
                kb.ts("dve", lg[:, tb, :], lg[:, tb, :], top8[:, tb, 1:2], cw[:, tb, 1:2],
                      op0=ALU.is_equal, op1=ALU.mult)
                kb.tt("dve", comb[:, tb, :], comb[:, tb, :], lg[:, tb, :], ALU.add)
            pT = next_ps()
            for tb in range(nb):
                kb.mm(pT[0:8, tb * 128:(tb + 1) * 128], comb[:, tb, :], ident[:, :], start=True, stop=True)
            kb.copy("dve", combT[:, 0:W], pT[0:8, 0:W])
            for half in range(2):
                for el in range(4):
                    e = half * 4 + el
                    pcb = next_ps()
                    kb.mm(pcb[:, 0:W], sel[:, e, :], combT[:, 0:W], start=True, stop=True)
                    cb = combB[e % 2]
                    kb.copy("act", cb[:, 0:W], pcb[:, 0:W])
                    for pc in range(6):
                        c0 = pc * 256
                        cw_ = min(256, D_FFE - c0)
                        wpg = next_wp()
                        kb.dma("pool", wpg[:, :, 0:cw_], weg[e, pc, :, :, 0:cw_])
                        wpu = next_wp()
                        kb.dma("pool", wpu[:, :, 0:cw_], weu[e, pc, :, :, 0:cw_])
                        for mm_ in range(cw_ // 128):
                            fc = pc * 2 + mm_
                            pg = next_ps()
                            for kc in range(KC):
                                kb.mm(pg[:, 0:W], wpg[:, kc, mm_ * 128:(mm_ + 1) * 128], hn[:, kc, 0:W],
                                      start=(kc == 0), stop=(kc == KC - 1))
                            pu = next_ps()
                            for kc in range(KC):
                                kb.mm(pu[:, 0:W], wpu[:, kc, mm_ * 128:(mm_ + 1) * 128], hn[:, kc, 0:W],
                                      start=(kc == 0), stop=(kc == KC - 1))
                            sg = sgs[cnt[0] % 2]
                            tmp = tmps[cnt[0] % 2]
                            cnt[0] += 1
                            kb.act(sg[:, 0:W], pg[:, 0:W], AF.Silu)
                            kb.tt("dve", tmp[:, 0:W], sg[:, 0:W], pu[:, 0:W], ALU.mult)
                            kb.tt("dve", actb[:, el * NFE + fc, 0:W], tmp[:, 0:W], cb[:, 0:W], ALU.mult)
                for m in range(KC):
                    wd = wds[m % 2]
                    for el in range(4):
                        kb.dma("pool", wd[:, el * NFE:(el + 1) * NFE, :], wed[half * 4 + el, m, :, :, :])
                    po = next_ps()
                    for fc in range(NFF):
                        kb.mm(po[:, 0:W], wd[:, fc, :], actb[:, fc, 0:W], start=(fc == 0), stop=(fc == NFF - 1))
                    kb.tt("dve", h_sb[:, m, 0:W], h_sb[:, m, 0:W], po[:, 0:W], ALU.add)
        for kc in range(KC):
            kb.dma("sp", hTo[kc, :, t0:t0 + W], h_sb[:, kc, 0:W], final=True)
    kb.finish()
    return kb


FRONT = 112
W_SMALL = 8


def build_phaseA(NBLK, mixers=("gdn", "mla", "fox", "ssd")):
    from contextlib import ExitStack
    kb = KB()
    nc = kb.nc
    T = NBLK * 128
    dr = lambda n, s, k="ExternalInput", d=F32: Buf(kb, n, s, d, "dram", k)
    hT = dr("hT", [KC, 128, T])
    g1d = dr("g1", [128, KC])
    w_small_d = dr("w_small", [128, KC, W_SMALL])
    cst_d = dr("cst", [6, 128, 128])
    bo = dr("bo", [5, 128, T], "ExternalOutput")
    hnT = dr("hnT_scr", [KC, 128, T], "Internal", BF16)
    w_gdn_d = dr("w_gdn", [128, KC, 512])
    gdn_conv_d = dr("gdn_conv", [128, 3, 4])
    gdn_vec_d = dr("gdn_vec", [128, 2])
    gdn_ng_d = dr("gdn_ng", [128, 128])
    w_mla_d = dr("w_mla", [128, KC, 832])
    mla_wq_d = dr("mla_wq", [128, 4, 192])
    mla_wkv_d = dr("mla_wkv", [128, 2, 256])
    mla_g_d = dr("mla_g", [128, 10])
    rope_d = dr("rope", [2, 64, T])
    rotm_d = dr("rotm", [64, 64])
    w_fox_d = dr("w_fox", [128, KC, 384])
    fox_g_d = dr("fox_g", [128, 3])
    w_ssd_d = dr("w_ssd", [128, KC, 768])
    ssd_conv_d = dr("ssd_conv", [128, 4, 5])
    ssd_vec_d = dr("ssd_vec", [128, 8])
    ssd_row_d = dr("ssd_row", [2, 128, 256])

    sb = lambda n, s, d=F32, st=None: Buf(kb, n, s, d, stack=st)
    kb.eps_tile(EPS)
    kb.eps_tile(1.0)
    cst = sb("cst_sb", [128, 6, 128])
    kb.dma("sp", cst[:, :, :], bc(cst_d[:, :, :], cst_d.t.rearrange("c p f -> p c f")))
    cstb = sb("cst_bf", [128, 6, 128], BF16)
    kb.copy("dve", cstb[:, :, :], cst[:, :, :])
    ident, Umat, MnegT, strictT, causT, ones = [cst[:, i, :] for i in range(6)]
    ident_b, _, _, _, causT_b, ones_b = [cstb[:, i, :] for i in range(6)]
    g1 = sb("g1s", [128, KC])
    kb.dma("sp", g1[:, :], g1d[:, :])
    small_all = sb("small_all", [128, NBLK, W_SMALL])
    pss = [Buf(kb, "ps%d" % i, [128, 512], F32, "psum") for i in range(8)]
    psi = [0]

    def next_ps(n=8):
        p = pss[psi[0] % n]
        psi[0] += 1
        return p

    tiles = token_tiles(T)

    def sq_sum_rstd(srcs, W, scale, rstd, sqs, ps):
        for i, (v, P) in enumerate(srcs):
            sq = sqs[i % len(sqs)]
            kb.act(sq[0:P, 0:W], v, AF.Square)
            kb.mm(ps[:, 0:W], bc(ones_b, ones_b.ap[0:P, :]), sq[0:P, 0:W], start=(i == 0), stop=(i == len(srcs) - 1))
        kb.act(rstd[:, 0:W], ps[:, 0:W], AF.Sqrt, bias=kb.eps_tile(EPS), scale=scale)
        kb.recip(rstd[:, 0:W], rstd[:, 0:W])

    with ExitStack() as st:
        h_sb = sb("h_sb", [128, KC, 512], F32, st)
        hn = sb("hn0", [128, KC, 512], BF16, st)
        sqs = [sb("sq%d" % i, [128, 512], BF16, st) for i in range(2)]
        rstd = sb("rstd", [128, 512], F32, st)
        wsm = sb("wsm", [128, KC, W_SMALL], BF16, st)
        kb.dma("pool", wsm[:, :, :], w_small_d[:, :, :])
        import os
        STOP = int(os.environ.get("A0_STOP", "9"))
        for (t0, W) in tiles:
            if STOP < 1:
                break
            kb.dma("sp", h_sb[:, :, 0:W], bc(hT[:, :, t0:t0 + W], hT.t[:, :, t0:t0 + W].rearrange("k p w -> p k w")))
            if STOP < 2:
                continue
            sq_sum_rstd([(h_sb[:, kc, 0:W], 128) for kc in range(KC)], W, 1.0 / D_MODEL, rstd, sqs, pss[7])
            if STOP < 3:
                continue
            for kc in range(KC):
                kb.stt("dve", hn[:, kc, 0:W], h_sb[:, kc, 0:W], g1[:, kc:kc + 1], rstd[:, 0:W], ALU.mult, ALU.mult)
            if STOP < 4:
                continue
            for kc in range(KC):
                kb.dma("sp", hnT[kc, :, t0:t0 + W], hn[:, kc, 0:W])
            if STOP < 5:
                continue
            for tb in range(W // 128):
                blk = t0 // 128 + tb
                ps = next_ps(4)
                for kc in range(KC):
                    kb.mm(ps[:, 0:W_SMALL], hn[:, kc, tb * 128:(tb + 1) * 128], wsm[:, kc, :],
                          start=(kc == 0), stop=(kc == KC - 1))
                kb.copy("act", small_all[:, blk, :], ps[:, 0:W_SMALL])
    kb.barrier()

    def load_hn(hn_t, t0, W):
        for kc in range(KC):
            kb.dma("sp", hn_t[:, kc, 0:W], hnT[kc, :, t0:t0 + W])

    def proj_fm(ps, w, c0, ncols, hn_t, W):
        for kc in range(KC):
            kb.mm(ps[0:ncols, 0:W], w[:, kc, c0:c0 + ncols], hn_t[:, kc, 0:W], start=(kc == 0), stop=(kc == KC - 1))

    def proj_tm(psv, hn_t, tb, w, c0, ncols):
        for kc in range(KC):
            kb.mm(psv, hn_t[:, kc, tb * 128:(tb + 1) * 128], w[:, kc, c0:c0 + ncols], start=(kc == 0), stop=(kc == KC - 1))

    def softplus(out, in_, bias):
        kb.act(out, in_, AF.Exp, bias=bias)
        kb.act(out, out, AF.Ln, bias=kb.eps_tile(1.0))

    def conv4(acc, pre, wv, W):
        kb.ts("dve", acc[:, 0:W], pre[:, 3:3 + W], wv[:, 3:4], None, op0=ALU.mult)
        for i in (2, 1, 0):
            kb.stt("dve", acc[:, 0:W], pre[:, i:i + W], wv[:, i:i + 1], acc[:, 0:W], ALU.mult, ALU.add)

    def decay_prep(st, g_all, name):
        d = {}
        for nm in ("gcs", "ngcs", "eg", "e2e", "cd"):
            d[nm] = sb(name + nm, [128, NBLK], F32, st)
        ps = next_ps()
        kb.mm(ps[:, 0:NBLK], Umat, g_all[:, :], start=True, stop=True)
        kb.copy("dve", d["gcs"][:, :], ps[:, 0:NBLK])
        kb.ts("dve", d["ngcs"][:, :], d["gcs"][:, :], -1.0, None, op0=ALU.mult)
        kb.act(d["eg"][:, :], d["gcs"][:, :], AF.Exp)
        ps2 = next_ps()
        kb.mm(ps2[:, 0:NBLK], ones, g_all[:, :], start=True, stop=True)
        kb.act(d["cd"][:, :], ps2[:, 0:NBLK], AF.Exp)
        kb.tt("dve", d["e2e"][:, :], ps2[:, 0:NBLK], d["gcs"][:, :], ALU.subtract)
        kb.act(d["e2e"][:, :], d["e2e"][:, :], AF.Exp)
        return d

    def decay_mats(g_all, dec, n, grep, DmT, EGrow):
        kb.ts("dve", grep[:, :], ones, g_all[:, n:n + 1], None, op0=ALU.mult)
        ps = next_ps()
        kb.mm(ps[:, 0:128], grep[:, :], Umat, start=True, stop=True)
        kb.mm(ps[:, 128:256], grep[:, :], Umat, start=True, stop=False)
        kb.mm(ps[:, 128:256], ident, MnegT, start=False, stop=True)
        if EGrow is not None:
            kb.act(EGrow[:, :], ps[:, 0:128], AF.Exp)
        kb.act(DmT[:, :], ps[:, 128:256], AF.Exp, bias=dec["ngcs"][:, n:n + 1])

    def emit_out(slot, src_tm, ncol_chunks, blk, stage, stage_i, psl=None):
        for c in range(ncol_chunks):
            if psl is None:
                ps = next_ps()
            else:
                ps = psl[stage_i[0] % len(psl)]
            kb.mm(ps[:, 0:128], src_tm[:, c * 128:(c + 1) * 128], ident, start=True, stop=True)
            so = stage[stage_i[0] % len(stage)]
            stage_i[0] += 1
            kb.copy("act", so[:, :], ps[:, 0:128])
            kb.dma("sp", bo[slot + c, :, blk * 128:(blk + 1) * 128], so[:, :], final=True)

    try:
      if "gdn" in mixers:
          with ExitStack() as st:
              w = sb("w_gdn_s", [128, KC, 512], BF16, st)
              kb.dma("pool", w[:, :, :], w_gdn_d[:, :, :])
              convw = sb("gdn_convw", [128, 3, 4], F32, st)
              kb.dma("sp", convw[:, :, :], gdn_conv_d[:, :, :])
              gvec = sb("gdn_vec_s", [128, 2], F32, st)
              kb.dma("sp", gvec[:, :], gdn_vec_d[:, :])
              ngt = sb("gdn_ng_s", [128, 128], F32, st)
              kb.dma("sp", ngt[:, :], gdn_ng_d[:, :])
              hns = [sb("ghn%d" % i, [128, KC, 512], BF16, st) for i in range(2)]
              pre = sb("gpre", [128, 3, 515], F32, st)
              acc = [sb("gacc%d" % i, [128, 512], F32, st) for i in range(2)]
              sqs = [sb("gsq%d" % i, [128, 512], BF16, st) for i in range(2)]
              rstd = sb("grstd", [128, 512], F32, st)
              qT = sb("g_qT", [128, T], BF16, st)
              kT = sb("g_kT", [128, T], BF16, st)
              vT = sb("g_vT", [128, T], BF16, st)
              zg = sb("g_zg", [128, NBLK, 128], BF16, st)
              kb.memset("dve", pre[:, :, 0:3], 0.0)
              for ti, (t0, W) in enumerate(tiles):
                  hn_t = hns[ti % 2]
                  load_hn(hn_t, t0, W)
                  for c, dst in enumerate((qT, kT, vT)):
                      ps = next_ps()
                      proj_fm(ps, w, c * 128, 128, hn_t, W)
                      kb.copy("act", pre[:, c, 3:3 + W], ps[:, 0:W])
                      a = acc[c % 2]
                      conv4(a, bc(pre[:, c, :], pre.t[:, c, :]), bc(convw[:, c, :], convw.t[:, c, :]), W)
                      kb.copy("pool", pre[:, c, 0:3], pre[:, c, W:W + 3])
                      if c < 2:
                          kb.act(a[:, 0:W], a[:, 0:W], AF.Silu)
                          sq_sum_rstd([(a[:, 0:W], 128)], W, 1.0, rstd, sqs, pss[7])
                          kb.stt("dve", dst[:, t0:t0 + W], a[:, 0:W], (128.0 ** -0.5) if c == 0 else 1.0, rstd[:, 0:W],
                                 ALU.mult, ALU.mult)
                      else:
                          kb.act(dst[:, t0:t0 + W], a[:, 0:W], AF.Silu)
                  for tb in range(W // 128):
                      blk = t0 // 128 + tb
                      ps = next_ps()
                      proj_tm(ps[:, 0:128], hn_t, tb, w, 384, 128)
                      a = acc[tb % 2]
                      kb.act(a[:, 0:128], ps[:, 0:128], AF.Silu)
                      kb.tt("pool", zg[:, blk, :], a[:, 0:128], ngt[:, :], ALU.mult)
              kb.ck("gdn prep done")
              beta = sb("g_beta", [128, NBLK], F32, st)
              nbeta = sb("g_nbeta", [128, NBLK], F32, st)
              g_all = sb("g_gall", [128, NBLK], F32, st)
              expA = sb("g_expA", [128, 1], F32, st)
              kb.act(beta[:, :], bc(small_all[:, :, 0], small_all.t[:, :, 0]), AF.Sigmoid)
              kb.ts("dve", nbeta[:, :], beta[:, :], -1.0, None, op0=ALU.mult)
              kb.act(expA[:, :], gvec[:, 0:1], AF.Exp)
              kb.ts("dve", expA[:, :], expA[:, :], -1.0, None, op0=ALU.mult)
              softplus(g_all[:, :], bc(small_all[:, :, 1], small_all.t[:, :, 1]), gvec[:, 1:2])
              kb.ts("dve", g_all[:, :], g_all[:, :], expA[:, 0:1], None, op0=ALU.mult)
              kb.ck("gdn scalars")
              dec = decay_prep(st, g_all, "gd_")
              kb.ck("gdn decay_prep")
              NB2 = 4
              mk = lambda n, d=F32, shp=(128, 128): [sb("%s%d" % (n, i), list(shp), d, st) for i in range(NB2)]
              grep_, DmT_, EG_ = mk("g_grep"), mk("g_DmT"), mk("g_EG")
              t1_, Q_, P_, R_ = mk("g_t1"), [mk("g_Q%d" % k) for k in range(2)], [mk("g_P%d" % k) for k in range(2)], [mk("g_R%d" % k) for k in range(2)]
              attnT_, KeT_, QeT_ = mk("g_attnT", BF16), mk("g_KeT", BF16), mk("g_QeT", BF16)
              k2e_, Vt_ = mk("g_k2e", BF16), mk("g_Vt")
              R1_, vnew_ = mk("g_resid"), mk("g_vnew", BF16)
              o_, junk_ = mk("g_o"), mk("g_junk")
              ss_ = mk("g_ss", F32, (128, 1))
              S = sb("g_S", [128, 128], F32, st)
              S_bf = sb("g_Sbf", [128, 128], BF16, st)
              kb.memset("dve", S[:, :], 0.0)
              kb.memset("dve", S_bf[:, :], 0.0)
              stage = [sb("g_stage%d" % i, [128, 128], F32, st) for i in range(2)]
              stage_i = [0]
              def g_stage1(n, ctx):
                  i2 = n % NB2
                  cs = slice(n * 128, (n + 1) * 128)
                  grep, DmT, EG = grep_[i2], DmT_[i2], EG_[i2]
                  decay_mats(g_all, dec, n, grep, DmT, EG)
                  yield
                  psk = next_ps()
                  kb.mm(psk[:, 0:128], kT[:, cs], kT[:, cs], start=True, stop=True)
                  kb.mm(psk[:, 128:256], kT[:, cs], qT[:, cs], start=True, stop=True)
                  t1 = t1_[i2]
                  kb.tt("dve", t1[:, :], DmT[:, :], psk[:, 0:128], ALU.mult)
                  Q0 = Q_[0][i2]
                  kb.stt("dve", Q0[:, :], t1[:, :], nbeta[:, n:n + 1], strictT, ALU.mult, ALU.mult)
                  attnT = attnT_[i2]
                  kb.tt("dve", attnT[:, :], DmT[:, :], psk[:, 128:256], ALU.mult)
                  yield
                  pst = next_ps()
                  kb.mm(pst[:, 0:128], Q0[:, :], ident, start=True, stop=True)
                  P0 = P_[0][i2]
                  kb.copy("act", P0[:, :], pst[:, 0:128])
                  yield
                  Rc = R_[0][i2]
                  kb.tt("pool", Rc[:, :], Q0[:, :], ident, ALU.add)
                  yield
                  Qc, Pc = Q0, P0
                  for k in range(1, 7):
                      psq = next_ps()
                      Pn = P_[k % 2][i2]
                      kb.mm(psq[:, 0:128], Qc[:, :], Pc[:, :], start=True, stop=True)
                      if k < 6:
                          kb.mm(psq[:, 128:256], Pc[:, :], Qc[:, :], start=True, stop=True)
                      kb.copy("act", Pn[:, :], psq[:, 0:128])
                      if k < 6:
                          Qn = Q_[k % 2][i2]
                          kb.copy("dve", Qn[:, :], psq[:, 128:256])
                      yield
                      psr = next_ps()
                      kb.mm(psr[:, 0:128], Pn[:, :], Rc[:, :], start=True, stop=True)
                      Rn = R_[k % 2][i2]
                      kb.tt("dve", Rn[:, :], Rc[:, :], psr[:, 0:128], ALU.add)
                      yield
                      Rc, Pc = Rn, Pn
                      if k < 6:
                          Qc = Qn
                  yield
                  KeT, QeT = KeT_[i2], QeT_[i2]
                  kb.tt("pool", KeT[:, :], kT[:, cs], EG[:, :], ALU.mult)
                  kb.tt("pool", QeT[:, :], qT[:, cs], EG[:, :], ALU.mult)
                  pstk = next_ps()
                  kb.mm(pstk[:, 0:128], kT[:, cs], ident_b, start=True, stop=True)
                  kb.mm(pstk[:, 128:256], vT[:, cs], ident_b, start=True, stop=True)
                  k2e, Vt = k2e_[i2], Vt_[i2]
                  kb.ts("dve", k2e[:, :], pstk[:, 0:128], dec["e2e"][:, n:n + 1], None, op0=ALU.mult)
                  kb.copy("act", Vt[:, :], pstk[:, 128:256])
                  ctx.update(dict(KeT=KeT, QeT=QeT, k2e=k2e, Vt=Vt, Rc=Rc, attnT=attnT))
                  yield

              def g_stage2(n, ctx):
                  i2 = n % NB2
                  KeT, QeT, k2e, Vt, Rc, attnT = (ctx[k_] for k_ in ('KeT', 'QeT', 'k2e', 'Vt', 'Rc', 'attnT'))
                  psa = next_ps()
                  kb.mm(psa[:, 0:128], KeT[:, :], S_bf[:, :], start=True, stop=True)
                  R1 = R1_[i2]
                  kb.tt("dve", R1[:, :], Vt[:, :], psa[:, 0:128], ALU.subtract)
                  kb.mm(psa[:, 128:256], Rc[:, :], R1[:, :], start=True, stop=True)
                  vnew = vnew_[i2]
                  kb.ts("dve", vnew[:, :], psa[:, 128:256], beta[:, n:n + 1], None, op0=ALU.mult)
                  pso = next_ps()
                  kb.mm(pso[:, 0:128], QeT[:, :], S_bf[:, :], start=True, stop=False)
                  kb.mm(pso[:, 0:128], attnT[:, :], vnew[:, :], start=False, stop=True)
                  kb.mm(pso[:, 128:256], k2e[:, :], vnew[:, :], start=True, stop=True)
                  kb.stt("dve", S[:, :], S[:, :], dec["cd"][:, n:n + 1], pso[:, 128:256], ALU.mult, ALU.add)
                  kb.copy("act", S_bf[:, :], S[:, :])
                  ss, junk, o = ss_[i2], junk_[i2], o_[i2]
                  kb.memset("pool", ss[:, :], 0.0)
                  kb.act(junk[:, :], pso[:, 0:128], AF.Square, accum=ss[:, :])
                  kb.act(ss[:, :], ss[:, :], AF.Sqrt, bias=kb.eps_tile(EPS), scale=1.0 / 128)
                  kb.recip(ss[:, :], ss[:, :])
                  kb.stt("dve", o[:, :], pso[:, 0:128], ss[:, 0:1], zg[:, n, :], ALU.mult, ALU.mult)
                  emit_out(0, o, 1, n, stage, stage_i)


              for n0 in range(0, NBLK, NB2):
                  ns = [n for n in range(n0, min(n0 + NB2, NBLK))]
                  ctxs = [dict() for _ in ns]
                  gens = [g_stage1(n, c_) for n, c_ in zip(ns, ctxs)]
                  live = list(gens)
                  while live:
                      for g_ in list(live):
                          try:
                              next(g_)
                          except StopIteration:
                              live.remove(g_)
                  for n, c_ in zip(ns, ctxs):
                      g_stage2(n, c_)
          kb.barrier()
    except StopBuild:
        pass

    def attention(st, slot, q_parts, k_parts, Vaug, tab, name):
        G = 4
        pts = [sb("%s_pt%d" % (name, i), [128, 512], BF16, st) for i in range(3)]
        ot = [sb("%s_ot%d" % (name, i), [128, 128], F32, st) for i in range(2)]
        rc = [sb("%s_rc%d" % (name, i), [128, 1], F32, st) for i in range(2)]
        stage = [sb("%s_stage%d" % (name, i), [128, 128], F32, st) for i in range(2)]
        stage_i = [0]
        pti = [0]
        oi = [0]
        ps_s = [pss[0], pss[1]]
        ps_o4 = [pss[2], pss[3], pss[4], pss[5]]
        si = [0]
        for gi, i0 in enumerate(range(0, NBLK, G)):
            i1 = min(i0 + G, NBLK) - 1
            for j in range(0, i1 + 1):
                is_ = max(i0, j)
                Wq = (i1 - is_ + 1) * 128
                q0 = is_ * 128
                ps = ps_s[si[0] % 2]
                si[0] += 1
                for pi, ((qb, P), (kbuf, _)) in enumerate(zip(q_parts, k_parts)):
                    kb.mm(ps[:, 0:Wq], kbuf[0:P, j * 128:(j + 1) * 128], qb[0:P, q0:q0 + Wq],
                          start=(pi == 0), stop=(pi == len(q_parts) - 1))
                pt = pts[pti[0] % 3]
                pti[0] += 1
                if tab is None and not os.environ.get("NARROW"):
                    kb.act(pt[:, 0:Wq], ps[:, 0:Wq], AF.Exp)
                elif tab is None:
                    for ii in range(is_, i1 + 1):
                        c = (ii - is_) * 128
                        kb.act(pt[:, c:c + 128], ps[:, c:c + 128], AF.Exp)
                else:
                    for ii in range(is_, i1 + 1):
                        c = (ii - is_) * 128
                        kb.act(pt[:, c:c + 128], ps[:, c:c + 128], AF.Exp, bias=tab[:, ii, j:j + 1])
                if j >= i0:
                    kb.tt("pool", pt[:, 0:128], pt[:, 0:128], causT_b, ALU.mult)
                for ii in range(is_, i1 + 1):
                    c = (ii - is_) * 128
                    li = ii - i0
                    dst = ps_o4[li]
                    kb.mm(dst[:, 0:129], pt[:, c:c + 128],
                          bc(Vaug[:, j, :], Vaug.t[:, j, :]), start=(j == 0), stop=(j == ii))
            for ii in range(i0, i1 + 1):
                li = ii - i0
                src = ps_o4[li]
                c0 = 0
                r = rc[oi[0] % 2]
                o = ot[oi[0] % 2]
                oi[0] += 1
                kb.ts("dve", r[:, :], src[:, c0 + 128:c0 + 129], 1e-30, None, op0=ALU.max)
                kb.recip(r[:, :], r[:, :])
                kb.ts("dve", o[:, :], src[:, c0:c0 + 128], r[:, 0:1], None, op0=ALU.mult)
                emit_out(slot, o, 1, ii, stage, stage_i, psl=[pss[6], pss[7]])

    def make_vaug(st, name):
        Vaug = sb(name, [128, NBLK, 129], BF16, st)
        kb.memset("dve", bc(Vaug[:, :, 128:129], Vaug.t[:, :, 128:129]), 1.0)
        return Vaug

    def finish_vaug(Vaug):
        for p0, p1 in ((0, 32), (32, 64), (64, 96), (96, FRONT)):
            kb.memset("dve", bc(Vaug[:, 0, :], Vaug.t[p0:p1, 0, :]), 0.0)

    if "mla" in mixers:
        with ExitStack() as st:
            w = sb("w_mla_s", [128, KC, 832], BF16, st)
            kb.dma("pool", w[:, :, :], w_mla_d[:, :, :])
            wq = sb("mla_wq_s", [128, 4, 192], BF16, st)
            kb.dma("pool", wq[:, :, :], mla_wq_d[:, :, :])
            wkv = sb("mla_wkv_s", [128, 2, 256], BF16, st)
            kb.dma("pool", wkv[:, :, :], mla_wkv_d[:, :, :])
            mg = sb("mla_g_s", [128, 10], F32, st)
            kb.dma("sp", mg[:, :], mla_g_d[:, :])
            mgq = sb("mla_gq_s", [128, 2], F32, st)
            kb.ts("dve", mgq[:, :], mg[:, 6:8], 192.0 ** -0.5, None, op0=ALU.mult)
            ropes = [sb("rope_s%d" % i, [64, 2, 512], F32, st) for i in range(2)]
            rotm = sb("rotm_s", [64, 64], F32, st)
            kb.dma("sp", rotm[:, :], rotm_d[:, :])
            hns = [sb("mhn%d" % i, [128, KC, 512], BF16, st) for i in range(1)]
            lat = sb("m_lat", [128, 6, 512], F32, st)
            latn = sb("m_latn", [128, 6, 512], BF16, st)
            sqs = [sb("msq%d" % i, [128, 512], BF16, st) for i in range(2)]
            rstd = sb("mrstd", [128, 512], F32, st)
            raw = sb("m_raw", [128, 2, 512], F32, st)
            tr_ = sb("m_tr", [64, 512], F32, st)
            ta_ = sb("m_ta", [64, 512], F32, st)
            tb_ = sb("m_tb", [64, 512], F32, st)
            QTn = sb("m_QTn", [128, T], BF16, st)
            QTr = sb("m_QTr", [128, T], BF16, st)
            kb.memset("pool", QTr[64:128, :], 0.0)
            KTn = sb("m_KTn", [128, T], BF16, st)
            KTr = sb("m_KTr", [128, T], BF16, st)
            kb.memset("pool", KTr[64:128, :], 0.0)
            Vaug = make_vaug(st, "m_Vaug")

            def qk_finish(raw, gn, gr, dn, dr_, t0, W, rope):
                sq_sum_rstd([(raw[:, 0, 0:W], 128), (raw[0:64, 1, 0:W], 64)], W, 1.0 / 192, rstd, sqs, pss[7])
                kb.stt("dve", dn[:, t0:t0 + W], raw[:, 0, 0:W], gn, rstd[:, 0:W], ALU.mult, ALU.mult)
                kb.stt("dve", tr_[:, 0:W], raw[0:64, 1, 0:W], gr, rstd[0:64, 0:W], ALU.mult, ALU.mult)
                ps = next_ps(7)
                for c0 in range(0, W, 128):
                    kb.mm(ps[0:64, c0:c0 + 128], rotm[:, :], tr_[:, c0:c0 + 128], start=True, stop=True)
                kb.tt("dve", ta_[:, 0:W], tr_[:, 0:W], rope[:, 0, 0:W], ALU.mult)
                kb.tt("dve", tb_[:, 0:W], ps[0:64, 0:W], rope[:, 1, 0:W], ALU.mult)
                kb.tt("pool", dr_[0:64, t0:t0 + W], ta_[:, 0:W], tb_[:, 0:W], ALU.add)

            for ti, (t0, W) in enumerate(tiles):
                hn_t = hns[0]
                load_hn(hn_t, t0, W)
                rope = ropes[ti % 2]
                kb.dma("sp", rope[:, :, 0:W], bc(rope_d[:, :, t0:t0 + W], rope_d.t[:, :, t0:t0 + W].rearrange("c p t -> p c t")))
                for c in range(6):
                    ps = next_ps(7)
                    proj_fm(ps, w, c * 128, 128, hn_t, W)
                    kb.copy("act", lat[:, c, 0:W], ps[:, 0:W])
                sq_sum_rstd([(lat[:, c, 0:W], 128) for c in range(4)], W, 1.0 / 512, rstd, sqs, pss[7])
                for c in range(4):
                    kb.stt("dve", latn[:, c, 0:W], lat[:, c, 0:W], mg[:, c:c + 1], rstd[:, 0:W], ALU.mult, ALU.mult)
                sq_sum_rstd([(lat[:, c, 0:W], 128) for c in (4, 5)], W, 1.0 / 256, rstd, sqs, pss[7])
                for c in (4, 5):
                    kb.stt("dve", latn[:, c, 0:W], lat[:, c, 0:W], mg[:, c:c + 1], rstd[:, 0:W], ALU.mult, ALU.mult)
                ps = next_ps(7)
                for c in range(4):
                    kb.mm(ps[:, 0:W], wq[:, c, 0:128], latn[:, c, 0:W], start=(c == 0), stop=(c == 3))
                kb.copy("act", raw[:, 0, 0:W], ps[:, 0:W])
                ps = next_ps(7)
                for c in range(4):
                    kb.mm(ps[0:64, 0:W], wq[:, c, 128:192], latn[:, c, 0:W], start=(c == 0), stop=(c == 3))
                kb.copy("act", raw[0:64, 1, 0:W], ps[0:64, 0:W])
                qk_finish(raw, mgq[:, 0:1], mgq[0:64, 1:2], QTn, QTr, t0, W, rope)
                ps = next_ps(7)
                for c in range(2):
                    kb.mm(ps[:, 0:W], wkv[:, c, 0:128], latn[:, 4 + c, 0:W], start=(c == 0), stop=(c == 1))
                kb.copy("act", raw[:, 0, 0:W], ps[:, 0:W])
                ps = next_ps(7)
                proj_fm(ps, w, 768, 64, hn_t, W)
                kb.copy("act", raw[0:64, 1, 0:W], ps[0:64, 0:W])
                qk_finish(raw, mg[:, 8:9], mg[0:64, 9:10], KTn, KTr, t0, W, rope)
                for tb in range(W // 128):
                    blk = t0 // 128 + tb
                    ps = next_ps(7)
                    for c in range(2):
                        kb.mm(ps[:, 0:128], latn[:, 4 + c, tb * 128:(tb + 1) * 128], wkv[:, c, 128:256],
                              start=(c == 0), stop=(c == 1))
                    kb.copy("act", Vaug[:, blk, 0:128], ps[:, 0:128])
            finish_vaug(Vaug)
            if os.environ.get("DUMPQ"):
                kb.dma("pool", bo[3, :, :], QTn[:, :], final=True)
                kb.dma("pool", bo[4, :, :], QTr[:, :], final=True)
            attention(st, 1, [(QTn, 128), (QTr, 128)], [(KTn, 128), (KTr, 128)], Vaug, None, "ma")
        kb.barrier()

    if "fox" in mixers:
        with ExitStack() as st:
            w = sb("w_fox_s", [128, KC, 384], BF16, st)
            kb.dma("pool", w[:, :, :], w_fox_d[:, :, :])
            fg = sb("fox_g_s", [128, 3], F32, st)
            kb.dma("sp", fg[:, :], fox_g_d[:, :])
            fgq = sb("fox_gq", [128, 1], F32, st)
            kb.ts("dve", fgq[:, :], fg[:, 0:1], 128.0 ** -0.5, None, op0=ALU.mult)
            nbf = sb("fox_nbf", [128, 1], F32, st)
            kb.ts("dve", nbf[:, :], fg[:, 2:3], -1.0, None, op0=ALU.mult)
            hns = [sb("fhn%d" % i, [128, KC, 512], BF16, st) for i in range(2)]
            raws = [sb("f_raw%d" % i, [128, 512], F32, st) for i in range(2)]
            sqs = [sb("fsq%d" % i, [128, 512], BF16, st) for i in range(2)]
            rstd = sb("frstd", [128, 512], F32, st)
            QT = sb("f_QT", [128, T], BF16, st)
            KT = sb("f_KT", [128, T], BF16, st)
            Vaug = make_vaug(st, "f_Vaug")
            for ti, (t0, W) in enumerate(tiles):
                hn_t = hns[ti % 2]
                load_hn(hn_t, t0, W)
                for c, (dst, gv) in enumerate(((QT, fgq[:, 0:1]), (KT, fg[:, 1:2]))):
                    ps = next_ps(7)
                    proj_fm(ps, w, c * 128, 128, hn_t, W)
                    raw = raws[c]
                    kb.copy("act", raw[:, 0:W], ps[:, 0:W])
                    sq_sum_rstd([(raw[:, 0:W], 128)], W, 1.0 / 128, rstd, sqs, pss[7])
                    kb.stt("dve", dst[:, t0:t0 + W], raw[:, 0:W], gv, rstd[:, 0:W], ALU.mult, ALU.mult)
                for tb in range(W // 128):
                    blk = t0 // 128 + tb
                    ps = next_ps(7)
                    proj_tm(ps[:, 0:128], hn_t, tb, w, 256, 128)
                    kb.copy("act", Vaug[:, blk, 0:128], ps[:, 0:128])
            finish_vaug(Vaug)
            lf = sb("f_lf", [128, NBLK], F32, st)
            kb.act(lf[:, :], bc(small_all[:, :, 2], small_all.t[:, :, 2]), AF.Exp, bias=nbf[:, 0:1], scale=-1.0)
            kb.act(lf[:, :], lf[:, :], AF.Ln, bias=kb.eps_tile(1.0))
            kb.ts("dve", lf[:, :], lf[:, :], -1.0, None, op0=ALU.mult)
            within = sb("f_within", [128, NBLK], F32, st)
            totB = sb("f_totB", [128, NBLK], F32, st)
            ps = next_ps(7)
            kb.mm(ps[:, 0:NBLK], Umat, lf[:, :], start=True, stop=True)
            kb.copy("dve", within[:, :], ps[:, 0:NBLK])
            ps = next_ps(7)
            kb.mm(ps[:, 0:NBLK], ones, lf[:, :], start=True, stop=True)
            kb.copy("dve", totB[:, :], ps[:, 0:NBLK])
            tab = sb("f_tab", [128, NBLK, NBLK], F32, st)
            for i in range(NBLK):
                if i > 0:
                    kb.ts("dve", tab[:, i, 0:i], tab[:, i - 1, 0:i], totB[:, i:i + 1], None, op0=ALU.add)
                kb.tt("dve", tab[:, i, i:i + 1], totB[:, i:i + 1], within[:, i:i + 1], ALU.subtract)
            attention(st, 2, [(QT, 128)], [(KT, 128)], Vaug, tab, "fa")
        kb.barrier()

    if "ssd" in mixers:
        with ExitStack() as st:
            w = sb("w_ssd_s", [128, KC, 768], BF16, st)
            kb.dma("pool", w[:, :, :], w_ssd_d[:, :, :])
            scv = sb("ssd_conv_s", [128, 4, 5], F32, st)
            kb.dma("sp", scv[:, :, :], ssd_conv_d[:, :, :])
            svec = sb("ssd_vec_s", [128, 8], F32, st)
            kb.dma("sp", svec[:, :], ssd_vec_d[:, :])
            srow = sb("ssd_row_s", [128, 2, 256], F32, st)
            kb.dma("sp", srow[:, :, :], bc(ssd_row_d[:, :, :], ssd_row_d.t.rearrange("c p f -> p c f")))
            hns = [sb("shn%d" % i, [128, KC, 512], BF16, st) for i in range(1)]
            pre = sb("spre", [128, 4, 515], F32, st)
            acc = [sb("sacc%d" % i, [128, 512], F32, st) for i in range(2)]
            xT = [sb("s_xT%d" % i, [128, T], BF16, st) for i in range(2)]
            BT = sb("s_BT", [128, T], BF16, st)
            CT = sb("s_CT", [128, T], BF16, st)
            zs = sb("s_zs", [128, NBLK, 256], BF16, st)
            kb.memset("dve", pre[:, :, 0:3], 0.0)
            dsts = (xT[0], xT[1], BT, CT)
            for ti, (t0, W) in enumerate(tiles):
                hn_t = hns[0]
                load_hn(hn_t, t0, W)
                for c in range(4):
                    ps = next_ps()
                    proj_fm(ps, w, 256 + c * 128, 128, hn_t, W)
                    kb.copy("act", pre[:, c, 3:3 + W], ps[:, 0:W])
                    a = acc[c % 2]
                    conv4(a, bc(pre[:, c, :], pre.t[:, c, :]), bc(scv[:, c, :], scv.t[:, c, :]), W)
                    kb.copy("pool", pre[:, c, 0:3], pre[:, c, W:W + 3])
                    kb.act(dsts[c][:, t0:t0 + W], a[:, 0:W], AF.Silu, bias=scv[:, c, 4:5])
                for tb in range(W // 128):
                    blk = t0 // 128 + tb
                    ps = next_ps()
                    proj_tm(ps[:, 0:256], hn_t, tb, w, 0, 256)
                    kb.act(zs[:, blk, :], ps[:, 0:256], AF.Silu)
            for b_ in (xT[0], xT[1], BT):
                kb.memset("dve", b_[:, 0:FRONT], 0.0)
            negA = sb("s_negA", [128, 4], F32, st)
            kb.act(negA[:, :], svec[:, 4:8], AF.Exp)
            kb.ts("dve", negA[:, :], negA[:, :], -1.0, None, op0=ALU.mult)
            dts, decs, a_alls = [], [], []
            for h in range(4):
                dt = sb("s_dt%d" % h, [128, NBLK], F32, st)
                a_all = sb("s_a%d" % h, [128, NBLK], F32, st)
                softplus(dt[:, :], bc(small_all[:, :, 3 + h], small_all.t[:, :, 3 + h]), svec[:, h:h + 1])
                kb.ts("dve", a_all[:, :], dt[:, :], negA[:, h:h + 1], None, op0=ALU.mult)
                dts.append(dt)
                a_alls.append(a_all)
                decs.append(decay_prep(st, a_all, "sd%d_" % h))
            mk = lambda n, d=F32, shp=(128, 128), k=2: [sb("%s%d" % (n, i), list(shp), d, st) for i in range(k)]
            grep_, DmT_, EG_ = mk("s_grep", k=4), mk("s_DmT", k=4), mk("s_EG", k=4)
            attnT_, CeT_ = mk("s_attnT", BF16, k=8), mk("s_CeT", BF16, k=8)
            Btok_ = mk("s_Btok", BF16)
            Xtok_ = mk("s_Xtok", F32, (128, 256))
            Xdt_ = mk("s_Xdt", BF16, (128, 256))
            Xdec_ = mk("s_Xdec", BF16, (128, 256))
            CBt_ = mk("s_CBt")
            y1_ = mk("s_y1", F32, (128, 256))
            y2_ = mk("s_y2", F32, (128, 256))
            junk_ = mk("s_junk", F32, (128, 256))
            ss_ = mk("s_ss", F32, (128, 1))
            S = sb("s_S", [128, 256], F32, st)
            S_bf = sb("s_Sbf", [128, 256], BF16, st)
            kb.memset("dve", S[:, :], 0.0)
            kb.memset("dve", S_bf[:, :], 0.0)
            stage = [sb("s_stage%d" % i, [128, 128], F32, st) for i in range(2)]
            stage_i = [0]
            hh = [0]
            def s_stage1(n):
                i2 = n % 2
                cs = slice(n * 128, (n + 1) * 128)
                pst = next_ps()
                kb.mm(pst[:, 0:128], BT[:, cs], ident_b, start=True, stop=True)
                kb.mm(pst[:, 128:256], xT[0][:, cs], ident_b, start=True, stop=True)
                kb.mm(pst[:, 256:384], xT[1][:, cs], ident_b, start=True, stop=True)
                Btok, Xtok, Xdt, Xdec = Btok_[i2], Xtok_[i2], Xdt_[i2], Xdec_[i2]
                kb.copy("act", Btok[:, :], pst[:, 0:128])
                kb.copy("act", Xtok[:, :], pst[:, 128:384])
                for h in range(4):
                    hs = slice(h * 64, (h + 1) * 64)
                    kb.ts("dve", Xdt[:, hs], Xtok[:, hs], dts[h][:, n:n + 1], None, op0=ALU.mult)
                    kb.ts("dve", Xdec[:, hs], Xtok[:, hs], dts[h][:, n:n + 1], decs[h]["e2e"][:, n:n + 1],
                          op0=ALU.mult, op1=ALU.mult)
                psc = next_ps()
                kb.mm(psc[:, 0:128], BT[:, cs], CT[:, cs], start=True, stop=True)
                CBt = CBt_[i2]
                kb.copy("act", CBt[:, :], psc[:, 0:128])
                for h in range(4):
                    decay_mats(a_alls[h], decs[h], n, grep_[h], DmT_[h], EG_[h])
                for h in range(4):
                    kb.tt("dve", attnT_[i2 * 4 + h][:, :], CBt[:, :], DmT_[h][:, :], ALU.mult)
                    kb.tt("pool", CeT_[i2 * 4 + h][:, :], CT[:, cs], EG_[h][:, :], ALU.mult)
            def s_stage2(n):
                i2 = n % 2
                cs = slice(n * 128, (n + 1) * 128)
                Btok, Xtok, Xdt, Xdec, CBt = Btok_[i2], Xtok_[i2], Xdt_[i2], Xdec_[i2], CBt_[i2]
                psy = next_ps()
                for h in range(4):
                    hs = slice(h * 64, (h + 1) * 64)
                    kb.mm(psy[:, hs], CeT_[i2 * 4 + h][:, :], S_bf[:, hs], start=True, stop=False)
                    kb.mm(psy[:, hs], attnT_[i2 * 4 + h][:, :], Xdt[:, hs], start=False, stop=True)
                psn = next_ps()
                kb.mm(psn[:, 0:256], Btok[:, :], Xdec[:, :], start=True, stop=True)
                for h in range(4):
                    hs = slice(h * 64, (h + 1) * 64)
                    kb.stt("dve", S[:, hs], S[:, hs], decs[h]["cd"][:, n:n + 1], psn[:, hs], ALU.mult, ALU.add)
                kb.copy("act", S_bf[:, :], S[:, :])
                y1, y2, junk, ss = y1_[i2], y2_[i2], junk_[i2], ss_[i2]
                kb.tt("pool", y1[:, :], Xtok[:, :], bc(srow[:, 0, :], srow.t[:, 0, :]), ALU.mult)
                kb.tt("dve", y1[:, :], y1[:, :], psy[:, 0:256], ALU.add)
                kb.tt("pool", y2[:, :], y1[:, :], bc(zs[:, n, :], zs.t[:, n, :]), ALU.mult)
                kb.memset("pool", ss[:, :], 0.0)
                kb.act(junk[:, :], y2[:, :], AF.Square, accum=ss[:, :])
                kb.act(ss[:, :], ss[:, :], AF.Sqrt, bias=kb.eps_tile(EPS), scale=1.0 / 256)
                kb.recip(ss[:, :], ss[:, :])
                kb.stt("dve", y1[:, :], y2[:, :], ss[:, 0:1], bc(srow[:, 1, :], srow.t[:, 1, :]), ALU.mult, ALU.mult)

                emit_out(3, y1, 2, n, stage, stage_i)

            s_stage1(0)
            for n in range(NBLK):
                if n + 1 < NBLK:
                    s_stage1(n + 1)
                s_stage2(n)
        kb.barrier()
    kb.finish()
    return kb, locals()


IN_WIDTHS = (512, 512, 512, 512, 4, 4, 512, 256, 64, 512, 512, 512, 4, 512, 512, 256, 256, 8)
OFF = [0]
for _w in IN_WIDTHS:
    OFF.append(OFF[-1] + _w)


def r3(w):
    K, C = w.shape
    return np.ascontiguousarray(w.reshape(K // 128, 128, C).transpose(1, 0, 2))


def rep(v, n=128):
    v = np.asarray(v, np.float32).reshape(1, -1)
    return np.ascontiguousarray(np.broadcast_to(v, (n, v.shape[1])))


def make_consts(T):
    p = np.arange(128)[:, None]
    f = np.arange(128)[None, :]
    cst = np.stack([
        (p == f), (p <= f), np.where(f >= p, 0.0, -30000.0), (f > p), (f >= p), np.ones((128, 128)),
    ]).astype(np.float32)
    pos = np.maximum(np.arange(T) - FRONT, 0).astype(np.float32)
    inv = (1.0 / (10000.0 ** (np.arange(0, 64, 2, dtype=np.float32) / 64.0))).astype(np.float32)
    ang = pos[None, :] * np.concatenate([inv, inv])[:, None]
    rope = np.stack([np.cos(ang), np.sin(ang)]).astype(np.float32)
    rot = np.zeros((64, 64), np.float32)
    for ff in range(32):
        rot[ff, ff + 32] = -1.0
        rot[ff + 32, ff] = 1.0
    return cst, rope, np.ascontiguousarray(rot.T)


def phaseA_inputs(P, l, j, hT_b, T):
    G = j // 2
    w_in = P["w_in"][l]
    col = lambda o, a, n: w_in[:, OFF[o] + a:OFF[o] + a + n]
    cst, rope, rotT = make_consts(T)
    small = np.concatenate([col(4, j, 1), col(5, j, 1), col(12, j, 1), col(17, G * 4, 4),
                            np.zeros((2048, 1), np.float32)], axis=1)
    w_gdn = np.concatenate([col(0, j * 128, 128), col(1, j * 128, 128), col(2, j * 128, 128), col(3, j * 128, 128)], 1)
    cw = P["gdn_conv_w"][l]
    gdn_conv = np.stack([cw[:, j * 128:(j + 1) * 128].T, cw[:, 512 + j * 128:512 + (j + 1) * 128].T,
                         cw[:, 1024 + j * 128:1024 + (j + 1) * 128].T], axis=1)
    w_mla = np.concatenate([col(6, 0, 512), col(7, 0, 256), col(8, 0, 64)], 1)
    mla_g = np.zeros((128, 10), np.float32)
    mla_g[:, 0:4] = P["mla_qa_g"][l].reshape(4, 128).T
    mla_g[:, 4:6] = P["mla_kva_g"][l].reshape(2, 128).T
    mla_g[:, 6] = P["mla_qn_g"][l][0:128]
    mla_g[0:64, 7] = P["mla_qn_g"][l][128:192]
    mla_g[:, 8] = P["mla_kn_g"][l][0:128]
    mla_g[0:64, 9] = P["mla_kn_g"][l][128:192]
    w_fox = np.concatenate([col(9, j * 128, 128), col(10, j * 128, 128), col(11, j * 128, 128)], 1)
    fox_g = np.stack([P["fox_qn_g"][l], P["fox_kn_g"][l], np.full(128, P["fox_b_f"][l][j], np.float32)], 1)
    w_ssd = np.concatenate([col(13, G * 256, 256), col(14, G * 256, 256), col(15, G * 128, 128), col(16, G * 128, 128)], 1)
    sw = P["ssd_conv_w"][l]
    sbias = P["ssd_conv_b"][l]
    chs = [slice(G * 256, G * 256 + 128), slice(G * 256 + 128, G * 256 + 256),
           slice(512 + G * 128, 512 + (G + 1) * 128), slice(768 + G * 128, 768 + (G + 1) * 128)]
    ssd_conv = np.stack([np.concatenate([sw[:, c].T, sbias[c][:, None]], 1) for c in chs], axis=1)
    ssd_vec = rep(np.concatenate([P["ssd_dt_bias"][l][G * 4:G * 4 + 4], P["ssd_A_log"][l][G * 4:G * 4 + 4]]))
    ssd_row = np.stack([rep(np.repeat(P["ssd_D"][l][G * 4:G * 4 + 4], 64)), rep(P["ssd_norm_g"][l][G * 256:(G + 1) * 256])])
    f32c = lambda a: np.ascontiguousarray(a, dtype=np.float32)
    return {
        "hT": f32c(hT_b), "g1": f32c(P["mix_norm_g"][l].reshape(KC, 128).T), "w_small": r3(f32c(small)), "cst": cst,
        "w_gdn": r3(f32c(w_gdn)), "gdn_conv": f32c(gdn_conv),
        "gdn_vec": rep([P["gdn_A_log"][l][j], P["gdn_dt_bias"][l][j]]), "gdn_ng": rep(P["gdn_norm_g"][l]),
        "w_mla": r3(f32c(w_mla)), "mla_wq": r3(f32c(P["mla_wq_b"][l][:, j * 192:(j + 1) * 192])),
        "mla_wkv": r3(f32c(P["mla_wkv_b"][l][:, j * 256:(j + 1) * 256])), "mla_g": mla_g,
        "rope": rope, "rotm": rotT,
        "w_fox": r3(f32c(w_fox)), "fox_g": f32c(fox_g),
        "w_ssd": r3(f32c(w_ssd)), "ssd_conv": f32c(ssd_conv), "ssd_vec": ssd_vec, "ssd_row": f32c(ssd_row),
    }


def seq_to_hT(h_seq, T):
    L = h_seq.shape[0]
    out = np.zeros((2048, T), np.float32)
    out[:, FRONT:FRONT + L] = h_seq.T
    return out.reshape(KC, 128, T)


_PROGS = {}


def _prog(key, fn):
    if key not in _PROGS:
        _PROGS[key] = fn()
    return _PROGS[key]


def panels(w3, pw=256):
    p, K, C = w3.shape
    n = (C + pw - 1) // pw
    if n * pw != C:
        w3 = np.concatenate([w3, np.zeros((p, K, n * pw - C), w3.dtype)], axis=2)
    return np.ascontiguousarray(w3.reshape(p, K, n, pw).transpose(2, 0, 1, 3))


def phaseB_weights(P, l):
    f32c = lambda a: np.ascontiguousarray(a, dtype=np.float32)
    d = {
        "wgate": np.stack([panels(r3(f32c(P["w_gate"][l][b]))) for b in range(4)]),
        "wbr": np.stack([r3(f32c(P["w_branch"][l][b])) for b in range(4)]),
        "wo": panels(r3(f32c(P["w_o"][l]))),
        "g1": f32c(P["mix_norm_g"][l].reshape(KC, 128).T),
        "g2": f32c(P["ffn_norm_g"][l].reshape(KC, 128).T),
    }
    i = l // 2
    if l % 2 == 0:
        d.update({"wfg": panels(r3(f32c(P["dense_w_gate"][i]))), "wfu": panels(r3(f32c(P["dense_w_up"][i]))),
                  "wfd": panels(r3(f32c(P["dense_w_down"][i])), 128)})
    else:
        sel = np.zeros((N_EXP, N_EXP, 128), np.float32)
        for e in range(N_EXP):
            sel[e, e, :] = 1.0
        d.update({"wr": r3(f32c(P["router_w"][i])),
                  "weg": np.stack([panels(r3(f32c(P["moe_w_gate"][i][e]))) for e in range(N_EXP)]),
                  "weu": np.stack([panels(r3(f32c(P["moe_w_up"][i][e]))) for e in range(N_EXP)]),
                  "wed": np.stack([panels(r3(f32c(P["moe_w_down"][i][e])), 128) for e in range(N_EXP)]),
                  "sel": sel, "ident": np.eye(128, dtype=np.float32)})
    return d


def kernel(**inputs):
    P = {k: np.asarray(v) for k, v in inputs.items()}
    x = P["x"].astype(np.float32, copy=False)
    Bsz, S, D = x.shape
    NX = S // 128
    NBLK = NX + 1
    T = NBLK * 128
    NXB = NX // 4
    NTB = NXB + 1
    depth = P["w_in"].shape[0]
    meta = P["meta_tokens"].astype(np.float32)
    hT = [seq_to_hT(np.concatenate([meta, x[b]], 0), T) for b in range(Bsz)]
    cores = list(range(8))
    for l in range(depth):
        kbA, _ = _prog(("A", NBLK), lambda: build_phaseA(NBLK))
        in_maps = [phaseA_inputs(P, l, c % 4, hT[c // 4], T) for c in cores]
        resA = run_bass_kernel_spmd(kbA.nc, in_maps, core_ids=cores)
        br = []
        for b in range(Bsz):
            bos = [resA.results[b * 4 + j]["bo"] for j in range(4)]
            rows = [bos[j][0] for j in range(4)] + [bos[j][1] for j in range(4)] + [bos[j][2] for j in range(4)] \
                + [bos[j][3 + (j % 2)] for j in range(4)]
            br.append(np.stack(rows))
        del resA
        kind = "dense" if l % 2 == 0 else "moe"
        last = (l == depth - 1)
        ntb = NXB if last else NTB
        kbB = _prog(("B", ntb, kind), lambda: build_phaseB(ntb, kind))
        wts = phaseB_weights(P, l)
        in_maps = []
        for c in cores:
            b, q = c // 4, c % 4
            if last:
                cols = np.r_[128 + q * NXB * 128:128 + (q + 1) * NXB * 128]
            else:
                cols = np.r_[0:128, 128 + q * NXB * 128:128 + (q + 1) * NXB * 128]
            m = dict(wts)
            m["hT"] = np.ascontiguousarray(hT[b][:, :, cols])
            m["br"] = np.ascontiguousarray(br[b][:, :, cols])
            in_maps.append(m)
        resB = run_bass_kernel_spmd(kbB.nc, in_maps, core_ids=cores)
        for c in cores:
            b, q = c // 4, c % 4
            o = resB.results[c]["hTo"]
            if last:
                hT[b][:, :, 128 + q * NXB * 128:128 + (q + 1) * NXB * 128] = o
                continue
            if q == 0:
                hT[b][:, :, FRONT:128] = o[:, :, FRONT:128]
            hT[b][:, :, 128 + q * NXB * 128:128 + (q + 1) * NXB * 128] = o[:, :, 128:]
        del resB
    out = np.stack([np.ascontiguousarray(hT[b].reshape(D, T)[:, 128:].T) for b in range(Bsz)])
    return out.astype(np.float32)
```

```python
import os
import numpy as np
import concourse.bass as bass
import concourse.mybir as mybir
from concourse.bass_utils import run_bass_kernel_spmd

F32 = mybir.dt.float32
BF16 = mybir.dt.bfloat16
AF = mybir.ActivationFunctionType
ALU = mybir.AluOpType
AX = mybir.AxisListType


class Res:
    __slots__ = ("w", "r", "dsem", "dcnt")

    def __init__(self):
        self.w = None
        self.r = {}
        self.dsem = None
        self.dcnt = 0


class V:
    __slots__ = ("ap", "res", "space")

    def __init__(self, ap, res, space="sbuf"):
        self.ap = ap
        self.res = res
        self.space = space

    def __getitem__(self, idx):
        return V(self.ap[idx], self.res, self.space)


class Buf:
    def __init__(self, kb, name, shape, dtype, space="sbuf", kind=None, nres=1, stack=None):
        self.kb = kb
        nc = kb.nc
        self.name = name
        if space == "sbuf":
            if stack is not None:
                self.t = stack.enter_context(nc.sbuf_tensor(name, list(shape), dtype))
            else:
                self.t = nc.alloc_sbuf_tensor(name, list(shape), dtype)
        elif space == "psum":
            self.t = nc.alloc_psum_tensor(name, list(shape), dtype)
        else:
            self.t = nc.dram_tensor(name, list(shape), dtype, kind=kind or "Internal").ap()
        self.res = [Res() for _ in range(nres)]
        self.space = space

    def __getitem__(self, idx):
        return V(self.t[idx], self.res, self.space)

    def sub(self, ri, idx):
        return V(self.t[idx], [self.res[ri]], self.space)


def bc(v, ap):
    return V(ap, v.res, v.space)


class StopBuild(Exception):
    pass


class KB:
    ENG = ("pe", "act", "dve", "pool", "sp")

    def ck(self, name=""):
        import os
        lim = int(os.environ.get("STOP_AT", "0"))
        self._ck = getattr(self, "_ck", 0) + 1
        if lim and self._ck >= lim:
            print("STOP at checkpoint", self._ck, name)
            raise StopBuild()

    def __init__(self):
        self.nc = bass.Bass("TRN2", target_bir_lowering=False)
        nc = self.nc
        self.e = {"pe": nc.tensor, "act": nc.scalar, "dve": nc.vector, "pool": nc.gpsimd, "sp": nc.sync}
        self.sem = {}
        self.cnt = {}
        self.seen = {k: {} for k in self.ENG}
        self.semh = {}
        for k in self.ENG:
            h = nc.alloc_semaphore("sem_" + k)
            self.semh[k] = h
            self.cnt[k] = 0
        self.ndsem = 0
        self.dma_max = {}
        self.out_tokens = []
        self.ninstr = 0

    def _dsem(self, res):
        if res.dsem is None:
            key = "d%d" % self.ndsem
            self.ndsem += 1
            self.semh[key] = self.nc.alloc_semaphore("sem_" + key)
            res.dsem = key
        return res.dsem

    def _collect(self, eng, outs, ins):
        waits = {}

        def need(tok):
            if tok is None:
                return
            s, val = tok
            if waits.get(s, 0) < val:
                waits[s] = val

        for v in ins:
            for r in v.res:
                need(r.w)
                if v.space == "psum":
                    for s, val in r.r.items():
                        if s != eng:
                            need((s, val))
        for v in outs:
            for r in v.res:
                if r.w is not None:
                    need(r.w)
                for s, val in r.r.items():
                    need((s, val))
        if eng == "pe" and "pe" in waits:
            del waits["pe"]
        return waits

    def _dowaits(self, eng, waits):
        E = self.e[eng]
        seen = self.seen[eng]
        for s, val in waits.items():
            if seen.get(s, 0) >= val:
                continue
            E.wait_ge(self.semh[s], val)
            seen[s] = val

    def op(self, eng, fn, outs, ins):
        waits = self._collect(eng, outs, ins)
        self._dowaits(eng, waits)
        ins_ = fn()
        self.cnt[eng] += 1
        c = self.cnt[eng]
        ins_.then_inc(self.semh[eng], 1)
        for v in ins:
            for r in v.res:
                r.r[eng] = c
        for v in outs:
            for r in v.res:
                r.w = (eng, c)
                r.r = {}
        self.ninstr += 1
        return ins_

    def dma(self, q, out, in_, final=False):
        waits = self._collect("__dma__", [out], [in_])
        self._dowaits(q, waits)
        if out.space == "sbuf":
            sres = out.res[0]
        elif in_.space == "sbuf":
            sres = in_.res[0]
        else:
            sres = out.res[0]
        key = self._dsem(sres)
        ins_ = self.e[q].dma_start(out=out.ap, in_=in_.ap)
        sres.dcnt += 16
        ins_.then_inc(self.semh[key], 16)
        tok = (key, sres.dcnt)
        self.dma_max[key] = sres.dcnt
        for r in in_.res:
            r.r[key] = max(r.r.get(key, 0), sres.dcnt)
        for r in out.res:
            r.w = tok
            r.r = {}
        if final:
            self.out_tokens.append(tok)
        self.ninstr += 1
        return ins_

    def transfer(self, olds, news):
        toks = {}
        for b in olds:
            for r in b.res:
                if r.w is not None:
                    toks[r.w[0]] = max(toks.get(r.w[0], 0), r.w[1])
                for s, val in r.r.items():
                    toks[s] = max(toks.get(s, 0), val)
        for b in news:
            for r in b.res:
                r.w = None
                r.r = dict(toks)

    def barrier(self):
        allw = {k: self.cnt[k] for k in ("pe", "act", "dve", "pool") if self.cnt[k] > 0}
        for key, val in self.dma_max.items():
            allw[key] = val
        for e in self.ENG:
            w = {k: v for k, v in allw.items() if k != e}
            self._dowaits(e, w)

    def finish(self):
        waits = {}
        for s, val in self.out_tokens:
            waits[s] = max(waits.get(s, 0), val)
        self._dowaits("sp", waits)
        for k in ("pe", "act", "dve", "pool"):
            if self.cnt[k] > 0:
                self._dowaits("sp", {k: self.cnt[k]})

    def mm(self, out, lhsT, rhs, start=True, stop=True):
        nc = self.nc
        return self.op("pe", lambda: nc.tensor.matmul(out.ap, lhsT.ap, rhs.ap, start=start, stop=stop),
                       [out], [lhsT, rhs])

    def tr(self, out, in_, ident):
        nc = self.nc
        return self.op("pe", lambda: nc.tensor.transpose(out.ap, in_.ap, ident.ap), [out], [in_, ident])

    def act(self, out, in_, func, bias=None, scale=1.0, accum=None, eng="act"):
        nc = self.nc
        ins = [in_]
        kw = {}
        if bias is not None:
            if isinstance(bias, V):
                ins.append(bias)
                kw["bias"] = bias.ap
            else:
                kw["bias"] = bias
        if isinstance(scale, V):
            ins.append(scale)
            kw["scale"] = scale.ap
        else:
            kw["scale"] = scale
        outs = [out]
        if accum is not None:
            outs.append(accum)
            kw["accum_out"] = accum.ap
        return self.op("act", lambda: nc.scalar.activation(out=out.ap, in_=in_.ap, func=func, **kw), outs, ins)

    def tt(self, eng, out, a, b, op):
        E = self.e[eng]
        return self.op(eng, lambda: E.tensor_tensor(out=out.ap, in0=a.ap, in1=b.ap, op=op), [out], [a, b])

    def ts(self, eng, out, a, s1, s2=None, op0=ALU.mult, op1=None, accum=None):
        E = self.e[eng]
        ins = [a]
        a1 = s1
        a2 = s2
        if isinstance(s1, V):
            ins.append(s1)
            a1 = s1.ap
        if isinstance(s2, V):
            ins.append(s2)
            a2 = s2.ap
        kw = {}
        if op1 is not None:
            kw["op1"] = op1
        outs = [out]
        if accum is not None:
            outs.append(accum)
            kw["accum_out"] = accum.ap
        return self.op(eng, lambda: E.tensor_scalar(out=out.ap, in0=a.ap, scalar1=a1, scalar2=a2, op0=op0, **kw),
                       outs, ins)

    def stt(self, eng, out, a, s, b, op0, op1):
        E = self.e[eng]
        ins = [a, b]
        sa = s
        if isinstance(s, V):
            ins.append(s)
            sa = s.ap
        return self.op(eng, lambda: E.scalar_tensor_tensor(out=out.ap, in0=a.ap, scalar=sa, in1=b.ap, op0=op0, op1=op1),
                       [out], ins)

    def copy(self, eng, out, in_):
        if eng == "act":
            nc = self.nc
            return self.op("act", lambda: nc.scalar.copy(out=out.ap, in_=in_.ap), [out], [in_])
        E = self.e[eng]
        return self.op(eng, lambda: E.tensor_copy(out=out.ap, in_=in_.ap), [out], [in_])

    def recip(self, out, in_):
        nc = self.nc
        return self.op("dve", lambda: nc.vector.reciprocal(out=out.ap, in_=in_.ap), [out], [in_])

    def eps_tile(self, val):
        if not hasattr(self, "_eps"):
            self._eps = {}
        if val not in self._eps:
            b = Buf(self, "cst%d" % len(self._eps), [128, 1], F32)
            self.memset("dve", b[:, :], float(val))
            self._eps[val] = b
        return self._eps[val][:, :]

    def memset(self, eng, out, val):
        E = self.e[eng]
        return self.op(eng, lambda: E.memset(out.ap, val), [out], [])


class ABuf:
    def __init__(self, ap, space="sbuf", nres=1):
        self.t = ap
        self.res = [Res() for _ in range(nres)]
        self.space = space

    def __getitem__(self, idx):
        return V(self.t[idx], self.res, self.space)


D_MODEL = 2048
KC = 16
EPS = 1e-6
D_FF = 5632
NFF = D_FF // 128
N_EXP = 8
D_FFE = 1408
NFE = D_FFE // 128


def token_tiles(NT, W=512):
    out = []
    t = 0
    while t < NT:
        w = min(W, NT - t)
        out.append((t, w))
        t += w
    return out


def rmsnorm_fm(kb, h_sb, hn, g_sb, ones_bf, sqs, ps_ss, rstd, W, D=D_MODEL, post=None):
    nkc = D // 128
    for kc in range(nkc):
        sq = sqs[kc % len(sqs)]
        kb.act(sq[:, 0:W], h_sb[:, kc, 0:W], AF.Square)
        kb.mm(ps_ss[:, 0:W], ones_bf[:, :], sq[:, 0:W], start=(kc == 0), stop=(kc == nkc - 1))
    kb.act(rstd[:, 0:W], ps_ss[:, 0:W], AF.Sqrt, bias=kb.eps_tile(EPS))
    kb.recip(rstd[:, 0:W], rstd[:, 0:W])
    for kc in range(nkc):
        eng = "dve"
        if post is None:
            kb.stt(eng, hn[:, kc, 0:W], h_sb[:, kc, 0:W], g_sb[:, kc:kc + 1], rstd[:, 0:W], ALU.mult, ALU.mult)
        else:
            post(kc, eng)


def build_phaseB(NTB, kind):
    kb = KB()
    nc = kb.nc
    NT = NTB * 128
    D = D_MODEL
    dr = lambda n, s, k="ExternalInput": Buf(kb, n, s, F32, "dram", k)
    hT = dr("hT", [KC, 128, NT])
    br = dr("br", [16, 128, NT])
    wgate = dr("wgate", [4, D // 256, 128, KC, 256])
    wbr = dr("wbr", [4, 128, 4, D])
    wo = dr("wo", [D // 256, 128, KC, 256])
    g1d = dr("g1", [128, KC])
    g2d = dr("g2", [128, KC])
    if kind == "dense":
        wfg = dr("wfg", [D_FF // 256, 128, KC, 256])
        wfu = dr("wfu", [D_FF // 256, 128, KC, 256])
        wfd = dr("wfd", [KC, 128, NFF, 128])
    else:
        wr = dr("wr", [128, KC, N_EXP])
        weg = dr("weg", [N_EXP, 6, 128, KC, 256])
        weu = dr("weu", [N_EXP, 6, 128, KC, 256])
        wed = dr("wed", [N_EXP, KC, 128, NFE, 128])
        seld = dr("sel", [N_EXP, N_EXP, 128])
        identd = dr("ident", [128, 128])
    hTo = dr("hTo", [KC, 128, NT], "ExternalOutput")

    sb = lambda n, s, d=F32: Buf(kb, n, s, d)
    h_sb = sb("h_sb", [128, KC, 512])
    hn = sb("hn", [128, KC, 512], BF16)
    merged = sb("merged", [128, KC, 512], BF16)
    sqs = [sb("sq%d" % i, [128, 512], BF16) for i in range(2)]
    rstd = sb("rstd", [128, 512])
    X = nc.alloc_sbuf_tensor("X", [128, 12288], F32)
    macc = ABuf(X[:, 0:8192].rearrange("p (a b) -> p a b", b=512))
    br_sb = ABuf(X[:, 8192:12288].bitcast(BF16).rearrange("p (a b) -> p a b", b=512))
    actb = ABuf(X[:, 0:11264].bitcast(BF16).rearrange("p (a b) -> p a b", b=512))
    NWP = 3
    wps = [sb("wp%d" % i, [128, KC, 256], BF16) for i in range(NWP)]
    wbs = sb("wbs", [128, 4, D], BF16)
    wds = [sb("wd%d" % i, [128, NFF, 128], BF16) for i in range(2)]
    g1 = sb("g1s", [128, KC])
    g2 = sb("g2s", [128, KC])
    ones_bf = sb("ones_bf", [128, 128], BF16)
    sgs = [sb("sg%d" % i, [128, 512]) for i in range(2)]
    tmps = [sb("tmp%d" % i, [128, 512]) for i in range(2)]
    pss = [Buf(kb, "ps%d" % i, [128, 512], F32, "psum") for i in range(8)]
    if kind == "moe":
        stg = [sb("stg%d" % i, [128, 512]) for i in range(2)]
        wr_sb = sb("wr_sb", [128, KC, N_EXP])
        lg = sb("lg", [128, 4, 8])
        top8 = sb("top8", [128, 4, 8])
        comb = sb("comb", [128, 4, 8])
        cw = sb("cw", [128, 4, 8])
        combT = sb("combT", [8, 512], BF16)
        sel = sb("sel_sb", [N_EXP, N_EXP, 128], BF16)
        combB = [sb("combB%d" % i, [128, 512]) for i in range(2)]
        ident = sb("ident_sb", [128, 128])
        kb.dma("sp", wr_sb[:, :, :], wr[:, :, :])
        kb.dma("pool", sel[:, :, :], seld[:, :, :])
        kb.dma("sp", ident[:, :], identd[:, :])

    kb.dma("sp", g1[:, :], g1d[:, :])
    kb.dma("sp", g2[:, :], g2d[:, :])
    kb.memset("dve", ones_bf[:, :], 1.0 / D)

    wpi = [0]

    def next_wp():
        b = wps[wpi[0] % NWP]
        wpi[0] += 1
        return b

    psi = [0]

    def next_ps():
        p = pss[psi[0] % 6]
        psi[0] += 1
        return p

    ps_ss = pss[6]
    ps_misc = pss[7]
    cnt = [0]

    for (t0, W) in token_tiles(NT):
        for kc in range(KC):
            kb.dma("sp", h_sb[:, kc, 0:W], hT[kc, :, t0:t0 + W])
        rmsnorm_fm(kb, h_sb, hn, g1, ones_bf, sqs, ps_ss, rstd, W)
        kb.transfer([actb], [macc, br_sb])
        for i in range(16):
            kb.dma("pool", br_sb[:, i, 0:W], br[i, :, t0:t0 + W])
        for b in range(4):
            kb.dma("pool", wbs[:, :, :], wbr[b, :, :, :])
            for pc in range(D // 256):
                wp = next_wp()
                kb.dma("pool", wp[:, :, :], wgate[b, pc, :, :, :])
                for mm_ in range(2):
                    m = pc * 2 + mm_
                    pg = next_ps()
                    for kc in range(KC):
                        kb.mm(pg[:, 0:W], wp[:, kc, mm_ * 128:(mm_ + 1) * 128], hn[:, kc, 0:W],
                              start=(kc == 0), stop=(kc == KC - 1))
                    pb = next_ps()
                    for hh in range(4):
                        kb.mm(pb[:, 0:W], wbs[:, hh, m * 128:(m + 1) * 128], br_sb[:, b * 4 + hh, 0:W],
                              start=(hh == 0), stop=(hh == 3))
                    sg = sgs[cnt[0] % 2]
                    tmp = tmps[cnt[0] % 2]
                    cnt[0] += 1
                    kb.act(sg[:, 0:W], pg[:, 0:W], AF.Sigmoid)
                    if b == 0:
                        kb.tt("dve", macc[:, m, 0:W], sg[:, 0:W], pb[:, 0:W], ALU.mult)
                    elif b < 3:
                        kb.tt("dve", tmp[:, 0:W], sg[:, 0:W], pb[:, 0:W], ALU.mult)
                        kb.tt("dve", macc[:, m, 0:W], macc[:, m, 0:W], tmp[:, 0:W], ALU.add)
                    else:
                        kb.tt("dve", tmp[:, 0:W], sg[:, 0:W], pb[:, 0:W], ALU.mult)
                        kb.tt("dve", merged[:, m, 0:W], macc[:, m, 0:W], tmp[:, 0:W], ALU.add)
        for pc in range(D // 256):
            wp = next_wp()
            kb.dma("pool", wp[:, :, :], wo[pc, :, :, :])
            for mm_ in range(2):
                m = pc * 2 + mm_
                po = next_ps()
                for kc in range(KC):
                    kb.mm(po[:, 0:W], wp[:, kc, mm_ * 128:(mm_ + 1) * 128], merged[:, kc, 0:W],
                          start=(kc == 0), stop=(kc == KC - 1))
                kb.tt("dve", h_sb[:, m, 0:W], h_sb[:, m, 0:W], po[:, 0:W], ALU.add)
        kb.transfer([macc, br_sb], [actb])
        if kind == "dense":
            rmsnorm_fm(kb, h_sb, hn, g2, ones_bf, sqs, ps_ss, rstd, W)
            for pc in range(D_FF // 256):
                wpg = next_wp()
                kb.dma("pool", wpg[:, :, :], wfg[pc, :, :, :])
                wpu = next_wp()
                kb.dma("pool", wpu[:, :, :], wfu[pc, :, :, :])
                for mm_ in range(2):
                    fc = pc * 2 + mm_
                    pg = next_ps()
                    for kc in range(KC):
                        kb.mm(pg[:, 0:W], wpg[:, kc, mm_ * 128:(mm_ + 1) * 128], hn[:, kc, 0:W],
                              start=(kc == 0), stop=(kc == KC - 1))
                    pu = next_ps()
                    for kc in range(KC):
                        kb.mm(pu[:, 0:W], wpu[:, kc, mm_ * 128:(mm_ + 1) * 128], hn[:, kc, 0:W],
                              start=(kc == 0), stop=(kc == KC - 1))
                    sg = sgs[cnt[0] % 2]
                    cnt[0] += 1
                    kb.act(sg[:, 0:W], pg[:, 0:W], AF.Silu)
                    kb.tt("dve", actb[:, fc, 0:W], sg[:, 0:W], pu[:, 0:W], ALU.mult)
            for m in range(KC):
                wd = wds[m % 2]
                kb.dma("pool", wd[:, :, :], wfd[m, :, :, :])
                po = next_ps()
                for fc in range(NFF):
                    kb.mm(po[:, 0:W], wd[:, fc, :], actb[:, fc, 0:W], start=(fc == 0), stop=(fc == NFF - 1))
                kb.tt("dve", h_sb[:, m, 0:W], h_sb[:, m, 0:W], po[:, 0:W], ALU.add)
        else:
            nb = W // 128
            def post(kc, eng):
                st = stg[kc % 2]
                kb.stt("dve", st[:, 0:W], h_sb[:, kc, 0:W], g2[:, kc:kc + 1], rstd[:, 0:W], ALU.mult, ALU.mult)
                kb.copy("act", hn[:, kc, 0:W], st[:, 0:W])
                for tb in range(nb):
                    kb.mm(pss[tb][:, 0:8], st[:, tb * 128:(tb + 1) * 128], wr_sb[:, kc, :],
                          start=(kc == 0), stop=(kc == KC - 1))
            rmsnorm_fm(kb, h_sb, hn, g2, ones_bf, sqs, ps_ss, rstd, W, post=post)
            for tb in range(nb):
                kb.copy("dve", lg[:, tb, :], pss[tb][:, 0:8])
            for tb in range(nb):
                kb.op("dve", lambda tb=tb: nc.vector.max(out=top8.t[:, tb, :], in_=lg.t[:, tb, :]),
                      [top8[:, tb, :]], [lg[:, tb, :]])
            kb.tt("dve", cw[:, 0:nb, 0:1], top8[:, 0:nb, 0:1], top8[:, 0:nb, 1:2], ALU.subtract)
            kb.act(cw[:, 0:nb, 0:1], cw[:, 0:nb, 0:1], AF.Exp)
            kb.ts("dve", cw[:, 0:nb, 0:1], cw[:, 0:nb, 0:1], 1.0, None, op0=ALU.add)
            kb.op("dve", lambda: nc.vector.reciprocal(out=cw.t[:, 0:nb, 1:2], in_=cw.t[:, 0:nb, 0:1]),
                  [cw[:, 0:nb, 1:2]], [cw[:, 0:nb, 0:1]])
            kb.ts("dve", cw[:, 0:nb, 0:1], cw[:, 0:nb, 1:2], -1.0, 1.0, op0=ALU.mult, op1=ALU.add)
            for tb in range(nb):
                kb.ts("dve", comb[:, tb, :], lg[:, tb, :], top8[:, tb, 0:1], cw[:, tb, 0:1],
                      op0=ALU.is_equal, op1=ALU.mult)
                kb.ts("dve", lg[:, tb, :], lg[:, tb, :], top8[:, tb, 1:2], cw[:, tb, 1:2],
                      op0=ALU.is_equal, op1=ALU.mult)
                kb.tt("dve", comb[:, tb, :], comb[:, tb, :], lg[:, tb, :], ALU.add)
            pT = next_ps()
            for tb in range(nb):
                kb.mm(pT[0:8, tb * 128:(tb + 1) * 128], comb[:, tb, :], ident[:, :], start=True, stop=True)
            kb.copy("dve", combT[:, 0:W], pT[0:8, 0:W])
            for half in range(2):
                for el in range(4):
                    e = half * 4 + el
                    pcb = next_ps()
                    kb.mm(pcb[:, 0:W], sel[:, e, :], combT[:, 0:W], start=True, stop=True)
                    cb = combB[e % 2]
                    kb.copy("act", cb[:, 0:W], pcb[:, 0:W])
                    for pc in range(6):
                        c0 = pc * 256
                        cw_ = min(256, D_FFE - c0)
                        wpg = next_wp()
                        kb.dma("pool", wpg[:, :, 0:cw_], weg[e, pc, :, :, 0:cw_])
                        wpu = next_wp()
                        kb.dma("pool", wpu[:, :, 0:cw_], weu[e, pc, :, :, 0:cw_])
                        for mm_ in range(cw_ // 128):
                            fc = pc * 2 + mm_
                            pg = next_ps()
                            for kc in range(KC):
                                kb.mm(pg[:, 0:W], wpg[:, kc, mm_ * 128:(mm_ + 1) * 128], hn[:, kc, 0:W],
                                      start=(kc == 0), stop=(kc == KC - 1))
                            pu = next_ps()
                            for kc in range(KC):
                                kb.mm(pu[:, 0:W], wpu[:, kc, mm_ * 128:(mm_ + 1) * 128], hn[:, kc, 0:W],
                                      start=(kc == 0), stop=(kc == KC - 1))
                            sg = sgs[cnt[0] % 2]
                            tmp = tmps[cnt[0] % 2]
                            cnt[0] += 1
                            kb.act(sg[:, 0:W], pg[:, 0:W], AF.Silu)
                            kb.tt("dve", tmp[:, 0:W], sg[:, 0:W], pu[:, 0:W], ALU.mult)
                            kb.tt("dve", actb[:, el * NFE + fc, 0:W], tmp[:, 0:W], cb[:, 0:W], ALU.mult)
                for m in range(KC):
                    wd = wds[m % 2]
                    for el in range(4):
                        kb.dma("pool", wd[:, el * NFE:(el + 1) * NFE, :], wed[half * 4 + el, m, :, :, :])
                    po = next_ps()
                    for fc in range(NFF):
                        kb.mm(po[:, 0:W], wd[:, fc, :], actb[:, fc, 0:W], start=(fc == 0), stop=(fc == NFF - 1))
                    kb.tt("dve", h_sb[:, m, 0:W], h_sb[:, m, 0:W], po[:, 0:W], ALU.add)
        for kc in range(KC):
            kb.dma("sp", hTo[kc, :, t0:t0 + W], h_sb[:, kc, 0:W], final=True)
    kb.finish()
    return kb


FRONT = 112
W_SMALL = 8


def build_phaseA(NBLK, mixers=("gdn", "mla", "fox", "ssd")):
    from contextlib import ExitStack
    kb = KB()
    nc = kb.nc
    T = NBLK * 128
    dr = lambda n, s, k="ExternalInput", d=F32: Buf(kb, n, s, d, "dram", k)
    hT = dr("hT", [KC, 128, T])
    g1d = dr("g1", [128, KC])
    w_small_d = dr("w_small", [128, KC, W_SMALL])
    cst_d = dr("cst", [6, 128, 128])
    bo = dr("bo", [5, 128, T], "ExternalOutput")
    hnT = dr("hnT_scr", [KC, 128, T], "Internal", BF16)
    w_gdn_d = dr("w_gdn", [128, KC, 512])
    gdn_conv_d = dr("gdn_conv", [128, 3, 4])
    gdn_vec_d = dr("gdn_vec", [128, 2])
    gdn_ng_d = dr("gdn_ng", [128, 128])
    w_mla_d = dr("w_mla", [128, KC, 832])
    mla_wq_d = dr("mla_wq", [128, 4, 192])
    mla_wkv_d = dr("mla_wkv", [128, 2, 256])
    mla_g_d = dr("mla_g", [128, 10])
    rope_d = dr("rope", [2, 64, T])
    rotm_d = dr("rotm", [64, 64])
    w_fox_d = dr("w_fox", [128, KC, 384])
    fox_g_d = dr("fox_g", [128, 3])
    w_ssd_d = dr("w_ssd", [128, KC, 768])
    ssd_conv_d = dr("ssd_conv", [128, 4, 5])
    ssd_vec_d = dr("ssd_vec", [128, 8])
    ssd_row_d = dr("ssd_row", [2, 128, 256])

    sb = lambda n, s, d=F32, st=None: Buf(kb, n, s, d, stack=st)
    kb.eps_tile(EPS)
    kb.eps_tile(1.0)
    cst = sb("cst_sb", [128, 6, 128])
    kb.dma("sp", cst[:, :, :], bc(cst_d[:, :, :], cst_d.t.rearrange("c p f -> p c f")))
    cstb = sb("cst_bf", [128, 6, 128], BF16)
    kb.copy("dve", cstb[:, :, :], cst[:, :, :])
    ident, Umat, MnegT, strictT, causT, ones = [cst[:, i, :] for i in range(6)]
    ident_b, _, _, _, causT_b, ones_b = [cstb[:, i, :] for i in range(6)]
    g1 = sb("g1s", [128, KC])
    kb.dma("sp", g1[:, :], g1d[:, :])
    small_all = sb("small_all", [128, NBLK, W_SMALL])
    pss = [Buf(kb, "ps%d" % i, [128, 512], F32, "psum") for i in range(8)]
    psi = [0]

    def next_ps(n=8):
        p = pss[psi[0] % n]
        psi[0] += 1
        return p

    tiles = token_tiles(T)

    def sq_sum_rstd(srcs, W, scale, rstd, sqs, ps):
        for i, (v, P) in enumerate(srcs):
            sq = sqs[i % len(sqs)]
            kb.act(sq[0:P, 0:W], v, AF.Square)
            kb.mm(ps[:, 0:W], bc(ones_b, ones_b.ap[0:P, :]), sq[0:P, 0:W], start=(i == 0), stop=(i == len(srcs) - 1))
        kb.act(rstd[:, 0:W], ps[:, 0:W], AF.Sqrt, bias=kb.eps_tile(EPS), scale=scale)
        kb.recip(rstd[:, 0:W], rstd[:, 0:W])

    with ExitStack() as st:
        h_sbs = [sb("h_sb%d" % i, [128, KC, 512], F32, st) for i in range(2)]
        hn0s = [sb("hn0_%d" % i, [128, KC, 512], BF16, st) for i in range(2)]
        sqs = [sb("sq%d" % i, [128, 512], BF16, st) for i in range(2)]
        rstds = [sb("rstd%d" % i, [128, 512], F32, st) for i in range(2)]
        wsm = sb("wsm", [128, KC, W_SMALL], BF16, st)
        kb.dma("pool", wsm[:, :, :], w_small_d[:, :, :])
        import os
        STOP = int(os.environ.get("A0_STOP", "9"))
        for ti0, (t0, W) in enumerate(tiles):
            h_sb, hn, rstd = h_sbs[ti0 % 2], hn0s[ti0 % 2], rstds[ti0 % 2]
            if STOP < 1:
                break
            kb.dma("sp", h_sb[:, :, 0:W], bc(hT[:, :, t0:t0 + W], hT.t[:, :, t0:t0 + W].rearrange("k p w -> p k w")))
            if STOP < 2:
                continue
            sq_sum_rstd([(h_sb[:, kc, 0:W], 128) for kc in range(KC)], W, 1.0 / D_MODEL, rstd, sqs, pss[7])
            if STOP < 3:
                continue
            for kc in range(KC):
                kb.stt("dve", hn[:, kc, 0:W], h_sb[:, kc, 0:W], g1[:, kc:kc + 1], rstd[:, 0:W], ALU.mult, ALU.mult)
            if STOP < 4:
                continue
            for kc in range(KC):
                kb.dma("sp", hnT[kc, :, t0:t0 + W], hn[:, kc, 0:W])
            if STOP < 5:
                continue
            for tb in range(W // 128):
                blk = t0 // 128 + tb
                ps = next_ps(4)
                for kc in range(KC):
                    kb.mm(ps[:, 0:W_SMALL], hn[:, kc, tb * 128:(tb + 1) * 128], wsm[:, kc, :],
                          start=(kc == 0), stop=(kc == KC - 1))
                kb.copy("act", small_all[:, blk, :], ps[:, 0:W_SMALL])
    kb.barrier()

    def load_hn(hn_t, t0, W):
        for kc in range(KC):
            kb.dma("sp", hn_t[:, kc, 0:W], hnT[kc, :, t0:t0 + W])

    def proj_fm(ps, w, c0, ncols, hn_t, W):
        for kc in range(KC):
            kb.mm(ps[0:ncols, 0:W], w[:, kc, c0:c0 + ncols], hn_t[:, kc, 0:W], start=(kc == 0), stop=(kc == KC - 1))

    def proj_tm(psv, hn_t, tb, w, c0, ncols):
        for kc in range(KC):
            kb.mm(psv, hn_t[:, kc, tb * 128:(tb + 1) * 128], w[:, kc, c0:c0 + ncols], start=(kc == 0), stop=(kc == KC - 1))

    def softplus(out, in_, bias):
        kb.act(out, in_, AF.Exp, bias=bias)
        kb.act(out, out, AF.Ln, bias=kb.eps_tile(1.0))

    def conv4(acc, pre, wv, W):
        kb.ts("dve", acc[:, 0:W], pre[:, 3:3 + W], wv[:, 3:4], None, op0=ALU.mult)
        for i in (2, 1, 0):
            kb.stt("dve", acc[:, 0:W], pre[:, i:i + W], wv[:, i:i + 1], acc[:, 0:W], ALU.mult, ALU.add)

    def decay_prep(st, g_all, name):
        d = {}
        for nm in ("gcs", "ngcs", "eg", "e2e", "cd"):
            d[nm] = sb(name + nm, [128, NBLK], F32, st)
        ps = next_ps()
        kb.mm(ps[:, 0:NBLK], Umat, g_all[:, :], start=True, stop=True)
        kb.copy("dve", d["gcs"][:, :], ps[:, 0:NBLK])
        kb.ts("dve", d["ngcs"][:, :], d["gcs"][:, :], -1.0, None, op0=ALU.mult)
        kb.act(d["eg"][:, :], d["gcs"][:, :], AF.Exp)
        ps2 = next_ps()
        kb.mm(ps2[:, 0:NBLK], ones, g_all[:, :], start=True, stop=True)
        kb.act(d["cd"][:, :], ps2[:, 0:NBLK], AF.Exp)
        kb.tt("dve", d["e2e"][:, :], ps2[:, 0:NBLK], d["gcs"][:, :], ALU.subtract)
        kb.act(d["e2e"][:, :], d["e2e"][:, :], AF.Exp)
        return d

    def decay_mats(g_all, dec, n, grep, DmT, EGrow):
        kb.ts("dve", grep[:, :], ones, g_all[:, n:n + 1], None, op0=ALU.mult)
        ps = next_ps()
        kb.mm(ps[:, 0:128], grep[:, :], Umat, start=True, stop=True)
        kb.mm(ps[:, 128:256], grep[:, :], Umat, start=True, stop=False)
        kb.mm(ps[:, 128:256], ident, MnegT, start=False, stop=True)
        if EGrow is not None:
            kb.act(EGrow[:, :], ps[:, 0:128], AF.Exp)
        kb.act(DmT[:, :], ps[:, 128:256], AF.Exp, bias=dec["ngcs"][:, n:n + 1])

    def emit_out(slot, src_tm, ncol_chunks, blk, stage, stage_i, psl=None):
        for c in range(ncol_chunks):
            if psl is None:
                ps = next_ps()
            else:
                ps = psl[stage_i[0] % len(psl)]
            kb.mm(ps[:, 0:128], src_tm[:, c * 128:(c + 1) * 128], ident, start=True, stop=True)
            so = stage[stage_i[0] % len(stage)]
            stage_i[0] += 1
            kb.copy("act", so[:, :], ps[:, 0:128])
            kb.dma("sp", bo[slot + c, :, blk * 128:(blk + 1) * 128], so[:, :], final=True)

    try:
      if "gdn" in mixers:
          with ExitStack() as st:
              w = sb("w_gdn_s", [128, KC, 512], BF16, st)
              kb.dma("pool", w[:, :, :], w_gdn_d[:, :, :])
              convw = sb("gdn_convw", [128, 3, 4], F32, st)
              kb.dma("sp", convw[:, :, :], gdn_conv_d[:, :, :])
              gvec = sb("gdn_vec_s", [128, 2], F32, st)
              kb.dma("sp", gvec[:, :], gdn_vec_d[:, :])
              ngt = sb("gdn_ng_s", [128, 128], F32, st)
              kb.dma("sp", ngt[:, :], gdn_ng_d[:, :])
              hns = [sb("ghn%d" % i, [128, KC, 512], BF16, st) for i in range(2)]
              pre = sb("gpre", [128, 3, 515], F32, st)
              acc = [sb("gacc%d" % i, [128, 512], F32, st) for i in range(2)]
              sqs = [sb("gsq%d" % i, [128, 512], BF16, st) for i in range(2)]
              rstd = sb("grstd", [128, 512], F32, st)
              qT = sb("g_qT", [128, T], BF16, st)
              kT = sb("g_kT", [128, T], BF16, st)
              vT = sb("g_vT", [128, T], BF16, st)
              zg = sb("g_zg", [128, NBLK, 128], BF16, st)
              kb.memset("dve", pre[:, :, 0:3], 0.0)
              for ti, (t0, W) in enumerate(tiles):
                  hn_t = hns[ti % 2]
                  load_hn(hn_t, t0, W)
                  for c, dst in enumerate((qT, kT, vT)):
                      ps = next_ps()
                      proj_fm(ps, w, c * 128, 128, hn_t, W)
                      kb.copy("act", pre[:, c, 3:3 + W], ps[:, 0:W])
                      a = acc[c % 2]
                      conv4(a, bc(pre[:, c, :], pre.t[:, c, :]), bc(convw[:, c, :], convw.t[:, c, :]), W)
                      kb.copy("pool", pre[:, c, 0:3], pre[:, c, W:W + 3])
                      if c < 2:
                          kb.act(a[:, 0:W], a[:, 0:W], AF.Silu)
                          sq_sum_rstd([(a[:, 0:W], 128)], W, 1.0, rstd, sqs, pss[7])
                          kb.stt("dve", dst[:, t0:t0 + W], a[:, 0:W], (128.0 ** -0.5) if c == 0 else 1.0, rstd[:, 0:W],
                                 ALU.mult, ALU.mult)
                      else:
                          kb.act(dst[:, t0:t0 + W], a[:, 0:W], AF.Silu)
                  for tb in range(W // 128):
                      blk = t0 // 128 + tb
                      ps = next_ps()
                      proj_tm(ps[:, 0:128], hn_t, tb, w, 384, 128)
                      a = acc[tb % 2]
                      kb.act(a[:, 0:128], ps[:, 0:128], AF.Silu)
                      kb.tt("pool", zg[:, blk, :], a[:, 0:128], ngt[:, :], ALU.mult)
              kb.ck("gdn prep done")
              beta = sb("g_beta", [128, NBLK], F32, st)
              nbeta = sb("g_nbeta", [128, NBLK], F32, st)
              g_all = sb("g_gall", [128, NBLK], F32, st)
              expA = sb("g_expA", [128, 1], F32, st)
              kb.act(beta[:, :], bc(small_all[:, :, 0], small_all.t[:, :, 0]), AF.Sigmoid)
              kb.ts("dve", nbeta[:, :], beta[:, :], -1.0, None, op0=ALU.mult)
              kb.act(expA[:, :], gvec[:, 0:1], AF.Exp)
              kb.ts("dve", expA[:, :], expA[:, :], -1.0, None, op0=ALU.mult)
              softplus(g_all[:, :], bc(small_all[:, :, 1], small_all.t[:, :, 1]), gvec[:, 1:2])
              kb.ts("dve", g_all[:, :], g_all[:, :], expA[:, 0:1], None, op0=ALU.mult)
              kb.ck("gdn scalars")
              dec = decay_prep(st, g_all, "gd_")
              kb.ck("gdn decay_prep")
              NB2 = 4
              mk = lambda n, d=F32, shp=(128, 128): [sb("%s%d" % (n, i), list(shp), d, st) for i in range(NB2)]
              grep_, DmT_, EG_ = mk("g_grep"), mk("g_DmT"), mk("g_EG")
              t1_, Q_, P_, R_ = mk("g_t1"), [mk("g_Q%d" % k) for k in range(2)], [mk("g_P%d" % k) for k in range(2)], [mk("g_R%d" % k) for k in range(2)]
              attnT_, KeT_, QeT_ = mk("g_attnT", BF16), mk("g_KeT", BF16), mk("g_QeT", BF16)
              k2e_, Vt_ = mk("g_k2e", BF16), mk("g_Vt")
              R1_, vnew_ = mk("g_resid"), mk("g_vnew", BF16)
              o_, junk_ = mk("g_o"), mk("g_junk")
              ss_ = mk("g_ss", F32, (128, 1))
              S = sb("g_S", [128, 128], F32, st)
              S_bf = sb("g_Sbf", [128, 128], BF16, st)
              kb.memset("dve", S[:, :], 0.0)
              kb.memset("dve", S_bf[:, :], 0.0)
              stage = [sb("g_stage%d" % i, [128, 128], F32, st) for i in range(2)]
              stage_i = [0]
              def g_stage1(n, ctx):
                  i2 = n % NB2
                  cs = slice(n * 128, (n + 1) * 128)
                  grep, DmT, EG = grep_[i2], DmT_[i2], EG_[i2]
                  decay_mats(g_all, dec, n, grep, DmT, EG)
                  yield
                  psk = next_ps()
                  kb.mm(psk[:, 0:128], kT[:, cs], kT[:, cs], start=True, stop=True)
                  kb.mm(psk[:, 128:256], kT[:, cs], qT[:, cs], start=True, stop=True)
                  t1 = t1_[i2]
                  kb.tt("dve", t1[:, :], DmT[:, :], psk[:, 0:128], ALU.mult)
                  Q0 = Q_[0][i2]
                  kb.stt("dve", Q0[:, :], t1[:, :], nbeta[:, n:n + 1], strictT, ALU.mult, ALU.mult)
                  attnT = attnT_[i2]
                  kb.tt("dve", attnT[:, :], DmT[:, :], psk[:, 128:256], ALU.mult)
                  yield
                  pst = next_ps()
                  kb.mm(pst[:, 0:128], Q0[:, :], ident, start=True, stop=True)
                  P0 = P_[0][i2]
                  kb.copy("act", P0[:, :], pst[:, 0:128])
                  yield
                  Rc = R_[0][i2]
                  kb.tt("pool", Rc[:, :], Q0[:, :], ident, ALU.add)
                  yield
                  Qc, Pc = Q0, P0
                  for k in range(1, 7):
                      psq = next_ps()
                      Pn = P_[k % 2][i2]
                      kb.mm(psq[:, 0:128], Qc[:, :], Pc[:, :], start=True, stop=True)
                      if k < 6:
                          kb.mm(psq[:, 128:256], Pc[:, :], Qc[:, :], start=True, stop=True)
                      kb.copy("act", Pn[:, :], psq[:, 0:128])
                      if k < 6:
                          Qn = Q_[k % 2][i2]
                          kb.copy("dve", Qn[:, :], psq[:, 128:256])
                      yield
                      psr = next_ps()
                      kb.mm(psr[:, 0:128], Pn[:, :], Rc[:, :], start=True, stop=True)
                      Rn = R_[k % 2][i2]
                      kb.tt("dve", Rn[:, :], Rc[:, :], psr[:, 0:128], ALU.add)
                      yield
                      Rc, Pc = Rn, Pn
                      if k < 6:
                          Qc = Qn
                  yield
                  KeT, QeT = KeT_[i2], QeT_[i2]
                  kb.tt("pool", KeT[:, :], kT[:, cs], EG[:, :], ALU.mult)
                  kb.tt("pool", QeT[:, :], qT[:, cs], EG[:, :], ALU.mult)
                  pstk = next_ps()
                  kb.mm(pstk[:, 0:128], kT[:, cs], ident_b, start=True, stop=True)
                  kb.mm(pstk[:, 128:256], vT[:, cs], ident_b, start=True, stop=True)
                  k2e, Vt = k2e_[i2], Vt_[i2]
                  kb.ts("dve", k2e[:, :], pstk[:, 0:128], dec["e2e"][:, n:n + 1], None, op0=ALU.mult)
                  kb.copy("act", Vt[:, :], pstk[:, 128:256])
                  ctx.update(dict(KeT=KeT, QeT=QeT, k2e=k2e, Vt=Vt, Rc=Rc, attnT=attnT))
                  yield

              def g_stage2(n, ctx):
                  i2 = n % NB2
                  KeT, QeT, k2e, Vt, Rc, attnT = (ctx[k_] for k_ in ('KeT', 'QeT', 'k2e', 'Vt', 'Rc', 'attnT'))
                  psa = next_ps()
                  kb.mm(psa[:, 0:128], KeT[:, :], S_bf[:, :], start=True, stop=True)
                  R1 = R1_[i2]
                  kb.tt("dve", R1[:, :], Vt[:, :], psa[:, 0:128], ALU.subtract)
                  kb.mm(psa[:, 128:256], Rc[:, :], R1[:, :], start=True, stop=True)
                  vnew = vnew_[i2]
                  kb.ts("dve", vnew[:, :], psa[:, 128:256], beta[:, n:n + 1], None, op0=ALU.mult)
                  pso = next_ps()
                  kb.mm(pso[:, 0:128], QeT[:, :], S_bf[:, :], start=True, stop=False)
                  kb.mm(pso[:, 0:128], attnT[:, :], vnew[:, :], start=False, stop=True)
                  kb.mm(pso[:, 128:256], k2e[:, :], vnew[:, :], start=True, stop=True)
                  kb.stt("dve", S[:, :], S[:, :], dec["cd"][:, n:n + 1], pso[:, 128:256], ALU.mult, ALU.add)
                  kb.copy("act", S_bf[:, :], S[:, :])
                  ss, junk, o = ss_[i2], junk_[i2], o_[i2]
                  kb.memset("pool", ss[:, :], 0.0)
                  kb.act(junk[:, :], pso[:, 0:128], AF.Square, accum=ss[:, :])
                  kb.act(ss[:, :], ss[:, :], AF.Sqrt, bias=kb.eps_tile(EPS), scale=1.0 / 128)
                  kb.recip(ss[:, :], ss[:, :])
                  kb.stt("dve", o[:, :], pso[:, 0:128], ss[:, 0:1], zg[:, n, :], ALU.mult, ALU.mult)
                  emit_out(0, o, 1, n, stage, stage_i)


              for n0 in range(0, NBLK, NB2):
                  ns = [n for n in range(n0, min(n0 + NB2, NBLK))]
                  ctxs = [dict() for _ in ns]
                  gens = [g_stage1(n, c_) for n, c_ in zip(ns, ctxs)]
                  live = list(gens)
                  while live:
                      for g_ in list(live):
                          try:
                              next(g_)
                          except StopIteration:
                              live.remove(g_)
                  for n, c_ in zip(ns, ctxs):
                      g_stage2(n, c_)
          kb.barrier()
    except StopBuild:
        pass

    def attention(st, slot, q_parts, k_parts, Vaug, tab, name):
        G = 4
        pts = [sb("%s_pt%d" % (name, i), [128, 512], BF16, st) for i in range(3)]
        ot = [sb("%s_ot%d" % (name, i), [128, 128], F32, st) for i in range(2)]
        rc = [sb("%s_rc%d" % (name, i), [128, 1], F32, st) for i in range(2)]
        stage = [sb("%s_stage%d" % (name, i), [128, 128], F32, st) for i in range(2)]
        stage_i = [0]
        pti = [0]
        oi = [0]
        ps_s = [pss[0], pss[1]]
        ps_o4 = [pss[2], pss[3], pss[4], pss[5]]
        si = [0]
        for gi, i0 in enumerate(range(0, NBLK, G)):
            i1 = min(i0 + G, NBLK) - 1
            def stage_q(j):
                is_ = max(i0, j)
                Wq = (i1 - is_ + 1) * 128
                q0 = is_ * 128
                ps = ps_s[si[0] % 2]
                si[0] += 1
                for pi, ((qb, P), (kbuf, _)) in enumerate(zip(q_parts, k_parts)):
                    kb.mm(ps[:, 0:Wq], kbuf[0:P, j * 128:(j + 1) * 128], qb[0:P, q0:q0 + Wq],
                          start=(pi == 0), stop=(pi == len(q_parts) - 1))
                pt = pts[pti[0] % 3]
                pti[0] += 1
                if tab is None:
                    kb.act(pt[:, 0:Wq], ps[:, 0:Wq], AF.Exp)
                else:
                    for ii in range(is_, i1 + 1):
                        c = (ii - is_) * 128
                        kb.act(pt[:, c:c + 128], ps[:, c:c + 128], AF.Exp, bias=tab[:, ii, j:j + 1])
                if j >= i0:
                    kb.tt("pool", pt[:, 0:128], pt[:, 0:128], causT_b, ALU.mult)
                return pt

            def stage_p(j, pt):
                is_ = max(i0, j)
                for ii in range(is_, i1 + 1):
                    c = (ii - is_) * 128
                    li = ii - i0
                    dst = ps_o4[li]
                    kb.mm(dst[:, 0:129], pt[:, c:c + 128],
                          bc(Vaug[:, j, :], Vaug.t[:, j, :]), start=(j == 0), stop=(j == ii))

            prev = None
            for j in range(0, i1 + 1):
                pt_j = stage_q(j)
                if prev is not None:
                    stage_p(*prev)
                prev = (j, pt_j)
            stage_p(*prev)
            for ii in range(i0, i1 + 1):
                li = ii - i0
                src = ps_o4[li]
                c0 = 0
                r = rc[oi[0] % 2]
                o = ot[oi[0] % 2]
                oi[0] += 1
                kb.ts("dve", r[:, :], src[:, c0 + 128:c0 + 129], 1e-30, None, op0=ALU.max)
                kb.recip(r[:, :], r[:, :])
                kb.ts("dve", o[:, :], src[:, c0:c0 + 128], r[:, 0:1], None, op0=ALU.mult)
                emit_out(slot, o, 1, ii, stage, stage_i, psl=[pss[6], pss[7]])

    def make_vaug(st, name):
        Vaug = sb(name, [128, NBLK, 129], BF16, st)
        kb.memset("dve", bc(Vaug[:, :, 128:129], Vaug.t[:, :, 128:129]), 1.0)
        return Vaug

    def finish_vaug(Vaug):
        for p0, p1 in ((0, 32), (32, 64), (64, 96), (96, FRONT)):
            kb.memset("dve", bc(Vaug[:, 0, :], Vaug.t[p0:p1, 0, :]), 0.0)

    if "mla" in mixers:
        with ExitStack() as st:
            w = sb("w_mla_s", [128, KC, 832], BF16, st)
            kb.dma("pool", w[:, :, :], w_mla_d[:, :, :])
            wq = sb("mla_wq_s", [128, 4, 192], BF16, st)
            kb.dma("pool", wq[:, :, :], mla_wq_d[:, :, :])
            wkv = sb("mla_wkv_s", [128, 2, 256], BF16, st)
            kb.dma("pool", wkv[:, :, :], mla_wkv_d[:, :, :])
            mg = sb("mla_g_s", [128, 10], F32, st)
            kb.dma("sp", mg[:, :], mla_g_d[:, :])
            mgq = sb("mla_gq_s", [128, 2], F32, st)
            kb.ts("dve", mgq[:, :], mg[:, 6:8], 192.0 ** -0.5, None, op0=ALU.mult)
            ropes = [sb("rope_s%d" % i, [64, 2, 512], F32, st) for i in range(2)]
            rotm = sb("rotm_s", [64, 64], F32, st)
            kb.dma("sp", rotm[:, :], rotm_d[:, :])
            hns = [sb("mhn%d" % i, [128, KC, 512], BF16, st) for i in range(1)]
            lat = sb("m_lat", [128, 6, 512], F32, st)
            latn = sb("m_latn", [128, 6, 512], BF16, st)
            sqs = [sb("msq%d" % i, [128, 512], BF16, st) for i in range(2)]
            rstd = sb("mrstd", [128, 512], F32, st)
            raw = sb("m_raw", [128, 2, 512], F32, st)
            tr_ = sb("m_tr", [64, 512], F32, st)
            ta_ = sb("m_ta", [64, 512], F32, st)
            tb_ = sb("m_tb", [64, 512], F32, st)
            QTn = sb("m_QTn", [128, T], BF16, st)
            QTr = sb("m_QTr", [128, T], BF16, st)
            kb.memset("pool", QTr[64:128, :], 0.0)
            KTn = sb("m_KTn", [128, T], BF16, st)
            KTr = sb("m_KTr", [128, T], BF16, st)
            kb.memset("pool", KTr[64:128, :], 0.0)
            Vaug = make_vaug(st, "m_Vaug")

            def qk_finish(raw, gn, gr, dn, dr_, t0, W, rope):
                sq_sum_rstd([(raw[:, 0, 0:W], 128), (raw[0:64, 1, 0:W], 64)], W, 1.0 / 192, rstd, sqs, pss[7])
                kb.stt("dve", dn[:, t0:t0 + W], raw[:, 0, 0:W], gn, rstd[:, 0:W], ALU.mult, ALU.mult)
                kb.stt("dve", tr_[:, 0:W], raw[0:64, 1, 0:W], gr, rstd[0:64, 0:W], ALU.mult, ALU.mult)
                ps = next_ps(7)
                for c0 in range(0, W, 128):
                    kb.mm(ps[0:64, c0:c0 + 128], rotm[:, :], tr_[:, c0:c0 + 128], start=True, stop=True)
                kb.tt("dve", ta_[:, 0:W], tr_[:, 0:W], rope[:, 0, 0:W], ALU.mult)
                kb.tt("dve", tb_[:, 0:W], ps[0:64, 0:W], rope[:, 1, 0:W], ALU.mult)
                kb.tt("pool", dr_[0:64, t0:t0 + W], ta_[:, 0:W], tb_[:, 0:W], ALU.add)

            for ti, (t0, W) in enumerate(tiles):
                hn_t = hns[0]
                load_hn(hn_t, t0, W)
                rope = ropes[ti % 2]
                kb.dma("sp", rope[:, :, 0:W], bc(rope_d[:, :, t0:t0 + W], rope_d.t[:, :, t0:t0 + W].rearrange("c p t -> p c t")))
                for c in range(6):
                    ps = next_ps(7)
                    proj_fm(ps, w, c * 128, 128, hn_t, W)
                    kb.copy("act", lat[:, c, 0:W], ps[:, 0:W])
                sq_sum_rstd([(lat[:, c, 0:W], 128) for c in range(4)], W, 1.0 / 512, rstd, sqs, pss[7])
                for c in range(4):
                    kb.stt("dve", latn[:, c, 0:W], lat[:, c, 0:W], mg[:, c:c + 1], rstd[:, 0:W], ALU.mult, ALU.mult)
                sq_sum_rstd([(lat[:, c, 0:W], 128) for c in (4, 5)], W, 1.0 / 256, rstd, sqs, pss[7])
                for c in (4, 5):
                    kb.stt("dve", latn[:, c, 0:W], lat[:, c, 0:W], mg[:, c:c + 1], rstd[:, 0:W], ALU.mult, ALU.mult)
                ps = next_ps(7)
                for c in range(4):
                    kb.mm(ps[:, 0:W], wq[:, c, 0:128], latn[:, c, 0:W], start=(c == 0), stop=(c == 3))
                kb.copy("act", raw[:, 0, 0:W], ps[:, 0:W])
                ps = next_ps(7)
                for c in range(4):
                    kb.mm(ps[0:64, 0:W], wq[:, c, 128:192], latn[:, c, 0:W], start=(c == 0), stop=(c == 3))
                kb.copy("act", raw[0:64, 1, 0:W], ps[0:64, 0:W])
                qk_finish(raw, mgq[:, 0:1], mgq[0:64, 1:2], QTn, QTr, t0, W, rope)
                ps = next_ps(7)
                for c in range(2):
                    kb.mm(ps[:, 0:W], wkv[:, c, 0:128], latn[:, 4 + c, 0:W], start=(c == 0), stop=(c == 1))
                kb.copy("act", raw[:, 0, 0:W], ps[:, 0:W])
                ps = next_ps(7)
                proj_fm(ps, w, 768, 64, hn_t, W)
                kb.copy("act", raw[0:64, 1, 0:W], ps[0:64, 0:W])
                qk_finish(raw, mg[:, 8:9], mg[0:64, 9:10], KTn, KTr, t0, W, rope)
                for tb in range(W // 128):
                    blk = t0 // 128 + tb
                    ps = next_ps(7)
                    for c in range(2):
                        kb.mm(ps[:, 0:128], latn[:, 4 + c, tb * 128:(tb + 1) * 128], wkv[:, c, 128:256],
                              start=(c == 0), stop=(c == 1))
                    kb.copy("act", Vaug[:, blk, 0:128], ps[:, 0:128])
            finish_vaug(Vaug)
            if os.environ.get("DUMPQ"):
                kb.dma("pool", bo[3, :, :], QTn[:, :], final=True)
                kb.dma("pool", bo[4, :, :], QTr[:, :], final=True)
            attention(st, 1, [(QTn, 128), (QTr, 128)], [(KTn, 128), (KTr, 128)], Vaug, None, "ma")
        kb.barrier()

    if "fox" in mixers:
        with ExitStack() as st:
            w = sb("w_fox_s", [128, KC, 384], BF16, st)
            kb.dma("pool", w[:, :, :], w_fox_d[:, :, :])
            fg = sb("fox_g_s", [128, 3], F32, st)
            kb.dma("sp", fg[:, :], fox_g_d[:, :])
            fgq = sb("fox_gq", [128, 1], F32, st)
            kb.ts("dve", fgq[:, :], fg[:, 0:1], 128.0 ** -0.5, None, op0=ALU.mult)
            nbf = sb("fox_nbf", [128, 1], F32, st)
            kb.ts("dve", nbf[:, :], fg[:, 2:3], -1.0, None, op0=ALU.mult)
            hns = [sb("fhn%d" % i, [128, KC, 512], BF16, st) for i in range(2)]
            raws = [sb("f_raw%d" % i, [128, 512], F32, st) for i in range(2)]
            sqs = [sb("fsq%d" % i, [128, 512], BF16, st) for i in range(2)]
            rstd = sb("frstd", [128, 512], F32, st)
            QT = sb("f_QT", [128, T], BF16, st)
            KT = sb("f_KT", [128, T], BF16, st)
            Vaug = make_vaug(st, "f_Vaug")
            for ti, (t0, W) in enumerate(tiles):
                hn_t = hns[ti % 2]
                load_hn(hn_t, t0, W)
                for c, (dst, gv) in enumerate(((QT, fgq[:, 0:1]), (KT, fg[:, 1:2]))):
                    ps = next_ps(7)
                    proj_fm(ps, w, c * 128, 128, hn_t, W)
                    raw = raws[c]
                    kb.copy("act", raw[:, 0:W], ps[:, 0:W])
                    sq_sum_rstd([(raw[:, 0:W], 128)], W, 1.0 / 128, rstd, sqs, pss[7])
                    kb.stt("dve", dst[:, t0:t0 + W], raw[:, 0:W], gv, rstd[:, 0:W], ALU.mult, ALU.mult)
                for tb in range(W // 128):
                    blk = t0 // 128 + tb
                    ps = next_ps(7)
                    proj_tm(ps[:, 0:128], hn_t, tb, w, 256, 128)
                    kb.copy("act", Vaug[:, blk, 0:128], ps[:, 0:128])
            finish_vaug(Vaug)
            lf = sb("f_lf", [128, NBLK], F32, st)
            kb.act(lf[:, :], bc(small_all[:, :, 2], small_all.t[:, :, 2]), AF.Exp, bias=nbf[:, 0:1], scale=-1.0)
            kb.act(lf[:, :], lf[:, :], AF.Ln, bias=kb.eps_tile(1.0))
            kb.ts("dve", lf[:, :], lf[:, :], -1.0, None, op0=ALU.mult)
            within = sb("f_within", [128, NBLK], F32, st)
            totB = sb("f_totB", [128, NBLK], F32, st)
            ps = next_ps(7)
            kb.mm(ps[:, 0:NBLK], Umat, lf[:, :], start=True, stop=True)
            kb.copy("dve", within[:, :], ps[:, 0:NBLK])
            ps = next_ps(7)
            kb.mm(ps[:, 0:NBLK], ones, lf[:, :], start=True, stop=True)
            kb.copy("dve", totB[:, :], ps[:, 0:NBLK])
            tab = sb("f_tab", [128, NBLK, NBLK], F32, st)
            for i in range(NBLK):
                if i > 0:
                    kb.ts("dve", tab[:, i, 0:i], tab[:, i - 1, 0:i], totB[:, i:i + 1], None, op0=ALU.add)
                kb.tt("dve", tab[:, i, i:i + 1], totB[:, i:i + 1], within[:, i:i + 1], ALU.subtract)
            attention(st, 2, [(QT, 128)], [(KT, 128)], Vaug, tab, "fa")
        kb.barrier()

    if "ssd" in mixers:
        with ExitStack() as st:
            w = sb("w_ssd_s", [128, KC, 768], BF16, st)
            kb.dma("pool", w[:, :, :], w_ssd_d[:, :, :])
            scv = sb("ssd_conv_s", [128, 4, 5], F32, st)
            kb.dma("sp", scv[:, :, :], ssd_conv_d[:, :, :])
            svec = sb("ssd_vec_s", [128, 8], F32, st)
            kb.dma("sp", svec[:, :], ssd_vec_d[:, :])
            srow = sb("ssd_row_s", [128, 2, 256], F32, st)
            kb.dma("sp", srow[:, :, :], bc(ssd_row_d[:, :, :], ssd_row_d.t.rearrange("c p f -> p c f")))
            hns = [sb("shn%d" % i, [128, KC, 512], BF16, st) for i in range(1)]
            pre = sb("spre", [128, 4, 515], F32, st)
            acc = [sb("sacc%d" % i, [128, 512], F32, st) for i in range(2)]
            xT = [sb("s_xT%d" % i, [128, T], BF16, st) for i in range(2)]
            BT = sb("s_BT", [128, T], BF16, st)
            CT = sb("s_CT", [128, T], BF16, st)
            zs = sb("s_zs", [128, NBLK, 256], BF16, st)
            kb.memset("dve", pre[:, :, 0:3], 0.0)
            dsts = (xT[0], xT[1], BT, CT)
            for ti, (t0, W) in enumerate(tiles):
                hn_t = hns[0]
                load_hn(hn_t, t0, W)
                for c in range(4):
                    ps = next_ps()
                    proj_fm(ps, w, 256 + c * 128, 128, hn_t, W)
                    kb.copy("act", pre[:, c, 3:3 + W], ps[:, 0:W])
                    a = acc[c % 2]
                    conv4(a, bc(pre[:, c, :], pre.t[:, c, :]), bc(scv[:, c, :], scv.t[:, c, :]), W)
                    kb.copy("pool", pre[:, c, 0:3], pre[:, c, W:W + 3])
                    kb.act(dsts[c][:, t0:t0 + W], a[:, 0:W], AF.Silu, bias=scv[:, c, 4:5])
                for tb in range(W // 128):
                    blk = t0 // 128 + tb
                    ps = next_ps()
                    proj_tm(ps[:, 0:256], hn_t, tb, w, 0, 256)
                    kb.act(zs[:, blk, :], ps[:, 0:256], AF.Silu)
            for b_ in (xT[0], xT[1], BT):
                kb.memset("dve", b_[:, 0:FRONT], 0.0)
            negA = sb("s_negA", [128, 4], F32, st)
            kb.act(negA[:, :], svec[:, 4:8], AF.Exp)
            kb.ts("dve", negA[:, :], negA[:, :], -1.0, None, op0=ALU.mult)
            dts, decs, a_alls = [], [], []
            for h in range(4):
                dt = sb("s_dt%d" % h, [128, NBLK], F32, st)
                a_all = sb("s_a%d" % h, [128, NBLK], F32, st)
                softplus(dt[:, :], bc(small_all[:, :, 3 + h], small_all.t[:, :, 3 + h]), svec[:, h:h + 1])
                kb.ts("dve", a_all[:, :], dt[:, :], negA[:, h:h + 1], None, op0=ALU.mult)
                dts.append(dt)
                a_alls.append(a_all)
                decs.append(decay_prep(st, a_all, "sd%d_" % h))
            mk = lambda n, d=F32, shp=(128, 128), k=2: [sb("%s%d" % (n, i), list(shp), d, st) for i in range(k)]
            grep_, DmT_, EG_ = mk("s_grep", k=4), mk("s_DmT", k=4), mk("s_EG", k=4)
            attnT_, CeT_ = mk("s_attnT", BF16, k=8), mk("s_CeT", BF16, k=8)
            Btok_ = mk("s_Btok", BF16)
            Xtok_ = mk("s_Xtok", F32, (128, 256))
            Xdt_ = mk("s_Xdt", BF16, (128, 256))
            Xdec_ = mk("s_Xdec", BF16, (128, 256))
            CBt_ = mk("s_CBt")
            y1_ = mk("s_y1", F32, (128, 256))
            y2_ = mk("s_y2", F32, (128, 256))
            junk_ = mk("s_junk", F32, (128, 256))
            ss_ = mk("s_ss", F32, (128, 1))
            S = sb("s_S", [128, 256], F32, st)
            S_bf = sb("s_Sbf", [128, 256], BF16, st)
            kb.memset("dve", S[:, :], 0.0)
            kb.memset("dve", S_bf[:, :], 0.0)
            stage = [sb("s_stage%d" % i, [128, 128], F32, st) for i in range(2)]
            stage_i = [0]
            hh = [0]
            def s_stage1(n):
                i2 = n % 2
                cs = slice(n * 128, (n + 1) * 128)
                pst = next_ps()
                kb.mm(pst[:, 0:128], BT[:, cs], ident_b, start=True, stop=True)
                kb.mm(pst[:, 128:256], xT[0][:, cs], ident_b, start=True, stop=True)
                kb.mm(pst[:, 256:384], xT[1][:, cs], ident_b, start=True, stop=True)
                Btok, Xtok, Xdt, Xdec = Btok_[i2], Xtok_[i2], Xdt_[i2], Xdec_[i2]
                kb.copy("act", Btok[:, :], pst[:, 0:128])
                kb.copy("act", Xtok[:, :], pst[:, 128:384])
                for h in range(4):
                    hs = slice(h * 64, (h + 1) * 64)
                    kb.ts("dve", Xdt[:, hs], Xtok[:, hs], dts[h][:, n:n + 1], None, op0=ALU.mult)
                    kb.ts("dve", Xdec[:, hs], Xtok[:, hs], dts[h][:, n:n + 1], decs[h]["e2e"][:, n:n + 1],
                          op0=ALU.mult, op1=ALU.mult)
                psc = next_ps()
                kb.mm(psc[:, 0:128], BT[:, cs], CT[:, cs], start=True, stop=True)
                CBt = CBt_[i2]
                kb.copy("act", CBt[:, :], psc[:, 0:128])
                for h in range(4):
                    decay_mats(a_alls[h], decs[h], n, grep_[h], DmT_[h], EG_[h])
                for h in range(4):
                    kb.tt("dve", attnT_[i2 * 4 + h][:, :], CBt[:, :], DmT_[h][:, :], ALU.mult)
                    kb.tt("pool", CeT_[i2 * 4 + h][:, :], CT[:, cs], EG_[h][:, :], ALU.mult)
            def s_stage2(n):
                i2 = n % 2
                cs = slice(n * 128, (n + 1) * 128)
                Btok, Xtok, Xdt, Xdec, CBt = Btok_[i2], Xtok_[i2], Xdt_[i2], Xdec_[i2], CBt_[i2]
                psy = next_ps()
                for h in range(4):
                    hs = slice(h * 64, (h + 1) * 64)
                    kb.mm(psy[:, hs], CeT_[i2 * 4 + h][:, :], S_bf[:, hs], start=True, stop=False)
                    kb.mm(psy[:, hs], attnT_[i2 * 4 + h][:, :], Xdt[:, hs], start=False, stop=True)
                psn = next_ps()
                kb.mm(psn[:, 0:256], Btok[:, :], Xdec[:, :], start=True, stop=True)
                for h in range(4):
                    hs = slice(h * 64, (h + 1) * 64)
                    kb.stt("dve", S[:, hs], S[:, hs], decs[h]["cd"][:, n:n + 1], psn[:, hs], ALU.mult, ALU.add)
                kb.copy("act", S_bf[:, :], S[:, :])
                y1, y2, junk, ss = y1_[i2], y2_[i2], junk_[i2], ss_[i2]
                kb.tt("pool", y1[:, :], Xtok[:, :], bc(srow[:, 0, :], srow.t[:, 0, :]), ALU.mult)
                kb.tt("dve", y1[:, :], y1[:, :], psy[:, 0:256], ALU.add)
                kb.tt("pool", y2[:, :], y1[:, :], bc(zs[:, n, :], zs.t[:, n, :]), ALU.mult)
                kb.memset("pool", ss[:, :], 0.0)
                kb.act(junk[:, :], y2[:, :], AF.Square, accum=ss[:, :])
                kb.act(ss[:, :], ss[:, :], AF.Sqrt, bias=kb.eps_tile(EPS), scale=1.0 / 256)
                kb.recip(ss[:, :], ss[:, :])
                kb.stt("dve", y1[:, :], y2[:, :], ss[:, 0:1], bc(srow[:, 1, :], srow.t[:, 1, :]), ALU.mult, ALU.mult)

                emit_out(3, y1, 2, n, stage, stage_i)

            s_stage1(0)
            for n in range(NBLK):
                if n + 1 < NBLK:
                    s_stage1(n + 1)
                s_stage2(n)
        kb.barrier()
    kb.finish()
    return kb, locals()


IN_WIDTHS = (512, 512, 512, 512, 4, 4, 512, 256, 64, 512, 512, 512, 4, 512, 512, 256, 256, 8)
OFF = [0]
for _w in IN_WIDTHS:
    OFF.append(OFF[-1] + _w)


def r3(w):
    K, C = w.shape
    return np.ascontiguousarray(w.reshape(K // 128, 128, C).transpose(1, 0, 2))


def rep(v, n=128):
    v = np.asarray(v, np.float32).reshape(1, -1)
    return np.ascontiguousarray(np.broadcast_to(v, (n, v.shape[1])))


def make_consts(T):
    p = np.arange(128)[:, None]
    f = np.arange(128)[None, :]
    cst = np.stack([
        (p == f), (p <= f), np.where(f >= p, 0.0, -30000.0), (f > p), (f >= p), np.ones((128, 128)),
    ]).astype(np.float32)
    pos = np.maximum(np.arange(T) - FRONT, 0).astype(np.float32)
    inv = (1.0 / (10000.0 ** (np.arange(0, 64, 2, dtype=np.float32) / 64.0))).astype(np.float32)
    ang = pos[None, :] * np.concatenate([inv, inv])[:, None]
    rope = np.stack([np.cos(ang), np.sin(ang)]).astype(np.float32)
    rot = np.zeros((64, 64), np.float32)
    for ff in range(32):
        rot[ff, ff + 32] = -1.0
        rot[ff + 32, ff] = 1.0
    return cst, rope, np.ascontiguousarray(rot.T)


def phaseA_inputs(P, l, j, hT_b, T):
    G = j // 2
    w_in = P["w_in"][l]
    col = lambda o, a, n: w_in[:, OFF[o] + a:OFF[o] + a + n]
    cst, rope, rotT = make_consts(T)
    small = np.concatenate([col(4, j, 1), col(5, j, 1), col(12, j, 1), col(17, G * 4, 4),
                            np.zeros((2048, 1), np.float32)], axis=1)
    w_gdn = np.concatenate([col(0, j * 128, 128), col(1, j * 128, 128), col(2, j * 128, 128), col(3, j * 128, 128)], 1)
    cw = P["gdn_conv_w"][l]
    gdn_conv = np.stack([cw[:, j * 128:(j + 1) * 128].T, cw[:, 512 + j * 128:512 + (j + 1) * 128].T,
                         cw[:, 1024 + j * 128:1024 + (j + 1) * 128].T], axis=1)
    w_mla = np.concatenate([col(6, 0, 512), col(7, 0, 256), col(8, 0, 64)], 1)
    mla_g = np.zeros((128, 10), np.float32)
    mla_g[:, 0:4] = P["mla_qa_g"][l].reshape(4, 128).T
    mla_g[:, 4:6] = P["mla_kva_g"][l].reshape(2, 128).T
    mla_g[:, 6] = P["mla_qn_g"][l][0:128]
    mla_g[0:64, 7] = P["mla_qn_g"][l][128:192]
    mla_g[:, 8] = P["mla_kn_g"][l][0:128]
    mla_g[0:64, 9] = P["mla_kn_g"][l][128:192]
    w_fox = np.concatenate([col(9, j * 128, 128), col(10, j * 128, 128), col(11, j * 128, 128)], 1)
    fox_g = np.stack([P["fox_qn_g"][l], P["fox_kn_g"][l], np.full(128, P["fox_b_f"][l][j], np.float32)], 1)
    w_ssd = np.concatenate([col(13, G * 256, 256), col(14, G * 256, 256), col(15, G * 128, 128), col(16, G * 128, 128)], 1)
    sw = P["ssd_conv_w"][l]
    sbias = P["ssd_conv_b"][l]
    chs = [slice(G * 256, G * 256 + 128), slice(G * 256 + 128, G * 256 + 256),
           slice(512 + G * 128, 512 + (G + 1) * 128), slice(768 + G * 128, 768 + (G + 1) * 128)]
    ssd_conv = np.stack([np.concatenate([sw[:, c].T, sbias[c][:, None]], 1) for c in chs], axis=1)
    ssd_vec = rep(np.concatenate([P["ssd_dt_bias"][l][G * 4:G * 4 + 4], P["ssd_A_log"][l][G * 4:G * 4 + 4]]))
    ssd_row = np.stack([rep(np.repeat(P["ssd_D"][l][G * 4:G * 4 + 4], 64)), rep(P["ssd_norm_g"][l][G * 256:(G + 1) * 256])])
    f32c = lambda a: np.ascontiguousarray(a, dtype=np.float32)
    return {
        "hT": f32c(hT_b), "g1": f32c(P["mix_norm_g"][l].reshape(KC, 128).T), "w_small": r3(f32c(small)), "cst": cst,
        "w_gdn": r3(f32c(w_gdn)), "gdn_conv": f32c(gdn_conv),
        "gdn_vec": rep([P["gdn_A_log"][l][j], P["gdn_dt_bias"][l][j]]), "gdn_ng": rep(P["gdn_norm_g"][l]),
        "w_mla": r3(f32c(w_mla)), "mla_wq": r3(f32c(P["mla_wq_b"][l][:, j * 192:(j + 1) * 192])),
        "mla_wkv": r3(f32c(P["mla_wkv_b"][l][:, j * 256:(j + 1) * 256])), "mla_g": mla_g,
        "rope": rope, "rotm": rotT,
        "w_fox": r3(f32c(w_fox)), "fox_g": f32c(fox_g),
        "w_ssd": r3(f32c(w_ssd)), "ssd_conv": f32c(ssd_conv), "ssd_vec": ssd_vec, "ssd_row": f32c(ssd_row),
    }


def seq_to_hT(h_seq, T):
    L = h_seq.shape[0]
    out = np.zeros((2048, T), np.float32)
    out[:, FRONT:FRONT + L] = h_seq.T
    return out.reshape(KC, 128, T)


_PROGS = {}


def _prog(key, fn):
    if key not in _PROGS:
        _PROGS[key] = fn()
    return _PROGS[key]


def panels(w3, pw=256):
    p, K, C = w3.shape
    n = (C + pw - 1) // pw
    if n * pw != C:
        w3 = np.concatenate([w3, np.zeros((p, K, n * pw - C), w3.dtype)], axis=2)
    return np.ascontiguousarray(w3.reshape(p, K, n, pw).transpose(2, 0, 1, 3))


def phaseB_weights(P, l):
    f32c = lambda a: np.ascontiguousarray(a, dtype=np.float32)
    d = {
        "wgate": np.stack([panels(r3(f32c(P["w_gate"][l][b]))) for b in range(4)]),
        "wbr": np.stack([r3(f32c(P["w_branch"][l][b])) for b in range(4)]),
        "wo": panels(r3(f32c(P["w_o"][l]))),
        "g1": f32c(P["mix_norm_g"][l].reshape(KC, 128).T),
        "g2": f32c(P["ffn_norm_g"][l].reshape(KC, 128).T),
    }
    i = l // 2
    if l % 2 == 0:
        d.update({"wfg": panels(r3(f32c(P["dense_w_gate"][i]))), "wfu": panels(r3(f32c(P["dense_w_up"][i]))),
                  "wfd": panels(r3(f32c(P["dense_w_down"][i])), 128)})
    else:
        sel = np.zeros((N_EXP, N_EXP, 128), np.float32)
        for e in range(N_EXP):
            sel[e, e, :] = 1.0
        d.update({"wr": r3(f32c(P["router_w"][i])),
                  "weg": np.stack([panels(r3(f32c(P["moe_w_gate"][i][e]))) for e in range(N_EXP)]),
                  "weu": np.stack([panels(r3(f32c(P["moe_w_up"][i][e]))) for e in range(N_EXP)]),
                  "wed": np.stack([panels(r3(f32c(P["moe_w_down"][i][e])), 128) for e in range(N_EXP)]),
                  "sel": sel, "ident": np.eye(128, dtype=np.float32)})
    return d


def kernel(**inputs):
    P = {k: np.asarray(v) for k, v in inputs.items()}
    x = P["x"].astype(np.float32, copy=False)
    Bsz, S, D = x.shape
    NX = S // 128
    NBLK = NX + 1
    T = NBLK * 128
    NXB = NX // 4
    NTB = NXB + 1
    depth = P["w_in"].shape[0]
    meta = P["meta_tokens"].astype(np.float32)
    hT = [seq_to_hT(np.concatenate([meta, x[b]], 0), T) for b in range(Bsz)]
    cores = list(range(8))
    for l in range(depth):
        kbA, _ = _prog(("A", NBLK), lambda: build_phaseA(NBLK))
        in_maps = [phaseA_inputs(P, l, c % 4, hT[c // 4], T) for c in cores]
        resA = run_bass_kernel_spmd(kbA.nc, in_maps, core_ids=cores)
        br = []
        for b in range(Bsz):
            bos = [resA.results[b * 4 + j]["bo"] for j in range(4)]
            rows = [bos[j][0] for j in range(4)] + [bos[j][1] for j in range(4)] + [bos[j][2] for j in range(4)] \
                + [bos[j][3 + (j % 2)] for j in range(4)]
            br.append(np.stack(rows))
        del resA
        kind = "dense" if l % 2 == 0 else "moe"
        last = (l == depth - 1)
        ntb = NXB if last else NTB
        kbB = _prog(("B", ntb, kind), lambda: build_phaseB(ntb, kind))
        wts = phaseB_weights(P, l)
        in_maps = []
        for c in cores:
            b, q = c // 4, c % 4
            if last:
                cols = np.r_[128 + q * NXB * 128:128 + (q + 1) * NXB * 128]
            else:
                cols = np.r_[0:128, 128 + q * NXB * 128:128 + (q + 1) * NXB * 128]
            m = dict(wts)
            m["hT"] = np.ascontiguousarray(hT[b][:, :, cols])
            m["br"] = np.ascontiguousarray(br[b][:, :, cols])
            in_maps.append(m)
        resB = run_bass_kernel_spmd(kbB.nc, in_maps, core_ids=cores)
        for c in cores:
            b, q = c // 4, c % 4
            o = resB.results[c]["hTo"]
            if last:
                hT[b][:, :, 128 + q * NXB * 128:128 + (q + 1) * NXB * 128] = o
                continue
            if q == 0:
                hT[b][:, :, FRONT:128] = o[:, :, FRONT:128]
            hT[b][:, :, 128 + q * NXB * 128:128 + (q + 1) * NXB * 128] = o[:, :, 128:]
        del resB
    out = np.stack([np.ascontiguousarray(hT[b].reshape(D, T)[:, 128:].T) for b in range(Bsz)])
    return out.astype(np.float32)
```

```python
import os
import numpy as np
import concourse.bass as bass
import concourse.mybir as mybir
from concourse.bass_utils import run_bass_kernel_spmd

F32 = mybir.dt.float32
BF16 = mybir.dt.bfloat16
AF = mybir.ActivationFunctionType
ALU = mybir.AluOpType
AX = mybir.AxisListType


class Res:
    __slots__ = ("w", "r", "dsem", "dcnt")

    def __init__(self):
        self.w = None
        self.r = {}
        self.dsem = None
        self.dcnt = 0


class V:
    __slots__ = ("ap", "res", "space")

    def __init__(self, ap, res, space="sbuf"):
        self.ap = ap
        self.res = res
        self.space = space

    def __getitem__(self, idx):
        return V(self.ap[idx], self.res, self.space)


class Buf:
    def __init__(self, kb, name, shape, dtype, space="sbuf", kind=None, nres=1, stack=None):
        self.kb = kb
        nc = kb.nc
        self.name = name
        if space == "sbuf":
            if stack is not None:
                self.t = stack.enter_context(nc.sbuf_tensor(name, list(shape), dtype))
            else:
                self.t = nc.alloc_sbuf_tensor(name, list(shape), dtype)
        elif space == "psum":
            self.t = nc.alloc_psum_tensor(name, list(shape), dtype)
        else:
            self.t = nc.dram_tensor(name, list(shape), dtype, kind=kind or "Internal").ap()
        self.res = [Res() for _ in range(nres)]
        self.space = space

    def __getitem__(self, idx):
        return V(self.t[idx], self.res, self.space)

    def sub(self, ri, idx):
        return V(self.t[idx], [self.res[ri]], self.space)


def bc(v, ap):
    return V(ap, v.res, v.space)


class StopBuild(Exception):
    pass


class KB:
    ENG = ("pe", "act", "dve", "pool", "sp")

    def ck(self, name=""):
        import os
        lim = int(os.environ.get("STOP_AT", "0"))
        self._ck = getattr(self, "_ck", 0) + 1
        if lim and self._ck >= lim:
            print("STOP at checkpoint", self._ck, name)
            raise StopBuild()

    def __init__(self):
        self.nc = bass.Bass("TRN2", target_bir_lowering=False)
        nc = self.nc
        self.e = {"pe": nc.tensor, "act": nc.scalar, "dve": nc.vector, "pool": nc.gpsimd, "sp": nc.sync}
        self.sem = {}
        self.cnt = {}
        self.seen = {k: {} for k in self.ENG}
        self.semh = {}
        for k in self.ENG:
            h = nc.alloc_semaphore("sem_" + k)
            self.semh[k] = h
            self.cnt[k] = 0
        self.ndsem = 0
        self.dma_max = {}
        self.out_tokens = []
        self.ninstr = 0

    def _dsem(self, res):
        if res.dsem is None:
            key = "d%d" % self.ndsem
            self.ndsem += 1
            self.semh[key] = self.nc.alloc_semaphore("sem_" + key)
            res.dsem = key
        return res.dsem

    def _collect(self, eng, outs, ins):
        waits = {}

        def need(tok):
            if tok is None:
                return
            s, val = tok
            if waits.get(s, 0) < val:
                waits[s] = val

        for v in ins:
            for r in v.res:
                need(r.w)
                if v.space == "psum":
                    for s, val in r.r.items():
                        if s != eng:
                            need((s, val))
        for v in outs:
            for r in v.res:
                if r.w is not None:
                    need(r.w)
                for s, val in r.r.items():
                    need((s, val))
        if eng == "pe" and "pe" in waits:
            del waits["pe"]
        return waits

    def _dowaits(self, eng, waits):
        E = self.e[eng]
        seen = self.seen[eng]
        for s, val in waits.items():
            if seen.get(s, 0) >= val:
                continue
            E.wait_ge(self.semh[s], val)
            seen[s] = val

    def op(self, eng, fn, outs, ins):
        waits = self._collect(eng, outs, ins)
        self._dowaits(eng, waits)
        ins_ = fn()
        self.cnt[eng] += 1
        c = self.cnt[eng]
        ins_.then_inc(self.semh[eng], 1)
        for v in ins:
            for r in v.res:
                r.r[eng] = c
        for v in outs:
            for r in v.res:
                r.w = (eng, c)
                r.r = {}
        self.ninstr += 1
        return ins_

    def dma(self, q, out, in_, final=False):
        waits = self._collect("__dma__", [out], [in_])
        self._dowaits(q, waits)
        if out.space == "sbuf":
            sres = out.res[0]
        elif in_.space == "sbuf":
            sres = in_.res[0]
        else:
            sres = out.res[0]
        key = self._dsem(sres)
        ins_ = self.e[q].dma_start(out=out.ap, in_=in_.ap)
        sres.dcnt += 16
        ins_.then_inc(self.semh[key], 16)
        tok = (key, sres.dcnt)
        self.dma_max[key] = sres.dcnt
        for r in in_.res:
            r.r[key] = max(r.r.get(key, 0), sres.dcnt)
        for r in out.res:
            r.w = tok
            r.r = {}
        if final:
            self.out_tokens.append(tok)
        self.ninstr += 1
        return ins_

    def transfer(self, olds, news):
        toks = {}
        for b in olds:
            for r in b.res:
                if r.w is not None:
                    toks[r.w[0]] = max(toks.get(r.w[0], 0), r.w[1])
                for s, val in r.r.items():
                    toks[s] = max(toks.get(s, 0), val)
        for b in news:
            for r in b.res:
                r.w = None
                r.r = dict(toks)

    def barrier(self):
        allw = {k: self.cnt[k] for k in ("pe", "act", "dve", "pool") if self.cnt[k] > 0}
        for key, val in self.dma_max.items():
            allw[key] = val
        for e in self.ENG:
            w = {k: v for k, v in allw.items() if k != e}
            self._dowaits(e, w)

    def finish(self):
        waits = {}
        for s, val in self.out_tokens:
            waits[s] = max(waits.get(s, 0), val)
        self._dowaits("sp", waits)
        for k in ("pe", "act", "dve", "pool"):
            if self.cnt[k] > 0:
                self._dowaits("sp", {k: self.cnt[k]})

    def mm(self, out, lhsT, rhs, start=True, stop=True):
        nc = self.nc
        return self.op("pe", lambda: nc.tensor.matmul(out.ap, lhsT.ap, rhs.ap, start=start, stop=stop),
                       [out], [lhsT, rhs])

    def tr(self, out, in_, ident):
        nc = self.nc
        return self.op("pe", lambda: nc.tensor.transpose(out.ap, in_.ap, ident.ap), [out], [in_, ident])

    def act(self, out, in_, func, bias=None, scale=1.0, accum=None, eng="act"):
        nc = self.nc
        ins = [in_]
        kw = {}
        if bias is not None:
            if isinstance(bias, V):
                ins.append(bias)
                kw["bias"] = bias.ap
            else:
                kw["bias"] = bias
        if isinstance(scale, V):
            ins.append(scale)
            kw["scale"] = scale.ap
        else:
            kw["scale"] = scale
        outs = [out]
        if accum is not None:
            outs.append(accum)
            kw["accum_out"] = accum.ap
        return self.op("act", lambda: nc.scalar.activation(out=out.ap, in_=in_.ap, func=func, **kw), outs, ins)

    def tt(self, eng, out, a, b, op):
        E = self.e[eng]
        return self.op(eng, lambda: E.tensor_tensor(out=out.ap, in0=a.ap, in1=b.ap, op=op), [out], [a, b])

    def ts(self, eng, out, a, s1, s2=None, op0=ALU.mult, op1=None, accum=None):
        E = self.e[eng]
        ins = [a]
        a1 = s1
        a2 = s2
        if isinstance(s1, V):
            ins.append(s1)
            a1 = s1.ap
        if isinstance(s2, V):
            ins.append(s2)
            a2 = s2.ap
        kw = {}
        if op1 is not None:
            kw["op1"] = op1
        outs = [out]
        if accum is not None:
            outs.append(accum)
            kw["accum_out"] = accum.ap
        return self.op(eng, lambda: E.tensor_scalar(out=out.ap, in0=a.ap, scalar1=a1, scalar2=a2, op0=op0, **kw),
                       outs, ins)

    def stt(self, eng, out, a, s, b, op0, op1):
        E = self.e[eng]
        ins = [a, b]
        sa = s
        if isinstance(s, V):
            ins.append(s)
            sa = s.ap
        return self.op(eng, lambda: E.scalar_tensor_tensor(out=out.ap, in0=a.ap, scalar=sa, in1=b.ap, op0=op0, op1=op1),
                       [out], ins)

    def copy(self, eng, out, in_):
        if eng == "act":
            nc = self.nc
            return self.op("act", lambda: nc.scalar.copy(out=out.ap, in_=in_.ap), [out], [in_])
        E = self.e[eng]
        return self.op(eng, lambda: E.tensor_copy(out=out.ap, in_=in_.ap), [out], [in_])

    def recip(self, out, in_):
        nc = self.nc
        return self.op("dve", lambda: nc.vector.reciprocal(out=out.ap, in_=in_.ap), [out], [in_])

    def eps_tile(self, val):
        if not hasattr(self, "_eps"):
            self._eps = {}
        if val not in self._eps:
            b = Buf(self, "cst%d" % len(self._eps), [128, 1], F32)
            self.memset("dve", b[:, :], float(val))
            self._eps[val] = b
        return self._eps[val][:, :]

    def memset(self, eng, out, val):
        E = self.e[eng]
        return self.op(eng, lambda: E.memset(out.ap, val), [out], [])


class ABuf:
    def __init__(self, ap, space="sbuf", nres=1):
        self.t = ap
        self.res = [Res() for _ in range(nres)]
        self.space = space

    def __getitem__(self, idx):
        return V(self.t[idx], self.res, self.space)


D_MODEL = 2048
KC = 16
EPS = 1e-6
D_FF = 5632
NFF = D_FF // 128
N_EXP = 8
D_FFE = 1408
NFE = D_FFE // 128


def token_tiles(NT, W=512):
    out = []
    t = 0
    while t < NT:
        w = min(W, NT - t)
        out.append((t, w))
        t += w
    return out


def rmsnorm_fm(kb, h_sb, hn, g_sb, ones_bf, sqs, ps_ss, rstd, W, D=D_MODEL, post=None):
    nkc = D // 128
    for kc in range(nkc):
        sq = sqs[kc % len(sqs)]
        kb.act(sq[:, 0:W], h_sb[:, kc, 0:W], AF.Square)
        kb.mm(ps_ss[:, 0:W], ones_bf[:, :], sq[:, 0:W], start=(kc == 0), stop=(kc == nkc - 1))
    kb.act(rstd[:, 0:W], ps_ss[:, 0:W], AF.Sqrt, bias=kb.eps_tile(EPS))
    kb.recip(rstd[:, 0:W], rstd[:, 0:W])
    for kc in range(nkc):
        eng = "dve"
        if post is None:
            kb.stt(eng, hn[:, kc, 0:W], h_sb[:, kc, 0:W], g_sb[:, kc:kc + 1], rstd[:, 0:W], ALU.mult, ALU.mult)
        else:
            post(kc, eng)


def build_phaseB(NTB, kind):
    kb = KB()
    nc = kb.nc
    NT = NTB * 128
    D = D_MODEL
    dr = lambda n, s, k="ExternalInput": Buf(kb, n, s, F32, "dram", k)
    hT = dr("hT", [KC, 128, NT])
    br = dr("br", [16, 128, NT])
    wgate = dr("wgate", [4, D // 256, 128, KC, 256])
    wbr = dr("wbr", [4, 128, 4, D])
    wo = dr("wo", [D // 256, 128, KC, 256])
    g1d = dr("g1", [128, KC])
    g2d = dr("g2", [128, KC])
    if kind == "dense":
        wfg = dr("wfg", [D_FF // 256, 128, KC, 256])
        wfu = dr("wfu", [D_FF // 256, 128, KC, 256])
        wfd = dr("wfd", [KC, 128, NFF, 128])
    else:
        wr = dr("wr", [128, KC, N_EXP])
        weg = dr("weg", [N_EXP, 6, 128, KC, 256])
        weu = dr("weu", [N_EXP, 6, 128, KC, 256])
        wed = dr("wed", [N_EXP, KC, 128, NFE, 128])
        seld = dr("sel", [N_EXP, N_EXP, 128])
        identd = dr("ident", [128, 128])
    hTo = dr("hTo", [KC, 128, NT], "ExternalOutput")

    sb = lambda n, s, d=F32: Buf(kb, n, s, d)
    h_sb = sb("h_sb", [128, KC, 512])
    hn = sb("hn", [128, KC, 512], BF16)
    merged = sb("merged", [128, KC, 512], BF16)
    sqs = [sb("sq%d" % i, [128, 512], BF16) for i in range(2)]
    rstd = sb("rstd", [128, 512])
    X = nc.alloc_sbuf_tensor("X", [128, 12288], F32)
    macc = ABuf(X[:, 0:8192].rearrange("p (a b) -> p a b", b=512))
    br_sb = ABuf(X[:, 8192:12288].bitcast(BF16).rearrange("p (a b) -> p a b", b=512))
    actb = ABuf(X[:, 0:11264].bitcast(BF16).rearrange("p (a b) -> p a b", b=512))
    NWP = 3
    wps = [sb("wp%d" % i, [128, KC, 256], BF16) for i in range(NWP)]
    wbs = sb("wbs", [128, 4, D], BF16)
    wds = [sb("wd%d" % i, [128, NFF, 128], BF16) for i in range(2)]
    g1 = sb("g1s", [128, KC])
    g2 = sb("g2s", [128, KC])
    ones_bf = sb("ones_bf", [128, 128], BF16)
    sgs = [sb("sg%d" % i, [128, 512]) for i in range(2)]
    tmps = [sb("tmp%d" % i, [128, 512]) for i in range(2)]
    pss = [Buf(kb, "ps%d" % i, [128, 512], F32, "psum") for i in range(8)]
    if kind == "moe":
        stg = [sb("stg%d" % i, [128, 512]) for i in range(2)]
        wr_sb = sb("wr_sb", [128, KC, N_EXP])
        lg = sb("lg", [128, 4, 8])
        top8 = sb("top8", [128, 4, 8])
        comb = sb("comb", [128, 4, 8])
        cw = sb("cw", [128, 4, 8])
        combT = sb("combT", [8, 512], BF16)
        sel = sb("sel_sb", [N_EXP, N_EXP, 128], BF16)
        combB = [sb("combB%d" % i, [128, 512]) for i in range(2)]
        ident = sb("ident_sb", [128, 128])
        kb.dma("sp", wr_sb[:, :, :], wr[:, :, :])
        kb.dma("pool", sel[:, :, :], seld[:, :, :])
        kb.dma("sp", ident[:, :], identd[:, :])

    kb.dma("sp", g1[:, :], g1d[:, :])
    kb.dma("sp", g2[:, :], g2d[:, :])
    kb.memset("dve", ones_bf[:, :], 1.0 / D)

    wpi = [0]

    def next_wp():
        b = wps[wpi[0] % NWP]
        wpi[0] += 1
        return b

    psi = [0]

    def next_ps():
        p = pss[psi[0] % 6]
        psi[0] += 1
        return p

    ps_ss = pss[6]
    ps_misc = pss[7]
    cnt = [0]

    for (t0, W) in token_tiles(NT):
        for kc in range(KC):
            kb.dma("sp", h_sb[:, kc, 0:W], hT[kc, :, t0:t0 + W])
        rmsnorm_fm(kb, h_sb, hn, g1, ones_bf, sqs, ps_ss, rstd, W)
        kb.transfer([actb], [macc, br_sb])
        for i in range(16):
            kb.dma("pool", br_sb[:, i, 0:W], br[i, :, t0:t0 + W])
        for b in range(4):
            kb.dma("pool", wbs[:, :, :], wbr[b, :, :, :])
            for pc in range(D // 256):
                wp = next_wp()
                kb.dma("pool", wp[:, :, :], wgate[b, pc, :, :, :])
                for mm_ in range(2):
                    m = pc * 2 + mm_
                    pg = next_ps()
                    for kc in range(KC):
                        kb.mm(pg[:, 0:W], wp[:, kc, mm_ * 128:(mm_ + 1) * 128], hn[:, kc, 0:W],
                              start=(kc == 0), stop=(kc == KC - 1))
                    pb = next_ps()
                    for hh in range(4):
                        kb.mm(pb[:, 0:W], wbs[:, hh, m * 128:(m + 1) * 128], br_sb[:, b * 4 + hh, 0:W],
                              start=(hh == 0), stop=(hh == 3))
                    sg = sgs[cnt[0] % 2]
                    tmp = tmps[cnt[0] % 2]
                    cnt[0] += 1
                    kb.act(sg[:, 0:W], pg[:, 0:W], AF.Sigmoid)
                    if b == 0:
                        kb.tt("dve", macc[:, m, 0:W], sg[:, 0:W], pb[:, 0:W], ALU.mult)
                    elif b < 3:
                        kb.tt("dve", tmp[:, 0:W], sg[:, 0:W], pb[:, 0:W], ALU.mult)
                        kb.tt("dve", macc[:, m, 0:W], macc[:, m, 0:W], tmp[:, 0:W], ALU.add)
                    else:
                        kb.tt("dve", tmp[:, 0:W], sg[:, 0:W], pb[:, 0:W], ALU.mult)
                        kb.tt("dve", merged[:, m, 0:W], macc[:, m, 0:W], tmp[:, 0:W], ALU.add)
        for pc in range(D // 256):
            wp = next_wp()
            kb.dma("pool", wp[:, :, :], wo[pc, :, :, :])
            for mm_ in range(2):
                m = pc * 2 + mm_
                po = next_ps()
                for kc in range(KC):
                    kb.mm(po[:, 0:W], wp[:, kc, mm_ * 128:(mm_ + 1) * 128], merged[:, kc, 0:W],
                          start=(kc == 0), stop=(kc == KC - 1))
                kb.tt("dve", h_sb[:, m, 0:W], h_sb[:, m, 0:W], po[:, 0:W], ALU.add)
        kb.transfer([macc, br_sb], [actb])
        if kind == "dense":
            rmsnorm_fm(kb, h_sb, hn, g2, ones_bf, sqs, ps_ss, rstd, W)
            for pc in range(D_FF // 256):
                wpg = next_wp()
                kb.dma("pool", wpg[:, :, :], wfg[pc, :, :, :])
                wpu = next_wp()
                kb.dma("pool", wpu[:, :, :], wfu[pc, :, :, :])
                for mm_ in range(2):
                    fc = pc * 2 + mm_
                    pg = next_ps()
                    for kc in range(KC):
                        kb.mm(pg[:, 0:W], wpg[:, kc, mm_ * 128:(mm_ + 1) * 128], hn[:, kc, 0:W],
                              start=(kc == 0), stop=(kc == KC - 1))
                    pu = next_ps()
                    for kc in range(KC):
                        kb.mm(pu[:, 0:W], wpu[:, kc, mm_ * 128:(mm_ + 1) * 128], hn[:, kc, 0:W],
                              start=(kc == 0), stop=(kc == KC - 1))
                    sg = sgs[cnt[0] % 2]
                    cnt[0] += 1
                    kb.act(sg[:, 0:W], pg[:, 0:W], AF.Silu)
                    kb.tt("dve", actb[:, fc, 0:W], sg[:, 0:W], pu[:, 0:W], ALU.mult)
            for m in range(KC):
                wd = wds[m % 2]
                kb.dma("pool", wd[:, :, :], wfd[m, :, :, :])
                po = next_ps()
                for fc in range(NFF):
                    kb.mm(po[:, 0:W], wd[:, fc, :], actb[:, fc, 0:W], start=(fc == 0), stop=(fc == NFF - 1))
                kb.tt("dve", h_sb[:, m, 0:W], h_sb[:, m, 0:W], po[:, 0:W], ALU.add)
        else:
            nb = W // 128
            def post(kc, eng):
                st = stg[kc % 2]
                kb.stt("dve", st[:, 0:W], h_sb[:, kc, 0:W], g2[:, kc:kc + 1], rstd[:, 0:W], ALU.mult, ALU.mult)
                kb.copy("act", hn[:, kc, 0:W], st[:, 0:W])
                for tb in range(nb):
                    kb.mm(pss[tb][:, 0:8], st[:, tb * 128:(tb + 1) * 128], wr_sb[:, kc, :],
                          start=(kc == 0), stop=(kc == KC - 1))
            rmsnorm_fm(kb, h_sb, hn, g2, ones_bf, sqs, ps_ss, rstd, W, post=post)
            for tb in range(nb):
                kb.copy("dve", lg[:, tb, :], pss[tb][:, 0:8])
            for tb in range(nb):
                kb.op("dve", lambda tb=tb: nc.vector.max(out=top8.t[:, tb, :], in_=lg.t[:, tb, :]),
                      [top8[:, tb, :]], [lg[:, tb, :]])
            kb.tt("dve", cw[:, 0:nb, 0:1], top8[:, 0:nb, 0:1], top8[:, 0:nb, 1:2], ALU.subtract)
            kb.act(cw[:, 0:nb, 0:1], cw[:, 0:nb, 0:1], AF.Exp)
            kb.ts("dve", cw[:, 0:nb, 0:1], cw[:, 0:nb, 0:1], 1.0, None, op0=ALU.add)
            kb.op("dve", lambda: nc.vector.reciprocal(out=cw.t[:, 0:nb, 1:2], in_=cw.t[:, 0:nb, 0:1]),
                  [cw[:, 0:nb, 1:2]], [cw[:, 0:nb, 0:1]])
            kb.ts("dve", cw[:, 0:nb, 0:1], cw[:, 0:nb, 1:2], -1.0, 1.0, op0=ALU.mult, op1=ALU.add)
            for tb in range(nb):
                kb.ts("dve", comb[:, tb, :], lg[:, tb, :], top8[:, tb, 0:1], cw[:, tb, 0:1],
                      op0=ALU.is_equal, op1=ALU.mult)
                kb.ts("dve", lg[:, tb, :], lg[:, tb, :], top8[:, tb, 1:2], cw[:, tb, 1:2],
                      op0=ALU.is_equal, op1=ALU.mult)
                kb.tt("dve", comb[:, tb, :], comb[:, tb, :], lg[:, tb, :], ALU.add)
            pT = next_ps()
            for tb in range(nb):
                kb.mm(pT[0:8, tb * 128:(tb + 1) * 128], comb[:, tb, :], ident[:, :], start=True, stop=True)
            kb.copy("dve", combT[:, 0:W], pT[0:8, 0:W])
            for half in range(2):
                for el in range(4):
                    e = half * 4 + el
                    pcb = next_ps()
                    kb.mm(pcb[:, 0:W], sel[:, e, :], combT[:, 0:W], start=True, stop=True)
                    cb = combB[e % 2]
                    kb.copy("act", cb[:, 0:W], pcb[:, 0:W])
                    for pc in range(6):
                        c0 = pc * 256
                        cw_ = min(256, D_FFE - c0)
                        wpg = next_wp()
                        kb.dma("pool", wpg[:, :, 0:cw_], weg[e, pc, :, :, 0:cw_])
                        wpu = next_wp()
                        kb.dma("pool", wpu[:, :, 0:cw_], weu[e, pc, :, :, 0:cw_])
                        for mm_ in range(cw_ // 128):
                            fc = pc * 2 + mm_
                            pg = next_ps()
                            for kc in range(KC):
                                kb.mm(pg[:, 0:W], wpg[:, kc, mm_ * 128:(mm_ + 1) * 128], hn[:, kc, 0:W],
                                      start=(kc == 0), stop=(kc == KC - 1))
                            pu = next_ps()
                            for kc in range(KC):
                                kb.mm(pu[:, 0:W], wpu[:, kc, mm_ * 128:(mm_ + 1) * 128], hn[:, kc, 0:W],
                                      start=(kc == 0), stop=(kc == KC - 1))
                            sg = sgs[cnt[0] % 2]
                            tmp = tmps[cnt[0] % 2]
                            cnt[0] += 1
                            kb.act(sg[:, 0:W], pg[:, 0:W], AF.Silu)
                            kb.tt("dve", tmp[:, 0:W], sg[:, 0:W], pu[:, 0:W], ALU.mult)
                            kb.tt("dve", actb[:, el * NFE + fc, 0:W], tmp[:, 0:W], cb[:, 0:W], ALU.mult)
                for m in range(KC):
                    wd = wds[m % 2]
                    for el in range(4):
                        kb.dma("pool", wd[:, el * NFE:(el + 1) * NFE, :], wed[half * 4 + el, m, :, :, :])
                    po = next_ps()
                    for fc in range(NFF):
                        kb.mm(po[:, 0:W], wd[:, fc, :], actb[:, fc, 0:W], start=(fc == 0), stop=(fc == NFF - 1))
                    kb.tt("dve", h_sb[:, m, 0:W], h_sb[:, m, 0:W], po[:, 0:W], ALU.add)
        for kc in range(KC):
            kb.dma("sp", hTo[kc, :, t0:t0 + W], h_sb[:, kc, 0:W], final=True)
    kb.finish()
    return kb


FRONT = 112
W_SMALL = 8


def build_phaseA(NBLK, mixers=("gdn", "mla", "fox", "ssd")):
    from contextlib import ExitStack
    kb = KB()
    nc = kb.nc
    T = NBLK * 128
    dr = lambda n, s, k="ExternalInput", d=F32: Buf(kb, n, s, d, "dram", k)
    hT = dr("hT", [KC, 128, T])
    g1d = dr("g1", [128, KC])
    w_small_d = dr("w_small", [128, KC, W_SMALL])
    cst_d = dr("cst", [6, 128, 128])
    bo = dr("bo", [5, 128, T], "ExternalOutput")
    hnT = dr("hnT_scr", [KC, 128, T], "Internal", BF16)
    w_gdn_d = dr("w_gdn", [128, KC, 512])
    gdn_conv_d = dr("gdn_conv", [128, 3, 4])
    gdn_vec_d = dr("gdn_vec", [128, 2])
    gdn_ng_d = dr("gdn_ng", [128, 128])
    w_mla_d = dr("w_mla", [128, KC, 832])
    mla_wq_d = dr("mla_wq", [128, 4, 192])
    mla_wkv_d = dr("mla_wkv", [128, 2, 256])
    mla_g_d = dr("mla_g", [128, 10])
    rope_d = dr("rope", [2, 64, T])
    rotm_d = dr("rotm", [64, 64])
    w_fox_d = dr("w_fox", [128, KC, 384])
    fox_g_d = dr("fox_g", [128, 3])
    w_ssd_d = dr("w_ssd", [128, KC, 768])
    ssd_conv_d = dr("ssd_conv", [128, 4, 5])
    ssd_vec_d = dr("ssd_vec", [128, 8])
    ssd_row_d = dr("ssd_row", [2, 128, 256])

    sb = lambda n, s, d=F32, st=None: Buf(kb, n, s, d, stack=st)
    kb.eps_tile(EPS)
    kb.eps_tile(1.0)
    cst = sb("cst_sb", [128, 6, 128])
    kb.dma("sp", cst[:, :, :], bc(cst_d[:, :, :], cst_d.t.rearrange("c p f -> p c f")))
    cstb = sb("cst_bf", [128, 6, 128], BF16)
    kb.copy("dve", cstb[:, :, :], cst[:, :, :])
    ident, Umat, MnegT, strictT, causT, ones = [cst[:, i, :] for i in range(6)]
    ident_b, _, _, _, causT_b, ones_b = [cstb[:, i, :] for i in range(6)]
    g1 = sb("g1s", [128, KC])
    kb.dma("sp", g1[:, :], g1d[:, :])
    small_all = sb("small_all", [128, NBLK, W_SMALL])
    pss = [Buf(kb, "ps%d" % i, [128, 512], F32, "psum") for i in range(8)]
    psi = [0]

    def next_ps(n=8):
        p = pss[psi[0] % n]
        psi[0] += 1
        return p

    tiles = token_tiles(T)

    def sq_sum_rstd(srcs, W, scale, rstd, sqs, ps):
        for i, (v, P) in enumerate(srcs):
            sq = sqs[i % len(sqs)]
            kb.act(sq[0:P, 0:W], v, AF.Square)
            kb.mm(ps[:, 0:W], bc(ones_b, ones_b.ap[0:P, :]), sq[0:P, 0:W], start=(i == 0), stop=(i == len(srcs) - 1))
        kb.act(rstd[:, 0:W], ps[:, 0:W], AF.Sqrt, bias=kb.eps_tile(EPS), scale=scale)
        kb.recip(rstd[:, 0:W], rstd[:, 0:W])

    with ExitStack() as st:
        h_sbs = [sb("h_sb%d" % i, [128, KC, 512], F32, st) for i in range(2)]
        hn0s = [sb("hn0_%d" % i, [128, KC, 512], BF16, st) for i in range(2)]
        sqs = [sb("sq%d" % i, [128, 512], BF16, st) for i in range(2)]
        rstds = [sb("rstd%d" % i, [128, 512], F32, st) for i in range(2)]
        wsm = sb("wsm", [128, KC, W_SMALL], BF16, st)
        kb.dma("pool", wsm[:, :, :], w_small_d[:, :, :])
        import os
        STOP = int(os.environ.get("A0_STOP", "9"))
        for ti0, (t0, W) in enumerate(tiles):
            h_sb, hn, rstd = h_sbs[ti0 % 2], hn0s[ti0 % 2], rstds[ti0 % 2]
            if STOP < 1:
                break
            kb.dma("sp", h_sb[:, :, 0:W], bc(hT[:, :, t0:t0 + W], hT.t[:, :, t0:t0 + W].rearrange("k p w -> p k w")))
            if STOP < 2:
                continue
            sq_sum_rstd([(h_sb[:, kc, 0:W], 128) for kc in range(KC)], W, 1.0 / D_MODEL, rstd, sqs, pss[7])
            if STOP < 3:
                continue
            for kc in range(KC):
                kb.stt("dve", hn[:, kc, 0:W], h_sb[:, kc, 0:W], g1[:, kc:kc + 1], rstd[:, 0:W], ALU.mult, ALU.mult)
            if STOP < 4:
                continue
            for kc in range(KC):
                kb.dma("sp", hnT[kc, :, t0:t0 + W], hn[:, kc, 0:W])
            if STOP < 5:
                continue
            for tb in range(W // 128):
                blk = t0 // 128 + tb
                ps = next_ps(4)
                for kc in range(KC):
                    kb.mm(ps[:, 0:W_SMALL], hn[:, kc, tb * 128:(tb + 1) * 128], wsm[:, kc, :],
                          start=(kc == 0), stop=(kc == KC - 1))
                kb.copy("act", small_all[:, blk, :], ps[:, 0:W_SMALL])
    kb.barrier()

    def load_hn(hn_t, t0, W):
        for kc in range(KC):
            kb.dma("sp", hn_t[:, kc, 0:W], hnT[kc, :, t0:t0 + W])

    def proj_fm(ps, w, c0, ncols, hn_t, W):
        for kc in range(KC):
            kb.mm(ps[0:ncols, 0:W], w[:, kc, c0:c0 + ncols], hn_t[:, kc, 0:W], start=(kc == 0), stop=(kc == KC - 1))

    def proj_tm(psv, hn_t, tb, w, c0, ncols):
        for kc in range(KC):
            kb.mm(psv, hn_t[:, kc, tb * 128:(tb + 1) * 128], w[:, kc, c0:c0 + ncols], start=(kc == 0), stop=(kc == KC - 1))

    def softplus(out, in_, bias):
        kb.act(out, in_, AF.Exp, bias=bias)
        kb.act(out, out, AF.Ln, bias=kb.eps_tile(1.0))

    def conv4(acc, pre, wv, W):
        kb.ts("dve", acc[:, 0:W], pre[:, 3:3 + W], wv[:, 3:4], None, op0=ALU.mult)
        for i in (2, 1, 0):
            kb.stt("dve", acc[:, 0:W], pre[:, i:i + W], wv[:, i:i + 1], acc[:, 0:W], ALU.mult, ALU.add)

    def decay_prep(st, g_all, name):
        d = {}
        for nm in ("gcs", "ngcs", "eg", "e2e", "cd"):
            d[nm] = sb(name + nm, [128, NBLK], F32, st)
        ps = next_ps()
        kb.mm(ps[:, 0:NBLK], Umat, g_all[:, :], start=True, stop=True)
        kb.copy("dve", d["gcs"][:, :], ps[:, 0:NBLK])
        kb.ts("dve", d["ngcs"][:, :], d["gcs"][:, :], -1.0, None, op0=ALU.mult)
        kb.act(d["eg"][:, :], d["gcs"][:, :], AF.Exp)
        ps2 = next_ps()
        kb.mm(ps2[:, 0:NBLK], ones, g_all[:, :], start=True, stop=True)
        kb.act(d["cd"][:, :], ps2[:, 0:NBLK], AF.Exp)
        kb.tt("dve", d["e2e"][:, :], ps2[:, 0:NBLK], d["gcs"][:, :], ALU.subtract)
        kb.act(d["e2e"][:, :], d["e2e"][:, :], AF.Exp)
        return d

    def decay_mats(g_all, dec, n, grep, DmT, EGrow):
        kb.ts("dve", grep[:, :], ones, g_all[:, n:n + 1], None, op0=ALU.mult)
        ps = next_ps()
        kb.mm(ps[:, 0:128], grep[:, :], Umat, start=True, stop=True)
        kb.mm(ps[:, 128:256], grep[:, :], Umat, start=True, stop=False)
        kb.mm(ps[:, 128:256], ident, MnegT, start=False, stop=True)
        if EGrow is not None:
            kb.act(EGrow[:, :], ps[:, 0:128], AF.Exp)
        kb.act(DmT[:, :], ps[:, 128:256], AF.Exp, bias=dec["ngcs"][:, n:n + 1])

    def emit_out(slot, src_tm, ncol_chunks, blk, stage, stage_i, psl=None):
        for c in range(ncol_chunks):
            if psl is None:
                ps = next_ps()
            else:
                ps = psl[stage_i[0] % len(psl)]
            kb.mm(ps[:, 0:128], src_tm[:, c * 128:(c + 1) * 128], ident, start=True, stop=True)
            so = stage[stage_i[0] % len(stage)]
            stage_i[0] += 1
            kb.copy("act", so[:, :], ps[:, 0:128])
            kb.dma("sp", bo[slot + c, :, blk * 128:(blk + 1) * 128], so[:, :], final=True)

    try:
      if "gdn" in mixers:
          with ExitStack() as st:
              w = sb("w_gdn_s", [128, KC, 512], BF16, st)
              kb.dma("pool", w[:, :, :], w_gdn_d[:, :, :])
              convw = sb("gdn_convw", [128, 3, 4], F32, st)
              kb.dma("sp", convw[:, :, :], gdn_conv_d[:, :, :])
              gvec = sb("gdn_vec_s", [128, 2], F32, st)
              kb.dma("sp", gvec[:, :], gdn_vec_d[:, :])
              ngt = sb("gdn_ng_s", [128, 128], F32, st)
              kb.dma("sp", ngt[:, :], gdn_ng_d[:, :])
              hns = [sb("ghn%d" % i, [128, KC, 512], BF16, st) for i in range(2)]
              pre = sb("gpre", [128, 3, 515], F32, st)
              acc = [sb("gacc%d" % i, [128, 512], F32, st) for i in range(2)]
              sqs = [sb("gsq%d" % i, [128, 512], BF16, st) for i in range(2)]
              rstd = sb("grstd", [128, 512], F32, st)
              qT = sb("g_qT", [128, T], BF16, st)
              kT = sb("g_kT", [128, T], BF16, st)
              vT = sb("g_vT", [128, T], BF16, st)
              zg = sb("g_zg", [128, NBLK, 128], BF16, st)
              kb.memset("dve", pre[:, :, 0:3], 0.0)
              for ti, (t0, W) in enumerate(tiles):
                  hn_t = hns[ti % 2]
                  load_hn(hn_t, t0, W)
                  pq = []
                  for c in range(3):
                      ps = next_ps(7)
                      proj_fm(ps, w, c * 128, 128, hn_t, W)
                      pq.append(ps)
                  pz = []
                  for tb in range(W // 128):
                      ps = next_ps(7)
                      proj_tm(ps[:, 0:128], hn_t, tb, w, 384, 128)
                      pz.append(ps)
                  for c, dst in enumerate((qT, kT, vT)):
                      ps = pq[c]
                      kb.copy("act", pre[:, c, 3:3 + W], ps[:, 0:W])
                      a = acc[c % 2]
                      conv4(a, bc(pre[:, c, :], pre.t[:, c, :]), bc(convw[:, c, :], convw.t[:, c, :]), W)
                      kb.copy("pool", pre[:, c, 0:3], pre[:, c, W:W + 3])
                      if c < 2:
                          kb.act(a[:, 0:W], a[:, 0:W], AF.Silu)
                          sq_sum_rstd([(a[:, 0:W], 128)], W, 1.0, rstd, sqs, pss[7])
                          kb.stt("dve", dst[:, t0:t0 + W], a[:, 0:W], (128.0 ** -0.5) if c == 0 else 1.0, rstd[:, 0:W],
                                 ALU.mult, ALU.mult)
                      else:
                          kb.act(dst[:, t0:t0 + W], a[:, 0:W], AF.Silu)
                  for tb in range(W // 128):
                      blk = t0 // 128 + tb
                      ps = pz[tb]
                      a = acc[tb % 2]
                      kb.act(a[:, 0:128], ps[:, 0:128], AF.Silu)
                      kb.tt("pool", zg[:, blk, :], a[:, 0:128], ngt[:, :], ALU.mult)
              kb.ck("gdn prep done")
              beta = sb("g_beta", [128, NBLK], F32, st)
              nbeta = sb("g_nbeta", [128, NBLK], F32, st)
              g_all = sb("g_gall", [128, NBLK], F32, st)
              expA = sb("g_expA", [128, 1], F32, st)
              kb.act(beta[:, :], bc(small_all[:, :, 0], small_all.t[:, :, 0]), AF.Sigmoid)
              kb.ts("dve", nbeta[:, :], beta[:, :], -1.0, None, op0=ALU.mult)
              kb.act(expA[:, :], gvec[:, 0:1], AF.Exp)
              kb.ts("dve", expA[:, :], expA[:, :], -1.0, None, op0=ALU.mult)
              softplus(g_all[:, :], bc(small_all[:, :, 1], small_all.t[:, :, 1]), gvec[:, 1:2])
              kb.ts("dve", g_all[:, :], g_all[:, :], expA[:, 0:1], None, op0=ALU.mult)
              kb.ck("gdn scalars")
              dec = decay_prep(st, g_all, "gd_")
              kb.ck("gdn decay_prep")
              NB2 = 4
              mk = lambda n, d=F32, shp=(128, 128): [sb("%s%d" % (n, i), list(shp), d, st) for i in range(NB2)]
              grep_, DmT_, EG_ = mk("g_grep"), mk("g_DmT"), mk("g_EG")
              t1_, Q_, P_, R_ = mk("g_t1"), [mk("g_Q%d" % k) for k in range(2)], [mk("g_P%d" % k) for k in range(2)], [mk("g_R%d" % k) for k in range(2)]
              attnT_, KeT_, QeT_ = mk("g_attnT", BF16), mk("g_KeT", BF16), mk("g_QeT", BF16)
              k2e_, Vt_ = mk("g_k2e", BF16), mk("g_Vt")
              R1_, vnew_ = mk("g_resid"), mk("g_vnew", BF16)
              o_, junk_ = mk("g_o"), mk("g_junk")
              ss_ = mk("g_ss", F32, (128, 1))
              S = sb("g_S", [128, 128], F32, st)
              S_bf = sb("g_Sbf", [128, 128], BF16, st)
              kb.memset("dve", S[:, :], 0.0)
              kb.memset("dve", S_bf[:, :], 0.0)
              stage = [sb("g_stage%d" % i, [128, 128], F32, st) for i in range(2)]
              stage_i = [0]
              def g_stage1(n, ctx):
                  i2 = n % NB2
                  cs = slice(n * 128, (n + 1) * 128)
                  grep, DmT, EG = grep_[i2], DmT_[i2], EG_[i2]
                  decay_mats(g_all, dec, n, grep, DmT, EG)
                  yield
                  psk = next_ps()
                  kb.mm(psk[:, 0:128], kT[:, cs], kT[:, cs], start=True, stop=True)
                  kb.mm(psk[:, 128:256], kT[:, cs], qT[:, cs], start=True, stop=True)
                  t1 = t1_[i2]
                  kb.tt("dve", t1[:, :], DmT[:, :], psk[:, 0:128], ALU.mult)
                  Q0 = Q_[0][i2]
                  kb.stt("dve", Q0[:, :], t1[:, :], nbeta[:, n:n + 1], strictT, ALU.mult, ALU.mult)
                  attnT = attnT_[i2]
                  kb.tt("dve", attnT[:, :], DmT[:, :], psk[:, 128:256], ALU.mult)
                  yield
                  pst = next_ps()
                  kb.mm(pst[:, 0:128], Q0[:, :], ident, start=True, stop=True)
                  P0 = P_[0][i2]
                  kb.copy("act", P0[:, :], pst[:, 0:128])
                  yield
                  Rc = R_[0][i2]
                  kb.tt("pool", Rc[:, :], Q0[:, :], ident, ALU.add)
                  yield
                  Qc, Pc = Q0, P0
                  for k in range(1, 7):
                      psq = next_ps()
                      Pn = P_[k % 2][i2]
                      kb.mm(psq[:, 0:128], Qc[:, :], Pc[:, :], start=True, stop=True)
                      if k < 6:
                          kb.mm(psq[:, 128:256], Pc[:, :], Qc[:, :], start=True, stop=True)
                      kb.copy("act", Pn[:, :], psq[:, 0:128])
                      if k < 6:
                          Qn = Q_[k % 2][i2]
                          kb.copy("dve", Qn[:, :], psq[:, 128:256])
                      yield
                      psr = next_ps()
                      kb.mm(psr[:, 0:128], Pn[:, :], Rc[:, :], start=True, stop=True)
                      Rn = R_[k % 2][i2]
                      kb.tt("dve", Rn[:, :], Rc[:, :], psr[:, 0:128], ALU.add)
                      yield
                      Rc, Pc = Rn, Pn
                      if k < 6:
                          Qc = Qn
                  yield
                  KeT, QeT = KeT_[i2], QeT_[i2]
                  kb.tt("pool", KeT[:, :], kT[:, cs], EG[:, :], ALU.mult)
                  kb.tt("pool", QeT[:, :], qT[:, cs], EG[:, :], ALU.mult)
                  pstk = next_ps()
                  kb.mm(pstk[:, 0:128], kT[:, cs], ident_b, start=True, stop=True)
                  kb.mm(pstk[:, 128:256], vT[:, cs], ident_b, start=True, stop=True)
                  k2e, Vt = k2e_[i2], Vt_[i2]
                  kb.ts("dve", k2e[:, :], pstk[:, 0:128], dec["e2e"][:, n:n + 1], None, op0=ALU.mult)
                  kb.copy("act", Vt[:, :], pstk[:, 128:256])
                  ctx.update(dict(KeT=KeT, QeT=QeT, k2e=k2e, Vt=Vt, Rc=Rc, attnT=attnT))
                  yield

              def g_stage2(n, ctx):
                  i2 = n % NB2
                  KeT, QeT, k2e, Vt, Rc, attnT = (ctx[k_] for k_ in ('KeT', 'QeT', 'k2e', 'Vt', 'Rc', 'attnT'))
                  psa = next_ps()
                  kb.mm(psa[:, 0:128], KeT[:, :], S_bf[:, :], start=True, stop=True)
                  R1 = R1_[i2]
                  kb.tt("dve", R1[:, :], Vt[:, :], psa[:, 0:128], ALU.subtract)
                  kb.mm(psa[:, 128:256], Rc[:, :], R1[:, :], start=True, stop=True)
                  vnew = vnew_[i2]
                  kb.ts("dve", vnew[:, :], psa[:, 128:256], beta[:, n:n + 1], None, op0=ALU.mult)
                  pso = next_ps()
                  kb.mm(pso[:, 0:128], QeT[:, :], S_bf[:, :], start=True, stop=False)
                  kb.mm(pso[:, 0:128], attnT[:, :], vnew[:, :], start=False, stop=True)
                  kb.mm(pso[:, 128:256], k2e[:, :], vnew[:, :], start=True, stop=True)
                  kb.stt("dve", S[:, :], S[:, :], dec["cd"][:, n:n + 1], pso[:, 128:256], ALU.mult, ALU.add)
                  kb.copy("act", S_bf[:, :], S[:, :])
                  ss, junk, o = ss_[i2], junk_[i2], o_[i2]
                  kb.memset("pool", ss[:, :], 0.0)
                  kb.act(junk[:, :], pso[:, 0:128], AF.Square, accum=ss[:, :])
                  kb.act(ss[:, :], ss[:, :], AF.Sqrt, bias=kb.eps_tile(EPS), scale=1.0 / 128)
                  kb.recip(ss[:, :], ss[:, :])
                  kb.stt("dve", o[:, :], pso[:, 0:128], ss[:, 0:1], zg[:, n, :], ALU.mult, ALU.mult)
                  emit_out(0, o, 1, n, stage, stage_i)


              for n0 in range(0, NBLK, NB2):
                  ns = [n for n in range(n0, min(n0 + NB2, NBLK))]
                  ctxs = [dict() for _ in ns]
                  gens = [g_stage1(n, c_) for n, c_ in zip(ns, ctxs)]
                  live = list(gens)
                  while live:
                      for g_ in list(live):
                          try:
                              next(g_)
                          except StopIteration:
                              live.remove(g_)
                  for n, c_ in zip(ns, ctxs):
                      g_stage2(n, c_)
          kb.barrier()
    except StopBuild:
        pass

    def attention(st, slot, q_parts, k_parts, Vaug, tab, name):
        G = 4
        pts = [sb("%s_pt%d" % (name, i), [128, 512], BF16, st) for i in range(3)]
        ot = [sb("%s_ot%d" % (name, i), [128, 128], F32, st) for i in range(2)]
        rc = [sb("%s_rc%d" % (name, i), [128, 1], F32, st) for i in range(2)]
        stage = [sb("%s_stage%d" % (name, i), [128, 128], F32, st) for i in range(2)]
        stage_i = [0]
        pti = [0]
        oi = [0]
        ps_s = [pss[0], pss[1]]
        ps_o4 = [pss[2], pss[3], pss[4], pss[5]]
        si = [0]
        for gi, i0 in enumerate(range(0, NBLK, G)):
            i1 = min(i0 + G, NBLK) - 1
            def stage_q(j):
                is_ = max(i0, j)
                Wq = (i1 - is_ + 1) * 128
                q0 = is_ * 128
                ps = ps_s[si[0] % 2]
                si[0] += 1
                for pi, ((qb, P), (kbuf, _)) in enumerate(zip(q_parts, k_parts)):
                    kb.mm(ps[:, 0:Wq], kbuf[0:P, j * 128:(j + 1) * 128], qb[0:P, q0:q0 + Wq],
                          start=(pi == 0), stop=(pi == len(q_parts) - 1))
                pt = pts[pti[0] % 3]
                pti[0] += 1
                if tab is None:
                    kb.act(pt[:, 0:Wq], ps[:, 0:Wq], AF.Exp)
                else:
                    for ii in range(is_, i1 + 1):
                        c = (ii - is_) * 128
                        kb.act(pt[:, c:c + 128], ps[:, c:c + 128], AF.Exp, bias=tab[:, ii, j:j + 1])
                if j >= i0:
                    kb.tt("pool", pt[:, 0:128], pt[:, 0:128], causT_b, ALU.mult)
                return pt

            def stage_p(j, pt):
                is_ = max(i0, j)
                for ii in range(is_, i1 + 1):
                    c = (ii - is_) * 128
                    li = ii - i0
                    dst = ps_o4[li]
                    kb.mm(dst[:, 0:129], pt[:, c:c + 128],
                          bc(Vaug[:, j, :], Vaug.t[:, j, :]), start=(j == 0), stop=(j == ii))

            prev = None
            for j in range(0, i1 + 1):
                pt_j = stage_q(j)
                if prev is not None:
                    stage_p(*prev)
                prev = (j, pt_j)
            stage_p(*prev)
            for ii in range(i0, i1 + 1):
                li = ii - i0
                src = ps_o4[li]
                c0 = 0
                r = rc[oi[0] % 2]
                o = ot[oi[0] % 2]
                oi[0] += 1
                kb.ts("dve", r[:, :], src[:, c0 + 128:c0 + 129], 1e-30, None, op0=ALU.max)
                kb.recip(r[:, :], r[:, :])
                kb.ts("dve", o[:, :], src[:, c0:c0 + 128], r[:, 0:1], None, op0=ALU.mult)
                emit_out(slot, o, 1, ii, stage, stage_i, psl=[pss[6], pss[7]])

    def make_vaug(st, name):
        Vaug = sb(name, [128, NBLK, 129], BF16, st)
        kb.memset("dve", bc(Vaug[:, :, 128:129], Vaug.t[:, :, 128:129]), 1.0)
        return Vaug

    def finish_vaug(Vaug):
        for p0, p1 in ((0, 32), (32, 64), (64, 96), (96, FRONT)):
            kb.memset("dve", bc(Vaug[:, 0, :], Vaug.t[p0:p1, 0, :]), 0.0)

    if "mla" in mixers:
        with ExitStack() as st:
            w = sb("w_mla_s", [128, KC, 832], BF16, st)
            kb.dma("pool", w[:, :, :], w_mla_d[:, :, :])
            wq = sb("mla_wq_s", [128, 4, 192], BF16, st)
            kb.dma("pool", wq[:, :, :], mla_wq_d[:, :, :])
            wkv = sb("mla_wkv_s", [128, 2, 256], BF16, st)
            kb.dma("pool", wkv[:, :, :], mla_wkv_d[:, :, :])
            mg = sb("mla_g_s", [128, 10], F32, st)
            kb.dma("sp", mg[:, :], mla_g_d[:, :])
            mgq = sb("mla_gq_s", [128, 2], F32, st)
            kb.ts("dve", mgq[:, :], mg[:, 6:8], 192.0 ** -0.5, None, op0=ALU.mult)
            ropes = [sb("rope_s%d" % i, [64, 2, 512], F32, st) for i in range(2)]
            rotm = sb("rotm_s", [64, 64], F32, st)
            kb.dma("sp", rotm[:, :], rotm_d[:, :])
            hns = [sb("mhn%d" % i, [128, KC, 512], BF16, st) for i in range(2)]
            lat = sb("m_lat", [128, 6, 512], F32, st)
            latn = sb("m_latn", [128, 6, 512], BF16, st)
            sqs = [sb("msq%d" % i, [128, 512], BF16, st) for i in range(2)]
            rstd = sb("mrstd", [128, 512], F32, st)
            raw = sb("m_raw", [128, 2, 512], F32, st)
            tr_ = sb("m_tr", [64, 512], F32, st)
            ta_ = sb("m_ta", [64, 512], F32, st)
            tb_ = sb("m_tb", [64, 512], F32, st)
            QTn = sb("m_QTn", [128, T], BF16, st)
            QTr = sb("m_QTr", [128, T], BF16, st)
            kb.memset("pool", QTr[64:128, :], 0.0)
            KTn = sb("m_KTn", [128, T], BF16, st)
            KTr = sb("m_KTr", [128, T], BF16, st)
            kb.memset("pool", KTr[64:128, :], 0.0)
            Vaug = make_vaug(st, "m_Vaug")

            def qk_finish(raw, gn, gr, dn, dr_, t0, W, rope):
                sq_sum_rstd([(raw[:, 0, 0:W], 128), (raw[0:64, 1, 0:W], 64)], W, 1.0 / 192, rstd, sqs, pss[7])
                kb.stt("dve", dn[:, t0:t0 + W], raw[:, 0, 0:W], gn, rstd[:, 0:W], ALU.mult, ALU.mult)
                kb.stt("dve", tr_[:, 0:W], raw[0:64, 1, 0:W], gr, rstd[0:64, 0:W], ALU.mult, ALU.mult)
                ps = next_ps(7)
                for c0 in range(0, W, 128):
                    kb.mm(ps[0:64, c0:c0 + 128], rotm[:, :], tr_[:, c0:c0 + 128], start=True, stop=True)
                kb.tt("dve", ta_[:, 0:W], tr_[:, 0:W], rope[:, 0, 0:W], ALU.mult)
                kb.tt("dve", tb_[:, 0:W], ps[0:64, 0:W], rope[:, 1, 0:W], ALU.mult)
                kb.tt("pool", dr_[0:64, t0:t0 + W], ta_[:, 0:W], tb_[:, 0:W], ALU.add)

            for ti, (t0, W) in enumerate(tiles):
                hn_t = hns[ti % 2]
                load_hn(hn_t, t0, W)
                rope = ropes[ti % 2]
                kb.dma("sp", rope[:, :, 0:W], bc(rope_d[:, :, t0:t0 + W], rope_d.t[:, :, t0:t0 + W].rearrange("c p t -> p c t")))
                for c in range(6):
                    ps = next_ps(7)
                    proj_fm(ps, w, c * 128, 128, hn_t, W)
                    kb.copy("act", lat[:, c, 0:W], ps[:, 0:W])
                sq_sum_rstd([(lat[:, c, 0:W], 128) for c in range(4)], W, 1.0 / 512, rstd, sqs, pss[7])
                for c in range(4):
                    kb.stt("dve", latn[:, c, 0:W], lat[:, c, 0:W], mg[:, c:c + 1], rstd[:, 0:W], ALU.mult, ALU.mult)
                sq_sum_rstd([(lat[:, c, 0:W], 128) for c in (4, 5)], W, 1.0 / 256, rstd, sqs, pss[7])
                for c in (4, 5):
                    kb.stt("dve", latn[:, c, 0:W], lat[:, c, 0:W], mg[:, c:c + 1], rstd[:, 0:W], ALU.mult, ALU.mult)
                ps = next_ps(7)
                for c in range(4):
                    kb.mm(ps[:, 0:W], wq[:, c, 0:128], latn[:, c, 0:W], start=(c == 0), stop=(c == 3))
                kb.copy("act", raw[:, 0, 0:W], ps[:, 0:W])
                ps = next_ps(7)
                for c in range(4):
                    kb.mm(ps[0:64, 0:W], wq[:, c, 128:192], latn[:, c, 0:W], start=(c == 0), stop=(c == 3))
                kb.copy("act", raw[0:64, 1, 0:W], ps[0:64, 0:W])
                qk_finish(raw, mgq[:, 0:1], mgq[0:64, 1:2], QTn, QTr, t0, W, rope)
                ps = next_ps(7)
                for c in range(2):
                    kb.mm(ps[:, 0:W], wkv[:, c, 0:128], latn[:, 4 + c, 0:W], start=(c == 0), stop=(c == 1))
                kb.copy("act", raw[:, 0, 0:W], ps[:, 0:W])
                ps = next_ps(7)
                proj_fm(ps, w, 768, 64, hn_t, W)
                kb.copy("act", raw[0:64, 1, 0:W], ps[0:64, 0:W])
                qk_finish(raw, mg[:, 8:9], mg[0:64, 9:10], KTn, KTr, t0, W, rope)
                for tb in range(W // 128):
                    blk = t0 // 128 + tb
                    ps = next_ps(7)
                    for c in range(2):
                        kb.mm(ps[:, 0:128], latn[:, 4 + c, tb * 128:(tb + 1) * 128], wkv[:, c, 128:256],
                              start=(c == 0), stop=(c == 1))
                    kb.copy("act", Vaug[:, blk, 0:128], ps[:, 0:128])
            finish_vaug(Vaug)
            if os.environ.get("DUMPQ"):
                kb.dma("pool", bo[3, :, :], QTn[:, :], final=True)
                kb.dma("pool", bo[4, :, :], QTr[:, :], final=True)
            attention(st, 1, [(QTn, 128), (QTr, 128)], [(KTn, 128), (KTr, 128)], Vaug, None, "ma")
        kb.barrier()

    if "fox" in mixers:
        with ExitStack() as st:
            w = sb("w_fox_s", [128, KC, 384], BF16, st)
            kb.dma("pool", w[:, :, :], w_fox_d[:, :, :])
            fg = sb("fox_g_s", [128, 3], F32, st)
            kb.dma("sp", fg[:, :], fox_g_d[:, :])
            fgq = sb("fox_gq", [128, 1], F32, st)
            kb.ts("dve", fgq[:, :], fg[:, 0:1], 128.0 ** -0.5, None, op0=ALU.mult)
            nbf = sb("fox_nbf", [128, 1], F32, st)
            kb.ts("dve", nbf[:, :], fg[:, 2:3], -1.0, None, op0=ALU.mult)
            hns = [sb("fhn%d" % i, [128, KC, 512], BF16, st) for i in range(2)]
            raws = [sb("f_raw%d" % i, [128, 512], F32, st) for i in range(2)]
            sqs = [sb("fsq%d" % i, [128, 512], BF16, st) for i in range(2)]
            rstd = sb("frstd", [128, 512], F32, st)
            QT = sb("f_QT", [128, T], BF16, st)
            KT = sb("f_KT", [128, T], BF16, st)
            Vaug = make_vaug(st, "f_Vaug")
            for ti, (t0, W) in enumerate(tiles):
                hn_t = hns[ti % 2]
                load_hn(hn_t, t0, W)
                pq = []
                for c in range(2):
                    ps = next_ps(7)
                    proj_fm(ps, w, c * 128, 128, hn_t, W)
                    pq.append(ps)
                pv = []
                for tb in range(W // 128):
                    ps = next_ps(7)
                    proj_tm(ps[:, 0:128], hn_t, tb, w, 256, 128)
                    pv.append(ps)
                for c, (dst, gv) in enumerate(((QT, fgq[:, 0:1]), (KT, fg[:, 1:2]))):
                    ps = pq[c]
                    raw = raws[c]
                    kb.copy("act", raw[:, 0:W], ps[:, 0:W])
                    sq_sum_rstd([(raw[:, 0:W], 128)], W, 1.0 / 128, rstd, sqs, pss[7])
                    kb.stt("dve", dst[:, t0:t0 + W], raw[:, 0:W], gv, rstd[:, 0:W], ALU.mult, ALU.mult)
                for tb in range(W // 128):
                    blk = t0 // 128 + tb
                    kb.copy("act", Vaug[:, blk, 0:128], pv[tb][:, 0:128])
            finish_vaug(Vaug)
            lf = sb("f_lf", [128, NBLK], F32, st)
            kb.act(lf[:, :], bc(small_all[:, :, 2], small_all.t[:, :, 2]), AF.Exp, bias=nbf[:, 0:1], scale=-1.0)
            kb.act(lf[:, :], lf[:, :], AF.Ln, bias=kb.eps_tile(1.0))
            kb.ts("dve", lf[:, :], lf[:, :], -1.0, None, op0=ALU.mult)
            within = sb("f_within", [128, NBLK], F32, st)
            totB = sb("f_totB", [128, NBLK], F32, st)
            ps = next_ps(7)
            kb.mm(ps[:, 0:NBLK], Umat, lf[:, :], start=True, stop=True)
            kb.copy("dve", within[:, :], ps[:, 0:NBLK])
            ps = next_ps(7)
            kb.mm(ps[:, 0:NBLK], ones, lf[:, :], start=True, stop=True)
            kb.copy("dve", totB[:, :], ps[:, 0:NBLK])
            tab = sb("f_tab", [128, NBLK, NBLK], F32, st)
            for i in range(NBLK):
                if i > 0:
                    kb.ts("dve", tab[:, i, 0:i], tab[:, i - 1, 0:i], totB[:, i:i + 1], None, op0=ALU.add)
                kb.tt("dve", tab[:, i, i:i + 1], totB[:, i:i + 1], within[:, i:i + 1], ALU.subtract)
            attention(st, 2, [(QT, 128)], [(KT, 128)], Vaug, tab, "fa")
        kb.barrier()

    if "ssd" in mixers:
        with ExitStack() as st:
            w = sb("w_ssd_s", [128, KC, 768], BF16, st)
            kb.dma("pool", w[:, :, :], w_ssd_d[:, :, :])
            scv = sb("ssd_conv_s", [128, 4, 5], F32, st)
            kb.dma("sp", scv[:, :, :], ssd_conv_d[:, :, :])
            svec = sb("ssd_vec_s", [128, 8], F32, st)
            kb.dma("sp", svec[:, :], ssd_vec_d[:, :])
            srow = sb("ssd_row_s", [128, 2, 256], F32, st)
            kb.dma("sp", srow[:, :, :], bc(ssd_row_d[:, :, :], ssd_row_d.t.rearrange("c p f -> p c f")))
            hns = [sb("shn%d" % i, [128, KC, 512], BF16, st) for i in range(1)]
            pre = sb("spre", [128, 4, 515], F32, st)
            acc = [sb("sacc%d" % i, [128, 512], F32, st) for i in range(2)]
            xT = [sb("s_xT%d" % i, [128, T], BF16, st) for i in range(2)]
            BT = sb("s_BT", [128, T], BF16, st)
            CT = sb("s_CT", [128, T], BF16, st)
            zs = sb("s_zs", [128, NBLK, 256], BF16, st)
            kb.memset("dve", pre[:, :, 0:3], 0.0)
            dsts = (xT[0], xT[1], BT, CT)
            for ti, (t0, W) in enumerate(tiles):
                hn_t = hns[0]
                load_hn(hn_t, t0, W)
                for c in range(4):
                    ps = next_ps()
                    proj_fm(ps, w, 256 + c * 128, 128, hn_t, W)
                    kb.copy("act", pre[:, c, 3:3 + W], ps[:, 0:W])
                    a = acc[c % 2]
                    conv4(a, bc(pre[:, c, :], pre.t[:, c, :]), bc(scv[:, c, :], scv.t[:, c, :]), W)
                    kb.copy("pool", pre[:, c, 0:3], pre[:, c, W:W + 3])
                    kb.act(dsts[c][:, t0:t0 + W], a[:, 0:W], AF.Silu, bias=scv[:, c, 4:5])
                for tb in range(W // 128):
                    blk = t0 // 128 + tb
                    ps = next_ps()
                    proj_tm(ps[:, 0:256], hn_t, tb, w, 0, 256)
                    kb.act(zs[:, blk, :], ps[:, 0:256], AF.Silu)
            for b_ in (xT[0], xT[1], BT):
                kb.memset("dve", b_[:, 0:FRONT], 0.0)
            negA = sb("s_negA", [128, 4], F32, st)
            kb.act(negA[:, :], svec[:, 4:8], AF.Exp)
            kb.ts("dve", negA[:, :], negA[:, :], -1.0, None, op0=ALU.mult)
            dts, decs, a_alls = [], [], []
            for h in range(4):
                dt = sb("s_dt%d" % h, [128, NBLK], F32, st)
                a_all = sb("s_a%d" % h, [128, NBLK], F32, st)
                softplus(dt[:, :], bc(small_all[:, :, 3 + h], small_all.t[:, :, 3 + h]), svec[:, h:h + 1])
                kb.ts("dve", a_all[:, :], dt[:, :], negA[:, h:h + 1], None, op0=ALU.mult)
                dts.append(dt)
                a_alls.append(a_all)
                decs.append(decay_prep(st, a_all, "sd%d_" % h))
            mk = lambda n, d=F32, shp=(128, 128), k=2: [sb("%s%d" % (n, i), list(shp), d, st) for i in range(k)]
            grep_, DmT_, EG_ = mk("s_grep", k=4), mk("s_DmT", k=4), mk("s_EG", k=4)
            attnT_, CeT_ = mk("s_attnT", BF16, k=8), mk("s_CeT", BF16, k=8)
            Btok_ = mk("s_Btok", BF16)
            Xtok_ = mk("s_Xtok", F32, (128, 256))
            Xdt_ = mk("s_Xdt", BF16, (128, 256))
            Xdec_ = mk("s_Xdec", BF16, (128, 256))
            CBt_ = mk("s_CBt")
            y1_ = mk("s_y1", F32, (128, 256))
            y2_ = mk("s_y2", F32, (128, 256))
            junk_ = mk("s_junk", F32, (128, 256))
            ss_ = mk("s_ss", F32, (128, 1))
            S = sb("s_S", [128, 256], F32, st)
            S_bf = sb("s_Sbf", [128, 256], BF16, st)
            kb.memset("dve", S[:, :], 0.0)
            kb.memset("dve", S_bf[:, :], 0.0)
            stage = [sb("s_stage%d" % i, [128, 128], F32, st) for i in range(2)]
            stage_i = [0]
            hh = [0]
            def s_stage1(n):
                i2 = n % 2
                cs = slice(n * 128, (n + 1) * 128)
                pst = next_ps()
                kb.mm(pst[:, 0:128], BT[:, cs], ident_b, start=True, stop=True)
                kb.mm(pst[:, 128:256], xT[0][:, cs], ident_b, start=True, stop=True)
                kb.mm(pst[:, 256:384], xT[1][:, cs], ident_b, start=True, stop=True)
                Btok, Xtok, Xdt, Xdec = Btok_[i2], Xtok_[i2], Xdt_[i2], Xdec_[i2]
                kb.copy("act", Btok[:, :], pst[:, 0:128])
                kb.copy("act", Xtok[:, :], pst[:, 128:384])
                for h in range(4):
                    hs = slice(h * 64, (h + 1) * 64)
                    kb.ts("dve", Xdt[:, hs], Xtok[:, hs], dts[h][:, n:n + 1], None, op0=ALU.mult)
                    kb.ts("dve", Xdec[:, hs], Xtok[:, hs], dts[h][:, n:n + 1], decs[h]["e2e"][:, n:n + 1],
                          op0=ALU.mult, op1=ALU.mult)
                psc = next_ps()
                kb.mm(psc[:, 0:128], BT[:, cs], CT[:, cs], start=True, stop=True)
                CBt = CBt_[i2]
                kb.copy("act", CBt[:, :], psc[:, 0:128])
                for h in range(4):
                    decay_mats(a_alls[h], decs[h], n, grep_[h], DmT_[h], EG_[h])
                for h in range(4):
                    kb.tt("dve", attnT_[i2 * 4 + h][:, :], CBt[:, :], DmT_[h][:, :], ALU.mult)
                    kb.tt("pool", CeT_[i2 * 4 + h][:, :], CT[:, cs], EG_[h][:, :], ALU.mult)
            def s_stage2(n):
                i2 = n % 2
                cs = slice(n * 128, (n + 1) * 128)
                Btok, Xtok, Xdt, Xdec, CBt = Btok_[i2], Xtok_[i2], Xdt_[i2], Xdec_[i2], CBt_[i2]
                psy = next_ps()
                for h in range(4):
                    hs = slice(h * 64, (h + 1) * 64)
                    kb.mm(psy[:, hs], CeT_[i2 * 4 + h][:, :], S_bf[:, hs], start=True, stop=False)
                    kb.mm(psy[:, hs], attnT_[i2 * 4 + h][:, :], Xdt[:, hs], start=False, stop=True)
                psn = next_ps()
                kb.mm(psn[:, 0:256], Btok[:, :], Xdec[:, :], start=True, stop=True)
                for h in range(4):
                    hs = slice(h * 64, (h + 1) * 64)
                    kb.stt("dve", S[:, hs], S[:, hs], decs[h]["cd"][:, n:n + 1], psn[:, hs], ALU.mult, ALU.add)
                kb.copy("act", S_bf[:, :], S[:, :])
                y1, y2, junk, ss = y1_[i2], y2_[i2], junk_[i2], ss_[i2]
                kb.tt("pool", y1[:, :], Xtok[:, :], bc(srow[:, 0, :], srow.t[:, 0, :]), ALU.mult)
                kb.tt("dve", y1[:, :], y1[:, :], psy[:, 0:256], ALU.add)
                kb.tt("pool", y2[:, :], y1[:, :], bc(zs[:, n, :], zs.t[:, n, :]), ALU.mult)
                kb.memset("pool", ss[:, :], 0.0)
                kb.act(junk[:, :], y2[:, :], AF.Square, accum=ss[:, :])
                kb.act(ss[:, :], ss[:, :], AF.Sqrt, bias=kb.eps_tile(EPS), scale=1.0 / 256)
                kb.recip(ss[:, :], ss[:, :])
                kb.stt("dve", y1[:, :], y2[:, :], ss[:, 0:1], bc(srow[:, 1, :], srow.t[:, 1, :]), ALU.mult, ALU.mult)

                emit_out(3, y1, 2, n, stage, stage_i)

            s_stage1(0)
            for n in range(NBLK):
                if n + 1 < NBLK:
                    s_stage1(n + 1)
                s_stage2(n)
        kb.barrier()
    kb.finish()
    return kb, locals()


IN_WIDTHS = (512, 512, 512, 512, 4, 4, 512, 256, 64, 512, 512, 512, 4, 512, 512, 256, 256, 8)
OFF = [0]
for _w in IN_WIDTHS:
    OFF.append(OFF[-1] + _w)


def r3(w):
    K, C = w.shape
    return np.ascontiguousarray(w.reshape(K // 128, 128, C).transpose(1, 0, 2))


def rep(v, n=128):
    v = np.asarray(v, np.float32).reshape(1, -1)
    return np.ascontiguousarray(np.broadcast_to(v, (n, v.shape[1])))


def make_consts(T):
    p = np.arange(128)[:, None]
    f = np.arange(128)[None, :]
    cst = np.stack([
        (p == f), (p <= f), np.where(f >= p, 0.0, -30000.0), (f > p), (f >= p), np.ones((128, 128)),
    ]).astype(np.float32)
    pos = np.maximum(np.arange(T) - FRONT, 0).astype(np.float32)
    inv = (1.0 / (10000.0 ** (np.arange(0, 64, 2, dtype=np.float32) / 64.0))).astype(np.float32)
    ang = pos[None, :] * np.concatenate([inv, inv])[:, None]
    rope = np.stack([np.cos(ang), np.sin(ang)]).astype(np.float32)
    rot = np.zeros((64, 64), np.float32)
    for ff in range(32):
        rot[ff, ff + 32] = -1.0
        rot[ff + 32, ff] = 1.0
    return cst, rope, np.ascontiguousarray(rot.T)


def phaseA_inputs(P, l, j, hT_b, T):
    G = j // 2
    w_in = P["w_in"][l]
    col = lambda o, a, n: w_in[:, OFF[o] + a:OFF[o] + a + n]
    cst, rope, rotT = make_consts(T)
    small = np.concatenate([col(4, j, 1), col(5, j, 1), col(12, j, 1), col(17, G * 4, 4),
                            np.zeros((2048, 1), np.float32)], axis=1)
    w_gdn = np.concatenate([col(0, j * 128, 128), col(1, j * 128, 128), col(2, j * 128, 128), col(3, j * 128, 128)], 1)
    cw = P["gdn_conv_w"][l]
    gdn_conv = np.stack([cw[:, j * 128:(j + 1) * 128].T, cw[:, 512 + j * 128:512 + (j + 1) * 128].T,
                         cw[:, 1024 + j * 128:1024 + (j + 1) * 128].T], axis=1)
    w_mla = np.concatenate([col(6, 0, 512), col(7, 0, 256), col(8, 0, 64)], 1)
    mla_g = np.zeros((128, 10), np.float32)
    mla_g[:, 0:4] = P["mla_qa_g"][l].reshape(4, 128).T
    mla_g[:, 4:6] = P["mla_kva_g"][l].reshape(2, 128).T
    mla_g[:, 6] = P["mla_qn_g"][l][0:128]
    mla_g[0:64, 7] = P["mla_qn_g"][l][128:192]
    mla_g[:, 8] = P["mla_kn_g"][l][0:128]
    mla_g[0:64, 9] = P["mla_kn_g"][l][128:192]
    w_fox = np.concatenate([col(9, j * 128, 128), col(10, j * 128, 128), col(11, j * 128, 128)], 1)
    fox_g = np.stack([P["fox_qn_g"][l], P["fox_kn_g"][l], np.full(128, P["fox_b_f"][l][j], np.float32)], 1)
    w_ssd = np.concatenate([col(13, G * 256, 256), col(14, G * 256, 256), col(15, G * 128, 128), col(16, G * 128, 128)], 1)
    sw = P["ssd_conv_w"][l]
    sbias = P["ssd_conv_b"][l]
    chs = [slice(G * 256, G * 256 + 128), slice(G * 256 + 128, G * 256 + 256),
           slice(512 + G * 128, 512 + (G + 1) * 128), slice(768 + G * 128, 768 + (G + 1) * 128)]
    ssd_conv = np.stack([np.concatenate([sw[:, c].T, sbias[c][:, None]], 1) for c in chs], axis=1)
    ssd_vec = rep(np.concatenate([P["ssd_dt_bias"][l][G * 4:G * 4 + 4], P["ssd_A_log"][l][G * 4:G * 4 + 4]]))
    ssd_row = np.stack([rep(np.repeat(P["ssd_D"][l][G * 4:G * 4 + 4], 64)), rep(P["ssd_norm_g"][l][G * 256:(G + 1) * 256])])
    f32c = lambda a: np.ascontiguousarray(a, dtype=np.float32)
    return {
        "hT": f32c(hT_b), "g1": f32c(P["mix_norm_g"][l].reshape(KC, 128).T), "w_small": r3(f32c(small)), "cst": cst,
        "w_gdn": r3(f32c(w_gdn)), "gdn_conv": f32c(gdn_conv),
        "gdn_vec": rep([P["gdn_A_log"][l][j], P["gdn_dt_bias"][l][j]]), "gdn_ng": rep(P["gdn_norm_g"][l]),
        "w_mla": r3(f32c(w_mla)), "mla_wq": r3(f32c(P["mla_wq_b"][l][:, j * 192:(j + 1) * 192])),
        "mla_wkv": r3(f32c(P["mla_wkv_b"][l][:, j * 256:(j + 1) * 256])), "mla_g": mla_g,
        "rope": rope, "rotm": rotT,
        "w_fox": r3(f32c(w_fox)), "fox_g": f32c(fox_g),
        "w_ssd": r3(f32c(w_ssd)), "ssd_conv": f32c(ssd_conv), "ssd_vec": ssd_vec, "ssd_row": f32c(ssd_row),
    }


def seq_to_hT(h_seq, T):
    L = h_seq.shape[0]
    out = np.zeros((2048, T), np.float32)
    out[:, FRONT:FRONT + L] = h_seq.T
    return out.reshape(KC, 128, T)


_PROGS = {}


def _prog(key, fn):
    if key not in _PROGS:
        _PROGS[key] = fn()
    return _PROGS[key]


def panels(w3, pw=256):
    p, K, C = w3.shape
    n = (C + pw - 1) // pw
    if n * pw != C:
        w3 = np.concatenate([w3, np.zeros((p, K, n * pw - C), w3.dtype)], axis=2)
    return np.ascontiguousarray(w3.reshape(p, K, n, pw).transpose(2, 0, 1, 3))


def phaseB_weights(P, l):
    f32c = lambda a: np.ascontiguousarray(a, dtype=np.float32)
    d = {
        "wgate": np.stack([panels(r3(f32c(P["w_gate"][l][b]))) for b in range(4)]),
        "wbr": np.stack([r3(f32c(P["w_branch"][l][b])) for b in range(4)]),
        "wo": panels(r3(f32c(P["w_o"][l]))),
        "g1": f32c(P["mix_norm_g"][l].reshape(KC, 128).T),
        "g2": f32c(P["ffn_norm_g"][l].reshape(KC, 128).T),
    }
    i = l // 2
    if l % 2 == 0:
        d.update({"wfg": panels(r3(f32c(P["dense_w_gate"][i]))), "wfu": panels(r3(f32c(P["dense_w_up"][i]))),
                  "wfd": panels(r3(f32c(P["dense_w_down"][i])), 128)})
    else:
        sel = np.zeros((N_EXP, N_EXP, 128), np.float32)
        for e in range(N_EXP):
            sel[e, e, :] = 1.0
        d.update({"wr": r3(f32c(P["router_w"][i])),
                  "weg": np.stack([panels(r3(f32c(P["moe_w_gate"][i][e]))) for e in range(N_EXP)]),
                  "weu": np.stack([panels(r3(f32c(P["moe_w_up"][i][e]))) for e in range(N_EXP)]),
                  "wed": np.stack([panels(r3(f32c(P["moe_w_down"][i][e])), 128) for e in range(N_EXP)]),
                  "sel": sel, "ident": np.eye(128, dtype=np.float32)})
    return d


def kernel(**inputs):
    P = {k: np.asarray(v) for k, v in inputs.items()}
    x = P["x"].astype(np.float32, copy=False)
    Bsz, S, D = x.shape
    NX = S // 128
    NBLK = NX + 1
    T = NBLK * 128
    NXB = NX // 4
    NTB = NXB + 1
    depth = P["w_in"].shape[0]
    meta = P["meta_tokens"].astype(np.float32)
    hT = [seq_to_hT(np.concatenate([meta, x[b]], 0), T) for b in range(Bsz)]
    cores = list(range(8))
    for l in range(depth):
        kbA, _ = _prog(("A", NBLK), lambda: build_phaseA(NBLK))
        in_maps = [phaseA_inputs(P, l, c % 4, hT[c // 4], T) for c in cores]
        resA = run_bass_kernel_spmd(kbA.nc, in_maps, core_ids=cores)
        br = []
        for b in range(Bsz):
            bos = [resA.results[b * 4 + j]["bo"] for j in range(4)]
            rows = [bos[j][0] for j in range(4)] + [bos[j][1] for j in range(4)] + [bos[j][2] for j in range(4)] \
                + [bos[j][3 + (j % 2)] for j in range(4)]
            br.append(np.stack(rows))
        del resA
        kind = "dense" if l % 2 == 0 else "moe"
        last = (l == depth - 1)
        ntb = NXB if last else NTB
        kbB = _prog(("B", ntb, kind), lambda: build_phaseB(ntb, kind))
        wts = phaseB_weights(P, l)
        in_maps = []
        for c in cores:
            b, q = c // 4, c % 4
            if last:
                cols = np.r_[128 + q * NXB * 128:128 + (q + 1) * NXB * 128]
            else:
                cols = np.r_[0:128, 128 + q * NXB * 128:128 + (q + 1) * NXB * 128]
            m = dict(wts)
            m["hT"] = np.ascontiguousarray(hT[b][:, :, cols])
            m["br"] = np.ascontiguousarray(br[b][:, :, cols])
            in_maps.append(m)
        resB = run_bass_kernel_spmd(kbB.nc, in_maps, core_ids=cores)
        for c in cores:
            b, q = c // 4, c % 4
            o = resB.results[c]["hTo"]
            if last:
                hT[b][:, :, 128 + q * NXB * 128:128 + (q + 1) * NXB * 128] = o
                continue
            if q == 0:
                hT[b][:, :, FRONT:128] = o[:, :, FRONT:128]
            hT[b][:, :, 128 + q * NXB * 128:128 + (q + 1) * NXB * 128] = o[:, :, 128:]
        del resB
    out = np.stack([np.ascontiguousarray(hT[b].reshape(D, T)[:, 128:].T) for b in range(Bsz)])
    return out.astype(np.float32)
```
